# Optimizing a Trainium2 kernel written in Bass

```python
import math
import jax, jax.numpy as jnp
from jax import lax
import numpy as np

D_MODEL = 2048
BATCH = 1
SEQ = 8192
DEPTH = 2

GRID_W = 64
CTX_LEN = 256
EPS = 1e-6

A_HEADS = 8
A_HEAD_DIM = 64
A_V_DIM = 2 * A_HEAD_DIM
A_WIDTH = A_HEADS * A_V_DIM
QK_COLS = A_HEADS * 2 * A_HEAD_DIM
Q_BLOCK = 128
ROPE_THETA = 10000.0

B_GROUPS = 8
B_GROUP_W = 128
B_WIDTH = B_GROUPS * B_GROUP_W
CHUNK = 128

IN_COLS = 2 * QK_COLS + A_WIDTH + 2 * B_WIDTH
MIX_WIDTH = A_WIDTH + B_WIDTH

C_GROUPS = 4
C_GROUP_W = D_MODEL // C_GROUPS

N_EXPERTS = 16
CAPACITY_FACTOR = 2
F_EXPERT = D_MODEL // 2

N_EVEN = (DEPTH + 1) // 2
N_ODD = DEPTH // 2

kernel_name = "hybrid_diffattn_gmlp_fnet_ecmoe_dit"


def rms_norm(x, g):
    xf = x.astype(jnp.float32)
    y = xf * lax.rsqrt(jnp.mean(xf * xf, -1, keepdims=True) + EPS)
    return (y * g.astype(jnp.float32)).astype(x.dtype)


def layer_norm(x, g, b):
    xf = x.astype(jnp.float32)
    mu = jnp.mean(xf, -1, keepdims=True)
    var = jnp.mean(jnp.square(xf - mu), -1, keepdims=True)
    y = (xf - mu) * lax.rsqrt(var + EPS) * g.astype(jnp.float32) + b.astype(jnp.float32)
    return y.astype(x.dtype)


def modulate(h, shift, scale):
    return h * (1.0 + scale) + shift


def _rope_1d(x, pos):
    dim = x.shape[-1]
    inv = ROPE_THETA ** (-jnp.arange(0, dim, 2, dtype=jnp.float32) / dim)
    ang = pos.astype(jnp.float32)[:, None] * inv[None, :]
    cos = jnp.cos(ang)[None, :, None, None, :]
    sin = jnp.sin(ang)[None, :, None, None, :]
    x1, x2 = x[..., : dim // 2], x[..., dim // 2:]
    return jnp.concatenate([x1 * cos - x2 * sin, x2 * cos + x1 * sin], -1)


def axial_rope(x, row, col):
    half = A_HEAD_DIM // 2
    xf = x.astype(jnp.float32)
    out = jnp.concatenate([_rope_1d(xf[..., :half], row), _rope_1d(xf[..., half:], col)], -1)
    return out.astype(x.dtype)


def diff_attend(q, k, v, lam):
    s = jnp.einsum('bqhcd,bkhcd->bhcqk', q.astype(jnp.float32), k.astype(jnp.float32)) * (A_HEAD_DIM ** -0.5)
    p = jax.nn.softmax(s, axis=-1)
    a = p[:, :, 0] - lam * p[:, :, 1]
    return jnp.einsum('bhqk,bkhe->bqhe', a, v.astype(jnp.float32))


def split_projection(h, w_in):
    B_, n, _ = h.shape
    z = h @ w_in
    q = z[..., :QK_COLS].reshape(B_, n, A_HEADS, 2, A_HEAD_DIM)
    k = z[..., QK_COLS:2 * QK_COLS].reshape(B_, n, A_HEADS, 2, A_HEAD_DIM)
    v = z[..., 2 * QK_COLS:2 * QK_COLS + A_WIDTH].reshape(B_, n, A_HEADS, A_V_DIM)
    uv = jax.nn.gelu(z[..., 2 * QK_COLS + A_WIDTH:])
    return q, k, v, uv


def spatial_gate(uv, ln_g, ln_b, w_s, b_s):
    B_, n, _ = uv.shape
    u, v = uv[..., :B_WIDTH], uv[..., B_WIDTH:]
    v = v.reshape(B_, n // CHUNK, CHUNK, B_GROUPS, B_GROUP_W)
    v = layer_norm(v, ln_g.reshape(B_GROUPS, B_GROUP_W), ln_b.reshape(B_GROUPS, B_GROUP_W))
    mixed = jnp.einsum('gpq,bcqgd->bcpgd', w_s, v) + b_s.T[:, :, None]
    return u * mixed.reshape(B_, n, B_WIDTH)


def even_mixer(h_lat, h_ctx, row, col, w_in, w_out, lq1, lk1, lq2, lk2, g_subln,
               ln_g, ln_b, w_s, b_s, lam_init, need_ctx_out):
    f32 = jnp.float32
    lam = (jnp.exp(jnp.sum(lq1.astype(f32) * lk1.astype(f32)))
           - jnp.exp(jnp.sum(lq2.astype(f32) * lk2.astype(f32))) + lam_init)
    q_l, k_l, v_l, uv_l = split_projection(h_lat, w_in)
    q_c, k_c, v_c, uv_c = split_projection(h_ctx, w_in)
    q_l = axial_rope(q_l, row, col)
    k_l = axial_rope(k_l, row, col)
    k_all = jnp.concatenate([k_c, k_l], axis=1)
    v_all = jnp.concatenate([v_c, v_l], axis=1)
    B_, n = h_lat.shape[0], h_lat.shape[1]
    nb = n // Q_BLOCK
    qb = jnp.moveaxis(q_l.reshape(B_, nb, Q_BLOCK, A_HEADS, 2, A_HEAD_DIM), 1, 0)
    o = lax.map(lambda qi: diff_attend(qi, k_all, v_all, lam), qb)
    o = jnp.moveaxis(o, 0, 1).reshape(B_, n, A_HEADS, A_V_DIM)

    def finish(o_attn, uv):
        a = rms_norm(o_attn, g_subln) * (1.0 - lam_init)
        a = a.reshape(a.shape[0], a.shape[1], A_WIDTH).astype(uv.dtype)
        g = spatial_gate(uv, ln_g, ln_b, w_s, b_s)
        return jnp.concatenate([a, g], axis=-1) @ w_out

    y_lat = finish(o, uv_l)
    y_ctx = finish(diff_attend(q_c, k_c, v_c, lam), uv_c) if need_ctx_out else None
    return y_lat, y_ctx


def fourier_mix(h, w_o):
    B_, n, _ = h.shape
    hg = h.astype(jnp.float32).reshape(B_, n, C_GROUPS, C_GROUP_W)
    f = jnp.fft.fft2(hg, axes=(1, 3), norm="ortho").real
    return f.reshape(B_, n, D_MODEL).astype(h.dtype) @ w_o


def expert_choice_ffn(h, w_r, w_gate, w_up, w_down):
    B_, n, D = h.shape
    cap = CAPACITY_FACTOR * n // N_EXPERTS
    aff = jax.nn.softmax((h @ w_r).astype(jnp.float32), axis=-1)
    g, idx = lax.top_k(jnp.swapaxes(aff, 1, 2), cap)
    xs = jax.vmap(lambda hb, ib: hb[ib])(h, idx)
    a = jnp.einsum('becd,edf->becf', xs, w_gate)
    u = jnp.einsum('becd,edf->becf', xs, w_up)
    y = jnp.einsum('becf,efd->becd', jax.nn.silu(a) * u, w_down) * g[..., None].astype(h.dtype)
    return jax.vmap(lambda yb, ib: jnp.zeros((n, D), yb.dtype).at[ib.reshape(-1)].add(yb.reshape(-1, D)))(y, idx)


def setup_inputs(seed: int = 0) -> dict:
    key = jax.random.key(seed)
    ks = jax.random.split(key, 32)
    nrm = jax.random.normal
    D = D_MODEL
    return {
        "x": nrm(ks[0], (BATCH, SEQ, D), jnp.float32),
        "c": nrm(ks[1], (BATCH, D), jnp.float32),
        "ctx": nrm(ks[2], (BATCH, CTX_LEN, D), jnp.float32),
        "c_ctx": nrm(ks[3], (D,), jnp.float32),
        "w_mod": nrm(ks[4], (DEPTH, D, 6 * D), jnp.float32) * (0.5 * D ** -0.5),
        "b_mod": nrm(ks[5], (DEPTH, 6 * D), jnp.float32) * 0.02,
        "g_norm_mix": 1.0 + 0.02 * nrm(ks[6], (DEPTH, D), jnp.float32),
        "g_norm_ffn": 1.0 + 0.02 * nrm(ks[7], (DEPTH, D), jnp.float32),
        "w_in": nrm(ks[8], (N_EVEN, D, IN_COLS), jnp.float32) * D ** -0.5,
        "w_out": nrm(ks[9], (N_EVEN, MIX_WIDTH, D), jnp.float32) * MIX_WIDTH ** -0.5,
        "lam_q1": nrm(ks[10], (N_EVEN, A_HEAD_DIM), jnp.float32) * 0.1,
        "lam_k1": nrm(ks[11], (N_EVEN, A_HEAD_DIM), jnp.float32) * 0.1,
        "lam_q2": nrm(ks[12], (N_EVEN, A_HEAD_DIM), jnp.float32) * 0.1,
        "lam_k2": nrm(ks[13], (N_EVEN, A_HEAD_DIM), jnp.float32) * 0.1,
        "g_subln": 1.0 + 0.02 * nrm(ks[14], (N_EVEN, A_V_DIM), jnp.float32),
        "sgu_ln_g": 1.0 + 0.02 * nrm(ks[15], (N_EVEN, B_WIDTH), jnp.float32),
        "sgu_ln_b": 0.02 * nrm(ks[16], (N_EVEN, B_WIDTH), jnp.float32),
        "w_spatial": nrm(ks[17], (N_EVEN, B_GROUPS, CHUNK, CHUNK), jnp.float32) * CHUNK ** -0.5,
        "b_spatial": 1.0 + 0.02 * nrm(ks[18], (N_EVEN, B_GROUPS, CHUNK), jnp.float32),
        "w_fourier_out": nrm(ks[19], (N_ODD, D, D), jnp.float32) * D ** -0.5,
        "w_router": nrm(ks[20], (DEPTH, D, N_EXPERTS), jnp.float32) * D ** -0.5,
        "w_gate": nrm(ks[21], (DEPTH, N_EXPERTS, D, F_EXPERT), jnp.float32) * D ** -0.5,
        "w_up": nrm(ks[22], (DEPTH, N_EXPERTS, D, F_EXPERT), jnp.float32) * D ** -0.5,
        "w_down": nrm(ks[23], (DEPTH, N_EXPERTS, F_EXPERT, D), jnp.float32) * F_EXPERT ** -0.5,
        "g_final": 1.0 + 0.02 * nrm(ks[24], (D,), jnp.float32),
    }


def reference(x, c, ctx, c_ctx, w_mod, b_mod, g_norm_mix, g_norm_ffn, w_in, w_out,
              lam_q1, lam_k1, lam_q2, lam_k2, g_subln, sgu_ln_g, sgu_ln_b, w_spatial,
              b_spatial, w_fourier_out, w_router, w_gate, w_up, w_down, g_final):
    n = x.shape[1]
    rows = n // GRID_W
    row = jnp.repeat(jnp.arange(rows, dtype=jnp.int32), GRID_W)
    col = jnp.tile(jnp.arange(GRID_W, dtype=jnp.int32), rows)

    for i in range(DEPTH):
        last = i == DEPTH - 1
        j = i // 2
        m_lat = (jax.nn.silu(c) @ w_mod[i] + b_mod[i])[:, None, :]
        m_ctx = (jax.nn.silu(c_ctx) @ w_mod[i] + b_mod[i])[None, None, :]
        sm, cm, gm, sf, cf, gf = jnp.split(m_lat, 6, axis=-1)
        smc, cmc, gmc, sfc, cfc, gfc = jnp.split(m_ctx, 6, axis=-1)

        h_lat = modulate(rms_norm(x, g_norm_mix[i]), sm, cm)
        if i % 2 == 0:
            lam_init = 0.8 - 0.6 * math.exp(-0.3 * i)
            h_ctx = modulate(rms_norm(ctx, g_norm_mix[i]), smc, cmc)
            y_lat, y_ctx = even_mixer(h_lat, h_ctx, row, col, w_in[j], w_out[j],
                                      lam_q1[j], lam_k1[j], lam_q2[j], lam_k2[j], g_subln[j],
                                      sgu_ln_g[j], sgu_ln_b[j], w_spatial[j], b_spatial[j],
                                      lam_init, not last)
        else:
            y_lat = fourier_mix(h_lat, w_fourier_out[j])
            y_ctx = None if last else fourier_mix(modulate(rms_norm(ctx, g_norm_mix[i]), smc, cmc), w_fourier_out[j])

        x = x + gm * y_lat
        x = x + gf * expert_choice_ffn(modulate(rms_norm(x, g_norm_ffn[i]), sf, cf),
                                       w_router[i], w_gate[i], w_up[i], w_down[i])
        if not last:
            ctx = ctx + gmc * y_ctx
            ctx = ctx + gfc * expert_choice_ffn(modulate(rms_norm(ctx, g_norm_ffn[i]), sfc, cfc),
                                                w_router[i], w_gate[i], w_up[i], w_down[i])

    return rms_norm(x, g_final)
```

```python
import math
import numpy as np
from contextlib import ExitStack
import concourse.bass as bass
import concourse.mybir as mybir
from concourse.bass_utils import run_bass_kernel_spmd

F32 = mybir.dt.float32
BF16 = mybir.dt.bfloat16
I32 = mybir.dt.int32
AF = mybir.ActivationFunctionType
ALU = mybir.AluOpType
AX = mybir.AxisListType
NCORES = 8
D = 2048
SEQ = 8192
TPC = SEQ // NCORES
NT = TPC // 128
EPS = 1e-6


class Res:
    __slots__ = ("w", "r")

    def __init__(self):
        self.w = {}
        self.r = {}


class Prog:
    def __init__(self):
        self.nc = bass.Bass("TRN2", target_bir_lowering=False)
        self.es = ExitStack()
        nc = self.nc
        self.eng = {"pe": nc.tensor, "act": nc.scalar, "dve": nc.vector,
                    "pool": nc.gpsimd, "sp": nc.sync}
        self.sems = {}
        self.cnt = {}
        for e in self.eng:
            self.sems[e] = self.es.enter_context(nc.semaphore("s_" + e))
            self.cnt[e] = 0
        self.known = {e: {} for e in self.eng}
        self.res = {}
        self.nd = 0

    def sb(self, name, shape, dt):
        return self.es.enter_context(self.nc.sbuf_tensor(name, list(shape), dt))

    def ps(self, name, shape, dt):
        return self.es.enter_context(self.nc.psum_tensor(name, list(shape), dt))

    def din(self, name, shape, dt):
        return self.nc.dram_tensor(name, list(shape), dt, kind="ExternalInput").ap()

    def dout(self, name, shape, dt):
        return self.nc.dram_tensor(name, list(shape), dt, kind="ExternalOutput").ap()

    def dtmp(self, name, shape, dt):
        return self.nc.dram_tensor(name, list(shape), dt, kind="Internal").ap()

    def R(self, key):
        r = self.res.get(key)
        if r is None:
            r = self.res[key] = Res()
        return r

    def _wait(self, e, semkey, val):
        if val <= 0:
            return
        k = self.known[e]
        if k.get(semkey, 0) >= val:
            return
        k[semkey] = val
        self.eng[e].wait_ge(self.sems[semkey], val)

    def _deps(self, e, reads, writes, skip=None):
        for key in reads:
            for sk, v in self.R(key).w.items():
                if e == "pe" and sk == "pe":
                    continue
                self._wait(e, sk, v)
        for key in writes:
            r = self.R(key)
            for sk, v in r.w.items():
                if sk == skip or (e == "pe" and sk == "pe"):
                    continue
                self._wait(e, sk, v)
            for sk, v in r.r.items():
                if e == "pe" and sk == "pe":
                    continue
                self._wait(e, sk, v)

    def op(self, e, fn, reads=(), writes=()):
        self._deps(e, reads, writes)
        ins = fn()
        self.cnt[e] += 1
        c = self.cnt[e]
        ins.then_inc(self.sems[e], 1)
        for key in reads:
            self.R(key).r[e] = c
        for key in writes:
            r = self.R(key)
            r.w = {e: c}
            r.r = {}
        return ins

    def _dma_post(self, ins, sk, reads, writes):
        self.cnt[sk] += 16
        c = self.cnt[sk]
        ins.then_inc(self.sems[sk], 16)
        for key in reads:
            self.R(key).r[sk] = c
        for key in writes:
            r = self.R(key)
            if sk in r.w and len(r.w) == 1:
                r.w[sk] = c
            else:
                r.w = {sk: c}
            r.r = {}

    def _dsem(self, semkey, writes):
        sk = semkey if semkey is not None else ("d", writes[0])
        if sk not in self.sems:
            self.sems[sk] = self.es.enter_context(self.nc.semaphore("sd%d" % self.nd))
            self.nd += 1
            self.cnt[sk] = 0
        return sk

    def dma(self, q, out, in_, reads=(), writes=(), semkey=None, **kw):
        sk = self._dsem(semkey, writes)
        self._deps(q, reads, writes, skip=sk)
        ins = self.eng[q].dma_start(out=out, in_=in_, **kw)
        self._dma_post(ins, sk, reads, writes)
        return ins

    def idma(self, out, out_off, in_, in_off, reads=(), writes=(), semkey=None, **kw):
        sk = self._dsem(semkey, writes)
        self._deps("pool", reads, writes, skip=sk)
        ins = self.nc.gpsimd.indirect_dma_start(out, out_off, in_, in_off, **kw)
        self._dma_post(ins, sk, reads, writes)
        return ins

    def finish(self, out_keys, e="sp"):
        for key in out_keys:
            for sk, v in self.R(key).w.items():
                self._wait(e, sk, v)
        for o in self.eng:
            if o != e:
                self._wait(e, o, self.cnt[o])
        self.es.close()
        return self.nc


def make_ident(P, dt=BF16, name="ident"):
    nc = P.nc
    idf = P.sb(name + "f", [128, 128], F32)
    P.op("pool", lambda: nc.gpsimd.memset(idf[:], 1.0), writes=[name + "f"])
    P.op("pool", lambda: nc.gpsimd.affine_select(out=idf[:], in_=idf[:], pattern=[[-1, 128]],
                                                 compare_op=ALU.is_equal, fill=0.0, base=0,
                                                 channel_multiplier=1),
         reads=[name + "f"], writes=[name + "f"])
    if dt == F32:
        return idf, name + "f"
    idb = P.sb(name, [128, 128], dt)
    P.op("dve", lambda: nc.vector.tensor_copy(out=idb[:], in_=idf[:]), reads=[name + "f"], writes=[name])
    return idb, name


def bc128(v):
    v = np.asarray(v, dtype=np.float32).reshape(1, -1)
    return np.ascontiguousarray(np.broadcast_to(v, (128, v.shape[1])))


MC = 6 * D // NCORES


def build_mod():
    P = Prog()
    nc = P.nc
    c1 = P.din("c1", [128, 16], F32)
    c2 = P.din("c2", [128, 16], F32)
    wm = P.din("wm", [2, D, MC], F32)
    bm = P.din("bm", [2, 2, MC], F32)
    mo = P.dout("mo", [2, 2, MC], F32)
    c1s = P.sb("c1s", [128, 16], F32)
    c2s = P.sb("c2s", [128, 16], F32)
    cc = P.sb("cc", [128, 16, 2], F32)
    bs = P.sb("bs", [2, 2, MC], F32)
    ms = P.sb("ms", [2, 2, MC], F32)
    HC = MC // 2
    wb = [P.sb("wmb%d" % i, [128, 16, HC], F32) for i in range(2)]
    pz = [P.ps("pz%d" % i, [2, 384], F32) for i in range(2)]
    P.dma("sp", c1s[:], c1[:, :], writes=["c1s"])
    P.dma("sp", c2s[:], c2[:, :], writes=["c2s"])
    P.dma("sp", bs[:], bm.rearrange("l s n -> s l n"), writes=["bs"])
    P.op("act", lambda: nc.scalar.activation(out=cc[:, :, 0], in_=c1s[:], func=AF.Silu), reads=["c1s"], writes=["cc0"])
    P.op("act", lambda: nc.scalar.activation(out=cc[:, :, 1], in_=c2s[:], func=AF.Silu), reads=["c2s"], writes=["cc1"])
    ci = 0
    for l in range(2):
        wv = wm[l].rearrange("(p kc) n -> p kc n", kc=16)
        for hf in range(2):
            b = ci % 2
            q = "sp" if ci % 2 == 0 else "act"
            for kq in range(4):
                P.dma(q, wb[b][:, kq * 4:(kq + 1) * 4, :], wv[:, kq * 4:(kq + 1) * 4, hf * HC:(hf + 1) * HC], writes=[("wmb", b)])
            for nb in range(HC // 384):
                pb = (ci * 2 + nb) % 2
                for kc in range(16):
                    P.op("pe", lambda kc=kc: nc.tensor.matmul(pz[pb][:], lhsT=cc[:, kc, :], rhs=wb[b][:, kc, nb * 384:(nb + 1) * 384],
                                                               start=(kc == 0), stop=(kc == 15)),
                         reads=["cc0", "cc1", ("wmb", b)], writes=[("pz", pb)])
                n0 = hf * HC + nb * 384
                P.op("dve", lambda: nc.vector.tensor_tensor(out=ms[:, l, n0:n0 + 384], in0=pz[pb][:], in1=bs[:, l, n0:n0 + 384], op=ALU.add),
                     reads=[("pz", pb), "bs"], writes=["ms"])
            ci += 1
    P.dma("sp", mo.rearrange("l s n -> s l n"), ms[:], reads=["ms"], writes=["mo"])
    return P.finish(["mo"])


def run_mod(inp):
    nc = build_mod()
    c1 = np.ascontiguousarray(inp["c"].reshape(128, 16))
    c2 = np.ascontiguousarray(inp["c_ctx"].reshape(128, 16))
    maps = []
    for i in range(NCORES):
        wm = np.ascontiguousarray(inp["w_mod"][:, :, i * MC:(i + 1) * MC])
        bm = np.ascontiguousarray(np.broadcast_to(inp["b_mod"][:, None, i * MC:(i + 1) * MC], (2, 2, MC)))
        maps.append({"c1": c1, "c2": c2, "wm": wm, "bm": bm})
    res = run_bass_kernel_spmd(nc, maps, core_ids=list(range(NCORES)))
    m = np.concatenate([r["mo"] for r in res.results], axis=2)
    return m


NTK = TPC + 32
INC = 5120


def rms_modulate_tile(P, nc, xs, xkey, rows, ss, sskey, epsb, gam, sh, hb, hbkey):
    P.op("act", lambda: nc.scalar.activation(out=hb[:rows, :], in_=xs[:rows, :], func=AF.Square, accum_out=ss[:rows, 0:1]),
         reads=[xkey], writes=[hbkey, sskey])
    P.op("act", lambda: nc.scalar.activation(out=ss[:rows, 1:2], in_=ss[:rows, 0:1], func=AF.Sqrt, scale=1.0 / D, bias=epsb[:rows, 0:1]),
         reads=[sskey, "epsb"], writes=[sskey])
    P.op("dve", lambda: nc.vector.reciprocal(out=ss[:rows, 2:3], in_=ss[:rows, 1:2]), reads=[sskey], writes=[sskey])
    P.op("dve", lambda: nc.vector.scalar_tensor_tensor(out=xs[:rows, :], in0=xs[:rows, :], scalar=ss[:rows, 2:3], in1=gam[:rows, :],
                                                       op0=ALU.mult, op1=ALU.mult),
         reads=[xkey, sskey, "gam"], writes=[xkey])
    P.op("pool", lambda: nc.gpsimd.tensor_tensor(out=hb[:rows, :], in0=xs[:rows, :], in1=sh[:rows, :], op=ALU.add),
         reads=[xkey, "sh"], writes=[hbkey])


def build_A():
    P = Prog()
    nc = P.nc
    x = P.din("x", [TPC, D], F32)
    cx = P.din("cx", [32, D], F32)
    gmix = P.din("gmix", [128, D], F32)
    cm = P.din("cm", [128, D], F32)
    sm = P.din("sm", [128, D], F32)
    cmc = P.din("cmc", [128, D], F32)
    smc = P.din("smc", [128, D], F32)
    w_in = P.din("w_in", [D, INC], F32)
    cosd = P.din("cosE", [TPC, 256], F32)
    sind = P.din("sinE", [TPC, 256], F32)
    lngd = P.din("lng", [128, 1024], F32)
    lnbd = P.din("lnb", [128, 1024], F32)
    wsTd = P.din("wsT", [8, 128, 128], F32)
    bsbd = P.din("bsb", [128, 1024], F32)
    qT = P.dout("qT", [8, 128, TPC], BF16)
    kT = P.dout("kT", [8, 128, NTK], BF16)
    vo = P.dout("v", [NTK, 1024], BF16)
    gTo = P.dout("gT", [8, 128, TPC], BF16)

    ident, idk = make_ident(P)
    xs = [P.sb("xs%d" % i, [128, D], F32) for i in range(2)]
    gam = P.sb("gam", [128, D], F32)
    sh = P.sb("sh", [128, D], F32)
    hb = [P.sb("hb%d" % i, [128, D], BF16) for i in range(2)]
    hT = P.sb("hT", [128, 16, NTK], BF16)
    wblk = [P.sb("wblk%d" % i, [128, 16, 512], BF16) for i in range(2)]
    big = P.sb("big", [128, 4096], F32)
    cosE = big[:, 0:2048].rearrange("p (t f) -> p t f", t=8)
    sinE = big[:, 2048:4096].rearrange("p (t f) -> p t f", t=8)
    uT = big.bitcast(BF16).rearrange("p (g t) -> p g t", g=8)
    lng = P.sb("lngs", [128, 1024], F32)
    lnb = P.sb("lnbs", [128, 1024], F32)
    wsTf = P.sb("wsTf", [128, 8, 128], F32)
    wsTb = P.sb("wsTb", [128, 8, 128], BF16)
    bsb = P.sb("bsbs", [128, 8, 128], F32)
    stg = P.sb("stg", [128, 4, NTK], BF16)
    zf = [P.sb("zf%d" % i, [128, 512], F32) for i in range(2)]
    zsq = P.sb("zsq", [128, 512], F32)
    rt = [P.sb("rt%d" % i, [128, 256], F32) for i in range(4)]
    qr = [P.sb("qr%d" % i, [128, 512], BF16) for i in range(2)]
    vs = [P.sb("vs%d" % i, [128, 512], BF16) for i in range(2)]
    vlf = P.sb("vlf", [128, 512], F32)
    vlb = [P.sb("vlb%d" % i, [128, 512], BF16) for i in range(2)]
    tt = [P.sb("tt%d" % i, [128, 128], F32) for i in range(2)]
    ss = [P.sb("ss%d" % i, [128, 4], F32) for i in range(2)]
    st = [P.sb("st%d" % i, [128, 24], F32) for i in range(2)]
    epsb = P.sb("epsb", [128, 1], F32)
    pT = P.ps("pT", [128, D], BF16)
    pz = [P.ps("pz%d" % i, [128, 512], F32) for i in range(2)]
    pq = [P.ps("pq%d" % i, [128, 512], BF16) for i in range(2)]
    pm = [P.ps("pm%d" % i, [128, 128], F32) for i in range(2)]

    P.op("pool", lambda: nc.gpsimd.memset(epsb[:], EPS), writes=["epsb"])

    def load_mod(cmd, smd):
        P.dma("sp", gam[:], cmd[:, :], writes=["gam"])
        P.dma("act", xs[1][:], gmix[:, :], writes=[("xs", 1)])
        P.dma("sp", sh[:], smd[:, :], writes=["sh"])
        P.op("dve", lambda: nc.vector.scalar_tensor_tensor(out=gam[:], in0=gam[:], scalar=1.0, in1=xs[1][:], op0=ALU.add, op1=ALU.mult),
             reads=["gam", ("xs", 1)], writes=["gam"])

    def front_tile(src_ap, rows, tok0, b):
        P.dma("sp", xs[b][:rows, :], src_ap, writes=[("xs", b)])
        rms_modulate_tile(P, nc, xs[b], ("xs", b), rows, ss[b], ("ss", b), epsb, gam, sh, hb[b], ("hb", b))
        for kc in range(16):
            P.op("pe", lambda kc=kc: nc.tensor.transpose(pT[:, kc * 128:kc * 128 + rows], hb[b][:rows, kc * 128:(kc + 1) * 128], ident[:rows, :rows]),
                 reads=[("hb", b), idk], writes=["pT"])
        P.op("act", lambda: nc.scalar.copy(out=hT[:, :, tok0:tok0 + rows],
                                           in_=pT.rearrange("p (k t) -> p k t", k=16)[:, :, 0:rows]),
             reads=["pT"], writes=["hT"])

    load_mod(cmc, smc)
    front_tile(cx[:, :], 32, TPC, 0)
    load_mod(cm, sm)
    for ti in range(NT):
        front_tile(x[ti * 128:(ti + 1) * 128, :], 128, ti * 128, ti % 2)

    P.dma("sp", big[:, 0:2048].rearrange("p (t f) -> p t f", t=8), cosd.rearrange("(t p) f -> p t f", p=128), writes=["big"])
    P.dma("sp", big[:, 2048:4096].rearrange("p (t f) -> p t f", t=8), sind.rearrange("(t p) f -> p t f", p=128), writes=["big"])
    P.dma("act", lng[:], lngd[:, :], writes=["lng"])
    P.dma("act", lnb[:], lnbd[:, :], writes=["lnb"])
    P.dma("act", wsTf[:], wsTd.rearrange("g q p -> q g p"), writes=["wsTf"])
    P.dma("act", bsb[:], bsbd.rearrange("q (g p) -> q g p", g=8), writes=["bsb"])
    P.op("dve", lambda: nc.vector.tensor_copy(out=wsTb[:], in_=wsTf[:]), reads=["wsTf"], writes=["wsTb"])

    wv = w_in.rearrange("(kc p) n -> p kc n", p=128)
    zc = [0]

    def proj_tok(b, tok0, rows):
        j = zc[0] % 2
        zc[0] += 1
        for kc in range(16):
            P.op("pe", lambda kc=kc: nc.tensor.matmul(pz[j][:rows, :], lhsT=hT[:, kc, tok0:tok0 + rows], rhs=wblk[b][:, kc, :],
                                                       start=(kc == 0), stop=(kc == 15)),
                 reads=["hT", ("wblk", b)], writes=[("pz", j)])
        return j

    for cb in range(10):
        b = cb % 2
        for kq in range(4):
            P.dma("pool", wblk[b][:, kq * 4:(kq + 1) * 4, :], wv[:, kq * 4:(kq + 1) * 4, cb * 512:(cb + 1) * 512], writes=[("wblk", b)])
        if cb < 4:
            isk = cb >= 2
            for ti in range(NT):
                j = proj_tok(b, ti * 128, 128)
                zv = pz[j].rearrange("p (a h x f) -> p a h x f", a=8, h=2, x=2, f=16)
                x1 = zv[:, :, :, 0, :]
                x2 = zv[:, :, :, 1, :]
                cv = cosE[:, ti, :].rearrange("p (a h f) -> p a h f", a=8, h=2)
                sv = sinE[:, ti, :].rearrange("p (a h f) -> p a h f", a=8, h=2)
                r4 = [t.rearrange("p (a h f) -> p a h f", a=8, h=2) for t in rt]
                qv = qr[j].rearrange("p (a h x f) -> p a h x f", a=8, h=2, x=2, f=16)
                zk = ("pz", j)
                P.op("dve", lambda: nc.vector.tensor_tensor(out=r4[0], in0=x1, in1=cv, op=ALU.mult), reads=[zk, "big"], writes=["rt0"])
                P.op("dve", lambda: nc.vector.tensor_tensor(out=r4[1], in0=x2, in1=sv, op=ALU.mult), reads=[zk, "big"], writes=["rt1"])
                P.op("dve", lambda: nc.vector.tensor_tensor(out=r4[2], in0=x2, in1=cv, op=ALU.mult), reads=[zk, "big"], writes=["rt2"])
                P.op("dve", lambda: nc.vector.tensor_tensor(out=r4[3], in0=x1, in1=sv, op=ALU.mult), reads=[zk, "big"], writes=["rt3"])
                P.op("pool", lambda: nc.gpsimd.tensor_tensor(out=qv[:, :, :, 0, :], in0=r4[0], in1=r4[1], op=ALU.subtract),
                     reads=["rt0", "rt1"], writes=[("qr", j)])
                P.op("pool", lambda: nc.gpsimd.tensor_tensor(out=qv[:, :, :, 1, :], in0=r4[2], in1=r4[3], op=ALU.add),
                     reads=["rt2", "rt3"], writes=[("qr", j)])
                for s in range(4):
                    P.op("pe", lambda s=s: nc.tensor.transpose(pq[j][:, s * 128:(s + 1) * 128], qr[j][:, s * 128:(s + 1) * 128], ident[:]),
                         reads=[("qr", j), idk], writes=[("pq", j)])
                P.op("act", lambda: nc.scalar.copy(out=stg[:, :, ti * 128:(ti + 1) * 128], in_=pq[j].rearrange("p (s t) -> p s t", s=4)),
                     reads=[("pq", j)], writes=["stg"])
            if isk:
                j = proj_tok(b, TPC, 32)
                P.op("act", lambda: nc.scalar.copy(out=qr[j][:32, :], in_=pz[j][:32, :]), reads=[("pz", j)], writes=[("qr", j)])
                for s in range(4):
                    P.op("pe", lambda s=s: nc.tensor.transpose(pq[j][:, s * 128:s * 128 + 32], qr[j][:32, s * 128:(s + 1) * 128], ident[:32, :32]),
                         reads=[("qr", j), idk], writes=[("pq", j)])
                P.op("act", lambda: nc.scalar.copy(out=stg[:, :, TPC:TPC + 32], in_=pq[j].rearrange("p (s t) -> p s t", s=4)[:, :, 0:32]),
                     reads=[("pq", j)], writes=["stg"])
            h0 = (cb % 2) * 4
            if isk:
                P.dma("sp", kT[h0:h0 + 4].rearrange("h d t -> d h t"), stg[:], reads=["stg"], writes=["kT"])
            else:
                P.dma("sp", qT[h0:h0 + 4].rearrange("h d t -> d h t"), stg[:, :, 0:TPC], reads=["stg"], writes=["qT"])
        elif cb < 6:
            for ti in range(NT + 1):
                rows = 128 if ti < NT else 32
                tok0 = ti * 128
                j = proj_tok(b, tok0, rows)
                P.op("act", lambda: nc.scalar.copy(out=vs[j][:rows, :], in_=pz[j][:rows, :]), reads=[("pz", j)], writes=[("vs", j)])
                P.dma("sp", vo[tok0:tok0 + rows, (cb - 4) * 512:(cb - 3) * 512], vs[j][:rows, :], reads=[("vs", j)], writes=["vo"])
        elif cb < 8:
            for sub in range(4):
                G = (cb - 6) * 4 + sub
                for tg in range(2):
                    j = zc[0] % 2
                    zc[0] += 1
                    for kc in range(16):
                        P.op("pe", lambda kc=kc: nc.tensor.matmul(pz[j][:], lhsT=wblk[b][:, kc, sub * 128:(sub + 1) * 128],
                                                                   rhs=hT[:, kc, tg * 512:(tg + 1) * 512],
                                                                   start=(kc == 0), stop=(kc == 15)),
                             reads=["hT", ("wblk", b)], writes=[("pz", j)])
                    P.op("act", lambda: nc.scalar.activation(out=uT[:, G, tg * 512:(tg + 1) * 512], in_=pz[j][:], func=AF.Gelu_apprx_tanh),
                         reads=[("pz", j)], writes=["big"])
        else:
            for ti in range(NT):
                tok0 = ti * 128
                j = proj_tok(b, tok0, 128)
                z = zf[j]
                zk = ("zf", j)
                s_ = st[j]
                sk = ("st", j)
                P.op("act", lambda: nc.scalar.activation(out=z[:], in_=pz[j][:], func=AF.Gelu_apprx_tanh), reads=[("pz", j)], writes=[zk])
                P.op("dve", lambda: nc.vector.reduce_sum(out=s_[:, 0:4], in_=z.rearrange("p (g d) -> p g d", g=4), axis=AX.X),
                     reads=[zk], writes=[sk])
                P.op("act", lambda: nc.scalar.activation(out=zsq[:], in_=z[:], func=AF.Square), reads=[zk], writes=["zsq"])
                P.op("dve", lambda: nc.vector.reduce_sum(out=s_[:, 4:8], in_=zsq.rearrange("p (g d) -> p g d", g=4), axis=AX.X),
                     reads=["zsq"], writes=[sk])
                P.op("dve", lambda: nc.vector.tensor_scalar(out=s_[:, 8:12], in0=s_[:, 0:4], scalar1=1.0 / 128, scalar2=None, op0=ALU.mult),
                     reads=[sk], writes=[sk])
                P.op("dve", lambda: nc.vector.tensor_tensor(out=s_[:, 12:16], in0=s_[:, 8:12], in1=s_[:, 8:12], op=ALU.mult),
                     reads=[sk], writes=[sk])
                P.op("dve", lambda: nc.vector.scalar_tensor_tensor(out=s_[:, 16:20], in0=s_[:, 4:8], scalar=1.0 / 128, in1=s_[:, 12:16],
                                                                   op0=ALU.mult, op1=ALU.subtract),
                     reads=[sk], writes=[sk])
                P.op("act", lambda: nc.scalar.activation(out=s_[:, 20:24], in_=s_[:, 16:20], func=AF.Sqrt, bias=epsb[:, 0:1]),
                     reads=[sk, "epsb"], writes=[sk])
                P.op("dve", lambda: nc.vector.reciprocal(out=s_[:, 20:24], in_=s_[:, 20:24]), reads=[sk], writes=[sk])
                for g in range(4):
                    P.op("dve", lambda g=g: nc.vector.tensor_scalar(out=vlf[:, g * 128:(g + 1) * 128], in0=z[:, g * 128:(g + 1) * 128],
                                                                    scalar1=s_[:, 8 + g:9 + g], scalar2=s_[:, 20 + g:21 + g],
                                                                    op0=ALU.subtract, op1=ALU.mult),
                         reads=[zk, sk], writes=["vlf"])
                c0 = (cb - 8) * 512
                P.op("pool", lambda: nc.gpsimd.tensor_tensor(out=vlf[:], in0=vlf[:], in1=lng[:, c0:c0 + 512], op=ALU.mult),
                     reads=["vlf", "lng"], writes=["vlf"])
                P.op("pool", lambda: nc.gpsimd.tensor_tensor(out=vlb[j][:], in0=vlf[:], in1=lnb[:, c0:c0 + 512], op=ALU.add),
                     reads=["vlf", "lnb"], writes=[("vlb", j)])
                for g in range(4):
                    G = (cb - 8) * 4 + g
                    m = (ti * 4 + g) % 2
                    P.op("pe", lambda g=g, G=G, m=m: nc.tensor.matmul(pm[m][:], lhsT=vlb[j][:, g * 128:(g + 1) * 128], rhs=wsTb[:, G, :],
                                                                      start=True, stop=True),
                         reads=[("vlb", j), "wsTb"], writes=[("pm", m)])
                    P.op("dve", lambda G=G, m=m: nc.vector.tensor_tensor(out=tt[m][:], in0=pm[m][:], in1=bsb[:, G, :], op=ALU.add),
                         reads=[("pm", m), "bsb"], writes=[("tt", m)])
                    P.op("dve", lambda g=g, G=G, m=m: nc.vector.tensor_tensor(out=stg[:, g, tok0:tok0 + 128], in0=tt[m][:],
                                                                              in1=uT[:, G, tok0:tok0 + 128], op=ALU.mult),
                         reads=[("tt", m), "big"], writes=["stg"])
            g0 = (cb - 8) * 4
            P.dma("sp", gTo[g0:g0 + 4].rearrange("g d t -> d g t"), stg[:, :, 0:TPC], reads=["stg"], writes=["gTo"])
    return P.finish(["qT", "kT", "vo", "gTo"])


def rope_tables():
    t = np.arange(SEQ)
    row = (t // 64).astype(np.float32)
    col = (t % 64).astype(np.float32)
    inv = (10000.0 ** (-np.arange(0, 32, 2, dtype=np.float32) / 32)).astype(np.float32)
    ang = np.stack([row[:, None] * inv[None, :], col[:, None] * inv[None, :]], axis=1)
    cosE = np.tile(np.cos(ang).astype(np.float32).reshape(SEQ, 1, 32), (1, 8, 1)).reshape(SEQ, 256)
    sinE = np.tile(np.sin(ang).astype(np.float32).reshape(SEQ, 1, 32), (1, 8, 1)).reshape(SEQ, 256)
    return np.ascontiguousarray(cosE), np.ascontiguousarray(sinE)


def run_A(inp, m):
    nc = build_A()
    cosE, sinE = rope_tables()
    m0 = m[0]
    sm_, cm_ = m0[0, 0:D], m0[0, D:2 * D]
    smc_, cmc_ = m0[1, 0:D], m0[1, D:2 * D]
    common = {
        "gmix": bc128(inp["g_norm_mix"][0]), "cm": bc128(cm_), "sm": bc128(sm_), "cmc": bc128(cmc_), "smc": bc128(smc_),
        "w_in": np.ascontiguousarray(inp["w_in"][0]),
        "lng": bc128(inp["sgu_ln_g"][0]), "lnb": bc128(inp["sgu_ln_b"][0]),
        "wsT": np.ascontiguousarray(np.transpose(inp["w_spatial"][0], (0, 2, 1))),
        "bsb": bc128(inp["b_spatial"][0].reshape(-1)),
    }
    maps = []
    for i in range(NCORES):
        d = dict(common)
        d["x"] = np.ascontiguousarray(inp["x"][0, i * TPC:(i + 1) * TPC])
        d["cx"] = np.ascontiguousarray(inp["ctx"][0, i * 32:(i + 1) * 32])
        d["cosE"] = cosE[i * TPC:(i + 1) * TPC]
        d["sinE"] = sinE[i * TPC:(i + 1) * TPC]
        maps.append(d)
    res = run_bass_kernel_spmd(nc, maps, core_ids=list(range(NCORES)))
    return [r for r in res.results]


NKEY = SEQ + 256
NKT = NKEY // 128
LAM_INIT0 = 0.8 - 0.6 * math.exp(-0.3 * 0)


def build_B():
    P = Prog()
    nc = P.nc
    qTd = P.din("qT", [128, SEQ], BF16)
    kTd = P.din("kT", [128, NKEY], BF16)
    vd = P.din("v", [NKEY, 128], BF16)
    lamd = P.din("lamv", [128, 4, 64], F32)
    gsd = P.din("gsub", [128, 1], F32)
    aTo = P.dout("aT", [128, SEQ], BF16)

    qT = P.sb("qTs", [128, SEQ], BF16)
    kT = P.sb("kTs", [128, NKEY], BF16)
    vs = P.sb("vsb", [128, NKT, 128], BF16)
    lamv = P.sb("lamvs", [128, 4, 64], F32)
    lt = P.sb("lt", [128, 2, 64], F32)
    ls = P.sb("ls", [128, 8], F32)
    gs = P.sb("gs", [128, 1], F32)
    epsb = P.sb("epsb", [128, 1], F32)
    onesb = P.sb("onesb", [128, 128], BF16)
    onesf = P.sb("onesf", [128, 128], F32)
    pt = [P.sb("pt%d" % i, [128, 512], BF16) for i in range(3)]
    rz = [P.sb("rz%d" % i, [128, 512], F32) for i in range(2)]
    o = P.sb("o", [128, 512], F32)
    t1 = P.sb("t1", [128, 512], F32)
    osq = P.sb("osq", [128, 512], F32)
    rstd = P.sb("rstd", [128, 512], F32)
    ab = [P.sb("ab%d" % i, [128, 512], BF16) for i in range(2)]
    ps = [P.ps("ps%d" % i, [128, 512], F32) for i in range(3)]
    po = [P.ps("po%d" % i, [128, 512], F32) for i in range(2)]
    pzz = [P.ps("pzz%d" % i, [128, 512], F32) for i in range(2)]

    for i in range(4):
        q = "sp" if i % 2 == 0 else "act"
        P.dma(q, qT[:, i * 2048:(i + 1) * 2048], qTd[:, i * 2048:(i + 1) * 2048], writes=["qT"])
        P.dma(q, kT[:, i * 2112:(i + 1) * 2112], kTd[:, i * 2112:(i + 1) * 2112], writes=["kT"])
    P.dma("sp", vs[:, 0:33, :], vd[0:33 * 128, :].rearrange("(t p) e -> p t e", p=128), writes=["vs"])
    P.dma("act", vs[:, 33:66, :], vd[33 * 128:, :].rearrange("(t p) e -> p t e", p=128), writes=["vs"])
    P.dma("sp", lamv[:], lamd[:, :, :], writes=["lamv"])
    P.dma("sp", gs[:], gsd[:, :], writes=["gs"])
    P.op("pool", lambda: nc.gpsimd.memset(epsb[:], EPS), writes=["epsb"])
    P.op("pool", lambda: nc.gpsimd.memset(onesb[:], 1.0), writes=["onesb"])
    P.op("pool", lambda: nc.gpsimd.memset(onesf[:], 1.0 / 128), writes=["onesf"])
    P.op("dve", lambda: nc.vector.tensor_tensor(out=lt[:, 0, :], in0=lamv[:, 0, :], in1=lamv[:, 1, :], op=ALU.mult), reads=["lamv"], writes=["lt"])
    P.op("dve", lambda: nc.vector.tensor_tensor(out=lt[:, 1, :], in0=lamv[:, 2, :], in1=lamv[:, 3, :], op=ALU.mult), reads=["lamv", "lt"], writes=["lt"])
    P.op("dve", lambda: nc.vector.reduce_sum(out=ls[:, 0:2], in_=lt[:], axis=AX.X), reads=["lt"], writes=["ls"])
    P.op("act", lambda: nc.scalar.activation(out=ls[:, 2:4], in_=ls[:, 0:2], func=AF.Exp), reads=["ls"], writes=["ls"])
    P.op("dve", lambda: nc.vector.tensor_tensor(out=ls[:, 4:5], in0=ls[:, 3:4], in1=ls[:, 2:3], op=ALU.subtract), reads=["ls"], writes=["ls"])
    P.op("dve", lambda: nc.vector.tensor_scalar(out=ls[:, 4:5], in0=ls[:, 4:5], scalar1=-LAM_INIT0, scalar2=None, op0=ALU.add), reads=["ls"], writes=["ls"])
    P.op("dve", lambda: nc.vector.tensor_scalar(out=gs[:], in0=gs[:], scalar1=1.0 - LAM_INIT0, scalar2=None, op0=ALU.mult), reads=["gs"], writes=["gs"])

    it = 0
    for qb in range(SEQ // 512):
        q0 = qb * 512
        steps = [(kt, c) for kt in range(NKT) for c in range(2)]

        def qk(i, kt, c):
            j = i % 3
            P.op("pe", lambda: nc.tensor.matmul(ps[j][:], lhsT=kT[c * 64:(c + 1) * 64, kt * 128:(kt + 1) * 128],
                                                rhs=qT[c * 64:(c + 1) * 64, q0:q0 + 512], start=True, stop=True),
                 reads=["qT", "kT"], writes=[("ps", j)])

        for pre in range(2):
            qk(it + pre, *steps[pre])
        for si, (kt, c) in enumerate(steps):
            j = it % 3
            P.op("act", lambda: nc.scalar.activation(out=pt[j][:], in_=ps[j][:], func=AF.Exp, scale=0.125),
                 reads=[("ps", j)], writes=[("pt", j)])
            if si + 2 < len(steps):
                qk(it + 2, *steps[si + 2])
            P.op("pe", lambda: nc.tensor.matmul(po[c][:], lhsT=vs[:, kt, :], rhs=pt[j][:], start=(kt == 0), stop=(kt == NKT - 1)),
                 reads=["vs", ("pt", j)], writes=[("po", c)])
            P.op("pe", lambda: nc.tensor.matmul(pzz[c][:], lhsT=onesb[:], rhs=pt[j][:], start=(kt == 0), stop=(kt == NKT - 1)),
                 reads=["onesb", ("pt", j)], writes=[("pzz", c)])
            it += 1
        for c in range(2):
            P.op("dve", lambda c=c: nc.vector.reciprocal(out=rz[c][:], in_=pzz[c][:]), reads=[("pzz", c)], writes=[("rz", c)])
        P.op("dve", lambda: nc.vector.tensor_tensor(out=o[:], in0=po[0][:], in1=rz[0][:], op=ALU.mult), reads=[("po", 0), ("rz", 0)], writes=["o"])
        P.op("dve", lambda: nc.vector.tensor_tensor(out=t1[:], in0=po[1][:], in1=rz[1][:], op=ALU.mult), reads=[("po", 1), ("rz", 1)], writes=["t1"])
        P.op("dve", lambda: nc.vector.scalar_tensor_tensor(out=o[:], in0=t1[:], scalar=ls[:, 4:5], in1=o[:], op0=ALU.mult, op1=ALU.add),
             reads=["t1", "ls", "o"], writes=["o"])
        P.op("act", lambda: nc.scalar.activation(out=osq[:], in_=o[:], func=AF.Square), reads=["o"], writes=["osq"])
        jm = it % 3
        it += 1
        P.op("pe", lambda: nc.tensor.matmul(ps[jm][:], lhsT=onesf[:], rhs=osq[:], start=True, stop=True), reads=["onesf", "osq"], writes=[("ps", jm)])
        P.op("act", lambda: nc.scalar.activation(out=rstd[:], in_=ps[jm][:], func=AF.Sqrt, bias=epsb[:, 0:1]), reads=[("ps", jm), "epsb"], writes=["rstd"])
        P.op("dve", lambda: nc.vector.reciprocal(out=rstd[:], in_=rstd[:]), reads=["rstd"], writes=["rstd"])
        a = ab[qb % 2]
        P.op("dve", lambda: nc.vector.scalar_tensor_tensor(out=a[:], in0=o[:], scalar=gs[:, 0:1], in1=rstd[:], op0=ALU.mult, op1=ALU.mult),
             reads=["o", "gs", "rstd"], writes=[("ab", qb % 2)])
        P.dma("sp", aTo[:, q0:q0 + 512], a[:], reads=[("ab", qb % 2)], writes=["aTo"])
    return P.finish(["aTo"])


def run_B(inp, ra):
    nc = build_B()
    lamv = np.stack([inp["lam_q1"][0], inp["lam_k1"][0], inp["lam_q2"][0], inp["lam_k2"][0]], 0)
    lamv = np.ascontiguousarray(np.broadcast_to(lamv[None], (128, 4, 64))).astype(np.float32)
    gsub = np.ascontiguousarray(inp["g_subln"][0].reshape(128, 1))
    maps = []
    for h in range(NCORES):
        qT = np.concatenate([r["qT"][h] for r in ra], axis=1)
        kT = np.concatenate([r["kT"][h][:, :TPC] for r in ra] + [r["kT"][h][:, TPC:] for r in ra], axis=1)
        v = np.concatenate([r["v"][:TPC, h * 128:(h + 1) * 128] for r in ra] + [r["v"][TPC:, h * 128:(h + 1) * 128] for r in ra], axis=0)
        maps.append({"qT": np.ascontiguousarray(qT), "kT": np.ascontiguousarray(kT), "v": np.ascontiguousarray(v),
                     "lamv": lamv, "gsub": gsub})
    res = run_bass_kernel_spmd(nc, maps, core_ids=list(range(NCORES)))
    return [r["aT"] for r in res.results]


def build_proj():
    P = Prog()
    nc = P.nc
    fTd = P.din("fT", [16, 128, TPC], BF16)
    Wd = P.din("W", [D, D], F32)
    xd = P.din("x", [TPC, D], F32)
    gmd = P.din("gm", [128, D], F32)
    gfd = P.din("gffn", [128, D], F32)
    cfd = P.din("cf", [128, D], F32)
    sfd = P.din("sf", [128, D], F32)
    wrd = P.din("wr", [D, 16], F32)
    x1o = P.dout("x1", [TPC, D], F32)
    hfo = P.dout("hf", [TPC, D], BF16)
    affo = P.dout("aff", [TPC, 16], F32)

    identf, idk = make_ident(P, F32)
    Wb = P.sb("Wb", [128, 16, D], BF16)
    fT = [P.sb("fTs%d" % i, [128, 16, 128], BF16) for i in range(2)]
    gm = P.sb("gms", [128, D], F32)
    gam = P.sb("gam", [128, D], F32)
    sh = P.sb("sh", [128, D], F32)
    xs = [P.sb("xs%d" % i, [128, D], F32) for i in range(2)]
    hf32 = P.sb("hf32", [128, D], F32)
    hfb = [P.sb("hfb%d" % i, [128, D], BF16) for i in range(2)]
    hT32 = P.sb("hT32", [128, 16, 128], F32)
    tq = [P.sb("tq%d" % i, [128, 512], F32) for i in range(2)]
    wr = P.sb("wrs", [128, 16, 16], F32)
    ss = [P.sb("ss%d" % i, [128, 4], F32) for i in range(2)]
    sm = [P.sb("smx%d" % i, [128, 4], F32) for i in range(2)]
    ex = [P.sb("ex%d" % i, [128, 16], F32) for i in range(2)]
    epsb = P.sb("epsb", [128, 1], F32)
    pz = [P.ps("pz%d" % i, [128, 512], F32) for i in range(2)]
    pT = P.ps("pT", [128, D], F32)
    pl = P.ps("pl", [128, 16], F32)

    P.op("pool", lambda: nc.gpsimd.memset(epsb[:], EPS), writes=["epsb"])
    wv = Wd.rearrange("(kc p) n -> p kc n", p=128)
    for kc in range(16):
        P.dma("pool", Wb[:, kc, :], wv[:, kc, :], writes=["Wb"])
    P.dma("sp", gm[:], gmd[:, :], writes=["gm"])
    P.dma("sp", gam[:], cfd[:, :], writes=["gam"])
    P.dma("act", xs[1][:], gfd[:, :], writes=[("xs", 1)])
    P.dma("act", sh[:], sfd[:, :], writes=["sh"])
    P.dma("act", wr[:], wrd.rearrange("(kc p) e -> p kc e", p=128), writes=["wr"])
    P.op("dve", lambda: nc.vector.scalar_tensor_tensor(out=gam[:], in0=gam[:], scalar=1.0, in1=xs[1][:], op0=ALU.add, op1=ALU.mult),
         reads=["gam", ("xs", 1)], writes=["gam"])

    zc = 0
    for ti in range(NT):
        b = ti % 2
        tok0 = ti * 128
        xk = ("xs", b)
        P.dma("sp", xs[b][:], xd[tok0:tok0 + 128, :], writes=[xk])
        P.dma("act", fT[b][:], fTd[:, :, tok0:tok0 + 128].rearrange("k d t -> d k t"), writes=[("fT", b)])
        for nb in range(4):
            j = zc % 2
            zc += 1
            for kc in range(16):
                P.op("pe", lambda kc=kc: nc.tensor.matmul(pz[j][:], lhsT=fT[b][:, kc, :], rhs=Wb[:, kc, nb * 512:(nb + 1) * 512],
                                                           start=(kc == 0), stop=(kc == 15)),
                     reads=[("fT", b), "Wb"], writes=[("pz", j)])
            P.op("dve", lambda: nc.vector.tensor_tensor(out=tq[j][:], in0=pz[j][:], in1=gm[:, nb * 512:(nb + 1) * 512], op=ALU.mult),
                 reads=[("pz", j), "gm"], writes=[("tq", j)])
            P.op("pool", lambda: nc.gpsimd.tensor_tensor(out=xs[b][:, nb * 512:(nb + 1) * 512], in0=xs[b][:, nb * 512:(nb + 1) * 512],
                                                         in1=tq[j][:], op=ALU.add),
                 reads=[xk, ("tq", j)], writes=[xk])
        P.dma("sp", x1o[tok0:tok0 + 128, :], xs[b][:], reads=[xk], writes=["x1o"])
        s_ = ss[b]
        sk = ("ss", b)
        P.op("act", lambda: nc.scalar.activation(out=hfb[b][:], in_=xs[b][:], func=AF.Square, accum_out=s_[:, 0:1]),
             reads=[xk], writes=[("hfb", b), sk])
        P.op("act", lambda: nc.scalar.activation(out=s_[:, 1:2], in_=s_[:, 0:1], func=AF.Sqrt, scale=1.0 / D, bias=epsb[:, 0:1]),
             reads=[sk, "epsb"], writes=[sk])
        P.op("dve", lambda: nc.vector.reciprocal(out=s_[:, 2:3], in_=s_[:, 1:2]), reads=[sk], writes=[sk])
        P.op("dve", lambda: nc.vector.scalar_tensor_tensor(out=hf32[:], in0=xs[b][:], scalar=s_[:, 2:3], in1=gam[:], op0=ALU.mult, op1=ALU.mult),
             reads=[xk, sk, "gam"], writes=["hf32"])
        P.op("pool", lambda: nc.gpsimd.tensor_tensor(out=hf32[:], in0=hf32[:], in1=sh[:], op=ALU.add), reads=["hf32", "sh"], writes=["hf32"])
        P.op("act", lambda: nc.scalar.copy(out=hfb[b][:], in_=hf32[:]), reads=["hf32"], writes=[("hfb", b)])
        P.dma("sp", hfo[tok0:tok0 + 128, :], hfb[b][:], reads=[("hfb", b)], writes=["hfo"])
        for kc in range(16):
            P.op("pe", lambda kc=kc: nc.tensor.transpose(pT[:, kc * 128:(kc + 1) * 128], hf32[:, kc * 128:(kc + 1) * 128], identf[:]),
                 reads=["hf32", idk], writes=["pT"])
        P.op("dve", lambda: nc.vector.tensor_copy(out=hT32[:], in_=pT.rearrange("p (k t) -> p k t", k=16)), reads=["pT"], writes=["hT32"])
        for kc in range(16):
            P.op("pe", lambda kc=kc: nc.tensor.matmul(pl[:], lhsT=hT32[:, kc, :], rhs=wr[:, kc, :], start=(kc == 0), stop=(kc == 15)),
                 reads=["hT32", "wr"], writes=["pl"])
        m_ = sm[b]
        mk = ("sm", b)
        P.op("dve", lambda: nc.vector.reduce_max(out=m_[:, 0:1], in_=pl[:], axis=AX.X), reads=["pl"], writes=[mk])
        P.op("dve", lambda: nc.vector.tensor_scalar(out=m_[:, 1:2], in0=m_[:, 0:1], scalar1=-1.0, scalar2=None, op0=ALU.mult), reads=[mk], writes=[mk])
        P.op("act", lambda: nc.scalar.activation(out=ex[b][:], in_=pl[:], func=AF.Exp, bias=m_[:, 1:2], accum_out=m_[:, 2:3]),
             reads=["pl", mk], writes=[("ex", b), mk])
        P.op("dve", lambda: nc.vector.reciprocal(out=m_[:, 3:4], in_=m_[:, 2:3]), reads=[mk], writes=[mk])
        P.op("dve", lambda: nc.vector.tensor_scalar(out=ex[b][:], in0=ex[b][:], scalar1=m_[:, 3:4], scalar2=None, op0=ALU.mult),
             reads=[("ex", b), mk], writes=[("ex", b)])
        P.dma("sp", affo[tok0:tok0 + 128, :], ex[b][:], reads=[("ex", b)], writes=["affo"])
    return P.finish(["x1o", "hfo", "affo"])


_PROJ_NC = [None]


def run_proj(fT_list, W, x_full, gm_, gffn, cf_, sf_, w_r):
    nc = build_proj()
    common = {"W": np.ascontiguousarray(W), "gm": bc128(gm_), "gffn": bc128(gffn), "cf": bc128(cf_), "sf": bc128(sf_),
              "wr": np.ascontiguousarray(w_r)}
    maps = []
    for i in range(NCORES):
        d = dict(common)
        d["fT"] = np.ascontiguousarray(fT_list[i])
        d["x"] = np.ascontiguousarray(x_full[i * TPC:(i + 1) * TPC])
        maps.append(d)
    res = run_bass_kernel_spmd(nc, maps, core_ids=list(range(NCORES)))
    x1 = np.concatenate([r["x1"] for r in res.results], 0)
    hf = np.concatenate([r["hf"] for r in res.results], 0)
    aff = np.concatenate([r["aff"] for r in res.results], 0)
    return x1, hf, aff


CAP = 1024
FE = 1024
NBIS = 32
OOB = 30000.0


def build_E():
    P = Prog()
    nc = P.nc
    affd = P.din("affT", [128, 2, 64], F32)
    ebd = P.din("ebase", [128, 2], F32)
    hfd = P.din("hf", [SEQ, D], BF16)
    wgd = P.din("wg", [2, D, FE], F32)
    wud = P.din("wu", [2, D, FE], F32)
    wdd = P.din("wd", [2, FE, D], F32)
    Yo = P.dout("Y", [2, CAP, D], F32)
    sloto = P.dout("slot", [128, 2, 64], I32)

    ident, idk = make_ident(P)
    onesf = P.sb("onesf", [128, 128], F32)
    UT = P.sb("UT", [128, 128], F32)
    Lb = P.sb("Lb", [128, 128], F32)
    iot_i = P.sb("iot_i", [128, 1024], I32)
    iot = P.sb("iot", [128, 1024], F32)
    jp_i = P.sb("jp_i", [128, 64, 2], I32)
    jp = P.sb("jp", [128, 64, 2], BF16)
    a = P.sb("a", [128, 2, 64], F32)
    eb = P.sb("eb", [128, 2], F32)
    msk = P.sb("msk", [128, 2, 64], F32)
    bs = P.sb("bs", [128, 16], F32)
    exs = P.sb("exs", [128, 128], F32)
    ct = P.sb("ct", [128, 1], F32)
    CB = P.sb("CB", [128, 128], F32)
    rk = P.sb("rk", [128, 2, 64], F32)
    sg = P.sb("sg", [128, 2, 64], F32)
    sgi = P.sb("sgi", [128, 2, 64], I32)
    sel = [P.sb("sel%d" % i, [128, 1024], BF16) for i in range(2)]
    idxf = P.sb("idxf", [128, 2, 8], F32)
    pselS = P.sb("pselS", [128, 8, 2], F32)
    zb = P.sb("zb", [128, 128], BF16)
    idxi = P.sb("idxi", [128, 2, 8], I32)
    xg = [P.sb("xg%d" % i, [128, D], BF16) for i in range(2)]
    xsT = P.sb("xsT", [128, 16, CAP], BF16)
    hT = P.sb("hTe", [128, 8, CAP], BF16)
    wgb = [P.sb("wgb%d" % i, [128, 16, 256], BF16) for i in range(2)]
    wub = [P.sb("wub%d" % i, [128, 16, 256], BF16) for i in range(2)]
    wdb = [P.sb("wdb%d" % i, [128, 8, 512], BF16) for i in range(2)]
    sa = [P.sb("sa%d" % i, [128, 512], F32) for i in range(2)]
    ys = [P.sb("ys%d" % i, [128, 512], F32) for i in range(2)]
    pa = [P.ps("pa%d" % i, [128, 512], F32) for i in range(2)]
    pu = [P.ps("pu%d" % i, [128, 512], F32) for i in range(2)]
    py = [P.ps("py%d" % i, [128, 512], F32) for i in range(2)]
    pT = P.ps("pT", [128, D], BF16)

    P.op("pool", lambda: nc.gpsimd.memset(onesf[:], 1.0), writes=["onesf"])
    P.op("pool", lambda: nc.gpsimd.memset(UT[:], 1.0), writes=["UT"])
    P.op("pool", lambda: nc.gpsimd.affine_select(out=UT[:], in_=UT[:], pattern=[[1, 128]], compare_op=ALU.is_ge, fill=0.0, base=0,
                                                 channel_multiplier=-1), reads=["UT"], writes=["UT"])
    P.op("pool", lambda: nc.gpsimd.memset(Lb[:], 1.0), writes=["Lb"])
    P.op("pool", lambda: nc.gpsimd.affine_select(out=Lb[:], in_=Lb[:], pattern=[[1, 128]], compare_op=ALU.is_gt, fill=0.0, base=0,
                                                 channel_multiplier=-1), reads=["Lb"], writes=["Lb"])
    P.op("pool", lambda: nc.gpsimd.memset(Lb[0:64, 64:128], 0.0), reads=["Lb"], writes=["Lb"])
    P.op("pool", lambda: nc.gpsimd.iota(iot_i[:], pattern=[[1, 1024]], base=0, channel_multiplier=0), writes=["iot_i"])
    P.op("dve", lambda: nc.vector.tensor_copy(out=iot[:], in_=iot_i[:]), reads=["iot_i"], writes=["iot"])
    P.op("pool", lambda: nc.gpsimd.iota(jp_i[:, :, 0], pattern=[[1, 64]], base=0, channel_multiplier=0), writes=["jp_i"])
    P.op("pool", lambda: nc.gpsimd.iota(jp_i[:, :, 1], pattern=[[0, 64]], base=0, channel_multiplier=1), reads=["jp_i"], writes=["jp_i"])
    P.op("dve", lambda: nc.vector.tensor_copy(out=jp[:], in_=jp_i[:]), reads=["jp_i"], writes=["jp"])
    P.dma("sp", a[:], affd[:, :, :], writes=["a"])
    P.dma("sp", eb[:], ebd[:, :], writes=["eb"])
    P.op("pool", lambda: nc.gpsimd.memset(zb[:], 0.0), writes=["zb"])

    lo, hi, mid, cnt, ge, dd, tt_ = (bs[:, 0:2], bs[:, 2:4], bs[:, 4:6], bs[:, 6:8], bs[:, 8:10], bs[:, 10:12], bs[:, 12:14])
    P.op("dve", lambda: nc.vector.memset(bs[:, 0:2], 0.0), writes=["bs"])
    P.op("dve", lambda: nc.vector.memset(bs[:, 2:4], 1.001), reads=["bs"], writes=["bs"])
    tot = pa[0][:, 0:2]

    def V(fn, reads=("bs",), writes=("bs",)):
        P.op("dve", fn, reads=list(reads), writes=list(writes))

    for itb in range(NBIS):
        V(lambda: nc.vector.tensor_tensor(out=mid, in0=lo, in1=hi, op=ALU.add))
        V(lambda: nc.vector.tensor_scalar(out=mid, in0=mid, scalar1=0.5, scalar2=None, op0=ALU.mult))
        V(lambda: nc.vector.tensor_tensor(out=msk[:], in0=a[:], in1=mid.unsqueeze(2).to_broadcast([128, 2, 64]), op=ALU.is_ge),
          reads=["a", "bs"], writes=["msk"])
        V(lambda: nc.vector.reduce_sum(out=cnt, in_=msk[:], axis=AX.X), reads=["msk", "bs"], writes=["bs"])
        P.op("pe", lambda: nc.tensor.matmul(tot, lhsT=onesf[:], rhs=cnt, start=True, stop=True), reads=["onesf", "bs"], writes=[("pa", 0)])
        V(lambda: nc.vector.tensor_scalar(out=ge, in0=tot, scalar1=float(CAP) - 0.5, scalar2=None, op0=ALU.is_ge), reads=[("pa", 0), "bs"])
        V(lambda: nc.vector.tensor_tensor(out=dd, in0=mid, in1=lo, op=ALU.subtract))
        V(lambda: nc.vector.tensor_tensor(out=tt_, in0=dd, in1=ge, op=ALU.mult))
        V(lambda: nc.vector.tensor_tensor(out=lo, in0=lo, in1=tt_, op=ALU.add))
        V(lambda: nc.vector.tensor_tensor(out=dd, in0=mid, in1=hi, op=ALU.subtract))
        V(lambda: nc.vector.tensor_tensor(out=tt_, in0=dd, in1=ge, op=ALU.mult))
        V(lambda: nc.vector.tensor_tensor(out=hi, in0=mid, in1=tt_, op=ALU.subtract))
    V(lambda: nc.vector.tensor_tensor(out=msk[:], in0=a[:], in1=lo.unsqueeze(2).to_broadcast([128, 2, 64]), op=ALU.is_ge),
      reads=["a", "bs"], writes=["msk"])

    mflat = msk.rearrange("p e j -> p (e j)")
    pc = pa[1][:, 0:128]
    pex = pu[0][:, 0:128]
    pct = pu[1][:, 0:1]
    P.op("pe", lambda: nc.tensor.matmul(pc, lhsT=UT[:], rhs=mflat, start=True, stop=True), reads=["UT", "msk"], writes=[("pa", 1)])
    P.op("pe", lambda: nc.tensor.matmul(pct, lhsT=mflat, rhs=onesf[:, 0:1], start=True, stop=True), reads=["onesf", "msk"], writes=[("pu", 1)])
    P.op("dve", lambda: nc.vector.tensor_copy(out=ct[:], in_=pct), reads=[("pu", 1)], writes=["ct"])
    P.op("dve", lambda: nc.vector.tensor_scalar(out=CB[:], in0=onesf[:], scalar1=ct[:, 0:1], scalar2=None, op0=ALU.mult),
         reads=["onesf", "ct"], writes=["CB"])
    P.op("pe", lambda: nc.tensor.matmul(pex, lhsT=CB[:], rhs=Lb[:], start=True, stop=True), reads=["CB", "Lb"], writes=[("pu", 0)])
    P.op("dve", lambda: nc.vector.tensor_copy(out=exs[:], in_=pex), reads=[("pu", 0)], writes=["exs"])
    rkf = rk.rearrange("p e j -> p (e j)")
    P.op("dve", lambda: nc.vector.tensor_tensor(out=rkf, in0=pc, in1=exs[:], op=ALU.add), reads=[("pa", 1), "exs"], writes=["rk"])
    P.op("dve", lambda: nc.vector.tensor_tensor(out=rkf, in0=rkf, in1=mflat, op=ALU.mult), reads=["rk", "msk"], writes=["rk"])
    P.op("dve", lambda: nc.vector.tensor_scalar(out=rkf, in0=rkf, scalar1=-1.0, scalar2=None, op0=ALU.add), reads=["rk"], writes=["rk"])
    P.op("dve", lambda: nc.vector.tensor_tensor(out=sg[:], in0=rk[:], in1=eb[:, :].unsqueeze(2).to_broadcast([128, 2, 64]), op=ALU.add),
         reads=["rk", "eb"], writes=["sg"])
    P.op("dve", lambda: nc.vector.tensor_scalar(out=sg[:], in0=sg[:], scalar1=-OOB, scalar2=None, op0=ALU.add), reads=["sg"], writes=["sg"])
    P.op("dve", lambda: nc.vector.tensor_tensor(out=sg[:], in0=sg[:], in1=msk[:], op=ALU.mult), reads=["sg", "msk"], writes=["sg"])
    P.op("dve", lambda: nc.vector.tensor_scalar(out=sg[:], in0=sg[:], scalar1=OOB, scalar2=None, op0=ALU.add), reads=["sg"], writes=["sg"])
    P.op("dve", lambda: nc.vector.tensor_copy(out=sgi[:], in_=sg[:]), reads=["sg"], writes=["sgi"])
    P.dma("sp", sloto[:, :, :], sgi[:], reads=["sgi"], writes=["sloto"])

    for e in range(2):
        psel = py[e][:, 0:16].rearrange("p (s c) -> p s c", c=2)
        P.op("pe", lambda: nc.tensor.matmul(py[e][:, 0:16], lhsT=zb[:, 0:128], rhs=zb[:, 0:16], start=True, stop=False),
             reads=["zb"], writes=[("py", e)])
        for j in range(64):
            sb_ = sel[j % 2]
            P.op("dve", lambda: nc.vector.tensor_scalar(out=sb_[:], in0=iot[:], scalar1=rk[:, e, j:j + 1], scalar2=None, op0=ALU.is_equal),
                 reads=["iot", "rk"], writes=[("sel", j % 2)])
            for s in range(8):
                P.op("pe", lambda s=s: nc.tensor.matmul(psel[:, s, :], lhsT=sb_[:, s * 128:(s + 1) * 128], rhs=jp[:, j, :],
                                                         start=False, stop=(j == 63), skip_group_check=True),
                     reads=[("sel", j % 2), "jp"], writes=[("py", e)])
        P.op("dve", lambda: nc.vector.tensor_copy(out=pselS[:], in_=psel), reads=[("py", e)], writes=["pselS"])
        P.op("dve", lambda: nc.vector.scalar_tensor_tensor(out=idxf[:, e, :], in0=pselS[:, :, 0], scalar=128.0, in1=pselS[:, :, 1],
                                                           op0=ALU.mult, op1=ALU.add),
             reads=["pselS"], writes=["idxf"])
    P.op("dve", lambda: nc.vector.tensor_copy(out=idxi[:], in_=idxf[:]), reads=["idxf"], writes=["idxi"])

    gi = 0
    for e in range(2):
        for s in range(8):
            g_ = gi % 2
            gi += 1
            P.idma(xg[g_][:], None, hfd[:, :], bass.IndirectOffsetOnAxis(ap=idxi[:, e, s:s + 1], axis=0),
                   reads=["idxi"], writes=[("xg", g_)])
            for kc in range(16):
                P.op("pe", lambda kc=kc: nc.tensor.transpose(pT[:, kc * 128:(kc + 1) * 128], xg[g_][:, kc * 128:(kc + 1) * 128], ident[:]),
                     reads=[("xg", g_), idk], writes=["pT"])
            P.op("act", lambda: nc.scalar.copy(out=xsT[:, :, s * 128:(s + 1) * 128], in_=pT.rearrange("p (k t) -> p k t", k=16)),
                 reads=["pT"], writes=["xsT"])
        wgv = wgd[e].rearrange("(kc p) f -> p kc f", p=128)
        wuv = wud[e].rearrange("(kc p) f -> p kc f", p=128)
        wdv = wdd[e].rearrange("(fc p) d -> p fc d", p=128)
        zi = 0
        for f2 in range(4):
            b = f2 % 2
            for kq in range(4):
                P.dma("pool", wgb[b][:, kq * 4:(kq + 1) * 4, :], wgv[:, kq * 4:(kq + 1) * 4, f2 * 256:(f2 + 1) * 256], writes=[("wgb", b)])
                P.dma("pool", wub[b][:, kq * 4:(kq + 1) * 4, :], wuv[:, kq * 4:(kq + 1) * 4, f2 * 256:(f2 + 1) * 256], writes=[("wub", b)])
            for fs in range(2):
                fc = f2 * 2 + fs
                for hh in range(2):
                    z = zi % 2
                    zi += 1
                    for kc in range(16):
                        P.op("pe", lambda kc=kc: nc.tensor.matmul(pa[z][:], lhsT=wgb[b][:, kc, fs * 128:(fs + 1) * 128],
                                                                   rhs=xsT[:, kc, hh * 512:(hh + 1) * 512], start=(kc == 0), stop=(kc == 15)),
                             reads=[("wgb", b), "xsT"], writes=[("pa", z)])
                    for kc in range(16):
                        P.op("pe", lambda kc=kc: nc.tensor.matmul(pu[z][:], lhsT=wub[b][:, kc, fs * 128:(fs + 1) * 128],
                                                                   rhs=xsT[:, kc, hh * 512:(hh + 1) * 512], start=(kc == 0), stop=(kc == 15)),
                             reads=[("wub", b), "xsT"], writes=[("pu", z)])
                    P.op("act", lambda: nc.scalar.activation(out=sa[z][:], in_=pa[z][:], func=AF.Silu), reads=[("pa", z)], writes=[("sa", z)])
                    P.op("dve", lambda: nc.vector.tensor_tensor(out=hT[:, fc, hh * 512:(hh + 1) * 512], in0=pu[z][:], in1=sa[z][:], op=ALU.mult),
                         reads=[("pu", z), ("sa", z)], writes=["hT"])
        yi = 0
        for dc in range(4):
            b = dc % 2
            for fq in range(2):
                P.dma("pool", wdb[b][:, fq * 4:(fq + 1) * 4, :], wdv[:, fq * 4:(fq + 1) * 4, dc * 512:(dc + 1) * 512], writes=[("wdb", b)])
            for s in range(8):
                z = yi % 2
                yi += 1
                for fc in range(8):
                    P.op("pe", lambda fc=fc: nc.tensor.matmul(py[z][:], lhsT=hT[:, fc, s * 128:(s + 1) * 128], rhs=wdb[b][:, fc, :],
                                                               start=(fc == 0), stop=(fc == 7)),
                         reads=["hT", ("wdb", b)], writes=[("py", z)])
                P.op("act", lambda: nc.scalar.copy(out=ys[z][:], in_=py[z][:]), reads=[("py", z)], writes=[("ys", z)])
                P.dma("sp", Yo[e, s * 128:(s + 1) * 128, dc * 512:(dc + 1) * 512], ys[z][:], reads=[("ys", z)], writes=["Yo"])
    return P.finish(["Yo", "sloto"])


def run_E(aff, hf, wg, wu, wd):
    nc = build_E()
    hf = np.ascontiguousarray(hf)
    maps = []
    for c in range(NCORES):
        es = slice(2 * c, 2 * c + 2)
        affT = np.ascontiguousarray(aff[:, es].reshape(64, 128, 2).transpose(1, 2, 0))
        ebase = np.ascontiguousarray(np.broadcast_to(np.array([[2 * c * CAP, (2 * c + 1) * CAP]], np.float32), (128, 2)))
        maps.append({"affT": affT, "ebase": ebase, "hf": hf, "wg": np.ascontiguousarray(wg[es]), "wu": np.ascontiguousarray(wu[es]),
                     "wd": np.ascontiguousarray(wd[es])})
    res = run_bass_kernel_spmd(nc, maps, core_ids=list(range(NCORES)))
    Yall = np.concatenate([r["Y"].reshape(2 * CAP, D) for r in res.results], 0)
    slots = np.concatenate([r["slot"].transpose(2, 0, 1).reshape(SEQ, 2) for r in res.results], 1)
    return Yall, np.ascontiguousarray(slots)


NYROWS = 16 * CAP


def combine_setup(P):
    nc = P.nc
    c = {}
    c["x1d"] = P.din("x1", [TPC, D], F32)
    c["Yd"] = P.din("Yall", [NYROWS, D], F32)
    c["sld"] = P.din("slots", [TPC, 16], I32)
    c["afd"] = P.din("aff", [TPC, 16], F32)
    c["gfd"] = P.din("gf", [128, D], F32)
    c["sl"] = P.sb("sl", [128, NT, 16], I32)
    c["af"] = P.sb("af", [128, NT, 16], F32)
    c["gf"] = P.sb("gfs", [128, D], F32)
    c["gt"] = [P.sb("gt%d" % i, [128, D], F32) for i in range(3)]
    c["acc"] = [P.sb("acc%d" % i, [128, D], F32) for i in range(2)]
    c["xs"] = [P.sb("cxs%d" % i, [128, D], F32) for i in range(2)]
    P.dma("sp", c["sl"][:], c["sld"].rearrange("(t p) e -> p t e", p=128), writes=["sl"])
    P.dma("sp", c["af"][:], c["afd"].rearrange("(t p) e -> p t e", p=128), writes=["af"])
    P.dma("sp", c["gf"][:], c["gfd"][:, :], writes=["gfs"])
    c["gi"] = 0
    c["breg"] = nc.gpsimd.to_reg(NYROWS - 1)
    return c


def combine_tile(P, c, ti):
    nc = P.nc
    b = ti % 2
    acc = c["acc"][b]
    ak = ("acc", b)
    xs = c["xs"][b]
    xk = ("cxs", b)
    P.dma("sp", xs[:], c["x1d"][ti * 128:(ti + 1) * 128, :], writes=[xk])
    for e in range(16):
        g = c["gi"] % 3
        c["gi"] += 1
        gt = c["gt"][g]
        gk = ("gt", g)
        P.op("pool", lambda: nc.gpsimd.memset(gt[:], 0.0), writes=[gk])
        P.idma(gt[:], None, c["Yd"][:, :], bass.IndirectOffsetOnAxis(ap=c["sl"][:, ti, e:e + 1], axis=0),
               reads=["sl"], writes=[gk], semkey=("dg", g), bounds_check=c["breg"], oob_is_err=False)
        if e == 0:
            P.op("dve", lambda: nc.vector.tensor_scalar(out=acc[:], in0=gt[:], scalar1=c["af"][:, ti, e:e + 1], scalar2=None, op0=ALU.mult),
                 reads=[gk, "af"], writes=[ak])
        else:
            P.op("dve", lambda: nc.vector.scalar_tensor_tensor(out=acc[:], in0=gt[:], scalar=c["af"][:, ti, e:e + 1], in1=acc[:],
                                                               op0=ALU.mult, op1=ALU.add),
                 reads=[gk, "af", ak], writes=[ak])
    P.op("dve", lambda: nc.vector.tensor_tensor(out=acc[:], in0=acc[:], in1=c["gf"][:], op=ALU.mult), reads=[ak, "gfs"], writes=[ak])
    P.op("pool", lambda: nc.gpsimd.tensor_tensor(out=xs[:], in0=xs[:], in1=acc[:], op=ALU.add), reads=[xk, ak], writes=[xk])
    return xs, xk


def build_F():
    P = Prog()
    nc = P.nc
    c = combine_setup(P)
    gmixd = P.din("gmix", [128, D], F32)
    cmd = P.din("cm", [128, D], F32)
    smd = P.din("sm", [128, D], F32)
    Cd = P.din("Cch", [512, 512], BF16)
    Sd = P.din("Sch", [512, 512], BF16)
    x2o = P.dout("x2", [TPC, D], F32)
    Ao = P.dout("A", [TPC, D], BF16)
    Bo = P.dout("B", [TPC, D], BF16)
    ident, idk = make_ident(P)
    gam = P.sb("gam", [128, D], F32)
    sh = P.sb("sh", [128, D], F32)
    hb = [P.sb("hb%d" % i, [128, D], BF16) for i in range(2)]
    hTt = P.sb("hTt", [128, 16, 128], BF16)
    Cs = P.sb("Cs", [128, 4, 512], BF16)
    Ss = P.sb("Ss", [128, 4, 512], BF16)
    ao = [P.sb("ao%d" % i, [128, 512], BF16) for i in range(2)]
    ss = [P.sb("ss%d" % i, [128, 4], F32) for i in range(2)]
    epsb = P.sb("epsb", [128, 1], F32)
    pT = P.ps("pT", [128, D], BF16)
    pz = [P.ps("pz%d" % i, [128, 512], F32) for i in range(2)]
    P.op("pool", lambda: nc.gpsimd.memset(epsb[:], EPS), writes=["epsb"])
    P.dma("act", gam[:], cmd[:, :], writes=["gam"])
    P.dma("act", c["gt"][0][:], gmixd[:, :], writes=[("gt", 0)], semkey=("dg", 0))
    P.dma("act", sh[:], smd[:, :], writes=["sh"])
    P.dma("act", Cs[:], Cd.rearrange("(k p) n -> p k n", p=128), writes=["Cs"])
    P.dma("act", Ss[:], Sd.rearrange("(k p) n -> p k n", p=128), writes=["Ss"])
    P.op("dve", lambda: nc.vector.scalar_tensor_tensor(out=gam[:], in0=gam[:], scalar=1.0, in1=c["gt"][0][:], op0=ALU.add, op1=ALU.mult),
         reads=["gam", ("gt", 0)], writes=["gam"])
    zc = 0
    for ti in range(NT):
        b = ti % 2
        xs, xk = combine_tile(P, c, ti)
        P.dma("sp", x2o[ti * 128:(ti + 1) * 128, :], xs[:], reads=[xk], writes=["x2o"])
        rms_modulate_tile(P, nc, xs, xk, 128, ss[b], ("ss", b), epsb, gam, sh, hb[b], ("hb", b))
        for kc in range(16):
            P.op("pe", lambda kc=kc: nc.tensor.transpose(pT[:, kc * 128:(kc + 1) * 128], hb[b][:, kc * 128:(kc + 1) * 128], ident[:]),
                 reads=[("hb", b), idk], writes=["pT"])
        P.op("act", lambda: nc.scalar.copy(out=hTt[:], in_=pT.rearrange("p (k t) -> p k t", k=16)), reads=["pT"], writes=["hTt"])
        for g in range(4):
            for (W_, wk, outd) in ((Cs, "Cs", Ao), (Ss, "Ss", Bo)):
                j = zc % 2
                zc += 1
                for k in range(4):
                    P.op("pe", lambda k=k: nc.tensor.matmul(pz[j][:], lhsT=hTt[:, g * 4 + k, :], rhs=W_[:, k, :], start=(k == 0), stop=(k == 3)),
                         reads=["hTt", wk], writes=[("pz", j)])
                P.op("act", lambda: nc.scalar.copy(out=ao[j][:], in_=pz[j][:]), reads=[("pz", j)], writes=[("ao", j)])
                P.dma("sp", outd[ti * 128:(ti + 1) * 128, g * 512:(g + 1) * 512], ao[j][:], reads=[("ao", j)], writes=["AB"])
    return P.finish(["x2o", "AB"])


def bf16_np(a):
    import ml_dtypes
    return np.ascontiguousarray(np.asarray(a, np.float32).astype(ml_dtypes.bfloat16))


def dft_consts():
    c = np.arange(512)
    ang = 2 * np.pi * np.outer(c, c) / 512
    Cch = np.cos(ang) / math.sqrt(512)
    Sch = np.sin(ang) / math.sqrt(512)
    n1 = np.arange(128)
    a1 = 2 * np.pi * np.outer(n1, n1) / 128
    Cc = np.cos(a1) / math.sqrt(128)
    Ssn = np.sin(a1) / math.sqrt(128)
    n2 = np.arange(64)[:, None, None]
    k1 = np.arange(128)[None, :, None]
    k2 = np.arange(64)[None, None, :]
    th = 2 * np.pi * (n2 * k1 / 8192.0 + n2 * k2 / 64.0)
    Hr = np.cos(th) / 8.0
    Hni = np.sin(th) / 8.0
    return dict(Cch=bf16_np(Cch), Sch=bf16_np(Sch), Cc=bf16_np(Cc), nSs=bf16_np(-Ssn), nCc=bf16_np(-Cc), Hr=bf16_np(Hr), Hni=bf16_np(Hni))


def run_F(x1, Yall, slots, aff, gf_, gmix, cm_, sm_):
    nc = build_F()
    k = dft_consts()
    common = {"Yall": Yall, "gf": bc128(gf_), "gmix": bc128(gmix), "cm": bc128(cm_), "sm": bc128(sm_), "Cch": k["Cch"], "Sch": k["Sch"]}
    maps = []
    for i in range(NCORES):
        d = dict(common)
        sl = slice(i * TPC, (i + 1) * TPC)
        d["x1"] = np.ascontiguousarray(x1[sl])
        d["slots"] = np.ascontiguousarray(slots[sl])
        d["aff"] = np.ascontiguousarray(aff[sl])
        maps.append(d)
    res = run_bass_kernel_spmd(nc, maps, core_ids=list(range(NCORES)))
    x2 = np.concatenate([r["x2"] for r in res.results], 0)
    A = np.concatenate([r["A"] for r in res.results], 0)
    B = np.concatenate([r["B"] for r in res.results], 0)
    return x2, A, B


CPC = D // NCORES


def build_G():
    P = Prog()
    nc = P.nc
    zAd = P.din("zA", [128, 64, CPC], BF16)
    zBd = P.din("zB", [128, 64, CPC], BF16)
    Ccd = P.din("Cc", [128, 128], BF16)
    nSsd = P.din("nSs", [128, 128], BF16)
    nCcd = P.din("nCc", [128, 128], BF16)
    Hrd = P.din("Hr", [64, 128, 64], BF16)
    Hnid = P.din("Hni", [64, 128, 64], BF16)
    YTo = P.dout("YT", [CPC, SEQ], BF16)
    zA = P.sb("zAs", [128, 64, 128], BF16)
    zB = P.sb("zBs", [128, 64, 128], BF16)
    Cc = P.sb("Ccs", [128, 128], BF16)
    nSs = P.sb("nSss", [128, 128], BF16)
    nCc = P.sb("nCcs", [128, 128], BF16)
    Hr = P.sb("Hrs", [64, 128, 64], BF16)
    Hni = P.sb("Hnis", [64, 128, 64], BF16)
    Tr = P.sb("Tr", [64, 128, 128], BF16)
    Ti = P.sb("Ti", [64, 128, 128], BF16)
    YTs = P.sb("YTs", [128, 64, 128], BF16)
    pTr = [P.ps("pTr%d" % i, [64, 4, 128], F32) for i in range(2)]
    pTi = [P.ps("pTi%d" % i, [64, 4, 128], F32) for i in range(2)]
    pY = [P.ps("pY%d" % i, [128, 8, 64], F32) for i in range(2)]
    P.dma("sp", Cc[:], Ccd[:, :], writes=["Cc"])
    P.dma("sp", nSs[:], nSsd[:, :], writes=["nSs"])
    P.dma("sp", nCc[:], nCcd[:, :], writes=["nCc"])
    P.dma("act", Hr[:], Hrd[:, :, :], writes=["Hr"])
    P.dma("act", Hni[:], Hnid[:, :, :], writes=["Hni"])
    for hc in range(2):
        c0 = hc * 128
        P.dma("sp", zA[:], zAd[:, :, c0:c0 + 128], writes=["zA"])
        P.dma("act", zB[:], zBd[:, :, c0:c0 + 128], writes=["zB"])
        for cb in range(32):
            j = cb % 2
            for cc in range(4):
                c = cb * 4 + cc
                P.op("pe", lambda: nc.tensor.matmul(pTr[j][:, cc, :], lhsT=zA[:, :, c], rhs=Cc[:], start=True, stop=False),
                     reads=["zA", "Cc"], writes=[("pTr", j)])
                P.op("pe", lambda: nc.tensor.matmul(pTr[j][:, cc, :], lhsT=zB[:, :, c], rhs=nSs[:], start=False, stop=True),
                     reads=["zB", "nSs"], writes=[("pTr", j)])
                P.op("pe", lambda: nc.tensor.matmul(pTi[j][:, cc, :], lhsT=zB[:, :, c], rhs=nCc[:], start=True, stop=False),
                     reads=["zB", "nCc"], writes=[("pTi", j)])
                P.op("pe", lambda: nc.tensor.matmul(pTi[j][:, cc, :], lhsT=zA[:, :, c], rhs=nSs[:], start=False, stop=True),
                     reads=["zA", "nSs"], writes=[("pTi", j)])
            P.op("act", lambda: nc.scalar.copy(out=Tr[:, cb * 4:(cb + 1) * 4, :], in_=pTr[j][:]), reads=[("pTr", j)], writes=["Tr"])
            P.op("dve", lambda: nc.vector.tensor_copy(out=Ti[:, cb * 4:(cb + 1) * 4, :], in_=pTi[j][:]), reads=[("pTi", j)], writes=["Ti"])
        for kb in range(16):
            j = kb % 2
            for kk in range(8):
                k1 = kb * 8 + kk
                P.op("pe", lambda: nc.tensor.matmul(pY[j][:, kk, :], lhsT=Tr[:, :, k1], rhs=Hr[:, k1, :], start=True, stop=False),
                     reads=["Tr", "Hr"], writes=[("pY", j)])
                P.op("pe", lambda: nc.tensor.matmul(pY[j][:, kk, :], lhsT=Ti[:, :, k1], rhs=Hni[:, k1, :], start=False, stop=True),
                     reads=["Ti", "Hni"], writes=[("pY", j)])
            eng = "act" if kb % 2 == 0 else "dve"
            if eng == "act":
                P.op("act", lambda: nc.scalar.copy(out=YTs[:, :, kb * 8:(kb + 1) * 8], in_=pY[j].rearrange("p a b -> p b a")),
                     reads=[("pY", j)], writes=["YTs"])
            else:
                P.op("dve", lambda: nc.vector.tensor_copy(out=YTs[:, :, kb * 8:(kb + 1) * 8], in_=pY[j].rearrange("p a b -> p b a")),
                     reads=[("pY", j)], writes=["YTs"])
        P.dma("sp", YTo[c0:c0 + 128, :], YTs.rearrange("p a b -> p (a b)"), reads=["YTs"], writes=["YTo"])
    return P.finish(["YTo"])


def run_G(A, B):
    nc = build_G()
    k = dft_consts()
    maps = []
    for i in range(NCORES):
        cs = slice(i * CPC, (i + 1) * CPC)
        maps.append({"zA": np.ascontiguousarray(A[:, cs]).reshape(128, 64, CPC), "zB": np.ascontiguousarray(B[:, cs]).reshape(128, 64, CPC),
                     "Cc": k["Cc"], "nSs": k["nSs"], "nCc": k["nCc"], "Hr": k["Hr"], "Hni": k["Hni"]})
    res = run_bass_kernel_spmd(nc, maps, core_ids=list(range(NCORES)))
    YT = np.concatenate([r["YT"] for r in res.results], 0)
    return YT


def build_I():
    P = Prog()
    nc = P.nc
    c = combine_setup(P)
    gfin_d = P.din("gfin", [128, D], F32)
    outd = P.dout("out", [TPC, D], F32)
    gfin = P.sb("gfin_s", [128, D], F32)
    jk = P.sb("junk", [128, D], BF16)
    ss = [P.sb("ss%d" % i, [128, 4], F32) for i in range(2)]
    epsb = P.sb("epsb", [128, 1], F32)
    P.op("pool", lambda: nc.gpsimd.memset(epsb[:], EPS), writes=["epsb"])
    P.dma("act", gfin[:], gfin_d[:, :], writes=["gfin"])
    for ti in range(NT):
        b = ti % 2
        xs, xk = combine_tile(P, c, ti)
        s_ = ss[b]
        sk = ("ss", b)
        P.op("act", lambda: nc.scalar.activation(out=jk[:], in_=xs[:], func=AF.Square, accum_out=s_[:, 0:1]), reads=[xk], writes=["junk", sk])
        P.op("act", lambda: nc.scalar.activation(out=s_[:, 1:2], in_=s_[:, 0:1], func=AF.Sqrt, scale=1.0 / D, bias=epsb[:, 0:1]),
             reads=[sk, "epsb"], writes=[sk])
        P.op("dve", lambda: nc.vector.reciprocal(out=s_[:, 2:3], in_=s_[:, 1:2]), reads=[sk], writes=[sk])
        P.op("dve", lambda: nc.vector.scalar_tensor_tensor(out=xs[:], in0=xs[:], scalar=s_[:, 2:3], in1=gfin[:], op0=ALU.mult, op1=ALU.mult),
             reads=[xk, sk, "gfin"], writes=[xk])
        P.dma("sp", outd[ti * 128:(ti + 1) * 128, :], xs[:], reads=[xk], writes=["outd"])
    return P.finish(["outd"])


def run_I(x1, Yall, slots, aff, gf_, gfin):
    nc = build_I()
    common = {"Yall": Yall, "gf": bc128(gf_), "gfin": bc128(gfin)}
    maps = []
    for i in range(NCORES):
        d = dict(common)
        sl = slice(i * TPC, (i + 1) * TPC)
        d["x1"] = np.ascontiguousarray(x1[sl])
        d["slots"] = np.ascontiguousarray(slots[sl])
        d["aff"] = np.ascontiguousarray(aff[sl])
        maps.append(d)
    res = run_bass_kernel_spmd(nc, maps, core_ids=list(range(NCORES)))
    return np.concatenate([r["out"] for r in res.results], 0)


def kernel(**inp):
    inp = {k: np.asarray(v) for k, v in inp.items()}
    m = run_mod(inp)
    x0 = inp["x"][0]
    ra = run_A(inp, m)
    aT = run_B(inp, ra)
    fT = [np.concatenate([np.stack([aT[h][:, i * TPC:(i + 1) * TPC] for h in range(8)], 0), ra[i]["gT"]], 0) for i in range(NCORES)]
    m0 = m[0, 0]
    x1, hf, aff = run_proj(fT, inp["w_out"][0], x0, m0[2 * D:3 * D], inp["g_norm_ffn"][0], m0[4 * D:5 * D], m0[3 * D:4 * D],
                           inp["w_router"][0])
    Yall, slots = run_E(aff, hf, inp["w_gate"][0], inp["w_up"][0], inp["w_down"][0])
    m1 = m[1, 0]
    x2, A, B = run_F(x1, Yall, slots, aff, m0[5 * D:6 * D], inp["g_norm_mix"][1], m1[D:2 * D], m1[0:D])
    YT = run_G(A, B)
    fT = [np.ascontiguousarray(YT[:, i * TPC:(i + 1) * TPC]).reshape(16, 128, TPC) for i in range(NCORES)]
    x1, hf, aff = run_proj(fT, inp["w_fourier_out"][0], x2, m1[2 * D:3 * D], inp["g_norm_ffn"][1], m1[4 * D:5 * D], m1[3 * D:4 * D],
                           inp["w_router"][1])
    Yall, slots = run_E(aff, hf, inp["w_gate"][1], inp["w_up"][1], inp["w_down"][1])
    out = run_I(x1, Yall, slots, aff, m1[5 * D:6 * D], inp["g_final"])
    return np.ascontiguousarray(out.reshape(1, SEQ, D).astype(np.float32))
```

```python
import math
import numpy as np
from contextlib import ExitStack
import concourse.bass as bass
import concourse.mybir as mybir
from concourse.bass_utils import run_bass_kernel_spmd

F32 = mybir.dt.float32
BF16 = mybir.dt.bfloat16
I32 = mybir.dt.int32
AF = mybir.ActivationFunctionType
ALU = mybir.AluOpType
AX = mybir.AxisListType
NCORES = 8
D = 2048
SEQ = 8192
TPC = SEQ // NCORES
NT = TPC // 128
EPS = 1e-6


class Res:
    __slots__ = ("w", "r")

    def __init__(self):
        self.w = {}
        self.r = {}


class Prog:
    def __init__(self):
        self.nc = bass.Bass("TRN2", target_bir_lowering=False)
        self.es = ExitStack()
        nc = self.nc
        self.eng = {"pe": nc.tensor, "act": nc.scalar, "dve": nc.vector,
                    "pool": nc.gpsimd, "sp": nc.sync}
        self.sems = {}
        self.cnt = {}
        for e in self.eng:
            self.sems[e] = self.es.enter_context(nc.semaphore("s_" + e))
            self.cnt[e] = 0
        self.known = {e: {} for e in self.eng}
        self.res = {}
        self.nd = 0

    def sb(self, name, shape, dt):
        return self.es.enter_context(self.nc.sbuf_tensor(name, list(shape), dt))

    def ps(self, name, shape, dt):
        return self.es.enter_context(self.nc.psum_tensor(name, list(shape), dt))

    def din(self, name, shape, dt):
        return self.nc.dram_tensor(name, list(shape), dt, kind="ExternalInput").ap()

    def dout(self, name, shape, dt):
        return self.nc.dram_tensor(name, list(shape), dt, kind="ExternalOutput").ap()

    def dtmp(self, name, shape, dt):
        return self.nc.dram_tensor(name, list(shape), dt, kind="Internal").ap()

    def R(self, key):
        r = self.res.get(key)
        if r is None:
            r = self.res[key] = Res()
        return r

    def _wait(self, e, semkey, val):
        if val <= 0:
            return
        k = self.known[e]
        if k.get(semkey, 0) >= val:
            return
        k[semkey] = val
        if getattr(self, "_pend", None) is not None:
            self._pend.append((semkey, val))
        else:
            self.eng[e].wait_ge(self.sems[semkey], val)

    def _deps(self, e, reads, writes, skip=None):
        for key in reads:
            for sk, v in self.R(key).w.items():
                if e == "pe" and sk == "pe":
                    continue
                self._wait(e, sk, v)
        for key in writes:
            r = self.R(key)
            for sk, v in r.w.items():
                if sk == skip or (e == "pe" and sk == "pe"):
                    continue
                self._wait(e, sk, v)
            for sk, v in r.r.items():
                if e == "pe" and sk == "pe":
                    continue
                self._wait(e, sk, v)

    def op(self, e, fn, reads=(), writes=()):
        self._pend = []
        self._deps(e, reads, writes)
        pend, self._pend = self._pend, None
        for (sk, v) in pend[:-1]:
            self.eng[e].wait_ge(self.sems[sk], v)
        ins = fn()
        if pend:
            ins._wait_ge(self.sems[pend[-1][0]], pend[-1][1])
        self.cnt[e] += 1
        c = self.cnt[e]
        ins.then_inc(self.sems[e], 1)
        for key in reads:
            self.R(key).r[e] = c
        for key in writes:
            r = self.R(key)
            r.w = {e: c}
            r.r = {}
        return ins

    def _dma_post(self, ins, sk, reads, writes):
        self.cnt[sk] += 16
        c = self.cnt[sk]
        ins.then_inc(self.sems[sk], 16)
        for key in reads:
            self.R(key).r[sk] = c
        for key in writes:
            r = self.R(key)
            if sk in r.w and len(r.w) == 1:
                r.w[sk] = c
            else:
                r.w = {sk: c}
            r.r = {}

    def _dsem(self, semkey, writes):
        sk = semkey if semkey is not None else ("d", writes[0])
        if sk not in self.sems:
            self.sems[sk] = self.es.enter_context(self.nc.semaphore("sd%d" % self.nd))
            self.nd += 1
            self.cnt[sk] = 0
        return sk

    def dma(self, q, out, in_, reads=(), writes=(), semkey=None, **kw):
        sk = self._dsem(semkey, writes)
        self._deps(q, reads, writes, skip=sk)
        ins = self.eng[q].dma_start(out=out, in_=in_, **kw)
        self._dma_post(ins, sk, reads, writes)
        return ins

    def idma(self, out, out_off, in_, in_off, reads=(), writes=(), semkey=None, **kw):
        sk = self._dsem(semkey, writes)
        self._deps("pool", reads, writes, skip=sk)
        ins = self.nc.gpsimd.indirect_dma_start(out, out_off, in_, in_off, **kw)
        self._dma_post(ins, sk, reads, writes)
        return ins

    def finish(self, out_keys, e="sp"):
        for key in out_keys:
            for sk, v in self.R(key).w.items():
                self._wait(e, sk, v)
        for o in self.eng:
            if o != e:
                self._wait(e, o, self.cnt[o])
        self.es.close()
        return self.nc


class CastLoader:
    def __init__(self, P, nelem, nbuf=4, engines=("pool", "dve", "pool")):
        self.P = P
        self.n = nelem
        self.stg = [P.sb("cstg%d" % i, [128, nelem], F32) for i in range(nbuf)]
        self.i = 0
        self.engines = engines
        self.fifo = []

    def load(self, dst_ap, dst_key, src_ap, shape):
        P, nc = self.P, self.P.nc
        k = self.i % len(self.stg)
        e = self.engines[self.i % len(self.engines)]
        self.i += 1
        a, b = shape
        sv = self.stg[k].rearrange("p (a b) -> p a b", a=a)
        P.dma("sp", sv, src_ap, writes=[("cstg", k)])
        if e == "pool":
            P.op("pool", lambda: nc.gpsimd.tensor_copy(out=dst_ap, in_=sv), reads=[("cstg", k)], writes=[dst_key])
        else:
            P.op("dve", lambda: nc.vector.tensor_copy(out=dst_ap, in_=sv), reads=[("cstg", k)], writes=[dst_key])

    def enqueue(self, *args):
        self.fifo.append(args)

    def pump(self, n=1):
        for _ in range(n):
            if not self.fifo:
                return
            self.load(*self.fifo.pop(0))

    def flush(self):
        self.pump(len(self.fifo))


def make_ident(P, dt=BF16, name="ident"):
    nc = P.nc
    idf = P.sb(name + "f", [128, 128], F32)
    P.op("pool", lambda: nc.gpsimd.memset(idf[:], 1.0), writes=[name + "f"])
    P.op("pool", lambda: nc.gpsimd.affine_select(out=idf[:], in_=idf[:], pattern=[[-1, 128]],
                                                 compare_op=ALU.is_equal, fill=0.0, base=0,
                                                 channel_multiplier=1),
         reads=[name + "f"], writes=[name + "f"])
    if dt == F32:
        return idf, name + "f"
    idb = P.sb(name, [128, 128], dt)
    P.op("dve", lambda: nc.vector.tensor_copy(out=idb[:], in_=idf[:]), reads=[name + "f"], writes=[name])
    return idb, name


def bc128(v):
    v = np.asarray(v, dtype=np.float32).reshape(1, -1)
    return np.ascontiguousarray(np.broadcast_to(v, (128, v.shape[1])))


MC = 6 * D // NCORES


def build_mod():
    P = Prog()
    nc = P.nc
    c1 = P.din("c1", [128, 16], F32)
    c2 = P.din("c2", [128, 16], F32)
    wm = P.din("wm", [2, D, MC], F32)
    bm = P.din("bm", [2, 2, MC], F32)
    mo = P.dout("mo", [2, 2, MC], F32)
    c1s = P.sb("c1s", [128, 16], F32)
    c2s = P.sb("c2s", [128, 16], F32)
    cc = P.sb("cc", [128, 16, 2], F32)
    bs = P.sb("bs", [2, 2, MC], F32)
    ms = P.sb("ms", [2, 2, MC], F32)
    HC = MC // 2
    wb = [P.sb("wmb%d" % i, [128, 16, HC], F32) for i in range(2)]
    pz = [P.ps("pz%d" % i, [2, 384], F32) for i in range(2)]
    P.dma("sp", c1s[:], c1[:, :], writes=["c1s"])
    P.dma("sp", c2s[:], c2[:, :], writes=["c2s"])
    P.dma("sp", bs[:], bm.rearrange("l s n -> s l n"), writes=["bs"])
    P.op("act", lambda: nc.scalar.activation(out=cc[:, :, 0], in_=c1s[:], func=AF.Silu), reads=["c1s"], writes=["cc0"])
    P.op("act", lambda: nc.scalar.activation(out=cc[:, :, 1], in_=c2s[:], func=AF.Silu), reads=["c2s"], writes=["cc1"])
    ci = 0
    for l in range(2):
        wv = wm[l].rearrange("(p kc) n -> p kc n", kc=16)
        for hf in range(2):
            b = ci % 2
            q = "sp" if ci % 2 == 0 else "act"
            for kq in range(4):
                P.dma(q, wb[b][:, kq * 4:(kq + 1) * 4, :], wv[:, kq * 4:(kq + 1) * 4, hf * HC:(hf + 1) * HC], writes=[("wmb", b)])
            for nb in range(HC // 384):
                pb = (ci * 2 + nb) % 2
                for kc in range(16):
                    P.op("pe", lambda kc=kc: nc.tensor.matmul(pz[pb][:], lhsT=cc[:, kc, :], rhs=wb[b][:, kc, nb * 384:(nb + 1) * 384],
                                                               start=(kc == 0), stop=(kc == 15)),
                         reads=["cc0", "cc1", ("wmb", b)], writes=[("pz", pb)])
                n0 = hf * HC + nb * 384
                P.op("dve", lambda: nc.vector.tensor_tensor(out=ms[:, l, n0:n0 + 384], in0=pz[pb][:], in1=bs[:, l, n0:n0 + 384], op=ALU.add),
                     reads=[("pz", pb), "bs"], writes=["ms"])
            ci += 1
    P.dma("sp", mo.rearrange("l s n -> s l n"), ms[:], reads=["ms"], writes=["mo"])
    return P.finish(["mo"])


def run_mod(inp):
    nc = build_mod()
    c1 = np.ascontiguousarray(inp["c"].reshape(128, 16))
    c2 = np.ascontiguousarray(inp["c_ctx"].reshape(128, 16))
    maps = []
    for i in range(NCORES):
        wm = np.ascontiguousarray(inp["w_mod"][:, :, i * MC:(i + 1) * MC])
        bm = np.ascontiguousarray(np.broadcast_to(inp["b_mod"][:, None, i * MC:(i + 1) * MC], (2, 2, MC)))
        maps.append({"c1": c1, "c2": c2, "wm": wm, "bm": bm})
    res = run_bass_kernel_spmd(nc, maps, core_ids=list(range(NCORES)))
    m = np.concatenate([r["mo"] for r in res.results], axis=2)
    return m


NTK = TPC + 32
INC = 5120


def rms_modulate_tile(P, nc, xs, xkey, rows, ss, sskey, epsb, gam, sh, hb, hbkey):
    P.op("act", lambda: nc.scalar.activation(out=hb[:rows, :], in_=xs[:rows, :], func=AF.Square, accum_out=ss[:rows, 0:1]),
         reads=[xkey], writes=[hbkey, sskey])
    P.op("act", lambda: nc.scalar.activation(out=ss[:rows, 1:2], in_=ss[:rows, 0:1], func=AF.Sqrt, scale=1.0 / D, bias=epsb[:rows, 0:1]),
         reads=[sskey, "epsb"], writes=[sskey])
    P.op("dve", lambda: nc.vector.reciprocal(out=ss[:rows, 2:3], in_=ss[:rows, 1:2]), reads=[sskey], writes=[sskey])
    P.op("dve", lambda: nc.vector.scalar_tensor_tensor(out=xs[:rows, :], in0=xs[:rows, :], scalar=ss[:rows, 2:3], in1=gam[:rows, :],
                                                       op0=ALU.mult, op1=ALU.mult),
         reads=[xkey, sskey, "gam"], writes=[xkey])
    P.op("pool", lambda: nc.gpsimd.tensor_tensor(out=hb[:rows, :], in0=xs[:rows, :], in1=sh[:rows, :], op=ALU.add),
         reads=[xkey, "sh"], writes=[hbkey])


def build_A():
    P = Prog()
    nc = P.nc
    x = P.din("x", [TPC, D], F32)
    cx = P.din("cx", [32, D], F32)
    gmix = P.din("gmix", [128, D], F32)
    cm = P.din("cm", [128, D], F32)
    sm = P.din("sm", [128, D], F32)
    cmc = P.din("cmc", [128, D], F32)
    smc = P.din("smc", [128, D], F32)
    w_in = P.din("w_in", [D, INC], F32)
    cosd = P.din("cosE", [TPC, 256], F32)
    sind = P.din("sinE", [TPC, 256], F32)
    lngd = P.din("lng", [128, 1024], F32)
    lnbd = P.din("lnb", [128, 1024], F32)
    wsTd = P.din("wsT", [8, 128, 128], F32)
    bsbd = P.din("bsb", [128, 1024], F32)
    qT = P.dout("qT", [8, 128, TPC], BF16)
    kT = P.dout("kT", [8, 128, NTK], BF16)
    vo = P.dout("v", [NTK, 1024], BF16)
    gTo = P.dout("gT", [8, 128, TPC], BF16)

    ident, idk = make_ident(P)
    xs = [P.sb("xs%d" % i, [128, D], F32) for i in range(2)]
    gam = P.sb("gam", [128, D], F32)
    sh = P.sb("sh", [128, D], F32)
    hb = [P.sb("hb%d" % i, [128, D], BF16) for i in range(2)]
    hT = P.sb("hT", [128, 16, NTK], BF16)
    wblk = [P.sb("wblk%d" % i, [128, 16, 512], BF16) for i in range(2)]
    big = P.sb("big", [128, 4096], F32)
    cosE = big[:, 0:2048].rearrange("p (t f) -> p t f", t=8)
    sinE = big[:, 2048:4096].rearrange("p (t f) -> p t f", t=8)
    uT = big.bitcast(BF16).rearrange("p (g t) -> p g t", g=8)
    lng = P.sb("lngs", [128, 1024], F32)
    lnb = P.sb("lnbs", [128, 1024], F32)
    wsTf = P.sb("wsTf", [128, 8, 128], F32)
    wsTb = P.sb("wsTb", [128, 8, 128], BF16)
    bsb = P.sb("bsbs", [128, 8, 128], F32)
    stg = P.sb("stg", [128, 4, NTK], BF16)
    zf = [P.sb("zf%d" % i, [128, 512], F32) for i in range(2)]
    zsq = P.sb("zsq", [128, 512], F32)
    rt = [P.sb("rt%d" % i, [128, 256], F32) for i in range(4)]
    qr = [P.sb("qr%d" % i, [128, 512], BF16) for i in range(2)]
    vs = [P.sb("vs%d" % i, [128, 512], BF16) for i in range(2)]
    vlf = P.sb("vlf", [128, 512], F32)
    vlb = [P.sb("vlb%d" % i, [128, 512], BF16) for i in range(2)]
    tt = [P.sb("tt%d" % i, [128, 128], F32) for i in range(2)]
    ss = [P.sb("ss%d" % i, [128, 4], F32) for i in range(2)]
    st = [P.sb("st%d" % i, [128, 24], F32) for i in range(2)]
    epsb = P.sb("epsb", [128, 1], F32)
    pT = P.ps("pT", [128, D], BF16)
    pz = [P.ps("pz%d" % i, [128, 512], F32) for i in range(2)]
    pq = [P.ps("pq%d" % i, [128, 512], BF16) for i in range(2)]
    pm = [P.ps("pm%d" % i, [128, 128], F32) for i in range(2)]

    P.op("pool", lambda: nc.gpsimd.memset(epsb[:], EPS), writes=["epsb"])

    def load_mod(cmd, smd):
        P.dma("sp", gam[:], cmd[:, :], writes=["gam"])
        P.dma("act", xs[1][:], gmix[:, :], writes=[("xs", 1)])
        P.dma("sp", sh[:], smd[:, :], writes=["sh"])
        P.op("dve", lambda: nc.vector.scalar_tensor_tensor(out=gam[:], in0=gam[:], scalar=1.0, in1=xs[1][:], op0=ALU.add, op1=ALU.mult),
             reads=["gam", ("xs", 1)], writes=["gam"])

    def front_tile(src_ap, rows, tok0, b):
        P.dma("sp", xs[b][:rows, :], src_ap, writes=[("xs", b)])
        rms_modulate_tile(P, nc, xs[b], ("xs", b), rows, ss[b], ("ss", b), epsb, gam, sh, hb[b], ("hb", b))
        for kc in range(16):
            P.op("pe", lambda kc=kc: nc.tensor.transpose(pT[:, kc * 128:kc * 128 + rows], hb[b][:rows, kc * 128:(kc + 1) * 128], ident[:rows, :rows]),
                 reads=[("hb", b), idk], writes=["pT"])
        P.op("act", lambda: nc.scalar.copy(out=hT[:, :, tok0:tok0 + rows],
                                           in_=pT.rearrange("p (k t) -> p k t", k=16)[:, :, 0:rows]),
             reads=["pT"], writes=["hT"])

    load_mod(cmc, smc)
    front_tile(cx[:, :], 32, TPC, 0)
    load_mod(cm, sm)
    for ti in range(NT):
        front_tile(x[ti * 128:(ti + 1) * 128, :], 128, ti * 128, ti % 2)

    P.dma("sp", big[:, 0:2048].rearrange("p (t f) -> p t f", t=8), cosd.rearrange("(t p) f -> p t f", p=128), writes=["big"])
    P.dma("sp", big[:, 2048:4096].rearrange("p (t f) -> p t f", t=8), sind.rearrange("(t p) f -> p t f", p=128), writes=["big"])
    P.dma("act", lng[:], lngd[:, :], writes=["lng"])
    P.dma("act", lnb[:], lnbd[:, :], writes=["lnb"])
    P.dma("act", wsTf[:], wsTd.rearrange("g q p -> q g p"), writes=["wsTf"])
    P.dma("act", bsb[:], bsbd.rearrange("q (g p) -> q g p", g=8), writes=["bsb"])
    P.op("dve", lambda: nc.vector.tensor_copy(out=wsTb[:], in_=wsTf[:]), reads=["wsTf"], writes=["wsTb"])

    wv = w_in.rearrange("(kc p) n -> p kc n", p=128)
    CL = CastLoader(P, 1024, engines=("pool", "dve"))
    zc = [0]

    def proj_tok(b, tok0, rows):
        j = zc[0] % 2
        zc[0] += 1
        for kc in range(16):
            P.op("pe", lambda kc=kc: nc.tensor.matmul(pz[j][:rows, :], lhsT=hT[:, kc, tok0:tok0 + rows], rhs=wblk[b][:, kc, :],
                                                       start=(kc == 0), stop=(kc == 15)),
                 reads=["hT", ("wblk", b)], writes=[("pz", j)])
        CL.pump(1)
        return j

    def queue_block(cb):
        b = cb % 2
        for kq in range(8):
            CL.enqueue(wblk[b][:, kq * 2:(kq + 1) * 2, :], ("wblk", b), wv[:, kq * 2:(kq + 1) * 2, cb * 512:(cb + 1) * 512], (2, 512))

    queue_block(0)
    CL.flush()
    for cb in range(10):
        b = cb % 2
        CL.flush()
        if cb + 1 < 10:
            queue_block(cb + 1)
        if cb < 4:
            isk = cb >= 2
            for ti in range(NT):
                j = proj_tok(b, ti * 128, 128)
                zv = pz[j].rearrange("p (a h x f) -> p a h x f", a=8, h=2, x=2, f=16)
                x1 = zv[:, :, :, 0, :]
                x2 = zv[:, :, :, 1, :]
                cv = cosE[:, ti, :].rearrange("p (a h f) -> p a h f", a=8, h=2)
                sv = sinE[:, ti, :].rearrange("p (a h f) -> p a h f", a=8, h=2)
                r4 = [t.rearrange("p (a h f) -> p a h f", a=8, h=2) for t in rt]
                qv = qr[j].rearrange("p (a h x f) -> p a h x f", a=8, h=2, x=2, f=16)
                zk = ("pz", j)
                P.op("dve", lambda: nc.vector.tensor_tensor(out=r4[0], in0=x1, in1=cv, op=ALU.mult), reads=[zk, "big"], writes=["rt0"])
                P.op("dve", lambda: nc.vector.tensor_tensor(out=r4[1], in0=x2, in1=sv, op=ALU.mult), reads=[zk, "big"], writes=["rt1"])
                P.op("dve", lambda: nc.vector.tensor_tensor(out=r4[2], in0=x2, in1=cv, op=ALU.mult), reads=[zk, "big"], writes=["rt2"])
                P.op("dve", lambda: nc.vector.tensor_tensor(out=r4[3], in0=x1, in1=sv, op=ALU.mult), reads=[zk, "big"], writes=["rt3"])
                P.op("pool", lambda: nc.gpsimd.tensor_tensor(out=qv[:, :, :, 0, :], in0=r4[0], in1=r4[1], op=ALU.subtract),
                     reads=["rt0", "rt1"], writes=[("qr", j)])
                P.op("pool", lambda: nc.gpsimd.tensor_tensor(out=qv[:, :, :, 1, :], in0=r4[2], in1=r4[3], op=ALU.add),
                     reads=["rt2", "rt3"], writes=[("qr", j)])
                for s in range(4):
                    P.op("pe", lambda s=s: nc.tensor.transpose(pq[j][:, s * 128:(s + 1) * 128], qr[j][:, s * 128:(s + 1) * 128], ident[:]),
                         reads=[("qr", j), idk], writes=[("pq", j)])
                P.op("act", lambda: nc.scalar.copy(out=stg[:, :, ti * 128:(ti + 1) * 128], in_=pq[j].rearrange("p (s t) -> p s t", s=4)),
                     reads=[("pq", j)], writes=["stg"])
            if isk:
                j = proj_tok(b, TPC, 32)
                P.op("act", lambda: nc.scalar.copy(out=qr[j][:32, :], in_=pz[j][:32, :]), reads=[("pz", j)], writes=[("qr", j)])
                for s in range(4):
                    P.op("pe", lambda s=s: nc.tensor.transpose(pq[j][:, s * 128:s * 128 + 32], qr[j][:32, s * 128:(s + 1) * 128], ident[:32, :32]),
                         reads=[("qr", j), idk], writes=[("pq", j)])
                P.op("act", lambda: nc.scalar.copy(out=stg[:, :, TPC:TPC + 32], in_=pq[j].rearrange("p (s t) -> p s t", s=4)[:, :, 0:32]),
                     reads=[("pq", j)], writes=["stg"])
            h0 = (cb % 2) * 4
            if isk:
                P.dma("act", kT[h0:h0 + 4].rearrange("h d t -> d h t"), stg[:], reads=["stg"], writes=["kT"])
            else:
                P.dma("act", qT[h0:h0 + 4].rearrange("h d t -> d h t"), stg[:, :, 0:TPC], reads=["stg"], writes=["qT"])
        elif cb < 6:
            for ti in range(NT + 1):
                rows = 128 if ti < NT else 32
                tok0 = ti * 128
                j = proj_tok(b, tok0, rows)
                P.op("act", lambda: nc.scalar.copy(out=vs[j][:rows, :], in_=pz[j][:rows, :]), reads=[("pz", j)], writes=[("vs", j)])
                P.dma("act", vo[tok0:tok0 + rows, (cb - 4) * 512:(cb - 3) * 512], vs[j][:rows, :], reads=[("vs", j)], writes=["vo"])
        elif cb < 8:
            for sub in range(4):
                G = (cb - 6) * 4 + sub
                for tg in range(2):
                    j = zc[0] % 2
                    zc[0] += 1
                    for kc in range(16):
                        P.op("pe", lambda kc=kc: nc.tensor.matmul(pz[j][:], lhsT=wblk[b][:, kc, sub * 128:(sub + 1) * 128],
                                                                   rhs=hT[:, kc, tg * 512:(tg + 1) * 512],
                                                                   start=(kc == 0), stop=(kc == 15)),
                             reads=["hT", ("wblk", b)], writes=[("pz", j)])
                    P.op("act", lambda: nc.scalar.activation(out=uT[:, G, tg * 512:(tg + 1) * 512], in_=pz[j][:], func=AF.Gelu_apprx_tanh),
                         reads=[("pz", j)], writes=["big"])
                    CL.pump(1)
        else:
            for ti in range(NT):
                tok0 = ti * 128
                j = proj_tok(b, tok0, 128)
                z = zf[j]
                zk = ("zf", j)
                s_ = st[j]
                sk = ("st", j)
                P.op("act", lambda: nc.scalar.activation(out=z[:], in_=pz[j][:], func=AF.Gelu_apprx_tanh), reads=[("pz", j)], writes=[zk])
                P.op("dve", lambda: nc.vector.reduce_sum(out=s_[:, 0:4], in_=z.rearrange("p (g d) -> p g d", g=4), axis=AX.X),
                     reads=[zk], writes=[sk])
                P.op("act", lambda: nc.scalar.activation(out=zsq[:], in_=z[:], func=AF.Square), reads=[zk], writes=["zsq"])
                P.op("dve", lambda: nc.vector.reduce_sum(out=s_[:, 4:8], in_=zsq.rearrange("p (g d) -> p g d", g=4), axis=AX.X),
                     reads=["zsq"], writes=[sk])
                P.op("dve", lambda: nc.vector.tensor_scalar(out=s_[:, 8:12], in0=s_[:, 0:4], scalar1=1.0 / 128, scalar2=None, op0=ALU.mult),
                     reads=[sk], writes=[sk])
                P.op("dve", lambda: nc.vector.tensor_tensor(out=s_[:, 12:16], in0=s_[:, 8:12], in1=s_[:, 8:12], op=ALU.mult),
                     reads=[sk], writes=[sk])
                P.op("dve", lambda: nc.vector.scalar_tensor_tensor(out=s_[:, 16:20], in0=s_[:, 4:8], scalar=1.0 / 128, in1=s_[:, 12:16],
                                                                   op0=ALU.mult, op1=ALU.subtract),
                     reads=[sk], writes=[sk])
                P.op("act", lambda: nc.scalar.activation(out=s_[:, 20:24], in_=s_[:, 16:20], func=AF.Sqrt, bias=epsb[:, 0:1]),
                     reads=[sk, "epsb"], writes=[sk])
                P.op("dve", lambda: nc.vector.reciprocal(out=s_[:, 20:24], in_=s_[:, 20:24]), reads=[sk], writes=[sk])
                for g in range(4):
                    P.op("dve", lambda g=g: nc.vector.tensor_scalar(out=vlf[:, g * 128:(g + 1) * 128], in0=z[:, g * 128:(g + 1) * 128],
                                                                    scalar1=s_[:, 8 + g:9 + g], scalar2=s_[:, 20 + g:21 + g],
                                                                    op0=ALU.subtract, op1=ALU.mult),
                         reads=[zk, sk], writes=["vlf"])
                c0 = (cb - 8) * 512
                P.op("pool", lambda: nc.gpsimd.tensor_tensor(out=vlf[:], in0=vlf[:], in1=lng[:, c0:c0 + 512], op=ALU.mult),
                     reads=["vlf", "lng"], writes=["vlf"])
                P.op("pool", lambda: nc.gpsimd.tensor_tensor(out=vlb[j][:], in0=vlf[:], in1=lnb[:, c0:c0 + 512], op=ALU.add),
                     reads=["vlf", "lnb"], writes=[("vlb", j)])
                for g in range(4):
                    G = (cb - 8) * 4 + g
                    m = (ti * 4 + g) % 2
                    P.op("pe", lambda g=g, G=G, m=m: nc.tensor.matmul(pm[m][:], lhsT=vlb[j][:, g * 128:(g + 1) * 128], rhs=wsTb[:, G, :],
                                                                      start=True, stop=True),
                         reads=[("vlb", j), "wsTb"], writes=[("pm", m)])
                    P.op("dve", lambda G=G, m=m: nc.vector.tensor_tensor(out=tt[m][:], in0=pm[m][:], in1=bsb[:, G, :], op=ALU.add),
                         reads=[("pm", m), "bsb"], writes=[("tt", m)])
                    P.op("dve", lambda g=g, G=G, m=m: nc.vector.tensor_tensor(out=stg[:, g, tok0:tok0 + 128], in0=tt[m][:],
                                                                              in1=uT[:, G, tok0:tok0 + 128], op=ALU.mult),
                         reads=[("tt", m), "big"], writes=["stg"])
            g0 = (cb - 8) * 4
            P.dma("pool", gTo[g0:g0 + 4].rearrange("g d t -> d g t"), stg[:, :, 0:TPC], reads=["stg"], writes=["gTo"])
    return P.finish(["qT", "kT", "vo", "gTo"])


def rope_tables():
    t = np.arange(SEQ)
    row = (t // 64).astype(np.float32)
    col = (t % 64).astype(np.float32)
    inv = (10000.0 ** (-np.arange(0, 32, 2, dtype=np.float32) / 32)).astype(np.float32)
    ang = np.stack([row[:, None] * inv[None, :], col[:, None] * inv[None, :]], axis=1)
    cosE = np.tile(np.cos(ang).astype(np.float32).reshape(SEQ, 1, 32), (1, 8, 1)).reshape(SEQ, 256)
    sinE = np.tile(np.sin(ang).astype(np.float32).reshape(SEQ, 1, 32), (1, 8, 1)).reshape(SEQ, 256)
    return np.ascontiguousarray(cosE), np.ascontiguousarray(sinE)


def run_A(inp, m):
    nc = build_A()
    cosE, sinE = rope_tables()
    m0 = m[0]
    sm_, cm_ = m0[0, 0:D], m0[0, D:2 * D]
    smc_, cmc_ = m0[1, 0:D], m0[1, D:2 * D]
    common = {
        "gmix": bc128(inp["g_norm_mix"][0]), "cm": bc128(cm_), "sm": bc128(sm_), "cmc": bc128(cmc_), "smc": bc128(smc_),
        "w_in": np.ascontiguousarray(inp["w_in"][0]),
        "lng": bc128(inp["sgu_ln_g"][0]), "lnb": bc128(inp["sgu_ln_b"][0]),
        "wsT": np.ascontiguousarray(np.transpose(inp["w_spatial"][0], (0, 2, 1))),
        "bsb": bc128(inp["b_spatial"][0].reshape(-1)),
    }
    maps = []
    for i in range(NCORES):
        d = dict(common)
        d["x"] = np.ascontiguousarray(inp["x"][0, i * TPC:(i + 1) * TPC])
        d["cx"] = np.ascontiguousarray(inp["ctx"][0, i * 32:(i + 1) * 32])
        d["cosE"] = cosE[i * TPC:(i + 1) * TPC]
        d["sinE"] = sinE[i * TPC:(i + 1) * TPC]
        maps.append(d)
    res = run_bass_kernel_spmd(nc, maps, core_ids=list(range(NCORES)))
    return [r for r in res.results]


NKEY = SEQ + 256
NKT = NKEY // 128
LAM_INIT0 = 0.8 - 0.6 * math.exp(-0.3 * 0)


def build_B():
    P = Prog()
    nc = P.nc
    qTd = P.din("qT", [128, SEQ], BF16)
    kTd = P.din("kT", [128, NKEY], BF16)
    vd = P.din("v", [NKEY, 128], BF16)
    lamd = P.din("lamv", [128, 4, 64], F32)
    gsd = P.din("gsub", [128, 1], F32)
    aTo = P.dout("aT", [128, SEQ], BF16)

    qz = [P.sb("qz%d" % i, [128, SEQ], BF16) for i in range(2)]
    kT = P.sb("kTs", [128, NKEY], BF16)
    vs = P.sb("vsb", [128, NKT, 128], BF16)
    lamv = P.sb("lamvs", [128, 4, 64], F32)
    lt = P.sb("lt", [128, 2, 64], F32)
    ls = P.sb("ls", [128, 8], F32)
    gs = P.sb("gs", [128, 1], F32)
    epsb = P.sb("epsb", [128, 1], F32)
    onesb = P.sb("onesb", [128, 128], BF16)
    onesf = P.sb("onesf", [128, 128], F32)
    pt = [P.sb("pt%d" % i, [128, 512], BF16) for i in range(3)]
    rz = [P.sb("rz%d" % i, [128, 512], F32) for i in range(2)]
    o = P.sb("o", [128, 512], F32)
    t1 = P.sb("t1", [128, 512], F32)
    osq = P.sb("osq", [128, 512], F32)
    rstd = P.sb("rstd", [128, 512], F32)
    ab = [P.sb("ab%d" % i, [128, 512], BF16) for i in range(2)]
    ps = [P.ps("ps%d" % i, [128, 512], F32) for i in range(3)]
    po = [P.ps("po%d" % i, [128, 512], F32) for i in range(2)]
    pzz = [P.ps("pzz%d" % i, [128, 512], F32) for i in range(2)]

    for i in range(4):
        q = "sp" if i % 2 == 0 else "act"
        P.dma(q, qz[0][0:64, i * 2048:(i + 1) * 2048], qTd[0:64, i * 2048:(i + 1) * 2048], writes=["qT"])
        P.dma(q, qz[1][64:128, i * 2048:(i + 1) * 2048], qTd[64:128, i * 2048:(i + 1) * 2048], writes=["qT"])
        P.dma(q, kT[:, i * 2112:(i + 1) * 2112], kTd[:, i * 2112:(i + 1) * 2112], writes=["kT"])
    P.dma("sp", vs[:, 0:33, :], vd[0:33 * 128, :].rearrange("(t p) e -> p t e", p=128), writes=["vs"])
    P.dma("act", vs[:, 33:66, :], vd[33 * 128:, :].rearrange("(t p) e -> p t e", p=128), writes=["vs"])
    P.dma("sp", lamv[:], lamd[:, :, :], writes=["lamv"])
    P.dma("sp", gs[:], gsd[:, :], writes=["gs"])
    P.op("pool", lambda: nc.gpsimd.memset(epsb[:], EPS), writes=["epsb"])
    P.op("pool", lambda: nc.gpsimd.memset(onesb[:], 1.0), writes=["onesb"])
    P.op("pool", lambda: nc.gpsimd.memset(onesf[:], 1.0 / 128), writes=["onesf"])
    P.op("pool", lambda: nc.gpsimd.memset(qz[0][64:128, :], 0.0), writes=["qz0pad"])
    P.op("dve", lambda: nc.vector.memset(qz[1][0:64, :], 0.0), writes=["qz1pad"])
    P.op("dve", lambda: nc.vector.tensor_tensor(out=lt[:, 0, :], in0=lamv[:, 0, :], in1=lamv[:, 1, :], op=ALU.mult), reads=["lamv"], writes=["lt"])
    P.op("dve", lambda: nc.vector.tensor_tensor(out=lt[:, 1, :], in0=lamv[:, 2, :], in1=lamv[:, 3, :], op=ALU.mult), reads=["lamv", "lt"], writes=["lt"])
    P.op("dve", lambda: nc.vector.reduce_sum(out=ls[:, 0:2], in_=lt[:], axis=AX.X), reads=["lt"], writes=["ls"])
    P.op("act", lambda: nc.scalar.activation(out=ls[:, 2:4], in_=ls[:, 0:2], func=AF.Exp), reads=["ls"], writes=["ls"])
    P.op("dve", lambda: nc.vector.tensor_tensor(out=ls[:, 4:5], in0=ls[:, 3:4], in1=ls[:, 2:3], op=ALU.subtract), reads=["ls"], writes=["ls"])
    P.op("dve", lambda: nc.vector.tensor_scalar(out=ls[:, 4:5], in0=ls[:, 4:5], scalar1=-LAM_INIT0, scalar2=None, op0=ALU.add), reads=["ls"], writes=["ls"])
    P.op("dve", lambda: nc.vector.tensor_scalar(out=gs[:], in0=gs[:], scalar1=1.0 - LAM_INIT0, scalar2=None, op0=ALU.mult), reads=["gs"], writes=["gs"])

    it = 0
    for qb in range(SEQ // 512):
        q0 = qb * 512
        steps = [(kt, c) for kt in range(NKT) for c in range(2)]

        def qk(i, kt, c):
            j = i % 3
            P.op("pe", lambda: nc.tensor.matmul(ps[j][:], lhsT=kT[:, kt * 128:(kt + 1) * 128],
                                                rhs=qz[c][:, q0:q0 + 512], start=True, stop=True),
                 reads=["qT", "kT", "qz0pad", "qz1pad"], writes=[("ps", j)])

        for pre in range(2):
            qk(it + pre, *steps[pre])
        for si, (kt, c) in enumerate(steps):
            j = it % 3
            P.op("act", lambda: nc.scalar.activation(out=pt[j][:], in_=ps[j][:], func=AF.Exp, scale=0.125),
                 reads=[("ps", j)], writes=[("pt", j)])
            if si + 2 < len(steps):
                qk(it + 2, *steps[si + 2])
            P.op("pe", lambda: nc.tensor.matmul(po[c][:], lhsT=vs[:, kt, :], rhs=pt[j][:], start=(kt == 0), stop=(kt == NKT - 1)),
                 reads=["vs", ("pt", j)], writes=[("po", c)])
            P.op("pe", lambda: nc.tensor.matmul(pzz[c][:], lhsT=onesb[:], rhs=pt[j][:], start=(kt == 0), stop=(kt == NKT - 1)),
                 reads=["onesb", ("pt", j)], writes=[("pzz", c)])
            it += 1
        for c in range(2):
            P.op("dve", lambda c=c: nc.vector.reciprocal(out=rz[c][:], in_=pzz[c][:]), reads=[("pzz", c)], writes=[("rz", c)])
        P.op("dve", lambda: nc.vector.tensor_tensor(out=o[:], in0=po[0][:], in1=rz[0][:], op=ALU.mult), reads=[("po", 0), ("rz", 0)], writes=["o"])
        P.op("dve", lambda: nc.vector.tensor_tensor(out=t1[:], in0=po[1][:], in1=rz[1][:], op=ALU.mult), reads=[("po", 1), ("rz", 1)], writes=["t1"])
        P.op("dve", lambda: nc.vector.scalar_tensor_tensor(out=o[:], in0=t1[:], scalar=ls[:, 4:5], in1=o[:], op0=ALU.mult, op1=ALU.add),
             reads=["t1", "ls", "o"], writes=["o"])
        P.op("act", lambda: nc.scalar.activation(out=osq[:], in_=o[:], func=AF.Square), reads=["o"], writes=["osq"])
        jm = it % 3
        it += 1
        P.op("pe", lambda: nc.tensor.matmul(ps[jm][:], lhsT=onesf[:], rhs=osq[:], start=True, stop=True), reads=["onesf", "osq"], writes=[("ps", jm)])
        P.op("act", lambda: nc.scalar.activation(out=rstd[:], in_=ps[jm][:], func=AF.Sqrt, bias=epsb[:, 0:1]), reads=[("ps", jm), "epsb"], writes=["rstd"])
        P.op("dve", lambda: nc.vector.reciprocal(out=rstd[:], in_=rstd[:]), reads=["rstd"], writes=["rstd"])
        a = ab[qb % 2]
        P.op("dve", lambda: nc.vector.scalar_tensor_tensor(out=a[:], in0=o[:], scalar=gs[:, 0:1], in1=rstd[:], op0=ALU.mult, op1=ALU.mult),
             reads=["o", "gs", "rstd"], writes=[("ab", qb % 2)])
        P.dma("sp", aTo[:, q0:q0 + 512], a[:], reads=[("ab", qb % 2)], writes=["aTo"])
    return P.finish(["aTo"])


def run_B(inp, ra):
    nc = build_B()
    lamv = np.stack([inp["lam_q1"][0], inp["lam_k1"][0], inp["lam_q2"][0], inp["lam_k2"][0]], 0)
    lamv = np.ascontiguousarray(np.broadcast_to(lamv[None], (128, 4, 64))).astype(np.float32)
    gsub = np.ascontiguousarray(inp["g_subln"][0].reshape(128, 1))
    maps = []
    for h in range(NCORES):
        qT = np.concatenate([r["qT"][h] for r in ra], axis=1)
        kT = np.concatenate([r["kT"][h][:, :TPC] for r in ra] + [r["kT"][h][:, TPC:] for r in ra], axis=1)
        v = np.concatenate([r["v"][:TPC, h * 128:(h + 1) * 128] for r in ra] + [r["v"][TPC:, h * 128:(h + 1) * 128] for r in ra], axis=0)
        maps.append({"qT": np.ascontiguousarray(qT), "kT": np.ascontiguousarray(kT), "v": np.ascontiguousarray(v),
                     "lamv": lamv, "gsub": gsub})
    res = run_bass_kernel_spmd(nc, maps, core_ids=list(range(NCORES)))
    return [r["aT"] for r in res.results]


def build_proj():
    P = Prog()
    nc = P.nc
    fTd = P.din("fT", [16, 128, TPC], BF16)
    Wd = P.din("W", [D, D], F32)
    xd = P.din("x", [TPC, D], F32)
    gmd = P.din("gm", [128, D], F32)
    gfd = P.din("gffn", [128, D], F32)
    cfd = P.din("cf", [128, D], F32)
    sfd = P.din("sf", [128, D], F32)
    wrd = P.din("wr", [D, 16], F32)
    x1o = P.dout("x1", [TPC, D], F32)
    hfo = P.dout("hf", [TPC, D], BF16)
    affo = P.dout("aff", [TPC, 16], F32)

    identf, idk = make_ident(P, F32)
    Wb = P.sb("Wb", [128, 16, D], BF16)
    fT = [P.sb("fTs%d" % i, [128, 16, 128], BF16) for i in range(2)]
    gm = P.sb("gms", [128, D], F32)
    gam = P.sb("gam", [128, D], F32)
    sh = P.sb("sh", [128, D], F32)
    xs = [P.sb("xs%d" % i, [128, D], F32) for i in range(2)]
    hf32 = P.sb("hf32", [128, D], F32)
    hfb = [P.sb("hfb%d" % i, [128, D], BF16) for i in range(2)]
    hT32 = P.sb("hT32", [128, 16, 128], F32)
    tq = [P.sb("tq%d" % i, [128, 512], F32) for i in range(2)]
    wr = P.sb("wrs", [128, 16, 16], F32)
    ss = [P.sb("ss%d" % i, [128, 4], F32) for i in range(2)]
    sm = [P.sb("smx%d" % i, [128, 4], F32) for i in range(2)]
    ex = [P.sb("ex%d" % i, [128, 16], F32) for i in range(2)]
    epsb = P.sb("epsb", [128, 1], F32)
    pz = [P.ps("pz%d" % i, [128, 512], F32) for i in range(2)]
    pT = P.ps("pT", [128, D], F32)
    pl = P.ps("pl", [128, 16], F32)

    P.op("pool", lambda: nc.gpsimd.memset(epsb[:], EPS), writes=["epsb"])
    wv = Wd.rearrange("(kc p) n -> p kc n", p=128)
    CL = CastLoader(P, 1024, engines=("pool", "dve"))
    for hh in range(2):
        for kc in range(16):
            CL.load(Wb[:, kc:kc + 1, hh * 1024:(hh + 1) * 1024], ("Wb", hh), wv[:, kc:kc + 1, hh * 1024:(hh + 1) * 1024], (1, 1024))
    P.dma("sp", gm[:], gmd[:, :], writes=["gm"])
    P.dma("sp", gam[:], cfd[:, :], writes=["gam"])
    P.dma("act", xs[1][:], gfd[:, :], writes=[("xs", 1)])
    P.dma("act", sh[:], sfd[:, :], writes=["sh"])
    P.dma("act", wr[:], wrd.rearrange("(kc p) e -> p kc e", p=128), writes=["wr"])
    P.op("dve", lambda: nc.vector.scalar_tensor_tensor(out=gam[:], in0=gam[:], scalar=1.0, in1=xs[1][:], op0=ALU.add, op1=ALU.mult),
         reads=["gam", ("xs", 1)], writes=["gam"])

    zc = 0
    for ti in range(NT):
        b = ti % 2
        tok0 = ti * 128
        xk = ("xs", b)
        P.dma("sp", xs[b][:], xd[tok0:tok0 + 128, :], writes=[xk])
        P.dma("sp", fT[b][:], fTd[:, :, tok0:tok0 + 128].rearrange("k d t -> d k t"), writes=[("fT", b)])
        for nb in range(4):
            j = zc % 2
            zc += 1
            for kc in range(16):
                P.op("pe", lambda kc=kc: nc.tensor.matmul(pz[j][:], lhsT=fT[b][:, kc, :], rhs=Wb[:, kc, nb * 512:(nb + 1) * 512],
                                                           start=(kc == 0), stop=(kc == 15)),
                     reads=[("fT", b), ("Wb", nb // 2)], writes=[("pz", j)])
            P.op("dve", lambda: nc.vector.tensor_tensor(out=tq[j][:], in0=pz[j][:], in1=gm[:, nb * 512:(nb + 1) * 512], op=ALU.mult),
                 reads=[("pz", j), "gm"], writes=[("tq", j)])
            P.op("pool", lambda: nc.gpsimd.tensor_tensor(out=xs[b][:, nb * 512:(nb + 1) * 512], in0=xs[b][:, nb * 512:(nb + 1) * 512],
                                                         in1=tq[j][:], op=ALU.add),
                 reads=[xk, ("tq", j)], writes=[xk])
        P.dma("pool", x1o[tok0:tok0 + 128, :], xs[b][:], reads=[xk], writes=["x1o"])
        s_ = ss[b]
        sk = ("ss", b)
        P.op("act", lambda: nc.scalar.activation(out=hfb[b][:], in_=xs[b][:], func=AF.Square, accum_out=s_[:, 0:1]),
             reads=[xk], writes=[("hfb", b), sk])
        P.op("act", lambda: nc.scalar.activation(out=s_[:, 1:2], in_=s_[:, 0:1], func=AF.Sqrt, scale=1.0 / D, bias=epsb[:, 0:1]),
             reads=[sk, "epsb"], writes=[sk])
        P.op("dve", lambda: nc.vector.reciprocal(out=s_[:, 2:3], in_=s_[:, 1:2]), reads=[sk], writes=[sk])
        P.op("dve", lambda: nc.vector.scalar_tensor_tensor(out=hf32[:], in0=xs[b][:], scalar=s_[:, 2:3], in1=gam[:], op0=ALU.mult, op1=ALU.mult),
             reads=[xk, sk, "gam"], writes=["hf32"])
        P.op("pool", lambda: nc.gpsimd.tensor_tensor(out=hf32[:], in0=hf32[:], in1=sh[:], op=ALU.add), reads=["hf32", "sh"], writes=["hf32"])
        P.op("act", lambda: nc.scalar.copy(out=hfb[b][:], in_=hf32[:]), reads=["hf32"], writes=[("hfb", b)])
        P.dma("act", hfo[tok0:tok0 + 128, :], hfb[b][:], reads=[("hfb", b)], writes=["hfo"])
        for kc in range(16):
            P.op("pe", lambda kc=kc: nc.tensor.transpose(pT[:, kc * 128:(kc + 1) * 128], hf32[:, kc * 128:(kc + 1) * 128], identf[:]),
                 reads=["hf32", idk], writes=["pT"])
        P.op("dve", lambda: nc.vector.tensor_copy(out=hT32[:], in_=pT.rearrange("p (k t) -> p k t", k=16)), reads=["pT"], writes=["hT32"])
        for kc in range(16):
            P.op("pe", lambda kc=kc: nc.tensor.matmul(pl[:], lhsT=hT32[:, kc, :], rhs=wr[:, kc, :], start=(kc == 0), stop=(kc == 15)),
                 reads=["hT32", "wr"], writes=["pl"])
        m_ = sm[b]
        mk = ("sm", b)
        P.op("dve", lambda: nc.vector.reduce_max(out=m_[:, 0:1], in_=pl[:], axis=AX.X), reads=["pl"], writes=[mk])
        P.op("dve", lambda: nc.vector.tensor_scalar(out=m_[:, 1:2], in0=m_[:, 0:1], scalar1=-1.0, scalar2=None, op0=ALU.mult), reads=[mk], writes=[mk])
        P.op("act", lambda: nc.scalar.activation(out=ex[b][:], in_=pl[:], func=AF.Exp, bias=m_[:, 1:2], accum_out=m_[:, 2:3]),
             reads=["pl", mk], writes=[("ex", b), mk])
        P.op("dve", lambda: nc.vector.reciprocal(out=m_[:, 3:4], in_=m_[:, 2:3]), reads=[mk], writes=[mk])
        P.op("dve", lambda: nc.vector.tensor_scalar(out=ex[b][:], in0=ex[b][:], scalar1=m_[:, 3:4], scalar2=None, op0=ALU.mult),
             reads=[("ex", b), mk], writes=[("ex", b)])
        P.dma("pool", affo[tok0:tok0 + 128, :], ex[b][:], reads=[("ex", b)], writes=["affo"])
    return P.finish(["x1o", "hfo", "affo"])


_PROJ_NC = [None]


def run_proj(fT_list, W, x_full, gm_, gffn, cf_, sf_, w_r):
    nc = build_proj()
    common = {"W": np.ascontiguousarray(W), "gm": bc128(gm_), "gffn": bc128(gffn), "cf": bc128(cf_), "sf": bc128(sf_),
              "wr": np.ascontiguousarray(w_r)}
    maps = []
    for i in range(NCORES):
        d = dict(common)
        d["fT"] = np.ascontiguousarray(fT_list[i])
        d["x"] = np.ascontiguousarray(x_full[i * TPC:(i + 1) * TPC])
        maps.append(d)
    res = run_bass_kernel_spmd(nc, maps, core_ids=list(range(NCORES)))
    x1 = np.concatenate([r["x1"] for r in res.results], 0)
    hf = np.concatenate([r["hf"] for r in res.results], 0)
    aff = np.concatenate([r["aff"] for r in res.results], 0)
    return x1, hf, aff


CAP = 1024
FE = 1024
NBIS = 32
OOB = 30000.0


def build_E():
    P = Prog()
    nc = P.nc
    affd = P.din("affT", [128, 2, 64], F32)
    ebd = P.din("ebase", [128, 2], F32)
    hfd = P.din("hf", [SEQ, D], BF16)
    wgd = P.din("wg", [2, D, FE], F32)
    wud = P.din("wu", [2, D, FE], F32)
    wdd = P.din("wd", [2, FE, D], F32)
    Yo = P.dout("Y", [2, CAP, D], F32)
    sloto = P.dout("slot", [128, 2, 64], I32)

    ident, idk = make_ident(P)
    onesf = P.sb("onesf", [128, 128], F32)
    UT = P.sb("UT", [128, 128], F32)
    Lb = P.sb("Lb", [128, 128], F32)
    iot_i = P.sb("iot_i", [128, 1024], I32)
    iot = P.sb("iot", [128, 1024], F32)
    jp_i = P.sb("jp_i", [128, 64, 2], I32)
    jp = P.sb("jp", [128, 64, 2], BF16)
    a = P.sb("a", [128, 2, 64], F32)
    eb = P.sb("eb", [128, 2], F32)
    msk = P.sb("msk", [128, 2, 64], F32)
    bs = P.sb("bs", [128, 16], F32)
    exs = P.sb("exs", [128, 128], F32)
    kv_i = P.sb("kv_i", [128, 2, 16], I32)
    kv = P.sb("kv", [128, 2, 16], F32)
    thr = P.sb("thr", [128, 2, 16], F32)
    mk4 = P.sb("mk4", [128, 2, 16, 64], F32)
    cn4 = P.sb("cn4", [128, 2, 16], F32)
    ge4 = P.sb("ge4", [128, 2, 16], F32)
    ct = P.sb("ct", [128, 1], F32)
    CB = P.sb("CB", [128, 128], F32)
    rk = P.sb("rk", [128, 2, 64], F32)
    sg = P.sb("sg", [128, 2, 64], F32)
    sgi = P.sb("sgi", [128, 2, 64], I32)
    sel = [P.sb("sel%d" % i, [128, 1024], BF16) for i in range(2)]
    idxf = P.sb("idxf", [128, 2, 8], F32)
    pselS = P.sb("pselS", [128, 8, 2], F32)
    zb = P.sb("zb", [128, 128], BF16)
    idxi = P.sb("idxi", [128, 2, 8], I32)
    xg = [P.sb("xg%d" % i, [128, D], BF16) for i in range(2)]
    xsT = P.sb("xsT", [128, 16, CAP], BF16)
    hT = P.sb("hTe", [128, 8, CAP], BF16)
    wgb = [P.sb("wgb%d" % i, [128, 16, 256], BF16) for i in range(2)]
    wub = [P.sb("wub%d" % i, [128, 16, 256], BF16) for i in range(2)]
    wdb = [P.sb("wdb%d" % i, [128, 8, 512], BF16) for i in range(2)]
    sa = [P.sb("sa%d" % i, [128, 512], F32) for i in range(2)]
    ys = [P.sb("ys%d" % i, [128, 512], F32) for i in range(2)]
    pa = [P.ps("pa%d" % i, [128, 512], F32) for i in range(2)]
    pu = [P.ps("pu%d" % i, [128, 512], F32) for i in range(2)]
    py = [P.ps("py%d" % i, [128, 512], F32) for i in range(2)]
    pT = P.ps("pT", [128, D], BF16)

    P.op("pool", lambda: nc.gpsimd.memset(onesf[:], 1.0), writes=["onesf"])
    P.op("pool", lambda: nc.gpsimd.memset(UT[:], 1.0), writes=["UT"])
    P.op("pool", lambda: nc.gpsimd.affine_select(out=UT[:], in_=UT[:], pattern=[[1, 128]], compare_op=ALU.is_ge, fill=0.0, base=0,
                                                 channel_multiplier=-1), reads=["UT"], writes=["UT"])
    P.op("pool", lambda: nc.gpsimd.memset(Lb[:], 1.0), writes=["Lb"])
    P.op("pool", lambda: nc.gpsimd.affine_select(out=Lb[:], in_=Lb[:], pattern=[[1, 128]], compare_op=ALU.is_gt, fill=0.0, base=0,
                                                 channel_multiplier=-1), reads=["Lb"], writes=["Lb"])
    P.op("pool", lambda: nc.gpsimd.memset(Lb[0:64, 64:128], 0.0), reads=["Lb"], writes=["Lb"])
    P.op("pool", lambda: nc.gpsimd.iota(iot_i[:], pattern=[[1, 1024]], base=0, channel_multiplier=0), writes=["iot_i"])
    P.op("dve", lambda: nc.vector.tensor_copy(out=iot[:], in_=iot_i[:]), reads=["iot_i"], writes=["iot"])
    P.op("pool", lambda: nc.gpsimd.iota(jp_i[:, :, 0], pattern=[[1, 64]], base=0, channel_multiplier=0), writes=["jp_i"])
    P.op("pool", lambda: nc.gpsimd.iota(jp_i[:, :, 1], pattern=[[0, 64]], base=0, channel_multiplier=1), reads=["jp_i"], writes=["jp_i"])
    P.op("dve", lambda: nc.vector.tensor_copy(out=jp[:], in_=jp_i[:]), reads=["jp_i"], writes=["jp"])
    P.dma("sp", a[:], affd[:, :, :], writes=["a"])
    P.dma("sp", eb[:], ebd[:, :], writes=["eb"])
    P.op("pool", lambda: nc.gpsimd.memset(zb[:], 0.0), writes=["zb"])

    lo, stp, nn, tmp = bs[:, 0:2], bs[:, 2:4], bs[:, 4:6], bs[:, 6:8]
    P.op("dve", lambda: nc.vector.memset(bs[:, 0:2], 0.0), writes=["bs"])
    P.op("dve", lambda: nc.vector.memset(bs[:, 2:4], 1.001 / 16), reads=["bs"], writes=["bs"])
    P.op("pool", lambda: nc.gpsimd.iota(kv_i[:], pattern=[[0, 2], [1, 16]], base=0, channel_multiplier=0), writes=["kv_i"])
    P.op("dve", lambda: nc.vector.tensor_copy(out=kv[:], in_=kv_i[:]), reads=["kv_i"], writes=["kv"])
    tot = pa[0][:, 0:32].rearrange("p (e k) -> p e k", e=2)

    def V(fn, reads=("bs",), writes=("bs",)):
        P.op("dve", fn, reads=list(reads), writes=list(writes))

    for itb in range(8):
        V(lambda: nc.vector.tensor_tensor(out=thr[:], in0=kv[:], in1=stp.unsqueeze(2).to_broadcast([128, 2, 16]), op=ALU.mult),
          reads=["kv", "bs"], writes=["thr"])
        V(lambda: nc.vector.tensor_tensor(out=thr[:], in0=thr[:], in1=lo.unsqueeze(2).to_broadcast([128, 2, 16]), op=ALU.add),
          reads=["thr", "bs"], writes=["thr"])
        V(lambda: nc.vector.tensor_tensor(out=mk4[:], in0=a[:, :, :].unsqueeze(2).to_broadcast([128, 2, 16, 64]),
                                          in1=thr[:, :, :].unsqueeze(3).to_broadcast([128, 2, 16, 64]), op=ALU.is_ge),
          reads=["a", "thr"], writes=["mk4"])
        V(lambda: nc.vector.reduce_sum(out=cn4[:], in_=mk4[:], axis=AX.X), reads=["mk4"], writes=["cn4"])
        P.op("pe", lambda: nc.tensor.matmul(pa[0][:, 0:32], lhsT=onesf[:], rhs=cn4.rearrange("p e k -> p (e k)"), start=True, stop=True),
             reads=["onesf", "cn4"], writes=[("pa", 0)])
        V(lambda: nc.vector.tensor_scalar(out=ge4[:], in0=tot, scalar1=float(CAP) - 0.5, scalar2=None, op0=ALU.is_ge),
          reads=[("pa", 0)], writes=["ge4"])
        V(lambda: nc.vector.reduce_sum(out=nn, in_=ge4[:], axis=AX.X), reads=["ge4", "bs"], writes=["bs"])
        V(lambda: nc.vector.scalar_tensor_tensor(out=tmp, in0=nn, scalar=-1.0, in1=stp, op0=ALU.add, op1=ALU.mult))
        V(lambda: nc.vector.tensor_tensor(out=lo, in0=lo, in1=tmp, op=ALU.add))
        V(lambda: nc.vector.tensor_scalar(out=stp, in0=stp, scalar1=0.0625, scalar2=None, op0=ALU.mult))
    V(lambda: nc.vector.tensor_tensor(out=msk[:], in0=a[:], in1=lo.unsqueeze(2).to_broadcast([128, 2, 64]), op=ALU.is_ge),
      reads=["a", "bs"], writes=["msk"])

    mflat = msk.rearrange("p e j -> p (e j)")
    pc = pa[1][:, 0:128]
    pex = pu[0][:, 0:128]
    pct = pu[1][:, 0:1]
    P.op("pe", lambda: nc.tensor.matmul(pc, lhsT=UT[:], rhs=mflat, start=True, stop=True), reads=["UT", "msk"], writes=[("pa", 1)])
    P.op("pe", lambda: nc.tensor.matmul(pct, lhsT=mflat, rhs=onesf[:, 0:1], start=True, stop=True), reads=["onesf", "msk"], writes=[("pu", 1)])
    P.op("dve", lambda: nc.vector.tensor_copy(out=ct[:], in_=pct), reads=[("pu", 1)], writes=["ct"])
    P.op("dve", lambda: nc.vector.tensor_scalar(out=CB[:], in0=onesf[:], scalar1=ct[:, 0:1], scalar2=None, op0=ALU.mult),
         reads=["onesf", "ct"], writes=["CB"])
    P.op("pe", lambda: nc.tensor.matmul(pex, lhsT=CB[:], rhs=Lb[:], start=True, stop=True), reads=["CB", "Lb"], writes=[("pu", 0)])
    P.op("dve", lambda: nc.vector.tensor_copy(out=exs[:], in_=pex), reads=[("pu", 0)], writes=["exs"])
    rkf = rk.rearrange("p e j -> p (e j)")
    P.op("dve", lambda: nc.vector.tensor_tensor(out=rkf, in0=pc, in1=exs[:], op=ALU.add), reads=[("pa", 1), "exs"], writes=["rk"])
    P.op("dve", lambda: nc.vector.tensor_tensor(out=rkf, in0=rkf, in1=mflat, op=ALU.mult), reads=["rk", "msk"], writes=["rk"])
    P.op("dve", lambda: nc.vector.tensor_scalar(out=rkf, in0=rkf, scalar1=-1.0, scalar2=None, op0=ALU.add), reads=["rk"], writes=["rk"])
    P.op("dve", lambda: nc.vector.tensor_tensor(out=sg[:], in0=rk[:], in1=eb[:, :].unsqueeze(2).to_broadcast([128, 2, 64]), op=ALU.add),
         reads=["rk", "eb"], writes=["sg"])
    P.op("dve", lambda: nc.vector.tensor_scalar(out=sg[:], in0=sg[:], scalar1=-OOB, scalar2=None, op0=ALU.add), reads=["sg"], writes=["sg"])
    P.op("dve", lambda: nc.vector.tensor_tensor(out=sg[:], in0=sg[:], in1=msk[:], op=ALU.mult), reads=["sg", "msk"], writes=["sg"])
    P.op("dve", lambda: nc.vector.tensor_scalar(out=sg[:], in0=sg[:], scalar1=OOB, scalar2=None, op0=ALU.add), reads=["sg"], writes=["sg"])
    P.op("dve", lambda: nc.vector.tensor_copy(out=sgi[:], in_=sg[:]), reads=["sg"], writes=["sgi"])
    P.dma("sp", sloto[:, :, :], sgi[:], reads=["sgi"], writes=["sloto"])

    for e in range(2):
        psel = py[e][:, 0:16].rearrange("p (s c) -> p s c", c=2)
        P.op("pe", lambda: nc.tensor.matmul(py[e][:, 0:16], lhsT=zb[:, 0:128], rhs=zb[:, 0:16], start=True, stop=False),
             reads=["zb"], writes=[("py", e)])
        for j in range(64):
            sb_ = sel[j % 2]
            P.op("dve", lambda: nc.vector.tensor_scalar(out=sb_[:], in0=iot[:], scalar1=rk[:, e, j:j + 1], scalar2=None, op0=ALU.is_equal),
                 reads=["iot", "rk"], writes=[("sel", j % 2)])
            for s in range(8):
                P.op("pe", lambda s=s: nc.tensor.matmul(psel[:, s, :], lhsT=sb_[:, s * 128:(s + 1) * 128], rhs=jp[:, j, :],
                                                         start=False, stop=(j == 63), skip_group_check=True),
                     reads=[("sel", j % 2), "jp"], writes=[("py", e)])
        P.op("dve", lambda: nc.vector.tensor_copy(out=pselS[:], in_=psel), reads=[("py", e)], writes=["pselS"])
        P.op("dve", lambda: nc.vector.scalar_tensor_tensor(out=idxf[:, e, :], in0=pselS[:, :, 0], scalar=128.0, in1=pselS[:, :, 1],
                                                           op0=ALU.mult, op1=ALU.add),
             reads=["pselS"], writes=["idxf"])
    P.op("dve", lambda: nc.vector.tensor_copy(out=idxi[:], in_=idxf[:]), reads=["idxf"], writes=["idxi"])

    CL = CastLoader(P, 1024)
    gi = 0
    for e in range(2):
        for s in range(8):
            g_ = gi % 2
            gi += 1
            P.idma(xg[g_][:], None, hfd[:, :], bass.IndirectOffsetOnAxis(ap=idxi[:, e, s:s + 1], axis=0),
                   reads=["idxi"], writes=[("xg", g_)])
            for kc in range(16):
                P.op("pe", lambda kc=kc: nc.tensor.transpose(pT[:, kc * 128:(kc + 1) * 128], xg[g_][:, kc * 128:(kc + 1) * 128], ident[:]),
                     reads=[("xg", g_), idk], writes=["pT"])
            P.op("act", lambda: nc.scalar.copy(out=xsT[:, :, s * 128:(s + 1) * 128], in_=pT.rearrange("p (k t) -> p k t", k=16)),
                 reads=["pT"], writes=["xsT"])
        wgv = wgd[e].rearrange("(kc p) f -> p kc f", p=128)
        wuv = wud[e].rearrange("(kc p) f -> p kc f", p=128)
        wdv = wdd[e].rearrange("(fc p) d -> p fc d", p=128)
        zi = 0

        def queue_gu(f2, wgv=wgv, wuv=wuv):
            b = f2 % 2
            for kq in range(4):
                CL.enqueue(wgb[b][:, kq * 4:(kq + 1) * 4, :], ("wgb", b), wgv[:, kq * 4:(kq + 1) * 4, f2 * 256:(f2 + 1) * 256], (4, 256))
                CL.enqueue(wub[b][:, kq * 4:(kq + 1) * 4, :], ("wub", b), wuv[:, kq * 4:(kq + 1) * 4, f2 * 256:(f2 + 1) * 256], (4, 256))

        def queue_d(dc, wdv=wdv):
            b = dc % 2
            for fq in range(4):
                CL.enqueue(wdb[b][:, fq * 2:(fq + 1) * 2, :], ("wdb", b), wdv[:, fq * 2:(fq + 1) * 2, dc * 512:(dc + 1) * 512], (2, 512))

        if e == 0:
            queue_gu(0)
        for f2 in range(4):
            b = f2 % 2
            CL.flush()
            if f2 + 1 < 4:
                queue_gu(f2 + 1)
            else:
                queue_d(0)
            for fs in range(2):
                fc = f2 * 2 + fs
                for hh in range(2):
                    z = zi % 2
                    zi += 1
                    for kc in range(16):
                        P.op("pe", lambda kc=kc: nc.tensor.matmul(pa[z][:], lhsT=wgb[b][:, kc, fs * 128:(fs + 1) * 128],
                                                                   rhs=xsT[:, kc, hh * 512:(hh + 1) * 512], start=(kc == 0), stop=(kc == 15)),
                             reads=[("wgb", b), "xsT"], writes=[("pa", z)])
                    for kc in range(16):
                        P.op("pe", lambda kc=kc: nc.tensor.matmul(pu[z][:], lhsT=wub[b][:, kc, fs * 128:(fs + 1) * 128],
                                                                   rhs=xsT[:, kc, hh * 512:(hh + 1) * 512], start=(kc == 0), stop=(kc == 15)),
                             reads=[("wub", b), "xsT"], writes=[("pu", z)])
                    P.op("act", lambda: nc.scalar.activation(out=sa[z][:], in_=pa[z][:], func=AF.Silu), reads=[("pa", z)], writes=[("sa", z)])
                    P.op("dve", lambda: nc.vector.tensor_tensor(out=hT[:, fc, hh * 512:(hh + 1) * 512], in0=pu[z][:], in1=sa[z][:], op=ALU.mult),
                         reads=[("pu", z), ("sa", z)], writes=["hT"])
                    CL.pump(2)
        yi = 0
        for dc in range(4):
            b = dc % 2
            CL.flush()
            if dc + 1 < 4:
                queue_d(dc + 1)
            elif e == 0:
                wgv1 = wgd[1].rearrange("(kc p) f -> p kc f", p=128)
                wuv1 = wud[1].rearrange("(kc p) f -> p kc f", p=128)
                queue_gu(0, wgv1, wuv1)
            for s in range(8):
                z = yi % 2
                yi += 1
                for fc in range(8):
                    P.op("pe", lambda fc=fc: nc.tensor.matmul(py[z][:], lhsT=hT[:, fc, s * 128:(s + 1) * 128], rhs=wdb[b][:, fc, :],
                                                               start=(fc == 0), stop=(fc == 7)),
                         reads=["hT", ("wdb", b)], writes=[("py", z)])
                P.op("act", lambda: nc.scalar.copy(out=ys[z][:], in_=py[z][:]), reads=[("py", z)], writes=[("ys", z)])
                P.dma("act", Yo[e, s * 128:(s + 1) * 128, dc * 512:(dc + 1) * 512], ys[z][:], reads=[("ys", z)], writes=["Yo"])
                CL.pump(1)
    return P.finish(["Yo", "sloto"])


def run_E(aff, hf, wg, wu, wd):
    nc = build_E()
    hf = np.ascontiguousarray(hf)
    maps = []
    for c in range(NCORES):
        es = slice(2 * c, 2 * c + 2)
        affT = np.ascontiguousarray(aff[:, es].reshape(64, 128, 2).transpose(1, 2, 0))
        ebase = np.ascontiguousarray(np.broadcast_to(np.array([[2 * c * CAP, (2 * c + 1) * CAP]], np.float32), (128, 2)))
        maps.append({"affT": affT, "ebase": ebase, "hf": hf, "wg": np.ascontiguousarray(wg[es]), "wu": np.ascontiguousarray(wu[es]),
                     "wd": np.ascontiguousarray(wd[es])})
    res = run_bass_kernel_spmd(nc, maps, core_ids=list(range(NCORES)))
    Yall = np.concatenate([r["Y"].reshape(2 * CAP, D) for r in res.results], 0)
    slots = np.concatenate([r["slot"].transpose(2, 0, 1).reshape(SEQ, 2) for r in res.results], 1)
    return Yall, np.ascontiguousarray(slots)


NYROWS = 16 * CAP


def combine_setup(P):
    nc = P.nc
    c = {}
    c["x1d"] = P.din("x1", [TPC, D], F32)
    c["Yd"] = P.din("Yall", [NYROWS, D], F32)
    c["sld"] = P.din("slots", [TPC, 16], I32)
    c["afd"] = P.din("aff", [TPC, 16], F32)
    c["gfd"] = P.din("gf", [128, D], F32)
    c["sl"] = P.sb("sl", [128, NT, 16], I32)
    c["af"] = P.sb("af", [128, NT, 16], F32)
    c["gf"] = P.sb("gfs", [128, D], F32)
    c["gt"] = [P.sb("gt%d" % i, [128, D], F32) for i in range(3)]
    c["acc"] = [P.sb("acc%d" % i, [128, D], F32) for i in range(2)]
    c["xs"] = [P.sb("cxs%d" % i, [128, D], F32) for i in range(2)]
    P.dma("sp", c["sl"][:], c["sld"].rearrange("(t p) e -> p t e", p=128), writes=["sl"])
    P.dma("sp", c["af"][:], c["afd"].rearrange("(t p) e -> p t e", p=128), writes=["af"])
    P.dma("sp", c["gf"][:], c["gfd"][:, :], writes=["gfs"])
    c["gi"] = 0
    c["breg"] = nc.gpsimd.to_reg(NYROWS - 1)
    return c


def combine_tile(P, c, ti):
    nc = P.nc
    b = ti % 2
    acc = c["acc"][b]
    ak = ("acc", b)
    xs = c["xs"][b]
    xk = ("cxs", b)
    P.dma("sp", xs[:], c["x1d"][ti * 128:(ti + 1) * 128, :], writes=[xk])
    for e in range(16):
        g = c["gi"] % 3
        c["gi"] += 1
        gt = c["gt"][g]
        gk = ("gt", g)
        if e % 2 == 0:
            P.op("act", lambda: nc.scalar.memzero(gt[:]), writes=[gk])
        else:
            P.op("pool", lambda: nc.gpsimd.memset(gt[:], 0.0), writes=[gk])
        P.idma(gt[:], None, c["Yd"][:, :], bass.IndirectOffsetOnAxis(ap=c["sl"][:, ti, e:e + 1], axis=0),
               reads=["sl"], writes=[gk], semkey=("dg", g), bounds_check=c["breg"], oob_is_err=False)
        if e == 0:
            P.op("dve", lambda: nc.vector.tensor_scalar(out=acc[:], in0=gt[:], scalar1=c["af"][:, ti, e:e + 1], scalar2=None, op0=ALU.mult),
                 reads=[gk, "af"], writes=[ak])
        else:
            P.op("dve", lambda: nc.vector.scalar_tensor_tensor(out=acc[:], in0=gt[:], scalar=c["af"][:, ti, e:e + 1], in1=acc[:],
                                                               op0=ALU.mult, op1=ALU.add),
                 reads=[gk, "af", ak], writes=[ak])
    P.op("dve", lambda: nc.vector.tensor_tensor(out=acc[:], in0=acc[:], in1=c["gf"][:], op=ALU.mult), reads=[ak, "gfs"], writes=[ak])
    P.op("pool", lambda: nc.gpsimd.tensor_tensor(out=xs[:], in0=xs[:], in1=acc[:], op=ALU.add), reads=[xk, ak], writes=[xk])
    return xs, xk


def build_F():
    P = Prog()
    nc = P.nc
    c = combine_setup(P)
    gmixd = P.din("gmix", [128, D], F32)
    cmd = P.din("cm", [128, D], F32)
    smd = P.din("sm", [128, D], F32)
    Cd = P.din("Cch", [512, 512], BF16)
    Sd = P.din("Sch", [512, 512], BF16)
    x2o = P.dout("x2", [TPC, D], F32)
    Ao = P.dout("A", [TPC, D], BF16)
    Bo = P.dout("B", [TPC, D], BF16)
    ident, idk = make_ident(P)
    gam = P.sb("gam", [128, D], F32)
    sh = P.sb("sh", [128, D], F32)
    hb = [P.sb("hb%d" % i, [128, D], BF16) for i in range(2)]
    hTt = P.sb("hTt", [128, 16, 128], BF16)
    Cs = P.sb("Cs", [128, 4, 512], BF16)
    Ss = P.sb("Ss", [128, 4, 512], BF16)
    ao = [P.sb("ao%d" % i, [128, 512], BF16) for i in range(2)]
    ss = [P.sb("ss%d" % i, [128, 4], F32) for i in range(2)]
    epsb = P.sb("epsb", [128, 1], F32)
    pT = P.ps("pT", [128, D], BF16)
    pz = [P.ps("pz%d" % i, [128, 512], F32) for i in range(2)]
    P.op("pool", lambda: nc.gpsimd.memset(epsb[:], EPS), writes=["epsb"])
    P.dma("act", gam[:], cmd[:, :], writes=["gam"])
    P.dma("act", c["gt"][0][:], gmixd[:, :], writes=[("gt", 0)], semkey=("dg", 0))
    P.dma("act", sh[:], smd[:, :], writes=["sh"])
    P.dma("act", Cs[:], Cd.rearrange("(k p) n -> p k n", p=128), writes=["Cs"])
    P.dma("act", Ss[:], Sd.rearrange("(k p) n -> p k n", p=128), writes=["Ss"])
    P.op("dve", lambda: nc.vector.scalar_tensor_tensor(out=gam[:], in0=gam[:], scalar=1.0, in1=c["gt"][0][:], op0=ALU.add, op1=ALU.mult),
         reads=["gam", ("gt", 0)], writes=["gam"])
    zc = 0
    for ti in range(NT):
        b = ti % 2
        xs, xk = combine_tile(P, c, ti)
        P.dma("act", x2o[ti * 128:(ti + 1) * 128, :], xs[:], reads=[xk], writes=["x2o"])
        rms_modulate_tile(P, nc, xs, xk, 128, ss[b], ("ss", b), epsb, gam, sh, hb[b], ("hb", b))
        for kc in range(16):
            P.op("pe", lambda kc=kc: nc.tensor.transpose(pT[:, kc * 128:(kc + 1) * 128], hb[b][:, kc * 128:(kc + 1) * 128], ident[:]),
                 reads=[("hb", b), idk], writes=["pT"])
        P.op("act", lambda: nc.scalar.copy(out=hTt[:], in_=pT.rearrange("p (k t) -> p k t", k=16)), reads=["pT"], writes=["hTt"])
        for g in range(4):
            for (W_, wk, outd) in ((Cs, "Cs", Ao), (Ss, "Ss", Bo)):
                j = zc % 2
                zc += 1
                for k in range(4):
                    P.op("pe", lambda k=k: nc.tensor.matmul(pz[j][:], lhsT=hTt[:, g * 4 + k, :], rhs=W_[:, k, :], start=(k == 0), stop=(k == 3)),
                         reads=["hTt", wk], writes=[("pz", j)])
                P.op("act", lambda: nc.scalar.copy(out=ao[j][:], in_=pz[j][:]), reads=[("pz", j)], writes=[("ao", j)])
                P.dma("act", outd[ti * 128:(ti + 1) * 128, g * 512:(g + 1) * 512], ao[j][:], reads=[("ao", j)], writes=["AB"])
    return P.finish(["x2o", "AB"])


def bf16_np(a):
    import ml_dtypes
    return np.ascontiguousarray(np.asarray(a, np.float32).astype(ml_dtypes.bfloat16))


def dft_consts():
    c = np.arange(512)
    ang = 2 * np.pi * np.outer(c, c) / 512
    Cch = np.cos(ang) / math.sqrt(512)
    Sch = np.sin(ang) / math.sqrt(512)
    n1 = np.arange(128)
    a1 = 2 * np.pi * np.outer(n1, n1) / 128
    Cc = np.cos(a1) / math.sqrt(128)
    Ssn = np.sin(a1) / math.sqrt(128)
    n2 = np.arange(64)[:, None, None]
    k1 = np.arange(128)[None, :, None]
    k2 = np.arange(64)[None, None, :]
    th = 2 * np.pi * (n2 * k1 / 8192.0 + n2 * k2 / 64.0)
    Hr = np.cos(th) / 8.0
    Hni = np.sin(th) / 8.0
    return dict(Cch=bf16_np(Cch), Sch=bf16_np(Sch), Cc=bf16_np(Cc), nSs=bf16_np(-Ssn), nCc=bf16_np(-Cc), Hr=bf16_np(Hr), Hni=bf16_np(Hni))


def run_F(x1, Yall, slots, aff, gf_, gmix, cm_, sm_):
    nc = build_F()
    k = dft_consts()
    common = {"Yall": Yall, "gf": bc128(gf_), "gmix": bc128(gmix), "cm": bc128(cm_), "sm": bc128(sm_), "Cch": k["Cch"], "Sch": k["Sch"]}
    maps = []
    for i in range(NCORES):
        d = dict(common)
        sl = slice(i * TPC, (i + 1) * TPC)
        d["x1"] = np.ascontiguousarray(x1[sl])
        d["slots"] = np.ascontiguousarray(slots[sl])
        d["aff"] = np.ascontiguousarray(aff[sl])
        maps.append(d)
    res = run_bass_kernel_spmd(nc, maps, core_ids=list(range(NCORES)))
    x2 = np.concatenate([r["x2"] for r in res.results], 0)
    A = np.concatenate([r["A"] for r in res.results], 0)
    B = np.concatenate([r["B"] for r in res.results], 0)
    return x2, A, B


CPC = D // NCORES


def build_G():
    P = Prog()
    nc = P.nc
    zAd = P.din("zA", [128, 64, CPC], BF16)
    zBd = P.din("zB", [128, 64, CPC], BF16)
    Ccd = P.din("Cc", [128, 128], BF16)
    nSsd = P.din("nSs", [128, 128], BF16)
    nCcd = P.din("nCc", [128, 128], BF16)
    Hrd = P.din("Hr", [64, 128, 64], BF16)
    Hnid = P.din("Hni", [64, 128, 64], BF16)
    YTo = P.dout("YT", [CPC, SEQ], BF16)
    zA = P.sb("zAs", [128, 64, 128], BF16)
    zB = P.sb("zBs", [128, 64, 128], BF16)
    Cc = P.sb("Ccs", [128, 128], BF16)
    nSs = P.sb("nSss", [128, 128], BF16)
    nCc = P.sb("nCcs", [128, 128], BF16)
    Hr = P.sb("Hrs", [64, 128, 64], BF16)
    Hni = P.sb("Hnis", [64, 128, 64], BF16)
    Tr = P.sb("Tr", [64, 128, 128], BF16)
    Ti = P.sb("Ti", [64, 128, 128], BF16)
    YTs = P.sb("YTs", [128, 64, 128], BF16)
    pTr = [P.ps("pTr%d" % i, [64, 4, 128], F32) for i in range(2)]
    pTi = [P.ps("pTi%d" % i, [64, 4, 128], F32) for i in range(2)]
    pY = [P.ps("pY%d" % i, [128, 8, 64], F32) for i in range(2)]
    P.dma("sp", Cc[:], Ccd[:, :], writes=["Cc"])
    P.dma("sp", nSs[:], nSsd[:, :], writes=["nSs"])
    P.dma("sp", nCc[:], nCcd[:, :], writes=["nCc"])
    P.dma("act", Hr[:], Hrd[:, :, :], writes=["Hr"])
    P.dma("act", Hni[:], Hnid[:, :, :], writes=["Hni"])
    for hc in range(2):
        c0 = hc * 128
        P.dma("sp", zA[:], zAd[:, :, c0:c0 + 128], writes=["zA"])
        P.dma("act", zB[:], zBd[:, :, c0:c0 + 128], writes=["zB"])
        for cb in range(32):
            j = cb % 2
            for cc in range(4):
                c = cb * 4 + cc
                P.op("pe", lambda: nc.tensor.matmul(pTr[j][:, cc, :], lhsT=zA[:, :, c], rhs=Cc[:], start=True, stop=False),
                     reads=["zA", "Cc"], writes=[("pTr", j)])
                P.op("pe", lambda: nc.tensor.matmul(pTr[j][:, cc, :], lhsT=zB[:, :, c], rhs=nSs[:], start=False, stop=True),
                     reads=["zB", "nSs"], writes=[("pTr", j)])
                P.op("pe", lambda: nc.tensor.matmul(pTi[j][:, cc, :], lhsT=zB[:, :, c], rhs=nCc[:], start=True, stop=False),
                     reads=["zB", "nCc"], writes=[("pTi", j)])
                P.op("pe", lambda: nc.tensor.matmul(pTi[j][:, cc, :], lhsT=zA[:, :, c], rhs=nSs[:], start=False, stop=True),
                     reads=["zA", "nSs"], writes=[("pTi", j)])
            P.op("act", lambda: nc.scalar.copy(out=Tr[:, cb * 4:(cb + 1) * 4, :], in_=pTr[j][:]), reads=[("pTr", j)], writes=["Tr"])
            P.op("dve", lambda: nc.vector.tensor_copy(out=Ti[:, cb * 4:(cb + 1) * 4, :], in_=pTi[j][:]), reads=[("pTi", j)], writes=["Ti"])
        for kb in range(16):
            j = kb % 2
            for kk in range(8):
                k1 = kb * 8 + kk
                P.op("pe", lambda: nc.tensor.matmul(pY[j][:, kk, :], lhsT=Tr[:, :, k1], rhs=Hr[:, k1, :], start=True, stop=False),
                     reads=["Tr", "Hr"], writes=[("pY", j)])
                P.op("pe", lambda: nc.tensor.matmul(pY[j][:, kk, :], lhsT=Ti[:, :, k1], rhs=Hni[:, k1, :], start=False, stop=True),
                     reads=["Ti", "Hni"], writes=[("pY", j)])
            eng = "act" if kb % 2 == 0 else "dve"
            if eng == "act":
                P.op("act", lambda: nc.scalar.copy(out=YTs[:, :, kb * 8:(kb + 1) * 8], in_=pY[j].rearrange("p a b -> p b a")),
                     reads=[("pY", j)], writes=["YTs"])
            else:
                P.op("dve", lambda: nc.vector.tensor_copy(out=YTs[:, :, kb * 8:(kb + 1) * 8], in_=pY[j].rearrange("p a b -> p b a")),
                     reads=[("pY", j)], writes=["YTs"])
        P.dma("sp", YTo[c0:c0 + 128, :], YTs.rearrange("p a b -> p (a b)"), reads=["YTs"], writes=["YTo"])
    return P.finish(["YTo"])


def run_G(A, B):
    nc = build_G()
    k = dft_consts()
    maps = []
    for i in range(NCORES):
        cs = slice(i * CPC, (i + 1) * CPC)
        maps.append({"zA": np.ascontiguousarray(A[:, cs]).reshape(128, 64, CPC), "zB": np.ascontiguousarray(B[:, cs]).reshape(128, 64, CPC),
                     "Cc": k["Cc"], "nSs": k["nSs"], "nCc": k["nCc"], "Hr": k["Hr"], "Hni": k["Hni"]})
    res = run_bass_kernel_spmd(nc, maps, core_ids=list(range(NCORES)))
    YT = np.concatenate([r["YT"] for r in res.results], 0)
    return YT


def build_I():
    P = Prog()
    nc = P.nc
    c = combine_setup(P)
    gfin_d = P.din("gfin", [128, D], F32)
    outd = P.dout("out", [TPC, D], F32)
    gfin = P.sb("gfin_s", [128, D], F32)
    jk = P.sb("junk", [128, D], BF16)
    ss = [P.sb("ss%d" % i, [128, 4], F32) for i in range(2)]
    epsb = P.sb("epsb", [128, 1], F32)
    P.op("pool", lambda: nc.gpsimd.memset(epsb[:], EPS), writes=["epsb"])
    P.dma("act", gfin[:], gfin_d[:, :], writes=["gfin"])
    for ti in range(NT):
        b = ti % 2
        xs, xk = combine_tile(P, c, ti)
        s_ = ss[b]
        sk = ("ss", b)
        P.op("act", lambda: nc.scalar.activation(out=jk[:], in_=xs[:], func=AF.Square, accum_out=s_[:, 0:1]), reads=[xk], writes=["junk", sk])
        P.op("act", lambda: nc.scalar.activation(out=s_[:, 1:2], in_=s_[:, 0:1], func=AF.Sqrt, scale=1.0 / D, bias=epsb[:, 0:1]),
             reads=[sk, "epsb"], writes=[sk])
        P.op("dve", lambda: nc.vector.reciprocal(out=s_[:, 2:3], in_=s_[:, 1:2]), reads=[sk], writes=[sk])
        P.op("dve", lambda: nc.vector.scalar_tensor_tensor(out=xs[:], in0=xs[:], scalar=s_[:, 2:3], in1=gfin[:], op0=ALU.mult, op1=ALU.mult),
             reads=[xk, sk, "gfin"], writes=[xk])
        P.dma("act", outd[ti * 128:(ti + 1) * 128, :], xs[:], reads=[xk], writes=["outd"])
    return P.finish(["outd"])


def run_I(x1, Yall, slots, aff, gf_, gfin):
    nc = build_I()
    common = {"Yall": Yall, "gf": bc128(gf_), "gfin": bc128(gfin)}
    maps = []
    for i in range(NCORES):
        d = dict(common)
        sl = slice(i * TPC, (i + 1) * TPC)
        d["x1"] = np.ascontiguousarray(x1[sl])
        d["slots"] = np.ascontiguousarray(slots[sl])
        d["aff"] = np.ascontiguousarray(aff[sl])
        maps.append(d)
    res = run_bass_kernel_spmd(nc, maps, core_ids=list(range(NCORES)))
    return np.concatenate([r["out"] for r in res.results], 0)


def kernel(**inp):
    inp = {k: np.asarray(v) for k, v in inp.items()}
    m = run_mod(inp)
    x0 = inp["x"][0]
    ra = run_A(inp, m)
    aT = run_B(inp, ra)
    fT = [np.concatenate([np.stack([aT[h][:, i * TPC:(i + 1) * TPC] for h in range(8)], 0), ra[i]["gT"]], 0) for i in range(NCORES)]
    m0 = m[0, 0]
    x1, hf, aff = run_proj(fT, inp["w_out"][0], x0, m0[2 * D:3 * D], inp["g_norm_ffn"][0], m0[4 * D:5 * D], m0[3 * D:4 * D],
                           inp["w_router"][0])
    Yall, slots = run_E(aff, hf, inp["w_gate"][0], inp["w_up"][0], inp["w_down"][0])
    m1 = m[1, 0]
    x2, A, B = run_F(x1, Yall, slots, aff, m0[5 * D:6 * D], inp["g_norm_mix"][1], m1[D:2 * D], m1[0:D])
    YT = run_G(A, B)
    fT = [np.ascontiguousarray(YT[:, i * TPC:(i + 1) * TPC]).reshape(16, 128, TPC) for i in range(NCORES)]
    x1, hf, aff = run_proj(fT, inp["w_fourier_out"][0], x2, m1[2 * D:3 * D], inp["g_norm_ffn"][1], m1[4 * D:5 * D], m1[3 * D:4 * D],
                           inp["w_router"][1])
    Yall, slots = run_E(aff, hf, inp["w_gate"][1], inp["w_up"][1], inp["w_down"][1])
    out = run_I(x1, Yall, slots, aff, m1[5 * D:6 * D], inp["g_final"])
    return np.ascontiguousarray(out.reshape(1, SEQ, D).astype(np.float32))
```

```python
import math
import numpy as np
from contextlib import ExitStack
import concourse.bass as bass
import concourse.mybir as mybir
from concourse.bass_utils import run_bass_kernel_spmd

F32 = mybir.dt.float32
BF16 = mybir.dt.bfloat16
I32 = mybir.dt.int32
AF = mybir.ActivationFunctionType
ALU = mybir.AluOpType
AX = mybir.AxisListType
NCORES = 8
D = 2048
SEQ = 8192
TPC = SEQ // NCORES
NT = TPC // 128
EPS = 1e-6


class Res:
    __slots__ = ("w", "r")

    def __init__(self):
        self.w = {}
        self.r = {}


class Prog:
    def __init__(self):
        self.nc = bass.Bass("TRN2", target_bir_lowering=False)
        self.es = ExitStack()
        nc = self.nc
        self.eng = {"pe": nc.tensor, "act": nc.scalar, "dve": nc.vector,
                    "pool": nc.gpsimd, "sp": nc.sync}
        self.sems = {}
        self.cnt = {}
        for e in self.eng:
            self.sems[e] = self.es.enter_context(nc.semaphore("s_" + e))
            self.cnt[e] = 0
        self.known = {e: {} for e in self.eng}
        self.res = {}
        self.nd = 0

    def sb(self, name, shape, dt):
        return self.es.enter_context(self.nc.sbuf_tensor(name, list(shape), dt))

    def ps(self, name, shape, dt):
        return self.es.enter_context(self.nc.psum_tensor(name, list(shape), dt))

    def din(self, name, shape, dt):
        return self.nc.dram_tensor(name, list(shape), dt, kind="ExternalInput").ap()

    def dout(self, name, shape, dt):
        return self.nc.dram_tensor(name, list(shape), dt, kind="ExternalOutput").ap()

    def dtmp(self, name, shape, dt):
        return self.nc.dram_tensor(name, list(shape), dt, kind="Internal").ap()

    def R(self, key):
        r = self.res.get(key)
        if r is None:
            r = self.res[key] = Res()
        return r

    def _wait(self, e, semkey, val):
        if val <= 0:
            return
        k = self.known[e]
        if k.get(semkey, 0) >= val:
            return
        k[semkey] = val
        if getattr(self, "_pend", None) is not None:
            self._pend.append((semkey, val))
        else:
            self.eng[e].wait_ge(self.sems[semkey], val)

    def _deps(self, e, reads, writes, skip=None):
        for key in reads:
            for sk, v in self.R(key).w.items():
                if e == "pe" and sk == "pe":
                    continue
                self._wait(e, sk, v)
        for key in writes:
            r = self.R(key)
            for sk, v in r.w.items():
                if sk == skip or (e == "pe" and sk == "pe"):
                    continue
                self._wait(e, sk, v)
            for sk, v in r.r.items():
                if e == "pe" and sk == "pe":
                    continue
                self._wait(e, sk, v)

    def op(self, e, fn, reads=(), writes=()):
        self._pend = []
        self._deps(e, reads, writes)
        pend, self._pend = self._pend, None
        for (sk, v) in pend[:-1]:
            self.eng[e].wait_ge(self.sems[sk], v)
        ins = fn()
        if pend:
            ins._wait_ge(self.sems[pend[-1][0]], pend[-1][1])
        self.cnt[e] += 1
        c = self.cnt[e]
        ins.then_inc(self.sems[e], 1)
        for key in reads:
            self.R(key).r[e] = c
        for key in writes:
            r = self.R(key)
            r.w = {e: c}
            r.r = {}
        return ins

    def _dma_post(self, ins, sk, reads, writes):
        self.cnt[sk] += 16
        c = self.cnt[sk]
        ins.then_inc(self.sems[sk], 16)
        for key in reads:
            self.R(key).r[sk] = c
        for key in writes:
            r = self.R(key)
            if sk in r.w and len(r.w) == 1:
                r.w[sk] = c
            else:
                r.w = {sk: c}
            r.r = {}

    def _dsem(self, semkey, writes):
        sk = semkey if semkey is not None else ("d", writes[0])
        if sk not in self.sems:
            self.sems[sk] = self.es.enter_context(self.nc.semaphore("sd%d" % self.nd))
            self.nd += 1
            self.cnt[sk] = 0
        return sk

    def dma(self, q, out, in_, reads=(), writes=(), semkey=None, **kw):
        sk = self._dsem(semkey, writes)
        self._deps(q, reads, writes, skip=sk)
        ins = self.eng[q].dma_start(out=out, in_=in_, **kw)
        self._dma_post(ins, sk, reads, writes)
        return ins

    def idma(self, out, out_off, in_, in_off, reads=(), writes=(), semkey=None, **kw):
        sk = self._dsem(semkey, writes)
        self._deps("pool", reads, writes, skip=sk)
        ins = self.nc.gpsimd.indirect_dma_start(out, out_off, in_, in_off, **kw)
        self._dma_post(ins, sk, reads, writes)
        return ins

    def finish(self, out_keys, e="sp"):
        for key in out_keys:
            for sk, v in self.R(key).w.items():
                self._wait(e, sk, v)
        for o in self.eng:
            if o != e:
                self._wait(e, o, self.cnt[o])
        self.es.close()
        return self.nc


class CastLoader:
    def __init__(self, P, nelem, nbuf=4, engines=("pool", "dve", "pool")):
        self.P = P
        self.n = nelem
        self.stg = [P.sb("cstg%d" % i, [128, nelem], F32) for i in range(nbuf)]
        self.i = 0
        self.engines = engines
        self.fifo = []

    def load(self, dst_ap, dst_key, src_ap, shape):
        P, nc = self.P, self.P.nc
        k = self.i % len(self.stg)
        e = self.engines[self.i % len(self.engines)]
        self.i += 1
        a, b = shape
        sv = self.stg[k].rearrange("p (a b) -> p a b", a=a)
        P.dma("sp", sv, src_ap, writes=[("cstg", k)])
        if e == "pool":
            P.op("pool", lambda: nc.gpsimd.tensor_copy(out=dst_ap, in_=sv), reads=[("cstg", k)], writes=[dst_key])
        else:
            P.op("dve", lambda: nc.vector.tensor_copy(out=dst_ap, in_=sv), reads=[("cstg", k)], writes=[dst_key])

    def enqueue(self, *args):
        self.fifo.append(args)

    def pump(self, n=1):
        for _ in range(n):
            if not self.fifo:
                return
            self.load(*self.fifo.pop(0))

    def flush(self):
        self.pump(len(self.fifo))


def make_ident(P, dt=BF16, name="ident"):
    nc = P.nc
    idf = P.sb(name + "f", [128, 128], F32)
    P.op("pool", lambda: nc.gpsimd.memset(idf[:], 1.0), writes=[name + "f"])
    P.op("pool", lambda: nc.gpsimd.affine_select(out=idf[:], in_=idf[:], pattern=[[-1, 128]],
                                                 compare_op=ALU.is_equal, fill=0.0, base=0,
                                                 channel_multiplier=1),
         reads=[name + "f"], writes=[name + "f"])
    if dt == F32:
        return idf, name + "f"
    idb = P.sb(name, [128, 128], dt)
    P.op("dve", lambda: nc.vector.tensor_copy(out=idb[:], in_=idf[:]), reads=[name + "f"], writes=[name])
    return idb, name


def bc128(v):
    v = np.asarray(v, dtype=np.float32).reshape(1, -1)
    return np.ascontiguousarray(np.broadcast_to(v, (128, v.shape[1])))


MC = 6 * D // NCORES


def build_mod():
    P = Prog()
    nc = P.nc
    c1 = P.din("c1", [128, 16], F32)
    c2 = P.din("c2", [128, 16], F32)
    wm = P.din("wm", [2, D, MC], F32)
    bm = P.din("bm", [2, 2, MC], F32)
    mo = P.dout("mo", [2, 2, MC], F32)
    c1s = P.sb("c1s", [128, 16], F32)
    c2s = P.sb("c2s", [128, 16], F32)
    cc = P.sb("cc", [128, 16, 2], F32)
    bs = P.sb("bs", [2, 2, MC], F32)
    ms = P.sb("ms", [2, 2, MC], F32)
    HC = MC // 2
    wb = [P.sb("wmb%d" % i, [128, 16, HC], F32) for i in range(2)]
    pz = [P.ps("pz%d" % i, [2, 384], F32) for i in range(2)]
    P.dma("sp", c1s[:], c1[:, :], writes=["c1s"])
    P.dma("sp", c2s[:], c2[:, :], writes=["c2s"])
    P.dma("sp", bs[:], bm.rearrange("l s n -> s l n"), writes=["bs"])
    P.op("act", lambda: nc.scalar.activation(out=cc[:, :, 0], in_=c1s[:], func=AF.Silu), reads=["c1s"], writes=["cc0"])
    P.op("act", lambda: nc.scalar.activation(out=cc[:, :, 1], in_=c2s[:], func=AF.Silu), reads=["c2s"], writes=["cc1"])
    ci = 0
    for l in range(2):
        wv = wm[l].rearrange("(p kc) n -> p kc n", kc=16)
        for hf in range(2):
            b = ci % 2
            q = "sp" if ci % 2 == 0 else "act"
            for kq in range(4):
                P.dma(q, wb[b][:, kq * 4:(kq + 1) * 4, :], wv[:, kq * 4:(kq + 1) * 4, hf * HC:(hf + 1) * HC], writes=[("wmb", b)])
            for nb in range(HC // 384):
                pb = (ci * 2 + nb) % 2
                for kc in range(16):
                    P.op("pe", lambda kc=kc: nc.tensor.matmul(pz[pb][:], lhsT=cc[:, kc, :], rhs=wb[b][:, kc, nb * 384:(nb + 1) * 384],
                                                               start=(kc == 0), stop=(kc == 15)),
                         reads=["cc0", "cc1", ("wmb", b)], writes=[("pz", pb)])
                n0 = hf * HC + nb * 384
                P.op("dve", lambda: nc.vector.tensor_tensor(out=ms[:, l, n0:n0 + 384], in0=pz[pb][:], in1=bs[:, l, n0:n0 + 384], op=ALU.add),
                     reads=[("pz", pb), "bs"], writes=["ms"])
            ci += 1
    P.dma("sp", mo.rearrange("l s n -> s l n"), ms[:], reads=["ms"], writes=["mo"])
    return P.finish(["mo"])


def run_mod(inp):
    nc = build_mod()
    c1 = np.ascontiguousarray(inp["c"].reshape(128, 16))
    c2 = np.ascontiguousarray(inp["c_ctx"].reshape(128, 16))
    maps = []
    for i in range(NCORES):
        wm = np.ascontiguousarray(inp["w_mod"][:, :, i * MC:(i + 1) * MC])
        bm = np.ascontiguousarray(np.broadcast_to(inp["b_mod"][:, None, i * MC:(i + 1) * MC], (2, 2, MC)))
        maps.append({"c1": c1, "c2": c2, "wm": wm, "bm": bm})
    res = run_bass_kernel_spmd(nc, maps, core_ids=list(range(NCORES)))
    m = np.concatenate([r["mo"] for r in res.results], axis=2)
    return m


NTK = TPC + 32
INC = 5120


def rms_modulate_tile(P, nc, xs, xkey, rows, ss, sskey, epsb, gam, sh, hb, hbkey):
    P.op("act", lambda: nc.scalar.activation(out=hb[:rows, :], in_=xs[:rows, :], func=AF.Square, accum_out=ss[:rows, 0:1]),
         reads=[xkey], writes=[hbkey, sskey])
    P.op("act", lambda: nc.scalar.activation(out=ss[:rows, 1:2], in_=ss[:rows, 0:1], func=AF.Sqrt, scale=1.0 / D, bias=epsb[:rows, 0:1]),
         reads=[sskey, "epsb"], writes=[sskey])
    P.op("dve", lambda: nc.vector.reciprocal(out=ss[:rows, 2:3], in_=ss[:rows, 1:2]), reads=[sskey], writes=[sskey])
    P.op("dve", lambda: nc.vector.scalar_tensor_tensor(out=xs[:rows, :], in0=xs[:rows, :], scalar=ss[:rows, 2:3], in1=gam[:rows, :],
                                                       op0=ALU.mult, op1=ALU.mult),
         reads=[xkey, sskey, "gam"], writes=[xkey])
    P.op("pool", lambda: nc.gpsimd.tensor_tensor(out=hb[:rows, :], in0=xs[:rows, :], in1=sh[:rows, :], op=ALU.add),
         reads=[xkey, "sh"], writes=[hbkey])


def build_A():
    P = Prog()
    nc = P.nc
    x = P.din("x", [TPC, D], F32)
    cx = P.din("cx", [32, D], F32)
    gmix = P.din("gmix", [128, D], F32)
    cm = P.din("cm", [128, D], F32)
    sm = P.din("sm", [128, D], F32)
    cmc = P.din("cmc", [128, D], F32)
    smc = P.din("smc", [128, D], F32)
    w_in = P.din("w_in", [D, INC], F32)
    cosd = P.din("cosE", [TPC, 256], F32)
    sind = P.din("sinE", [TPC, 256], F32)
    lngd = P.din("lng", [128, 1024], F32)
    lnbd = P.din("lnb", [128, 1024], F32)
    wsTd = P.din("wsT", [8, 128, 128], F32)
    bsbd = P.din("bsb", [128, 1024], F32)
    qT = P.dout("qT", [8, 128, TPC], BF16)
    kT = P.dout("kT", [8, 128, NTK], BF16)
    vo = P.dout("v", [NTK, 1024], BF16)
    gTo = P.dout("gT", [8, 128, TPC], BF16)

    ident, idk = make_ident(P)
    xs = [P.sb("xs%d" % i, [128, D], F32) for i in range(2)]
    gam = P.sb("gam", [128, D], F32)
    sh = P.sb("sh", [128, D], F32)
    hb = [P.sb("hb%d" % i, [128, D], BF16) for i in range(2)]
    hT = P.sb("hT", [128, 16, NTK], BF16)
    wblk = [P.sb("wblk%d" % i, [128, 16, 512], BF16) for i in range(2)]
    big = P.sb("big", [128, 4096], F32)
    cosE = big[:, 0:2048].rearrange("p (t f) -> p t f", t=8)
    sinE = big[:, 2048:4096].rearrange("p (t f) -> p t f", t=8)
    uT = big.bitcast(BF16).rearrange("p (g t) -> p g t", g=8)
    lng = P.sb("lngs", [128, 1024], F32)
    lnb = P.sb("lnbs", [128, 1024], F32)
    wsTf = P.sb("wsTf", [128, 8, 128], F32)
    wsTb = P.sb("wsTb", [128, 8, 128], BF16)
    bsb = P.sb("bsbs", [128, 8, 128], F32)
    stg = P.sb("stg", [128, 4, NTK], BF16)
    zf = [P.sb("zf%d" % i, [128, 512], F32) for i in range(2)]
    zsq = P.sb("zsq", [128, 512], F32)
    rt = [P.sb("rt%d" % i, [128, 256], F32) for i in range(8)]
    qr = [P.sb("qr%d" % i, [128, 512], BF16) for i in range(2)]
    vs = [P.sb("vs%d" % i, [128, 512], BF16) for i in range(2)]
    vlf = P.sb("vlf", [128, 512], F32)
    vlb = [P.sb("vlb%d" % i, [128, 512], BF16) for i in range(2)]
    tt = [P.sb("tt%d" % i, [128, 128], F32) for i in range(2)]
    ss = [P.sb("ss%d" % i, [128, 4], F32) for i in range(2)]
    st = [P.sb("st%d" % i, [128, 24], F32) for i in range(2)]
    epsb = P.sb("epsb", [128, 1], F32)
    pT = P.ps("pT", [128, D], BF16)
    pz = [P.ps("pz%d" % i, [128, 512], F32) for i in range(2)]
    pq = [P.ps("pq%d" % i, [128, 512], BF16) for i in range(2)]
    pm = [P.ps("pm%d" % i, [128, 128], F32) for i in range(2)]

    P.op("pool", lambda: nc.gpsimd.memset(epsb[:], EPS), writes=["epsb"])

    def load_mod(cmd, smd):
        P.dma("sp", gam[:], cmd[:, :], writes=["gam"])
        P.dma("act", xs[1][:], gmix[:, :], writes=[("xs", 1)])
        P.dma("sp", sh[:], smd[:, :], writes=["sh"])
        P.op("dve", lambda: nc.vector.scalar_tensor_tensor(out=gam[:], in0=gam[:], scalar=1.0, in1=xs[1][:], op0=ALU.add, op1=ALU.mult),
             reads=["gam", ("xs", 1)], writes=["gam"])

    def front_tile(src_ap, rows, tok0, b):
        P.dma("sp", xs[b][:rows, :], src_ap, writes=[("xs", b)])
        rms_modulate_tile(P, nc, xs[b], ("xs", b), rows, ss[b], ("ss", b), epsb, gam, sh, hb[b], ("hb", b))
        for kc in range(16):
            P.op("pe", lambda kc=kc: nc.tensor.transpose(pT[:, kc * 128:kc * 128 + rows], hb[b][:rows, kc * 128:(kc + 1) * 128], ident[:rows, :rows]),
                 reads=[("hb", b), idk], writes=["pT"])
        P.op("act", lambda: nc.scalar.copy(out=hT[:, :, tok0:tok0 + rows],
                                           in_=pT.rearrange("p (k t) -> p k t", k=16)[:, :, 0:rows]),
             reads=["pT"], writes=["hT"])

    load_mod(cmc, smc)
    front_tile(cx[:, :], 32, TPC, 0)
    load_mod(cm, sm)
    for ti in range(NT):
        front_tile(x[ti * 128:(ti + 1) * 128, :], 128, ti * 128, ti % 2)

    P.dma("sp", big[:, 0:2048].rearrange("p (t f) -> p t f", t=8), cosd.rearrange("(t p) f -> p t f", p=128), writes=["big"])
    P.dma("sp", big[:, 2048:4096].rearrange("p (t f) -> p t f", t=8), sind.rearrange("(t p) f -> p t f", p=128), writes=["big"])
    P.dma("act", lng[:], lngd[:, :], writes=["lng"])
    P.dma("act", lnb[:], lnbd[:, :], writes=["lnb"])
    P.dma("act", wsTf[:], wsTd.rearrange("g q p -> q g p"), writes=["wsTf"])
    P.dma("act", bsb[:], bsbd.rearrange("q (g p) -> q g p", g=8), writes=["bsb"])
    P.op("dve", lambda: nc.vector.tensor_copy(out=wsTb[:], in_=wsTf[:]), reads=["wsTf"], writes=["wsTb"])

    wv = w_in.rearrange("(kc p) n -> p kc n", p=128)
    CL = CastLoader(P, 1024, engines=("pool", "dve"))
    zc = [0]

    def proj_tok(b, tok0, rows):
        j = zc[0] % 2
        zc[0] += 1
        for kc in range(16):
            P.op("pe", lambda kc=kc: nc.tensor.matmul(pz[j][:rows, :], lhsT=hT[:, kc, tok0:tok0 + rows], rhs=wblk[b][:, kc, :],
                                                       start=(kc == 0), stop=(kc == 15)),
                 reads=["hT", ("wblk", b)], writes=[("pz", j)])
        CL.pump(1)
        return j

    def queue_block(cb):
        b = cb % 2
        for kq in range(8):
            CL.enqueue(wblk[b][:, kq * 2:(kq + 1) * 2, :], ("wblk", b), wv[:, kq * 2:(kq + 1) * 2, cb * 512:(cb + 1) * 512], (2, 512))

    queue_block(0)
    CL.flush()
    for cb in range(10):
        b = cb % 2
        CL.flush()
        if cb + 1 < 10:
            queue_block(cb + 1)
        if cb < 4:
            isk = cb >= 2
            for ti in range(NT):
                j = proj_tok(b, ti * 128, 128)
                zv = pz[j].rearrange("p (a h x f) -> p a h x f", a=8, h=2, x=2, f=16)
                x1 = zv[:, :, :, 0, :]
                x2 = zv[:, :, :, 1, :]
                cv = cosE[:, ti, :].rearrange("p (a h f) -> p a h f", a=8, h=2)
                sv = sinE[:, ti, :].rearrange("p (a h f) -> p a h f", a=8, h=2)
                ro = (ti % 2) * 4
                r4 = [t.rearrange("p (a h f) -> p a h f", a=8, h=2) for t in rt[ro:ro + 4]]
                rk_ = ["rt%d" % (ro + i) for i in range(4)]
                qv = qr[j].rearrange("p (a h x f) -> p a h x f", a=8, h=2, x=2, f=16)
                zk = ("pz", j)
                P.op("dve", lambda: nc.vector.tensor_tensor(out=r4[0], in0=x1, in1=cv, op=ALU.mult), reads=[zk, "big"], writes=[rk_[0]])
                P.op("dve", lambda: nc.vector.tensor_tensor(out=r4[1], in0=x2, in1=sv, op=ALU.mult), reads=[zk, "big"], writes=[rk_[1]])
                P.op("dve", lambda: nc.vector.tensor_tensor(out=r4[2], in0=x2, in1=cv, op=ALU.mult), reads=[zk, "big"], writes=[rk_[2]])
                P.op("dve", lambda: nc.vector.tensor_tensor(out=r4[3], in0=x1, in1=sv, op=ALU.mult), reads=[zk, "big"], writes=[rk_[3]])
                P.op("pool", lambda: nc.gpsimd.tensor_tensor(out=qv[:, :, :, 0, :], in0=r4[0], in1=r4[1], op=ALU.subtract),
                     reads=[rk_[0], rk_[1]], writes=[("qr", j)])
                P.op("pool", lambda: nc.gpsimd.tensor_tensor(out=qv[:, :, :, 1, :], in0=r4[2], in1=r4[3], op=ALU.add),
                     reads=[rk_[2], rk_[3]], writes=[("qr", j)])
                for s in range(4):
                    P.op("pe", lambda s=s: nc.tensor.transpose(pq[j][:, s * 128:(s + 1) * 128], qr[j][:, s * 128:(s + 1) * 128], ident[:]),
                         reads=[("qr", j), idk], writes=[("pq", j)])
                P.op("act", lambda: nc.scalar.copy(out=stg[:, :, ti * 128:(ti + 1) * 128], in_=pq[j].rearrange("p (s t) -> p s t", s=4)),
                     reads=[("pq", j)], writes=["stg"])
            if isk:
                j = proj_tok(b, TPC, 32)
                P.op("act", lambda: nc.scalar.copy(out=qr[j][:32, :], in_=pz[j][:32, :]), reads=[("pz", j)], writes=[("qr", j)])
                for s in range(4):
                    P.op("pe", lambda s=s: nc.tensor.transpose(pq[j][:, s * 128:s * 128 + 32], qr[j][:32, s * 128:(s + 1) * 128], ident[:32, :32]),
                         reads=[("qr", j), idk], writes=[("pq", j)])
                P.op("act", lambda: nc.scalar.copy(out=stg[:, :, TPC:TPC + 32], in_=pq[j].rearrange("p (s t) -> p s t", s=4)[:, :, 0:32]),
                     reads=[("pq", j)], writes=["stg"])
            h0 = (cb % 2) * 4
            if isk:
                P.dma("act", kT[h0:h0 + 4].rearrange("h d t -> d h t"), stg[:], reads=["stg"], writes=["kT"])
            else:
                P.dma("act", qT[h0:h0 + 4].rearrange("h d t -> d h t"), stg[:, :, 0:TPC], reads=["stg"], writes=["qT"])
        elif cb < 6:
            for ti in range(NT + 1):
                rows = 128 if ti < NT else 32
                tok0 = ti * 128
                j = proj_tok(b, tok0, rows)
                P.op("act", lambda: nc.scalar.copy(out=vs[j][:rows, :], in_=pz[j][:rows, :]), reads=[("pz", j)], writes=[("vs", j)])
                P.dma("act", vo[tok0:tok0 + rows, (cb - 4) * 512:(cb - 3) * 512], vs[j][:rows, :], reads=[("vs", j)], writes=["vo"])
        elif cb < 8:
            for sub in range(4):
                G = (cb - 6) * 4 + sub
                for tg in range(2):
                    j = zc[0] % 2
                    zc[0] += 1
                    for kc in range(16):
                        P.op("pe", lambda kc=kc: nc.tensor.matmul(pz[j][:], lhsT=wblk[b][:, kc, sub * 128:(sub + 1) * 128],
                                                                   rhs=hT[:, kc, tg * 512:(tg + 1) * 512],
                                                                   start=(kc == 0), stop=(kc == 15)),
                             reads=["hT", ("wblk", b)], writes=[("pz", j)])
                    P.op("act", lambda: nc.scalar.activation(out=uT[:, G, tg * 512:(tg + 1) * 512], in_=pz[j][:], func=AF.Gelu_apprx_tanh),
                         reads=[("pz", j)], writes=["big"])
                    CL.pump(1)
        else:
            for ti in range(NT):
                tok0 = ti * 128
                j = proj_tok(b, tok0, 128)
                z = zf[j]
                zk = ("zf", j)
                s_ = st[j]
                sk = ("st", j)
                P.op("act", lambda: nc.scalar.activation(out=z[:], in_=pz[j][:], func=AF.Gelu_apprx_tanh), reads=[("pz", j)], writes=[zk])
                P.op("dve", lambda: nc.vector.reduce_sum(out=s_[:, 0:4], in_=z.rearrange("p (g d) -> p g d", g=4), axis=AX.X),
                     reads=[zk], writes=[sk])
                P.op("act", lambda: nc.scalar.activation(out=zsq[:], in_=z[:], func=AF.Square), reads=[zk], writes=["zsq"])
                P.op("dve", lambda: nc.vector.reduce_sum(out=s_[:, 4:8], in_=zsq.rearrange("p (g d) -> p g d", g=4), axis=AX.X),
                     reads=["zsq"], writes=[sk])
                P.op("dve", lambda: nc.vector.tensor_scalar(out=s_[:, 8:12], in0=s_[:, 0:4], scalar1=1.0 / 128, scalar2=None, op0=ALU.mult),
                     reads=[sk], writes=[sk])
                P.op("dve", lambda: nc.vector.tensor_tensor(out=s_[:, 12:16], in0=s_[:, 8:12], in1=s_[:, 8:12], op=ALU.mult),
                     reads=[sk], writes=[sk])
                P.op("dve", lambda: nc.vector.scalar_tensor_tensor(out=s_[:, 16:20], in0=s_[:, 4:8], scalar=1.0 / 128, in1=s_[:, 12:16],
                                                                   op0=ALU.mult, op1=ALU.subtract),
                     reads=[sk], writes=[sk])
                P.op("act", lambda: nc.scalar.activation(out=s_[:, 20:24], in_=s_[:, 16:20], func=AF.Sqrt, bias=epsb[:, 0:1]),
                     reads=[sk, "epsb"], writes=[sk])
                P.op("dve", lambda: nc.vector.reciprocal(out=s_[:, 20:24], in_=s_[:, 20:24]), reads=[sk], writes=[sk])
                for g in range(4):
                    P.op("dve", lambda g=g: nc.vector.tensor_scalar(out=vlf[:, g * 128:(g + 1) * 128], in0=z[:, g * 128:(g + 1) * 128],
                                                                    scalar1=s_[:, 8 + g:9 + g], scalar2=s_[:, 20 + g:21 + g],
                                                                    op0=ALU.subtract, op1=ALU.mult),
                         reads=[zk, sk], writes=["vlf"])
                c0 = (cb - 8) * 512
                P.op("pool", lambda: nc.gpsimd.tensor_tensor(out=vlf[:], in0=vlf[:], in1=lng[:, c0:c0 + 512], op=ALU.mult),
                     reads=["vlf", "lng"], writes=["vlf"])
                P.op("pool", lambda: nc.gpsimd.tensor_tensor(out=vlb[j][:], in0=vlf[:], in1=lnb[:, c0:c0 + 512], op=ALU.add),
                     reads=["vlf", "lnb"], writes=[("vlb", j)])
                for g in range(4):
                    G = (cb - 8) * 4 + g
                    m = (ti * 4 + g) % 2
                    P.op("pe", lambda g=g, G=G, m=m: nc.tensor.matmul(pm[m][:], lhsT=vlb[j][:, g * 128:(g + 1) * 128], rhs=wsTb[:, G, :],
                                                                      start=True, stop=True),
                         reads=[("vlb", j), "wsTb"], writes=[("pm", m)])
                    P.op("dve", lambda G=G, m=m: nc.vector.tensor_tensor(out=tt[m][:], in0=pm[m][:], in1=bsb[:, G, :], op=ALU.add),
                         reads=[("pm", m), "bsb"], writes=[("tt", m)])
                    P.op("dve", lambda g=g, G=G, m=m: nc.vector.tensor_tensor(out=stg[:, g, tok0:tok0 + 128], in0=tt[m][:],
                                                                              in1=uT[:, G, tok0:tok0 + 128], op=ALU.mult),
                         reads=[("tt", m), "big"], writes=["stg"])
            g0 = (cb - 8) * 4
            P.dma("pool", gTo[g0:g0 + 4].rearrange("g d t -> d g t"), stg[:, :, 0:TPC], reads=["stg"], writes=["gTo"])
    return P.finish(["qT", "kT", "vo", "gTo"])


def rope_tables():
    t = np.arange(SEQ)
    row = (t // 64).astype(np.float32)
    col = (t % 64).astype(np.float32)
    inv = (10000.0 ** (-np.arange(0, 32, 2, dtype=np.float32) / 32)).astype(np.float32)
    ang = np.stack([row[:, None] * inv[None, :], col[:, None] * inv[None, :]], axis=1)
    cosE = np.tile(np.cos(ang).astype(np.float32).reshape(SEQ, 1, 32), (1, 8, 1)).reshape(SEQ, 256)
    sinE = np.tile(np.sin(ang).astype(np.float32).reshape(SEQ, 1, 32), (1, 8, 1)).reshape(SEQ, 256)
    return np.ascontiguousarray(cosE), np.ascontiguousarray(sinE)


def run_A(inp, m):
    nc = build_A()
    cosE, sinE = rope_tables()
    m0 = m[0]
    sm_, cm_ = m0[0, 0:D], m0[0, D:2 * D]
    smc_, cmc_ = m0[1, 0:D], m0[1, D:2 * D]
    common = {
        "gmix": bc128(inp["g_norm_mix"][0]), "cm": bc128(cm_), "sm": bc128(sm_), "cmc": bc128(cmc_), "smc": bc128(smc_),
        "w_in": np.ascontiguousarray(inp["w_in"][0]),
        "lng": bc128(inp["sgu_ln_g"][0]), "lnb": bc128(inp["sgu_ln_b"][0]),
        "wsT": np.ascontiguousarray(np.transpose(inp["w_spatial"][0], (0, 2, 1))),
        "bsb": bc128(inp["b_spatial"][0].reshape(-1)),
    }
    maps = []
    for i in range(NCORES):
        d = dict(common)
        d["x"] = np.ascontiguousarray(inp["x"][0, i * TPC:(i + 1) * TPC])
        d["cx"] = np.ascontiguousarray(inp["ctx"][0, i * 32:(i + 1) * 32])
        d["cosE"] = cosE[i * TPC:(i + 1) * TPC]
        d["sinE"] = sinE[i * TPC:(i + 1) * TPC]
        maps.append(d)
    res = run_bass_kernel_spmd(nc, maps, core_ids=list(range(NCORES)))
    return [r for r in res.results]


NKEY = SEQ + 256
NKT = NKEY // 128
LAM_INIT0 = 0.8 - 0.6 * math.exp(-0.3 * 0)


def build_B():
    P = Prog()
    nc = P.nc
    qTd = P.din("qT", [128, SEQ], BF16)
    kTd = P.din("kT", [128, NKEY], BF16)
    vd = P.din("v", [NKEY, 128], BF16)
    lamd = P.din("lamv", [128, 4, 64], F32)
    gsd = P.din("gsub", [128, 1], F32)
    aTo = P.dout("aT", [128, SEQ], BF16)

    qz = [P.sb("qz%d" % i, [128, SEQ], BF16) for i in range(2)]
    kT = P.sb("kTs", [128, NKEY], BF16)
    vs = P.sb("vsb", [128, NKT, 128], BF16)
    lamv = P.sb("lamvs", [128, 4, 64], F32)
    lt = P.sb("lt", [128, 2, 64], F32)
    ls = P.sb("ls", [128, 8], F32)
    gs = P.sb("gs", [128, 1], F32)
    epsb = P.sb("epsb", [128, 1], F32)
    onesb = P.sb("onesb", [128, 128], BF16)
    onesf = P.sb("onesf", [128, 128], F32)
    pt = [P.sb("pt%d" % i, [128, 512], BF16) for i in range(3)]
    rz = [P.sb("rz%d" % i, [128, 512], F32) for i in range(2)]
    o = P.sb("o", [128, 512], F32)
    t1 = P.sb("t1", [128, 512], F32)
    osq = P.sb("osq", [128, 512], F32)
    rstd = P.sb("rstd", [128, 512], F32)
    ab = [P.sb("ab%d" % i, [128, 512], BF16) for i in range(2)]
    ps = [P.ps("ps%d" % i, [128, 512], F32) for i in range(3)]
    po = [P.ps("po%d" % i, [128, 512], F32) for i in range(2)]
    pzz = [P.ps("pzz%d" % i, [128, 512], F32) for i in range(2)]

    for i in range(4):
        q = "sp" if i % 2 == 0 else "act"
        P.dma(q, qz[0][0:64, i * 2048:(i + 1) * 2048], qTd[0:64, i * 2048:(i + 1) * 2048], writes=["qT"])
        P.dma(q, qz[1][64:128, i * 2048:(i + 1) * 2048], qTd[64:128, i * 2048:(i + 1) * 2048], writes=["qT"])
        P.dma(q, kT[:, i * 2112:(i + 1) * 2112], kTd[:, i * 2112:(i + 1) * 2112], writes=["kT"])
    P.dma("sp", vs[:, 0:33, :], vd[0:33 * 128, :].rearrange("(t p) e -> p t e", p=128), writes=["vs"])
    P.dma("act", vs[:, 33:66, :], vd[33 * 128:, :].rearrange("(t p) e -> p t e", p=128), writes=["vs"])
    P.dma("sp", lamv[:], lamd[:, :, :], writes=["lamv"])
    P.dma("sp", gs[:], gsd[:, :], writes=["gs"])
    P.op("pool", lambda: nc.gpsimd.memset(epsb[:], EPS), writes=["epsb"])
    P.op("pool", lambda: nc.gpsimd.memset(onesb[:], 1.0), writes=["onesb"])
    P.op("pool", lambda: nc.gpsimd.memset(onesf[:], 1.0 / 128), writes=["onesf"])
    P.op("pool", lambda: nc.gpsimd.memset(qz[0][64:128, :], 0.0), writes=["qz0pad"])
    P.op("dve", lambda: nc.vector.memset(qz[1][0:64, :], 0.0), writes=["qz1pad"])
    P.op("dve", lambda: nc.vector.tensor_tensor(out=lt[:, 0, :], in0=lamv[:, 0, :], in1=lamv[:, 1, :], op=ALU.mult), reads=["lamv"], writes=["lt"])
    P.op("dve", lambda: nc.vector.tensor_tensor(out=lt[:, 1, :], in0=lamv[:, 2, :], in1=lamv[:, 3, :], op=ALU.mult), reads=["lamv", "lt"], writes=["lt"])
    P.op("dve", lambda: nc.vector.reduce_sum(out=ls[:, 0:2], in_=lt[:], axis=AX.X), reads=["lt"], writes=["ls"])
    P.op("act", lambda: nc.scalar.activation(out=ls[:, 2:4], in_=ls[:, 0:2], func=AF.Exp), reads=["ls"], writes=["ls"])
    P.op("dve", lambda: nc.vector.tensor_tensor(out=ls[:, 4:5], in0=ls[:, 3:4], in1=ls[:, 2:3], op=ALU.subtract), reads=["ls"], writes=["ls"])
    P.op("dve", lambda: nc.vector.tensor_scalar(out=ls[:, 4:5], in0=ls[:, 4:5], scalar1=-LAM_INIT0, scalar2=None, op0=ALU.add), reads=["ls"], writes=["ls"])
    P.op("dve", lambda: nc.vector.tensor_scalar(out=gs[:], in0=gs[:], scalar1=1.0 - LAM_INIT0, scalar2=None, op0=ALU.mult), reads=["gs"], writes=["gs"])

    it = 0
    for qb in range(SEQ // 512):
        q0 = qb * 512
        steps = [(kt, c) for kt in range(NKT) for c in range(2)]

        def qk(i, kt, c):
            j = i % 3
            P.op("pe", lambda: nc.tensor.matmul(ps[j][:], lhsT=kT[:, kt * 128:(kt + 1) * 128],
                                                rhs=qz[c][:, q0:q0 + 512], start=True, stop=True),
                 reads=["qT", "kT", "qz0pad", "qz1pad"], writes=[("ps", j)])

        for pre in range(2):
            qk(it + pre, *steps[pre])
        for si, (kt, c) in enumerate(steps):
            j = it % 3
            P.op("act", lambda: nc.scalar.activation(out=pt[j][:], in_=ps[j][:], func=AF.Exp, scale=0.125),
                 reads=[("ps", j)], writes=[("pt", j)])
            if si + 2 < len(steps):
                qk(it + 2, *steps[si + 2])
            P.op("pe", lambda: nc.tensor.matmul(po[c][:], lhsT=vs[:, kt, :], rhs=pt[j][:], start=(kt == 0), stop=(kt == NKT - 1)),
                 reads=["vs", ("pt", j)], writes=[("po", c)])
            P.op("pe", lambda: nc.tensor.matmul(pzz[c][:], lhsT=onesb[:], rhs=pt[j][:], start=(kt == 0), stop=(kt == NKT - 1)),
                 reads=["onesb", ("pt", j)], writes=[("pzz", c)])
            it += 1
        for c in range(2):
            P.op("dve", lambda c=c: nc.vector.reciprocal(out=rz[c][:], in_=pzz[c][:]), reads=[("pzz", c)], writes=[("rz", c)])
        P.op("dve", lambda: nc.vector.tensor_tensor(out=o[:], in0=po[0][:], in1=rz[0][:], op=ALU.mult), reads=[("po", 0), ("rz", 0)], writes=["o"])
        P.op("dve", lambda: nc.vector.tensor_tensor(out=t1[:], in0=po[1][:], in1=rz[1][:], op=ALU.mult), reads=[("po", 1), ("rz", 1)], writes=["t1"])
        P.op("dve", lambda: nc.vector.scalar_tensor_tensor(out=o[:], in0=t1[:], scalar=ls[:, 4:5], in1=o[:], op0=ALU.mult, op1=ALU.add),
             reads=["t1", "ls", "o"], writes=["o"])
        P.op("act", lambda: nc.scalar.activation(out=osq[:], in_=o[:], func=AF.Square), reads=["o"], writes=["osq"])
        jm = it % 3
        it += 1
        P.op("pe", lambda: nc.tensor.matmul(ps[jm][:], lhsT=onesf[:], rhs=osq[:], start=True, stop=True), reads=["onesf", "osq"], writes=[("ps", jm)])
        P.op("act", lambda: nc.scalar.activation(out=rstd[:], in_=ps[jm][:], func=AF.Sqrt, bias=epsb[:, 0:1]), reads=[("ps", jm), "epsb"], writes=["rstd"])
        P.op("dve", lambda: nc.vector.reciprocal(out=rstd[:], in_=rstd[:]), reads=["rstd"], writes=["rstd"])
        a = ab[qb % 2]
        P.op("dve", lambda: nc.vector.scalar_tensor_tensor(out=a[:], in0=o[:], scalar=gs[:, 0:1], in1=rstd[:], op0=ALU.mult, op1=ALU.mult),
             reads=["o", "gs", "rstd"], writes=[("ab", qb % 2)])
        P.dma("sp", aTo[:, q0:q0 + 512], a[:], reads=[("ab", qb % 2)], writes=["aTo"])
    return P.finish(["aTo"])


def run_B(inp, ra):
    nc = build_B()
    lamv = np.stack([inp["lam_q1"][0], inp["lam_k1"][0], inp["lam_q2"][0], inp["lam_k2"][0]], 0)
    lamv = np.ascontiguousarray(np.broadcast_to(lamv[None], (128, 4, 64))).astype(np.float32)
    gsub = np.ascontiguousarray(inp["g_subln"][0].reshape(128, 1))
    maps = []
    for h in range(NCORES):
        qT = np.concatenate([r["qT"][h] for r in ra], axis=1)
        kT = np.concatenate([r["kT"][h][:, :TPC] for r in ra] + [r["kT"][h][:, TPC:] for r in ra], axis=1)
        v = np.concatenate([r["v"][:TPC, h * 128:(h + 1) * 128] for r in ra] + [r["v"][TPC:, h * 128:(h + 1) * 128] for r in ra], axis=0)
        maps.append({"qT": np.ascontiguousarray(qT), "kT": np.ascontiguousarray(kT), "v": np.ascontiguousarray(v),
                     "lamv": lamv, "gsub": gsub})
    res = run_bass_kernel_spmd(nc, maps, core_ids=list(range(NCORES)))
    return [r["aT"] for r in res.results]


def build_proj():
    P = Prog()
    nc = P.nc
    fTd = P.din("fT", [16, 128, TPC], BF16)
    Wd = P.din("W", [D, D], F32)
    xd = P.din("x", [TPC, D], F32)
    gmd = P.din("gm", [128, D], F32)
    gfd = P.din("gffn", [128, D], F32)
    cfd = P.din("cf", [128, D], F32)
    sfd = P.din("sf", [128, D], F32)
    wrd = P.din("wr", [D, 16], F32)
    x1o = P.dout("x1", [TPC, D], F32)
    hfo = P.dout("hf", [TPC, D], BF16)
    affo = P.dout("aff", [TPC, 16], F32)

    identf, idk = make_ident(P, F32)
    Wb = P.sb("Wb", [128, 16, D], BF16)
    fT = [P.sb("fTs%d" % i, [128, 16, 128], BF16) for i in range(2)]
    gm = P.sb("gms", [128, D], F32)
    gam = P.sb("gam", [128, D], F32)
    sh = P.sb("sh", [128, D], F32)
    xs = [P.sb("xs%d" % i, [128, D], F32) for i in range(2)]
    hf32s = [P.sb("hf32_%d" % i, [128, D], F32) for i in range(2)]
    hfb = [P.sb("hfb%d" % i, [128, D], BF16) for i in range(2)]
    hT32 = P.sb("hT32", [128, 16, 128], F32)
    tq = [P.sb("tq%d" % i, [128, 512], F32) for i in range(2)]
    wr = P.sb("wrs", [128, 16, 16], F32)
    ss = [P.sb("ss%d" % i, [128, 4], F32) for i in range(2)]
    sm = [P.sb("smx%d" % i, [128, 4], F32) for i in range(2)]
    ex = [P.sb("ex%d" % i, [128, 16], F32) for i in range(2)]
    epsb = P.sb("epsb", [128, 1], F32)
    pz = [P.ps("pz%d" % i, [128, 512], F32) for i in range(2)]
    pT = P.ps("pT", [128, D], F32)
    pl = P.ps("pl", [128, 16], F32)

    P.op("pool", lambda: nc.gpsimd.memset(epsb[:], EPS), writes=["epsb"])
    wv = Wd.rearrange("(kc p) n -> p kc n", p=128)
    CL = CastLoader(P, 1024, engines=("pool", "dve"))
    for hh in range(2):
        for kc in range(16):
            CL.load(Wb[:, kc:kc + 1, hh * 1024:(hh + 1) * 1024], ("Wb", hh), wv[:, kc:kc + 1, hh * 1024:(hh + 1) * 1024], (1, 1024))
    P.dma("sp", gm[:], gmd[:, :], writes=["gm"])
    P.dma("sp", gam[:], cfd[:, :], writes=["gam"])
    P.dma("act", xs[1][:], gfd[:, :], writes=[("xs", 1)])
    P.dma("act", sh[:], sfd[:, :], writes=["sh"])
    P.dma("act", wr[:], wrd.rearrange("(kc p) e -> p kc e", p=128), writes=["wr"])
    P.op("dve", lambda: nc.vector.scalar_tensor_tensor(out=gam[:], in0=gam[:], scalar=1.0, in1=xs[1][:], op0=ALU.add, op1=ALU.mult),
         reads=["gam", ("xs", 1)], writes=["gam"])

    pending = []

    def router(ti, b):
        tok0 = ti * 128
        hf32 = hf32s[b]
        hk = ("hf32", b)
        for kc in range(16):
            P.op("pe", lambda kc=kc: nc.tensor.transpose(pT[:, kc * 128:(kc + 1) * 128], hf32[:, kc * 128:(kc + 1) * 128], identf[:]),
                 reads=[hk, idk], writes=["pT"])
        P.op("dve", lambda: nc.vector.tensor_copy(out=hT32[:], in_=pT.rearrange("p (k t) -> p k t", k=16)), reads=["pT"], writes=["hT32"])
        for kc in range(16):
            P.op("pe", lambda kc=kc: nc.tensor.matmul(pl[:], lhsT=hT32[:, kc, :], rhs=wr[:, kc, :], start=(kc == 0), stop=(kc == 15)),
                 reads=["hT32", "wr"], writes=["pl"])
        m_ = sm[b]
        mk = ("sm", b)
        P.op("dve", lambda: nc.vector.reduce_max(out=m_[:, 0:1], in_=pl[:], axis=AX.X), reads=["pl"], writes=[mk])
        P.op("dve", lambda: nc.vector.tensor_scalar(out=m_[:, 1:2], in0=m_[:, 0:1], scalar1=-1.0, scalar2=None, op0=ALU.mult), reads=[mk], writes=[mk])
        P.op("act", lambda: nc.scalar.activation(out=ex[b][:], in_=pl[:], func=AF.Exp, bias=m_[:, 1:2], accum_out=m_[:, 2:3]),
             reads=["pl", mk], writes=[("ex", b), mk])
        P.op("dve", lambda: nc.vector.reciprocal(out=m_[:, 3:4], in_=m_[:, 2:3]), reads=[mk], writes=[mk])
        P.op("dve", lambda: nc.vector.tensor_scalar(out=ex[b][:], in0=ex[b][:], scalar1=m_[:, 3:4], scalar2=None, op0=ALU.mult),
             reads=[("ex", b), mk], writes=[("ex", b)])
        P.dma("pool", affo[tok0:tok0 + 128, :], ex[b][:], reads=[("ex", b)], writes=["affo"])

    zc = 0
    for ti in range(NT):
        b = ti % 2
        tok0 = ti * 128
        xk = ("xs", b)
        hf32 = hf32s[b]
        P.dma("sp", xs[b][:], xd[tok0:tok0 + 128, :], writes=[xk])
        P.dma("sp", fT[b][:], fTd[:, :, tok0:tok0 + 128].rearrange("k d t -> d k t"), writes=[("fT", b)])
        for nb in range(4):
            j = zc % 2
            zc += 1
            for kc in range(16):
                P.op("pe", lambda kc=kc: nc.tensor.matmul(pz[j][:], lhsT=fT[b][:, kc, :], rhs=Wb[:, kc, nb * 512:(nb + 1) * 512],
                                                           start=(kc == 0), stop=(kc == 15)),
                     reads=[("fT", b), ("Wb", nb // 2)], writes=[("pz", j)])
            P.op("dve", lambda: nc.vector.tensor_tensor(out=tq[j][:], in0=pz[j][:], in1=gm[:, nb * 512:(nb + 1) * 512], op=ALU.mult),
                 reads=[("pz", j), "gm"], writes=[("tq", j)])
            P.op("pool", lambda: nc.gpsimd.tensor_tensor(out=xs[b][:, nb * 512:(nb + 1) * 512], in0=xs[b][:, nb * 512:(nb + 1) * 512],
                                                         in1=tq[j][:], op=ALU.add),
                 reads=[xk, ("tq", j)], writes=[xk])
        P.dma("pool", x1o[tok0:tok0 + 128, :], xs[b][:], reads=[xk], writes=["x1o"])
        s_ = ss[b]
        sk = ("ss", b)
        P.op("act", lambda: nc.scalar.activation(out=hfb[b][:], in_=xs[b][:], func=AF.Square, accum_out=s_[:, 0:1]),
             reads=[xk], writes=[("hfb", b), sk])
        P.op("act", lambda: nc.scalar.activation(out=s_[:, 1:2], in_=s_[:, 0:1], func=AF.Sqrt, scale=1.0 / D, bias=epsb[:, 0:1]),
             reads=[sk, "epsb"], writes=[sk])
        P.op("dve", lambda: nc.vector.reciprocal(out=s_[:, 2:3], in_=s_[:, 1:2]), reads=[sk], writes=[sk])
        P.op("dve", lambda: nc.vector.scalar_tensor_tensor(out=hf32[:], in0=xs[b][:], scalar=s_[:, 2:3], in1=gam[:], op0=ALU.mult, op1=ALU.mult),
             reads=[xk, sk, "gam"], writes=[("hf32", b)])
        P.op("pool", lambda: nc.gpsimd.tensor_tensor(out=hf32[:], in0=hf32[:], in1=sh[:], op=ALU.add), reads=[("hf32", b), "sh"], writes=[("hf32", b)])
        P.op("act", lambda: nc.scalar.copy(out=hfb[b][:], in_=hf32[:]), reads=[("hf32", b)], writes=[("hfb", b)])
        P.dma("act", hfo[tok0:tok0 + 128, :], hfb[b][:], reads=[("hfb", b)], writes=["hfo"])
        pending.append((ti, b))
        if len(pending) > 1:
            router(*pending.pop(0))
    while pending:
        router(*pending.pop(0))
    return P.finish(["x1o", "hfo", "affo"])


_PROJ_NC = [None]


def run_proj(fT_list, W, x_full, gm_, gffn, cf_, sf_, w_r):
    nc = build_proj()
    common = {"W": np.ascontiguousarray(W), "gm": bc128(gm_), "gffn": bc128(gffn), "cf": bc128(cf_), "sf": bc128(sf_),
              "wr": np.ascontiguousarray(w_r)}
    maps = []
    for i in range(NCORES):
        d = dict(common)
        d["fT"] = np.ascontiguousarray(fT_list[i])
        d["x"] = np.ascontiguousarray(x_full[i * TPC:(i + 1) * TPC])
        maps.append(d)
    res = run_bass_kernel_spmd(nc, maps, core_ids=list(range(NCORES)))
    x1 = np.concatenate([r["x1"] for r in res.results], 0)
    hf = np.concatenate([r["hf"] for r in res.results], 0)
    aff = np.concatenate([r["aff"] for r in res.results], 0)
    return x1, hf, aff


CAP = 1024
FE = 1024
NBIS = 32
OOB = 30000.0


def build_E():
    P = Prog()
    nc = P.nc
    affd = P.din("affT", [128, 2, 64], F32)
    ebd = P.din("ebase", [128, 2], F32)
    hfd = P.din("hf", [SEQ, D], BF16)
    wgd = P.din("wg", [2, D, FE], F32)
    wud = P.din("wu", [2, D, FE], F32)
    wdd = P.din("wd", [2, FE, D], F32)
    Yo = P.dout("Y", [2, CAP, D], F32)
    sloto = P.dout("slot", [128, 2, 64], I32)

    ident, idk = make_ident(P)
    onesf = P.sb("onesf", [128, 128], F32)
    UT = P.sb("UT", [128, 128], F32)
    Lb = P.sb("Lb", [128, 128], F32)
    iot_i = P.sb("iot_i", [128, 1024], I32)
    iot = P.sb("iot", [128, 1024], F32)
    jp_i = P.sb("jp_i", [128, 64, 2], I32)
    jp = P.sb("jp", [128, 64, 2], BF16)
    a = P.sb("a", [128, 2, 64], F32)
    eb = P.sb("eb", [128, 2], F32)
    msk = P.sb("msk", [128, 2, 64], F32)
    bs = P.sb("bs", [128, 16], F32)
    exs = P.sb("exs", [128, 128], F32)
    kv_i = P.sb("kv_i", [128, 2, 16], I32)
    kv = P.sb("kv", [128, 2, 16], F32)
    thr = P.sb("thr", [128, 2, 16], F32)
    mk4 = P.sb("mk4", [128, 2, 16, 64], F32)
    cn4 = P.sb("cn4", [128, 2, 16], F32)
    ge4 = P.sb("ge4", [128, 2, 16], F32)
    ct = P.sb("ct", [128, 1], F32)
    CB = P.sb("CB", [128, 128], F32)
    rk = P.sb("rk", [128, 2, 64], F32)
    sg = P.sb("sg", [128, 2, 64], F32)
    sgi = P.sb("sgi", [128, 2, 64], I32)
    sel = [P.sb("sel%d" % i, [128, 1024], BF16) for i in range(3)]
    idxf = P.sb("idxf", [128, 2, 8], F32)
    pselS = P.sb("pselS", [128, 8, 2], F32)
    zb = P.sb("zb", [128, 128], BF16)
    idxi = P.sb("idxi", [128, 2, 8], I32)
    xg = [P.sb("xg%d" % i, [128, D], BF16) for i in range(2)]
    xsT = P.sb("xsT", [128, 16, CAP], BF16)
    hT = P.sb("hTe", [128, 8, CAP], BF16)
    wgb = [P.sb("wgb%d" % i, [128, 16, 256], BF16) for i in range(2)]
    wub = [P.sb("wub%d" % i, [128, 16, 256], BF16) for i in range(2)]
    wdb = [P.sb("wdb%d" % i, [128, 8, 512], BF16) for i in range(2)]
    sa = [P.sb("sa%d" % i, [128, 512], F32) for i in range(2)]
    ys = [P.sb("ys%d" % i, [128, 512], F32) for i in range(2)]
    pa = [P.ps("pa%d" % i, [128, 512], F32) for i in range(2)]
    pu = [P.ps("pu%d" % i, [128, 512], F32) for i in range(2)]
    py = [P.ps("py%d" % i, [128, 512], F32) for i in range(2)]
    pT = P.ps("pT", [128, D], BF16)

    P.op("pool", lambda: nc.gpsimd.memset(onesf[:], 1.0), writes=["onesf"])
    P.op("pool", lambda: nc.gpsimd.memset(UT[:], 1.0), writes=["UT"])
    P.op("pool", lambda: nc.gpsimd.affine_select(out=UT[:], in_=UT[:], pattern=[[1, 128]], compare_op=ALU.is_ge, fill=0.0, base=0,
                                                 channel_multiplier=-1), reads=["UT"], writes=["UT"])
    P.op("pool", lambda: nc.gpsimd.memset(Lb[:], 1.0), writes=["Lb"])
    P.op("pool", lambda: nc.gpsimd.affine_select(out=Lb[:], in_=Lb[:], pattern=[[1, 128]], compare_op=ALU.is_gt, fill=0.0, base=0,
                                                 channel_multiplier=-1), reads=["Lb"], writes=["Lb"])
    P.op("pool", lambda: nc.gpsimd.memset(Lb[0:64, 64:128], 0.0), reads=["Lb"], writes=["Lb"])
    P.op("pool", lambda: nc.gpsimd.iota(iot_i[:], pattern=[[1, 1024]], base=0, channel_multiplier=0), writes=["iot_i"])
    P.op("dve", lambda: nc.vector.tensor_copy(out=iot[:], in_=iot_i[:]), reads=["iot_i"], writes=["iot"])
    P.op("pool", lambda: nc.gpsimd.iota(jp_i[:, :, 0], pattern=[[1, 64]], base=0, channel_multiplier=0), writes=["jp_i"])
    P.op("pool", lambda: nc.gpsimd.iota(jp_i[:, :, 1], pattern=[[0, 64]], base=0, channel_multiplier=1), reads=["jp_i"], writes=["jp_i"])
    P.op("dve", lambda: nc.vector.tensor_copy(out=jp[:], in_=jp_i[:]), reads=["jp_i"], writes=["jp"])
    P.dma("sp", a[:], affd[:, :, :], writes=["a"])
    P.dma("sp", eb[:], ebd[:, :], writes=["eb"])
    P.op("pool", lambda: nc.gpsimd.memset(zb[:], 0.0), writes=["zb"])

    lo, stp, nn, tmp = bs[:, 0:2], bs[:, 2:4], bs[:, 4:6], bs[:, 6:8]
    P.op("dve", lambda: nc.vector.memset(bs[:, 0:2], 0.0), writes=["bs"])
    P.op("dve", lambda: nc.vector.memset(bs[:, 2:4], 1.001 / 16), reads=["bs"], writes=["bs"])
    P.op("pool", lambda: nc.gpsimd.iota(kv_i[:], pattern=[[0, 2], [1, 16]], base=0, channel_multiplier=0), writes=["kv_i"])
    P.op("dve", lambda: nc.vector.tensor_copy(out=kv[:], in_=kv_i[:]), reads=["kv_i"], writes=["kv"])
    tot = pa[0][:, 0:32].rearrange("p (e k) -> p e k", e=2)

    def V(fn, reads=("bs",), writes=("bs",)):
        P.op("dve", fn, reads=list(reads), writes=list(writes))

    for itb in range(8):
        V(lambda: nc.vector.tensor_tensor(out=thr[:], in0=kv[:], in1=stp.unsqueeze(2).to_broadcast([128, 2, 16]), op=ALU.mult),
          reads=["kv", "bs"], writes=["thr"])
        V(lambda: nc.vector.tensor_tensor(out=thr[:], in0=thr[:], in1=lo.unsqueeze(2).to_broadcast([128, 2, 16]), op=ALU.add),
          reads=["thr", "bs"], writes=["thr"])
        V(lambda: nc.vector.tensor_tensor(out=mk4[:], in0=a[:, :, :].unsqueeze(2).to_broadcast([128, 2, 16, 64]),
                                          in1=thr[:, :, :].unsqueeze(3).to_broadcast([128, 2, 16, 64]), op=ALU.is_ge),
          reads=["a", "thr"], writes=["mk4"])
        V(lambda: nc.vector.reduce_sum(out=cn4[:], in_=mk4[:], axis=AX.X), reads=["mk4"], writes=["cn4"])
        P.op("pe", lambda: nc.tensor.matmul(pa[0][:, 0:32], lhsT=onesf[:], rhs=cn4.rearrange("p e k -> p (e k)"), start=True, stop=True),
             reads=["onesf", "cn4"], writes=[("pa", 0)])
        V(lambda: nc.vector.tensor_scalar(out=ge4[:], in0=tot, scalar1=float(CAP) - 0.5, scalar2=None, op0=ALU.is_ge),
          reads=[("pa", 0)], writes=["ge4"])
        V(lambda: nc.vector.reduce_sum(out=nn, in_=ge4[:], axis=AX.X), reads=["ge4", "bs"], writes=["bs"])
        V(lambda: nc.vector.scalar_tensor_tensor(out=tmp, in0=nn, scalar=-1.0, in1=stp, op0=ALU.add, op1=ALU.mult))
        V(lambda: nc.vector.tensor_tensor(out=lo, in0=lo, in1=tmp, op=ALU.add))
        V(lambda: nc.vector.tensor_scalar(out=stp, in0=stp, scalar1=0.0625, scalar2=None, op0=ALU.mult))
    V(lambda: nc.vector.tensor_tensor(out=msk[:], in0=a[:], in1=lo.unsqueeze(2).to_broadcast([128, 2, 64]), op=ALU.is_ge),
      reads=["a", "bs"], writes=["msk"])

    mflat = msk.rearrange("p e j -> p (e j)")
    pc = pa[1][:, 0:128]
    pex = pu[0][:, 0:128]
    pct = pu[1][:, 0:1]
    P.op("pe", lambda: nc.tensor.matmul(pc, lhsT=UT[:], rhs=mflat, start=True, stop=True), reads=["UT", "msk"], writes=[("pa", 1)])
    P.op("pe", lambda: nc.tensor.matmul(pct, lhsT=mflat, rhs=onesf[:, 0:1], start=True, stop=True), reads=["onesf", "msk"], writes=[("pu", 1)])
    P.op("dve", lambda: nc.vector.tensor_copy(out=ct[:], in_=pct), reads=[("pu", 1)], writes=["ct"])
    P.op("dve", lambda: nc.vector.tensor_scalar(out=CB[:], in0=onesf[:], scalar1=ct[:, 0:1], scalar2=None, op0=ALU.mult),
         reads=["onesf", "ct"], writes=["CB"])
    P.op("pe", lambda: nc.tensor.matmul(pex, lhsT=CB[:], rhs=Lb[:], start=True, stop=True), reads=["CB", "Lb"], writes=[("pu", 0)])
    P.op("dve", lambda: nc.vector.tensor_copy(out=exs[:], in_=pex), reads=[("pu", 0)], writes=["exs"])
    rkf = rk.rearrange("p e j -> p (e j)")
    P.op("dve", lambda: nc.vector.tensor_tensor(out=rkf, in0=pc, in1=exs[:], op=ALU.add), reads=[("pa", 1), "exs"], writes=["rk"])
    P.op("dve", lambda: nc.vector.tensor_tensor(out=rkf, in0=rkf, in1=mflat, op=ALU.mult), reads=["rk", "msk"], writes=["rk"])
    P.op("dve", lambda: nc.vector.tensor_scalar(out=rkf, in0=rkf, scalar1=-1.0, scalar2=None, op0=ALU.add), reads=["rk"], writes=["rk"])
    P.op("dve", lambda: nc.vector.tensor_tensor(out=sg[:], in0=rk[:], in1=eb[:, :].unsqueeze(2).to_broadcast([128, 2, 64]), op=ALU.add),
         reads=["rk", "eb"], writes=["sg"])
    P.op("dve", lambda: nc.vector.tensor_scalar(out=sg[:], in0=sg[:], scalar1=-OOB, scalar2=None, op0=ALU.add), reads=["sg"], writes=["sg"])
    P.op("dve", lambda: nc.vector.tensor_tensor(out=sg[:], in0=sg[:], in1=msk[:], op=ALU.mult), reads=["sg", "msk"], writes=["sg"])
    P.op("dve", lambda: nc.vector.tensor_scalar(out=sg[:], in0=sg[:], scalar1=OOB, scalar2=None, op0=ALU.add), reads=["sg"], writes=["sg"])
    P.op("dve", lambda: nc.vector.tensor_copy(out=sgi[:], in_=sg[:]), reads=["sg"], writes=["sgi"])
    P.dma("sp", sloto[:, :, :], sgi[:], reads=["sgi"], writes=["sloto"])

    for e in range(2):
        psel = py[e][:, 0:16].rearrange("p (s c) -> p s c", c=2)
        P.op("pe", lambda: nc.tensor.matmul(py[e][:, 0:16], lhsT=zb[:, 0:128], rhs=zb[:, 0:16], start=True, stop=False),
             reads=["zb"], writes=[("py", e)])
        for j in range(64):
            sb_ = sel[j % 3]
            P.op("dve", lambda: nc.vector.tensor_scalar(out=sb_[:], in0=iot[:], scalar1=rk[:, e, j:j + 1], scalar2=None, op0=ALU.is_equal),
                 reads=["iot", "rk"], writes=[("sel", j % 3)])
            for s in range(8):
                P.op("pe", lambda s=s: nc.tensor.matmul(psel[:, s, :], lhsT=sb_[:, s * 128:(s + 1) * 128], rhs=jp[:, j, :],
                                                         start=False, stop=(j == 63), skip_group_check=True),
                     reads=[("sel", j % 3), "jp"], writes=[("py", e)])
        P.op("dve", lambda: nc.vector.tensor_copy(out=pselS[:], in_=psel), reads=[("py", e)], writes=["pselS"])
        P.op("dve", lambda: nc.vector.scalar_tensor_tensor(out=idxf[:, e, :], in0=pselS[:, :, 0], scalar=128.0, in1=pselS[:, :, 1],
                                                           op0=ALU.mult, op1=ALU.add),
             reads=["pselS"], writes=["idxf"])
    P.op("dve", lambda: nc.vector.tensor_copy(out=idxi[:], in_=idxf[:]), reads=["idxf"], writes=["idxi"])

    CL = CastLoader(P, 1024)
    gi = 0
    for e in range(2):
        for s in range(8):
            g_ = gi % 2
            gi += 1
            P.idma(xg[g_][:], None, hfd[:, :], bass.IndirectOffsetOnAxis(ap=idxi[:, e, s:s + 1], axis=0),
                   reads=["idxi"], writes=[("xg", g_)])
            for kc in range(16):
                P.op("pe", lambda kc=kc: nc.tensor.transpose(pT[:, kc * 128:(kc + 1) * 128], xg[g_][:, kc * 128:(kc + 1) * 128], ident[:]),
                     reads=[("xg", g_), idk], writes=["pT"])
            P.op("act", lambda: nc.scalar.copy(out=xsT[:, :, s * 128:(s + 1) * 128], in_=pT.rearrange("p (k t) -> p k t", k=16)),
                 reads=["pT"], writes=["xsT"])
        wgv = wgd[e].rearrange("(kc p) f -> p kc f", p=128)
        wuv = wud[e].rearrange("(kc p) f -> p kc f", p=128)
        wdv = wdd[e].rearrange("(fc p) d -> p fc d", p=128)
        zi = 0

        def queue_gu(f2, wgv=wgv, wuv=wuv):
            b = f2 % 2
            for kq in range(4):
                CL.enqueue(wgb[b][:, kq * 4:(kq + 1) * 4, :], ("wgb", b), wgv[:, kq * 4:(kq + 1) * 4, f2 * 256:(f2 + 1) * 256], (4, 256))
                CL.enqueue(wub[b][:, kq * 4:(kq + 1) * 4, :], ("wub", b), wuv[:, kq * 4:(kq + 1) * 4, f2 * 256:(f2 + 1) * 256], (4, 256))

        def queue_d(dc, wdv=wdv):
            b = dc % 2
            for fq in range(4):
                CL.enqueue(wdb[b][:, fq * 2:(fq + 1) * 2, :], ("wdb", b), wdv[:, fq * 2:(fq + 1) * 2, dc * 512:(dc + 1) * 512], (2, 512))

        if e == 0:
            queue_gu(0)
        for f2 in range(4):
            b = f2 % 2
            CL.flush()
            if f2 + 1 < 4:
                queue_gu(f2 + 1)
            else:
                queue_d(0)
            for fs in range(2):
                fc = f2 * 2 + fs
                for hh in range(2):
                    z = zi % 2
                    zi += 1
                    for kc in range(16):
                        P.op("pe", lambda kc=kc: nc.tensor.matmul(pa[z][:], lhsT=wgb[b][:, kc, fs * 128:(fs + 1) * 128],
                                                                   rhs=xsT[:, kc, hh * 512:(hh + 1) * 512], start=(kc == 0), stop=(kc == 15)),
                             reads=[("wgb", b), "xsT"], writes=[("pa", z)])
                    for kc in range(16):
                        P.op("pe", lambda kc=kc: nc.tensor.matmul(pu[z][:], lhsT=wub[b][:, kc, fs * 128:(fs + 1) * 128],
                                                                   rhs=xsT[:, kc, hh * 512:(hh + 1) * 512], start=(kc == 0), stop=(kc == 15)),
                             reads=[("wub", b), "xsT"], writes=[("pu", z)])
                    P.op("act", lambda: nc.scalar.activation(out=sa[z][:], in_=pa[z][:], func=AF.Silu), reads=[("pa", z)], writes=[("sa", z)])
                    P.op("dve", lambda: nc.vector.tensor_tensor(out=hT[:, fc, hh * 512:(hh + 1) * 512], in0=pu[z][:], in1=sa[z][:], op=ALU.mult),
                         reads=[("pu", z), ("sa", z)], writes=["hT"])
                    CL.pump(2)
        yi = 0
        for dc in range(4):
            b = dc % 2
            CL.flush()
            if dc + 1 < 4:
                queue_d(dc + 1)
            elif e == 0:
                wgv1 = wgd[1].rearrange("(kc p) f -> p kc f", p=128)
                wuv1 = wud[1].rearrange("(kc p) f -> p kc f", p=128)
                queue_gu(0, wgv1, wuv1)
            for s in range(8):
                z = yi % 2
                yi += 1
                for fc in range(8):
                    P.op("pe", lambda fc=fc: nc.tensor.matmul(py[z][:], lhsT=hT[:, fc, s * 128:(s + 1) * 128], rhs=wdb[b][:, fc, :],
                                                               start=(fc == 0), stop=(fc == 7)),
                         reads=["hT", ("wdb", b)], writes=[("py", z)])
                P.op("act", lambda: nc.scalar.copy(out=ys[z][:], in_=py[z][:]), reads=[("py", z)], writes=[("ys", z)])
                P.dma("act", Yo[e, s * 128:(s + 1) * 128, dc * 512:(dc + 1) * 512], ys[z][:], reads=[("ys", z)], writes=["Yo"])
                CL.pump(1)
    return P.finish(["Yo", "sloto"])


def run_E(aff, hf, wg, wu, wd):
    nc = build_E()
    hf = np.ascontiguousarray(hf)
    maps = []
    for c in range(NCORES):
        es = slice(2 * c, 2 * c + 2)
        affT = np.ascontiguousarray(aff[:, es].reshape(64, 128, 2).transpose(1, 2, 0))
        ebase = np.ascontiguousarray(np.broadcast_to(np.array([[2 * c * CAP, (2 * c + 1) * CAP]], np.float32), (128, 2)))
        maps.append({"affT": affT, "ebase": ebase, "hf": hf, "wg": np.ascontiguousarray(wg[es]), "wu": np.ascontiguousarray(wu[es]),
                     "wd": np.ascontiguousarray(wd[es])})
    res = run_bass_kernel_spmd(nc, maps, core_ids=list(range(NCORES)))
    Yall = np.concatenate([r["Y"].reshape(2 * CAP, D) for r in res.results], 0)
    slots = np.concatenate([r["slot"].transpose(2, 0, 1).reshape(SEQ, 2) for r in res.results], 1)
    return Yall, np.ascontiguousarray(slots)


NYROWS = 16 * CAP
NGT = 5


def combine_setup(P):
    nc = P.nc
    c = {}
    c["x1d"] = P.din("x1", [TPC, D], F32)
    c["Yd"] = P.din("Yall", [NYROWS, D], F32)
    c["sld"] = P.din("slots", [TPC, 16], I32)
    c["afd"] = P.din("aff", [TPC, 16], F32)
    c["gfd"] = P.din("gf", [128, D], F32)
    c["sl"] = P.sb("sl", [128, NT, 16], I32)
    c["af"] = P.sb("af", [128, NT, 16], F32)
    c["gf"] = P.sb("gfs", [128, D], F32)
    c["gt"] = [P.sb("gt%d" % i, [128, D], F32) for i in range(NGT)]
    c["acc"] = [P.sb("acc%d" % i, [128, D], F32) for i in range(2)]
    c["xs"] = [P.sb("cxs%d" % i, [128, D], F32) for i in range(2)]
    P.dma("sp", c["sl"][:], c["sld"].rearrange("(t p) e -> p t e", p=128), writes=["sl"])
    P.dma("sp", c["af"][:], c["afd"].rearrange("(t p) e -> p t e", p=128), writes=["af"])
    P.dma("sp", c["gf"][:], c["gfd"][:, :], writes=["gfs"])
    c["gi"] = 0
    c["breg"] = nc.gpsimd.to_reg(NYROWS - 1)
    return c


def combine_tile(P, c, ti):
    nc = P.nc
    b = ti % 2
    acc = c["acc"][b]
    ak = ("acc", b)
    xs = c["xs"][b]
    xk = ("cxs", b)
    P.dma("sp", xs[:], c["x1d"][ti * 128:(ti + 1) * 128, :], writes=[xk])
    for e in range(16):
        g = c["gi"] % NGT
        c["gi"] += 1
        gt = c["gt"][g]
        gk = ("gt", g)
        P.op("act", lambda: nc.scalar.memzero(gt[:]), writes=[gk])
        P.idma(gt[:], None, c["Yd"][:, :], bass.IndirectOffsetOnAxis(ap=c["sl"][:, ti, e:e + 1], axis=0),
               reads=["sl"], writes=[gk], semkey=("dg", g), bounds_check=c["breg"], oob_is_err=False)
        if e == 0:
            P.op("dve", lambda: nc.vector.tensor_scalar(out=acc[:], in0=gt[:], scalar1=c["af"][:, ti, e:e + 1], scalar2=None, op0=ALU.mult),
                 reads=[gk, "af"], writes=[ak])
        else:
            P.op("dve", lambda: nc.vector.scalar_tensor_tensor(out=acc[:], in0=gt[:], scalar=c["af"][:, ti, e:e + 1], in1=acc[:],
                                                               op0=ALU.mult, op1=ALU.add),
                 reads=[gk, "af", ak], writes=[ak])
    P.op("dve", lambda: nc.vector.tensor_tensor(out=acc[:], in0=acc[:], in1=c["gf"][:], op=ALU.mult), reads=[ak, "gfs"], writes=[ak])
    P.op("pool", lambda: nc.gpsimd.tensor_tensor(out=xs[:], in0=xs[:], in1=acc[:], op=ALU.add), reads=[xk, ak], writes=[xk])
    return xs, xk


def build_F():
    P = Prog()
    nc = P.nc
    c = combine_setup(P)
    gmixd = P.din("gmix", [128, D], F32)
    cmd = P.din("cm", [128, D], F32)
    smd = P.din("sm", [128, D], F32)
    Cd = P.din("Cch", [512, 512], BF16)
    Sd = P.din("Sch", [512, 512], BF16)
    x2o = P.dout("x2", [TPC, D], F32)
    Ao = P.dout("A", [TPC, D], BF16)
    Bo = P.dout("B", [TPC, D], BF16)
    ident, idk = make_ident(P)
    gam = P.sb("gam", [128, D], F32)
    sh = P.sb("sh", [128, D], F32)
    hb = [P.sb("hb%d" % i, [128, D], BF16) for i in range(2)]
    hTt = P.sb("hTt", [128, 16, 128], BF16)
    Cs = P.sb("Cs", [128, 4, 512], BF16)
    Ss = P.sb("Ss", [128, 4, 512], BF16)
    ao = [P.sb("ao%d" % i, [128, 512], BF16) for i in range(2)]
    ss = [P.sb("ss%d" % i, [128, 4], F32) for i in range(2)]
    epsb = P.sb("epsb", [128, 1], F32)
    pT = P.ps("pT", [128, D], BF16)
    pz = [P.ps("pz%d" % i, [128, 512], F32) for i in range(2)]
    P.op("pool", lambda: nc.gpsimd.memset(epsb[:], EPS), writes=["epsb"])
    P.dma("act", gam[:], cmd[:, :], writes=["gam"])
    P.dma("act", c["gt"][0][:], gmixd[:, :], writes=[("gt", 0)], semkey=("dg", 0))
    P.dma("act", sh[:], smd[:, :], writes=["sh"])
    P.dma("act", Cs[:], Cd.rearrange("(k p) n -> p k n", p=128), writes=["Cs"])
    P.dma("act", Ss[:], Sd.rearrange("(k p) n -> p k n", p=128), writes=["Ss"])
    P.op("dve", lambda: nc.vector.scalar_tensor_tensor(out=gam[:], in0=gam[:], scalar=1.0, in1=c["gt"][0][:], op0=ALU.add, op1=ALU.mult),
         reads=["gam", ("gt", 0)], writes=["gam"])
    zc = 0
    for ti in range(NT):
        b = ti % 2
        xs, xk = combine_tile(P, c, ti)
        P.dma("act", x2o[ti * 128:(ti + 1) * 128, :], xs[:], reads=[xk], writes=["x2o"])
        rms_modulate_tile(P, nc, xs, xk, 128, ss[b], ("ss", b), epsb, gam, sh, hb[b], ("hb", b))
        for kc in range(16):
            P.op("pe", lambda kc=kc: nc.tensor.transpose(pT[:, kc * 128:(kc + 1) * 128], hb[b][:, kc * 128:(kc + 1) * 128], ident[:]),
                 reads=[("hb", b), idk], writes=["pT"])
        P.op("act", lambda: nc.scalar.copy(out=hTt[:], in_=pT.rearrange("p (k t) -> p k t", k=16)), reads=["pT"], writes=["hTt"])
        for g in range(4):
            for (W_, wk, outd) in ((Cs, "Cs", Ao), (Ss, "Ss", Bo)):
                j = zc % 2
                zc += 1
                for k in range(4):
                    P.op("pe", lambda k=k: nc.tensor.matmul(pz[j][:], lhsT=hTt[:, g * 4 + k, :], rhs=W_[:, k, :], start=(k == 0), stop=(k == 3)),
                         reads=["hTt", wk], writes=[("pz", j)])
                P.op("act", lambda: nc.scalar.copy(out=ao[j][:], in_=pz[j][:]), reads=[("pz", j)], writes=[("ao", j)])
                P.dma("act", outd[ti * 128:(ti + 1) * 128, g * 512:(g + 1) * 512], ao[j][:], reads=[("ao", j)], writes=["AB"])
    return P.finish(["x2o", "AB"])


def bf16_np(a):
    import ml_dtypes
    return np.ascontiguousarray(np.asarray(a, np.float32).astype(ml_dtypes.bfloat16))


def dft_consts():
    c = np.arange(512)
    ang = 2 * np.pi * np.outer(c, c) / 512
    Cch = np.cos(ang) / math.sqrt(512)
    Sch = np.sin(ang) / math.sqrt(512)
    n1 = np.arange(128)
    a1 = 2 * np.pi * np.outer(n1, n1) / 128
    Cc = np.cos(a1) / math.sqrt(128)
    Ssn = np.sin(a1) / math.sqrt(128)
    n2 = np.arange(64)[:, None, None]
    k1 = np.arange(128)[None, :, None]
    k2 = np.arange(64)[None, None, :]
    th = 2 * np.pi * (n2 * k1 / 8192.0 + n2 * k2 / 64.0)
    Hr = np.cos(th) / 8.0
    Hni = np.sin(th) / 8.0
    return dict(Cch=bf16_np(Cch), Sch=bf16_np(Sch), Cc=bf16_np(Cc), nSs=bf16_np(-Ssn), nCc=bf16_np(-Cc), Hr=bf16_np(Hr), Hni=bf16_np(Hni))


def run_F(x1, Yall, slots, aff, gf_, gmix, cm_, sm_):
    nc = build_F()
    k = dft_consts()
    common = {"Yall": Yall, "gf": bc128(gf_), "gmix": bc128(gmix), "cm": bc128(cm_), "sm": bc128(sm_), "Cch": k["Cch"], "Sch": k["Sch"]}
    maps = []
    for i in range(NCORES):
        d = dict(common)
        sl = slice(i * TPC, (i + 1) * TPC)
        d["x1"] = np.ascontiguousarray(x1[sl])
        d["slots"] = np.ascontiguousarray(slots[sl])
        d["aff"] = np.ascontiguousarray(aff[sl])
        maps.append(d)
    res = run_bass_kernel_spmd(nc, maps, core_ids=list(range(NCORES)))
    x2 = np.concatenate([r["x2"] for r in res.results], 0)
    A = np.concatenate([r["A"] for r in res.results], 0)
    B = np.concatenate([r["B"] for r in res.results], 0)
    return x2, A, B


CPC = D // NCORES


def build_G():
    P = Prog()
    nc = P.nc
    zAd = P.din("zA", [128, 64, CPC], BF16)
    zBd = P.din("zB", [128, 64, CPC], BF16)
    Ccd = P.din("Cc", [128, 128], BF16)
    nSsd = P.din("nSs", [128, 128], BF16)
    nCcd = P.din("nCc", [128, 128], BF16)
    Hrd = P.din("Hr", [64, 128, 64], BF16)
    Hnid = P.din("Hni", [64, 128, 64], BF16)
    YTo = P.dout("YT", [CPC, SEQ], BF16)
    zA = P.sb("zAs", [128, 64, 128], BF16)
    zB = P.sb("zBs", [128, 64, 128], BF16)
    Cc = P.sb("Ccs", [128, 128], BF16)
    nSs = P.sb("nSss", [128, 128], BF16)
    nCc = P.sb("nCcs", [128, 128], BF16)
    Hr = P.sb("Hrs", [64, 128, 64], BF16)
    Hni = P.sb("Hnis", [64, 128, 64], BF16)
    Tr = P.sb("Tr", [64, 128, 128], BF16)
    Ti = P.sb("Ti", [64, 128, 128], BF16)
    YTs = P.sb("YTs", [128, 64, 128], BF16)
    pTr = [P.ps("pTr%d" % i, [64, 4, 128], F32) for i in range(2)]
    pTi = [P.ps("pTi%d" % i, [64, 4, 128], F32) for i in range(2)]
    pY = [P.ps("pY%d" % i, [128, 8, 64], F32) for i in range(2)]
    P.dma("sp", Cc[:], Ccd[:, :], writes=["Cc"])
    P.dma("sp", nSs[:], nSsd[:, :], writes=["nSs"])
    P.dma("sp", nCc[:], nCcd[:, :], writes=["nCc"])
    P.dma("act", Hr[:], Hrd[:, :, :], writes=["Hr"])
    P.dma("act", Hni[:], Hnid[:, :, :], writes=["Hni"])
    for hc in range(2):
        c0 = hc * 128
        P.dma("sp", zA[:], zAd[:, :, c0:c0 + 128], writes=["zA"])
        P.dma("act", zB[:], zBd[:, :, c0:c0 + 128], writes=["zB"])
        for cb in range(32):
            j = cb % 2
            for cc in range(4):
                c = cb * 4 + cc
                P.op("pe", lambda: nc.tensor.matmul(pTr[j][:, cc, :], lhsT=zA[:, :, c], rhs=Cc[:], start=True, stop=False),
                     reads=["zA", "Cc"], writes=[("pTr", j)])
                P.op("pe", lambda: nc.tensor.matmul(pTr[j][:, cc, :], lhsT=zB[:, :, c], rhs=nSs[:], start=False, stop=True),
                     reads=["zB", "nSs"], writes=[("pTr", j)])
                P.op("pe", lambda: nc.tensor.matmul(pTi[j][:, cc, :], lhsT=zB[:, :, c], rhs=nCc[:], start=True, stop=False),
                     reads=["zB", "nCc"], writes=[("pTi", j)])
                P.op("pe", lambda: nc.tensor.matmul(pTi[j][:, cc, :], lhsT=zA[:, :, c], rhs=nSs[:], start=False, stop=True),
                     reads=["zA", "nSs"], writes=[("pTi", j)])
            P.op("act", lambda: nc.scalar.copy(out=Tr[:, cb * 4:(cb + 1) * 4, :], in_=pTr[j][:]), reads=[("pTr", j)], writes=["Tr"])
            P.op("dve", lambda: nc.vector.tensor_copy(out=Ti[:, cb * 4:(cb + 1) * 4, :], in_=pTi[j][:]), reads=[("pTi", j)], writes=["Ti"])
        for kb in range(16):
            j = kb % 2
            for kk in range(8):
                k1 = kb * 8 + kk
                P.op("pe", lambda: nc.tensor.matmul(pY[j][:, kk, :], lhsT=Tr[:, :, k1], rhs=Hr[:, k1, :], start=True, stop=False),
                     reads=["Tr", "Hr"], writes=[("pY", j)])
                P.op("pe", lambda: nc.tensor.matmul(pY[j][:, kk, :], lhsT=Ti[:, :, k1], rhs=Hni[:, k1, :], start=False, stop=True),
                     reads=["Ti", "Hni"], writes=[("pY", j)])
            eng = "act" if kb % 2 == 0 else "dve"
            if eng == "act":
                P.op("act", lambda: nc.scalar.copy(out=YTs[:, :, kb * 8:(kb + 1) * 8], in_=pY[j].rearrange("p a b -> p b a")),
                     reads=[("pY", j)], writes=["YTs"])
            else:
                P.op("dve", lambda: nc.vector.tensor_copy(out=YTs[:, :, kb * 8:(kb + 1) * 8], in_=pY[j].rearrange("p a b -> p b a")),
                     reads=[("pY", j)], writes=["YTs"])
        P.dma("sp", YTo[c0:c0 + 128, :], YTs.rearrange("p a b -> p (a b)"), reads=["YTs"], writes=["YTo"])
    return P.finish(["YTo"])


def run_G(A, B):
    nc = build_G()
    k = dft_consts()
    maps = []
    for i in range(NCORES):
        cs = slice(i * CPC, (i + 1) * CPC)
        maps.append({"zA": np.ascontiguousarray(A[:, cs]).reshape(128, 64, CPC), "zB": np.ascontiguousarray(B[:, cs]).reshape(128, 64, CPC),
                     "Cc": k["Cc"], "nSs": k["nSs"], "nCc": k["nCc"], "Hr": k["Hr"], "Hni": k["Hni"]})
    res = run_bass_kernel_spmd(nc, maps, core_ids=list(range(NCORES)))
    YT = np.concatenate([r["YT"] for r in res.results], 0)
    return YT


def build_I():
    P = Prog()
    nc = P.nc
    c = combine_setup(P)
    gfin_d = P.din("gfin", [128, D], F32)
    outd = P.dout("out", [TPC, D], F32)
    gfin = P.sb("gfin_s", [128, D], F32)
    jk = P.sb("junk", [128, D], BF16)
    ss = [P.sb("ss%d" % i, [128, 4], F32) for i in range(2)]
    epsb = P.sb("epsb", [128, 1], F32)
    P.op("pool", lambda: nc.gpsimd.memset(epsb[:], EPS), writes=["epsb"])
    P.dma("act", gfin[:], gfin_d[:, :], writes=["gfin"])
    for ti in range(NT):
        b = ti % 2
        xs, xk = combine_tile(P, c, ti)
        s_ = ss[b]
        sk = ("ss", b)
        P.op("act", lambda: nc.scalar.activation(out=jk[:], in_=xs[:], func=AF.Square, accum_out=s_[:, 0:1]), reads=[xk], writes=["junk", sk])
        P.op("act", lambda: nc.scalar.activation(out=s_[:, 1:2], in_=s_[:, 0:1], func=AF.Sqrt, scale=1.0 / D, bias=epsb[:, 0:1]),
             reads=[sk, "epsb"], writes=[sk])
        P.op("dve", lambda: nc.vector.reciprocal(out=s_[:, 2:3], in_=s_[:, 1:2]), reads=[sk], writes=[sk])
        P.op("dve", lambda: nc.vector.scalar_tensor_tensor(out=xs[:], in0=xs[:], scalar=s_[:, 2:3], in1=gfin[:], op0=ALU.mult, op1=ALU.mult),
             reads=[xk, sk, "gfin"], writes=[xk])
        P.dma("act", outd[ti * 128:(ti + 1) * 128, :], xs[:], reads=[xk], writes=["outd"])
    return P.finish(["outd"])


def run_I(x1, Yall, slots, aff, gf_, gfin):
    nc = build_I()
    common = {"Yall": Yall, "gf": bc128(gf_), "gfin": bc128(gfin)}
    maps = []
    for i in range(NCORES):
        d = dict(common)
        sl = slice(i * TPC, (i + 1) * TPC)
        d["x1"] = np.ascontiguousarray(x1[sl])
        d["slots"] = np.ascontiguousarray(slots[sl])
        d["aff"] = np.ascontiguousarray(aff[sl])
        maps.append(d)
    res = run_bass_kernel_spmd(nc, maps, core_ids=list(range(NCORES)))
    return np.concatenate([r["out"] for r in res.results], 0)


def kernel(**inp):
    inp = {k: np.asarray(v) for k, v in inp.items()}
    m = run_mod(inp)
    x0 = inp["x"][0]
    ra = run_A(inp, m)
    aT = run_B(inp, ra)
    fT = [np.concatenate([np.stack([aT[h][:, i * TPC:(i + 1) * TPC] for h in range(8)], 0), ra[i]["gT"]], 0) for i in range(NCORES)]
    m0 = m[0, 0]
    x1, hf, aff = run_proj(fT, inp["w_out"][0], x0, m0[2 * D:3 * D], inp["g_norm_ffn"][0], m0[4 * D:5 * D], m0[3 * D:4 * D],
                           inp["w_router"][0])
    Yall, slots = run_E(aff, hf, inp["w_gate"][0], inp["w_up"][0], inp["w_down"][0])
    m1 = m[1, 0]
    x2, A, B = run_F(x1, Yall, slots, aff, m0[5 * D:6 * D], inp["g_norm_mix"][1], m1[D:2 * D], m1[0:D])
    YT = run_G(A, B)
    fT = [np.ascontiguousarray(YT[:, i * TPC:(i + 1) * TPC]).reshape(16, 128, TPC) for i in range(NCORES)]
    x1, hf, aff = run_proj(fT, inp["w_fourier_out"][0], x2, m1[2 * D:3 * D], inp["g_norm_ffn"][1], m1[4 * D:5 * D], m1[3 * D:4 * D],
                           inp["w_router"][1])
    Yall, slots = run_E(aff, hf, inp["w_gate"][1], inp["w_up"][1], inp["w_down"][1])
    out = run_I(x1, Yall, slots, aff, m1[5 * D:6 * D], inp["g_final"])
    return np.ascontiguousarray(out.reshape(1, SEQ, D).astype(np.float32))
```

```python
import math
import numpy as np
from contextlib import ExitStack
import concourse.bass as bass
import concourse.mybir as mybir
from concourse.bass_utils import run_bass_kernel_spmd

F32 = mybir.dt.float32
BF16 = mybir.dt.bfloat16
I32 = mybir.dt.int32
AF = mybir.ActivationFunctionType
ALU = mybir.AluOpType
AX = mybir.AxisListType
NCORES = 8
D = 2048
SEQ = 8192
TPC = SEQ // NCORES
NT = TPC // 128
EPS = 1e-6


class Res:
    __slots__ = ("w", "r")

    def __init__(self):
        self.w = {}
        self.r = {}


class Prog:
    def __init__(self):
        self.nc = bass.Bass("TRN2", target_bir_lowering=False)
        self.es = ExitStack()
        nc = self.nc
        self.eng = {"pe": nc.tensor, "act": nc.scalar, "dve": nc.vector,
                    "pool": nc.gpsimd, "sp": nc.sync}
        self.sems = {}
        self.cnt = {}
        for e in self.eng:
            self.sems[e] = self.es.enter_context(nc.semaphore("s_" + e))
            self.cnt[e] = 0
        self.known = {e: {} for e in self.eng}
        self.res = {}
        self.nd = 0

    def sb(self, name, shape, dt):
        return self.es.enter_context(self.nc.sbuf_tensor(name, list(shape), dt))

    def ps(self, name, shape, dt):
        return self.es.enter_context(self.nc.psum_tensor(name, list(shape), dt))

    def din(self, name, shape, dt):
        return self.nc.dram_tensor(name, list(shape), dt, kind="ExternalInput").ap()

    def dout(self, name, shape, dt):
        return self.nc.dram_tensor(name, list(shape), dt, kind="ExternalOutput").ap()

    def dtmp(self, name, shape, dt):
        return self.nc.dram_tensor(name, list(shape), dt, kind="Internal").ap()

    def R(self, key):
        r = self.res.get(key)
        if r is None:
            r = self.res[key] = Res()
        return r

    def _wait(self, e, semkey, val):
        if val <= 0:
            return
        k = self.known[e]
        if k.get(semkey, 0) >= val:
            return
        k[semkey] = val
        if getattr(self, "_pend", None) is not None:
            self._pend.append((semkey, val))
        else:
            self.eng[e].wait_ge(self.sems[semkey], val)

    def _deps(self, e, reads, writes, skip=None):
        for key in reads:
            for sk, v in self.R(key).w.items():
                if e == "pe" and sk == "pe":
                    continue
                self._wait(e, sk, v)
        for key in writes:
            r = self.R(key)
            for sk, v in r.w.items():
                if sk == skip or (e == "pe" and sk == "pe"):
                    continue
                self._wait(e, sk, v)
            for sk, v in r.r.items():
                if e == "pe" and sk == "pe":
                    continue
                self._wait(e, sk, v)

    def op(self, e, fn, reads=(), writes=()):
        self._pend = []
        self._deps(e, reads, writes)
        pend, self._pend = self._pend, None
        for (sk, v) in pend[:-1]:
            self.eng[e].wait_ge(self.sems[sk], v)
        ins = fn()
        if pend:
            ins._wait_ge(self.sems[pend[-1][0]], pend[-1][1])
        self.cnt[e] += 1
        c = self.cnt[e]
        ins.then_inc(self.sems[e], 1)
        for key in reads:
            self.R(key).r[e] = c
        for key in writes:
            r = self.R(key)
            r.w = {e: c}
            r.r = {}
        return ins

    def _dma_post(self, ins, sk, reads, writes):
        self.cnt[sk] += 16
        c = self.cnt[sk]
        ins.then_inc(self.sems[sk], 16)
        for key in reads:
            self.R(key).r[sk] = c
        for key in writes:
            r = self.R(key)
            if sk in r.w and len(r.w) == 1:
                r.w[sk] = c
            else:
                r.w = {sk: c}
            r.r = {}

    def _dsem(self, semkey, writes):
        sk = semkey if semkey is not None else ("d", writes[0])
        if sk not in self.sems:
            self.sems[sk] = self.es.enter_context(self.nc.semaphore("sd%d" % self.nd))
            self.nd += 1
            self.cnt[sk] = 0
        return sk

    def dma(self, q, out, in_, reads=(), writes=(), semkey=None, **kw):
        sk = self._dsem(semkey, writes)
        self._deps(q, reads, writes, skip=sk)
        ins = self.eng[q].dma_start(out=out, in_=in_, **kw)
        self._dma_post(ins, sk, reads, writes)
        return ins

    def idma(self, out, out_off, in_, in_off, reads=(), writes=(), semkey=None, **kw):
        sk = self._dsem(semkey, writes)
        self._deps("pool", reads, writes, skip=sk)
        ins = self.nc.gpsimd.indirect_dma_start(out, out_off, in_, in_off, **kw)
        self._dma_post(ins, sk, reads, writes)
        return ins

    def finish(self, out_keys, e="sp"):
        for key in out_keys:
            for sk, v in self.R(key).w.items():
                self._wait(e, sk, v)
        for o in self.eng:
            if o != e:
                self._wait(e, o, self.cnt[o])
        self.es.close()
        return self.nc


class CastLoader:
    def __init__(self, P, nelem, nbuf=4, engines=("pool", "dve", "pool")):
        self.P = P
        self.n = nelem
        self.stg = [P.sb("cstg%d" % i, [128, nelem], F32) for i in range(nbuf)]
        self.i = 0
        self.engines = engines
        self.fifo = []

    def load(self, dst_ap, dst_key, src_ap, shape):
        P, nc = self.P, self.P.nc
        k = self.i % len(self.stg)
        e = self.engines[self.i % len(self.engines)]
        self.i += 1
        a, b = shape
        sv = self.stg[k].rearrange("p (a b) -> p a b", a=a)
        P.dma("sp", sv, src_ap, writes=[("cstg", k)])
        if e == "pool":
            P.op("pool", lambda: nc.gpsimd.tensor_copy(out=dst_ap, in_=sv), reads=[("cstg", k)], writes=[dst_key])
        else:
            P.op("dve", lambda: nc.vector.tensor_copy(out=dst_ap, in_=sv), reads=[("cstg", k)], writes=[dst_key])

    def enqueue(self, *args):
        self.fifo.append(args)

    def pump(self, n=1):
        for _ in range(n):
            if not self.fifo:
                return
            self.load(*self.fifo.pop(0))

    def flush(self):
        self.pump(len(self.fifo))


def make_ident(P, dt=BF16, name="ident"):
    nc = P.nc
    idf = P.sb(name + "f", [128, 128], F32)
    P.op("pool", lambda: nc.gpsimd.memset(idf[:], 1.0), writes=[name + "f"])
    P.op("pool", lambda: nc.gpsimd.affine_select(out=idf[:], in_=idf[:], pattern=[[-1, 128]],
                                                 compare_op=ALU.is_equal, fill=0.0, base=0,
                                                 channel_multiplier=1),
         reads=[name + "f"], writes=[name + "f"])
    if dt == F32:
        return idf, name + "f"
    idb = P.sb(name, [128, 128], dt)
    P.op("dve", lambda: nc.vector.tensor_copy(out=idb[:], in_=idf[:]), reads=[name + "f"], writes=[name])
    return idb, name


def bc128(v):
    v = np.asarray(v, dtype=np.float32).reshape(1, -1)
    return np.ascontiguousarray(np.broadcast_to(v, (128, v.shape[1])))


MC = 6 * D // NCORES


def build_mod():
    P = Prog()
    nc = P.nc
    c1 = P.din("c1", [128, 16], F32)
    c2 = P.din("c2", [128, 16], F32)
    wm = P.din("wm", [2, D, MC], F32)
    bm = P.din("bm", [2, 2, MC], F32)
    mo = P.dout("mo", [2, 2, MC], F32)
    c1s = P.sb("c1s", [128, 16], F32)
    c2s = P.sb("c2s", [128, 16], F32)
    cc = P.sb("cc", [128, 16, 2], F32)
    bs = P.sb("bs", [2, 2, MC], F32)
    ms = P.sb("ms", [2, 2, MC], F32)
    HC = MC // 2
    wb = [P.sb("wmb%d" % i, [128, 16, HC], F32) for i in range(2)]
    pz = [P.ps("pz%d" % i, [2, 384], F32) for i in range(2)]
    P.dma("sp", c1s[:], c1[:, :], writes=["c1s"])
    P.dma("sp", c2s[:], c2[:, :], writes=["c2s"])
    P.dma("sp", bs[:], bm.rearrange("l s n -> s l n"), writes=["bs"])
    P.op("act", lambda: nc.scalar.activation(out=cc[:, :, 0], in_=c1s[:], func=AF.Silu), reads=["c1s"], writes=["cc0"])
    P.op("act", lambda: nc.scalar.activation(out=cc[:, :, 1], in_=c2s[:], func=AF.Silu), reads=["c2s"], writes=["cc1"])
    ci = 0
    for l in range(2):
        wv = wm[l].rearrange("(p kc) n -> p kc n", kc=16)
        for hf in range(2):
            b = ci % 2
            q = "sp" if ci % 2 == 0 else "act"
            for kq in range(4):
                P.dma(q, wb[b][:, kq * 4:(kq + 1) * 4, :], wv[:, kq * 4:(kq + 1) * 4, hf * HC:(hf + 1) * HC], writes=[("wmb", b)])
            for nb in range(HC // 384):
                pb = (ci * 2 + nb) % 2
                for kc in range(16):
                    P.op("pe", lambda kc=kc: nc.tensor.matmul(pz[pb][:], lhsT=cc[:, kc, :], rhs=wb[b][:, kc, nb * 384:(nb + 1) * 384],
                                                               start=(kc == 0), stop=(kc == 15)),
                         reads=["cc0", "cc1", ("wmb", b)], writes=[("pz", pb)])
                n0 = hf * HC + nb * 384
                P.op("dve", lambda: nc.vector.tensor_tensor(out=ms[:, l, n0:n0 + 384], in0=pz[pb][:], in1=bs[:, l, n0:n0 + 384], op=ALU.add),
                     reads=[("pz", pb), "bs"], writes=["ms"])
            ci += 1
    P.dma("sp", mo.rearrange("l s n -> s l n"), ms[:], reads=["ms"], writes=["mo"])
    return P.finish(["mo"])


def run_mod(inp):
    nc = build_mod()
    c1 = np.ascontiguousarray(inp["c"].reshape(128, 16))
    c2 = np.ascontiguousarray(inp["c_ctx"].reshape(128, 16))
    maps = []
    for i in range(NCORES):
        wm = np.ascontiguousarray(inp["w_mod"][:, :, i * MC:(i + 1) * MC])
        bm = np.ascontiguousarray(np.broadcast_to(inp["b_mod"][:, None, i * MC:(i + 1) * MC], (2, 2, MC)))
        maps.append({"c1": c1, "c2": c2, "wm": wm, "bm": bm})
    res = run_bass_kernel_spmd(nc, maps, core_ids=list(range(NCORES)))
    m = np.concatenate([r["mo"] for r in res.results], axis=2)
    return m


NTK = TPC + 32
INC = 5120


def rms_modulate_tile(P, nc, xs, xkey, rows, ss, sskey, epsb, gam, sh, hb, hbkey):
    P.op("act", lambda: nc.scalar.activation(out=hb[:rows, :], in_=xs[:rows, :], func=AF.Square, accum_out=ss[:rows, 0:1]),
         reads=[xkey], writes=[hbkey, sskey])
    P.op("act", lambda: nc.scalar.activation(out=ss[:rows, 1:2], in_=ss[:rows, 0:1], func=AF.Sqrt, scale=1.0 / D, bias=epsb[:rows, 0:1]),
         reads=[sskey, "epsb"], writes=[sskey])
    P.op("dve", lambda: nc.vector.reciprocal(out=ss[:rows, 2:3], in_=ss[:rows, 1:2]), reads=[sskey], writes=[sskey])
    P.op("dve", lambda: nc.vector.scalar_tensor_tensor(out=xs[:rows, :], in0=xs[:rows, :], scalar=ss[:rows, 2:3], in1=gam[:rows, :],
                                                       op0=ALU.mult, op1=ALU.mult),
         reads=[xkey, sskey, "gam"], writes=[xkey])
    P.op("pool", lambda: nc.gpsimd.tensor_tensor(out=hb[:rows, :], in0=xs[:rows, :], in1=sh[:rows, :], op=ALU.add),
         reads=[xkey, "sh"], writes=[hbkey])


def build_A():
    P = Prog()
    nc = P.nc
    x = P.din("x", [TPC, D], F32)
    cx = P.din("cx", [32, D], F32)
    gmix = P.din("gmix", [128, D], F32)
    cm = P.din("cm", [128, D], F32)
    sm = P.din("sm", [128, D], F32)
    cmc = P.din("cmc", [128, D], F32)
    smc = P.din("smc", [128, D], F32)
    w_in = P.din("w_in", [D, INC], F32)
    cosd = P.din("cosE", [TPC, 256], F32)
    sind = P.din("sinE", [TPC, 256], F32)
    lngd = P.din("lng", [128, 1024], F32)
    lnbd = P.din("lnb", [128, 1024], F32)
    wsTd = P.din("wsT", [8, 128, 128], F32)
    bsbd = P.din("bsb", [128, 1024], F32)
    qT = P.dout("qT", [8, 128, TPC], BF16)
    kT = P.dout("kT", [8, 128, NTK], BF16)
    vo = P.dout("v", [NTK, 1024], BF16)
    gTo = P.dout("gT", [8, 128, TPC], BF16)

    ident, idk = make_ident(P)
    xs = [P.sb("xs%d" % i, [128, D], F32) for i in range(2)]
    gam = P.sb("gam", [128, D], F32)
    sh = P.sb("sh", [128, D], F32)
    hb = [P.sb("hb%d" % i, [128, D], BF16) for i in range(2)]
    hT = P.sb("hT", [128, 16, NTK], BF16)
    wblk = [P.sb("wblk%d" % i, [128, 16, 512], BF16) for i in range(2)]
    big = P.sb("big", [128, 4096], F32)
    cosE = big[:, 0:2048].rearrange("p (t f) -> p t f", t=8)
    sinE = big[:, 2048:4096].rearrange("p (t f) -> p t f", t=8)
    uT = big.bitcast(BF16).rearrange("p (g t) -> p g t", g=8)
    lng = P.sb("lngs", [128, 1024], F32)
    lnb = P.sb("lnbs", [128, 1024], F32)
    wsTf = P.sb("wsTf", [128, 8, 128], F32)
    wsTb = P.sb("wsTb", [128, 8, 128], BF16)
    bsb = P.sb("bsbs", [128, 8, 128], F32)
    stg = P.sb("stg", [128, 4, NTK], BF16)
    zf = [P.sb("zf%d" % i, [128, 512], F32) for i in range(2)]
    zsq = P.sb("zsq", [128, 512], F32)
    rt = [P.sb("rt%d" % i, [128, 256], F32) for i in range(8)]
    qr = [P.sb("qr%d" % i, [128, 512], BF16) for i in range(2)]
    vs = [P.sb("vs%d" % i, [128, 512], BF16) for i in range(2)]
    vlf = P.sb("vlf", [128, 512], F32)
    vlb = [P.sb("vlb%d" % i, [128, 512], BF16) for i in range(2)]
    tt = [P.sb("tt%d" % i, [128, 128], F32) for i in range(2)]
    ss = [P.sb("ss%d" % i, [128, 4], F32) for i in range(2)]
    st = [P.sb("st%d" % i, [128, 24], F32) for i in range(2)]
    epsb = P.sb("epsb", [128, 1], F32)
    pT = P.ps("pT", [128, D], BF16)
    pz = [P.ps("pz%d" % i, [128, 512], F32) for i in range(2)]
    pq = [P.ps("pq%d" % i, [128, 512], BF16) for i in range(2)]
    pm = [P.ps("pm%d" % i, [128, 128], F32) for i in range(2)]

    P.op("pool", lambda: nc.gpsimd.memset(epsb[:], EPS), writes=["epsb"])

    def load_mod(cmd, smd):
        P.dma("sp", gam[:], cmd[:, :], writes=["gam"])
        P.dma("act", xs[1][:], gmix[:, :], writes=[("xs", 1)])
        P.dma("sp", sh[:], smd[:, :], writes=["sh"])
        P.op("dve", lambda: nc.vector.scalar_tensor_tensor(out=gam[:], in0=gam[:], scalar=1.0, in1=xs[1][:], op0=ALU.add, op1=ALU.mult),
             reads=["gam", ("xs", 1)], writes=["gam"])

    def front_tile(src_ap, rows, tok0, b):
        P.dma("sp", xs[b][:rows, :], src_ap, writes=[("xs", b)])
        rms_modulate_tile(P, nc, xs[b], ("xs", b), rows, ss[b], ("ss", b), epsb, gam, sh, hb[b], ("hb", b))
        for kc in range(16):
            P.op("pe", lambda kc=kc: nc.tensor.transpose(pT[:, kc * 128:kc * 128 + rows], hb[b][:rows, kc * 128:(kc + 1) * 128], ident[:rows, :rows]),
                 reads=[("hb", b), idk], writes=["pT"])
        P.op("act", lambda: nc.scalar.copy(out=hT[:, :, tok0:tok0 + rows],
                                           in_=pT.rearrange("p (k t) -> p k t", k=16)[:, :, 0:rows]),
             reads=["pT"], writes=["hT"])

    load_mod(cmc, smc)
    front_tile(cx[:, :], 32, TPC, 0)
    load_mod(cm, sm)
    for ti in range(NT):
        front_tile(x[ti * 128:(ti + 1) * 128, :], 128, ti * 128, ti % 2)

    P.dma("sp", big[:, 0:2048].rearrange("p (t f) -> p t f", t=8), cosd.rearrange("(t p) f -> p t f", p=128), writes=["big"])
    P.dma("sp", big[:, 2048:4096].rearrange("p (t f) -> p t f", t=8), sind.rearrange("(t p) f -> p t f", p=128), writes=["big"])
    P.dma("act", lng[:], lngd[:, :], writes=["lng"])
    P.dma("act", lnb[:], lnbd[:, :], writes=["lnb"])
    P.dma("act", wsTf[:], wsTd.rearrange("g q p -> q g p"), writes=["wsTf"])
    P.dma("act", bsb[:], bsbd.rearrange("q (g p) -> q g p", g=8), writes=["bsb"])
    P.op("dve", lambda: nc.vector.tensor_copy(out=wsTb[:], in_=wsTf[:]), reads=["wsTf"], writes=["wsTb"])

    wv = w_in.rearrange("(kc p) n -> p kc n", p=128)
    CL = CastLoader(P, 1024, engines=("pool", "dve"))
    zc = [0]

    def proj_tok(b, tok0, rows):
        j = zc[0] % 2
        zc[0] += 1
        for kc in range(16):
            P.op("pe", lambda kc=kc: nc.tensor.matmul(pz[j][:rows, :], lhsT=hT[:, kc, tok0:tok0 + rows], rhs=wblk[b][:, kc, :],
                                                       start=(kc == 0), stop=(kc == 15)),
                 reads=["hT", ("wblk", b)], writes=[("pz", j)])
        CL.pump(1)
        return j

    def queue_block(cb):
        b = cb % 2
        for kq in range(8):
            CL.enqueue(wblk[b][:, kq * 2:(kq + 1) * 2, :], ("wblk", b), wv[:, kq * 2:(kq + 1) * 2, cb * 512:(cb + 1) * 512], (2, 512))

    queue_block(0)
    CL.flush()
    for cb in range(10):
        b = cb % 2
        CL.flush()
        if cb + 1 < 10:
            queue_block(cb + 1)
        if cb < 4:
            isk = cb >= 2
            for ti in range(NT):
                j = proj_tok(b, ti * 128, 128)
                zv = pz[j].rearrange("p (a h x f) -> p a h x f", a=8, h=2, x=2, f=16)
                x1 = zv[:, :, :, 0, :]
                x2 = zv[:, :, :, 1, :]
                cv = cosE[:, ti, :].rearrange("p (a h f) -> p a h f", a=8, h=2)
                sv = sinE[:, ti, :].rearrange("p (a h f) -> p a h f", a=8, h=2)
                ro = (ti % 2) * 4
                r4 = [t.rearrange("p (a h f) -> p a h f", a=8, h=2) for t in rt[ro:ro + 4]]
                rk_ = ["rt%d" % (ro + i) for i in range(4)]
                qv = qr[j].rearrange("p (a h x f) -> p a h x f", a=8, h=2, x=2, f=16)
                zk = ("pz", j)
                P.op("dve", lambda: nc.vector.tensor_tensor(out=r4[0], in0=x1, in1=cv, op=ALU.mult), reads=[zk, "big"], writes=[rk_[0]])
                P.op("dve", lambda: nc.vector.tensor_tensor(out=r4[1], in0=x2, in1=sv, op=ALU.mult), reads=[zk, "big"], writes=[rk_[1]])
                P.op("dve", lambda: nc.vector.tensor_tensor(out=r4[2], in0=x2, in1=cv, op=ALU.mult), reads=[zk, "big"], writes=[rk_[2]])
                P.op("dve", lambda: nc.vector.tensor_tensor(out=r4[3], in0=x1, in1=sv, op=ALU.mult), reads=[zk, "big"], writes=[rk_[3]])
                P.op("pool", lambda: nc.gpsimd.tensor_tensor(out=qv[:, :, :, 0, :], in0=r4[0], in1=r4[1], op=ALU.subtract),
                     reads=[rk_[0], rk_[1]], writes=[("qr", j)])
                P.op("pool", lambda: nc.gpsimd.tensor_tensor(out=qv[:, :, :, 1, :], in0=r4[2], in1=r4[3], op=ALU.add),
                     reads=[rk_[2], rk_[3]], writes=[("qr", j)])
                for s in range(4):
                    P.op("pe", lambda s=s: nc.tensor.transpose(pq[j][:, s * 128:(s + 1) * 128], qr[j][:, s * 128:(s + 1) * 128], ident[:]),
                         reads=[("qr", j), idk], writes=[("pq", j)])
                P.op("act", lambda: nc.scalar.copy(out=stg[:, :, ti * 128:(ti + 1) * 128], in_=pq[j].rearrange("p (s t) -> p s t", s=4)),
                     reads=[("pq", j)], writes=["stg"])
            if isk:
                j = proj_tok(b, TPC, 32)
                P.op("act", lambda: nc.scalar.copy(out=qr[j][:32, :], in_=pz[j][:32, :]), reads=[("pz", j)], writes=[("qr", j)])
                for s in range(4):
                    P.op("pe", lambda s=s: nc.tensor.transpose(pq[j][:, s * 128:s * 128 + 32], qr[j][:32, s * 128:(s + 1) * 128], ident[:32, :32]),
                         reads=[("qr", j), idk], writes=[("pq", j)])
                P.op("act", lambda: nc.scalar.copy(out=stg[:, :, TPC:TPC + 32], in_=pq[j].rearrange("p (s t) -> p s t", s=4)[:, :, 0:32]),
                     reads=[("pq", j)], writes=["stg"])
            h0 = (cb % 2) * 4
            if isk:
                P.dma("act", kT[h0:h0 + 4].rearrange("h d t -> d h t"), stg[:], reads=["stg"], writes=["kT"])
            else:
                P.dma("act", qT[h0:h0 + 4].rearrange("h d t -> d h t"), stg[:, :, 0:TPC], reads=["stg"], writes=["qT"])
        elif cb < 6:
            for ti in range(NT + 1):
                rows = 128 if ti < NT else 32
                tok0 = ti * 128
                j = proj_tok(b, tok0, rows)
                P.op("act", lambda: nc.scalar.copy(out=vs[j][:rows, :], in_=pz[j][:rows, :]), reads=[("pz", j)], writes=[("vs", j)])
                P.dma("act", vo[tok0:tok0 + rows, (cb - 4) * 512:(cb - 3) * 512], vs[j][:rows, :], reads=[("vs", j)], writes=["vo"])
        elif cb < 8:
            for sub in range(4):
                G = (cb - 6) * 4 + sub
                for tg in range(2):
                    j = zc[0] % 2
                    zc[0] += 1
                    for kc in range(16):
                        P.op("pe", lambda kc=kc: nc.tensor.matmul(pz[j][:], lhsT=wblk[b][:, kc, sub * 128:(sub + 1) * 128],
                                                                   rhs=hT[:, kc, tg * 512:(tg + 1) * 512],
                                                                   start=(kc == 0), stop=(kc == 15)),
                             reads=["hT", ("wblk", b)], writes=[("pz", j)])
                    P.op("act", lambda: nc.scalar.activation(out=uT[:, G, tg * 512:(tg + 1) * 512], in_=pz[j][:], func=AF.Gelu_apprx_tanh),
                         reads=[("pz", j)], writes=["big"])
                    CL.pump(1)
        else:
            for ti in range(NT):
                tok0 = ti * 128
                j = proj_tok(b, tok0, 128)
                z = zf[j]
                zk = ("zf", j)
                s_ = st[j]
                sk = ("st", j)
                P.op("act", lambda: nc.scalar.activation(out=z[:], in_=pz[j][:], func=AF.Gelu_apprx_tanh), reads=[("pz", j)], writes=[zk])
                P.op("dve", lambda: nc.vector.reduce_sum(out=s_[:, 0:4], in_=z.rearrange("p (g d) -> p g d", g=4), axis=AX.X),
                     reads=[zk], writes=[sk])
                P.op("act", lambda: nc.scalar.activation(out=zsq[:], in_=z[:], func=AF.Square), reads=[zk], writes=["zsq"])
                P.op("dve", lambda: nc.vector.reduce_sum(out=s_[:, 4:8], in_=zsq.rearrange("p (g d) -> p g d", g=4), axis=AX.X),
                     reads=["zsq"], writes=[sk])
                P.op("dve", lambda: nc.vector.tensor_scalar(out=s_[:, 8:12], in0=s_[:, 0:4], scalar1=1.0 / 128, scalar2=None, op0=ALU.mult),
                     reads=[sk], writes=[sk])
                P.op("dve", lambda: nc.vector.tensor_tensor(out=s_[:, 12:16], in0=s_[:, 8:12], in1=s_[:, 8:12], op=ALU.mult),
                     reads=[sk], writes=[sk])
                P.op("dve", lambda: nc.vector.scalar_tensor_tensor(out=s_[:, 16:20], in0=s_[:, 4:8], scalar=1.0 / 128, in1=s_[:, 12:16],
                                                                   op0=ALU.mult, op1=ALU.subtract),
                     reads=[sk], writes=[sk])
                P.op("act", lambda: nc.scalar.activation(out=s_[:, 20:24], in_=s_[:, 16:20], func=AF.Sqrt, bias=epsb[:, 0:1]),
                     reads=[sk, "epsb"], writes=[sk])
                P.op("dve", lambda: nc.vector.reciprocal(out=s_[:, 20:24], in_=s_[:, 20:24]), reads=[sk], writes=[sk])
                for g in range(4):
                    P.op("dve", lambda g=g: nc.vector.tensor_scalar(out=vlf[:, g * 128:(g + 1) * 128], in0=z[:, g * 128:(g + 1) * 128],
                                                                    scalar1=s_[:, 8 + g:9 + g], scalar2=s_[:, 20 + g:21 + g],
                                                                    op0=ALU.subtract, op1=ALU.mult),
                         reads=[zk, sk], writes=["vlf"])
                c0 = (cb - 8) * 512
                P.op("pool", lambda: nc.gpsimd.tensor_tensor(out=vlf[:], in0=vlf[:], in1=lng[:, c0:c0 + 512], op=ALU.mult),
                     reads=["vlf", "lng"], writes=["vlf"])
                P.op("pool", lambda: nc.gpsimd.tensor_tensor(out=vlb[j][:], in0=vlf[:], in1=lnb[:, c0:c0 + 512], op=ALU.add),
                     reads=["vlf", "lnb"], writes=[("vlb", j)])
                for g in range(4):
                    G = (cb - 8) * 4 + g
                    m = (ti * 4 + g) % 2
                    P.op("pe", lambda g=g, G=G, m=m: nc.tensor.matmul(pm[m][:], lhsT=vlb[j][:, g * 128:(g + 1) * 128], rhs=wsTb[:, G, :],
                                                                      start=True, stop=True),
                         reads=[("vlb", j), "wsTb"], writes=[("pm", m)])
                    P.op("dve", lambda G=G, m=m: nc.vector.tensor_tensor(out=tt[m][:], in0=pm[m][:], in1=bsb[:, G, :], op=ALU.add),
                         reads=[("pm", m), "bsb"], writes=[("tt", m)])
                    P.op("dve", lambda g=g, G=G, m=m: nc.vector.tensor_tensor(out=stg[:, g, tok0:tok0 + 128], in0=tt[m][:],
                                                                              in1=uT[:, G, tok0:tok0 + 128], op=ALU.mult),
                         reads=[("tt", m), "big"], writes=["stg"])
            g0 = (cb - 8) * 4
            P.dma("pool", gTo[g0:g0 + 4].rearrange("g d t -> d g t"), stg[:, :, 0:TPC], reads=["stg"], writes=["gTo"])
    return P.finish(["qT", "kT", "vo", "gTo"])


def rope_tables():
    t = np.arange(SEQ)
    row = (t // 64).astype(np.float32)
    col = (t % 64).astype(np.float32)
    inv = (10000.0 ** (-np.arange(0, 32, 2, dtype=np.float32) / 32)).astype(np.float32)
    ang = np.stack([row[:, None] * inv[None, :], col[:, None] * inv[None, :]], axis=1)
    cosE = np.tile(np.cos(ang).astype(np.float32).reshape(SEQ, 1, 32), (1, 8, 1)).reshape(SEQ, 256)
    sinE = np.tile(np.sin(ang).astype(np.float32).reshape(SEQ, 1, 32), (1, 8, 1)).reshape(SEQ, 256)
    return np.ascontiguousarray(cosE), np.ascontiguousarray(sinE)


def run_A(inp, m):
    nc = build_A()
    cosE, sinE = rope_tables()
    m0 = m[0]
    sm_, cm_ = m0[0, 0:D], m0[0, D:2 * D]
    smc_, cmc_ = m0[1, 0:D], m0[1, D:2 * D]
    common = {
        "gmix": bc128(inp["g_norm_mix"][0]), "cm": bc128(cm_), "sm": bc128(sm_), "cmc": bc128(cmc_), "smc": bc128(smc_),
        "w_in": np.ascontiguousarray(inp["w_in"][0]),
        "lng": bc128(inp["sgu_ln_g"][0]), "lnb": bc128(inp["sgu_ln_b"][0]),
        "wsT": np.ascontiguousarray(np.transpose(inp["w_spatial"][0], (0, 2, 1))),
        "bsb": bc128(inp["b_spatial"][0].reshape(-1)),
    }
    maps = []
    for i in range(NCORES):
        d = dict(common)
        d["x"] = np.ascontiguousarray(inp["x"][0, i * TPC:(i + 1) * TPC])
        d["cx"] = np.ascontiguousarray(inp["ctx"][0, i * 32:(i + 1) * 32])
        d["cosE"] = cosE[i * TPC:(i + 1) * TPC]
        d["sinE"] = sinE[i * TPC:(i + 1) * TPC]
        maps.append(d)
    res = run_bass_kernel_spmd(nc, maps, core_ids=list(range(NCORES)))
    return [r for r in res.results]


NKEY = SEQ + 256
NKT = NKEY // 128
LAM_INIT0 = 0.8 - 0.6 * math.exp(-0.3 * 0)


def build_B():
    P = Prog()
    nc = P.nc
    qTd = P.din("qT", [128, SEQ], BF16)
    kTd = P.din("kT", [128, NKEY], BF16)
    vd = P.din("v", [NKEY, 128], BF16)
    lamd = P.din("lamv", [128, 4, 64], F32)
    gsd = P.din("gsub", [128, 1], F32)
    aTo = P.dout("aT", [128, SEQ], BF16)

    qz = [P.sb("qz%d" % i, [128, SEQ], BF16) for i in range(2)]
    kT = P.sb("kTs", [128, NKEY], BF16)
    vs = P.sb("vsb", [128, NKT, 128], BF16)
    lamv = P.sb("lamvs", [128, 4, 64], F32)
    lt = P.sb("lt", [128, 2, 64], F32)
    ls = P.sb("ls", [128, 8], F32)
    gs = P.sb("gs", [128, 1], F32)
    epsb = P.sb("epsb", [128, 1], F32)
    onesb = P.sb("onesb", [128, 128], BF16)
    onesf = P.sb("onesf", [128, 128], F32)
    pt = [P.sb("pt%d" % i, [128, 512], BF16) for i in range(3)]
    rz = [P.sb("rz%d" % i, [128, 512], F32) for i in range(2)]
    o = P.sb("o", [128, 512], F32)
    t1 = P.sb("t1", [128, 512], F32)
    osq = P.sb("osq", [128, 512], F32)
    rstd = P.sb("rstd", [128, 512], F32)
    ab = [P.sb("ab%d" % i, [128, 512], BF16) for i in range(2)]
    ps = [P.ps("ps%d" % i, [128, 512], F32) for i in range(3)]
    po = [P.ps("po%d" % i, [128, 512], F32) for i in range(2)]
    pzz = [P.ps("pzz%d" % i, [128, 512], F32) for i in range(2)]
    pr = P.ps("pr", [128, 512], F32)
    zacc = [P.sb("zacc%d" % i, [128, 512], F32) for i in range(2)]
    zs = [P.sb("zs%d" % i, [128, 512], F32) for i in range(2)]
    os_ = [P.sb("os%d" % i, [128, 512], F32) for i in range(2)]
    ones1 = P.sb("ones1", [128, 128], F32)

    for i in range(4):
        q = "sp" if i % 2 == 0 else "act"
        P.dma(q, qz[0][0:64, i * 2048:(i + 1) * 2048], qTd[0:64, i * 2048:(i + 1) * 2048], writes=["qT"])
        P.dma(q, qz[1][64:128, i * 2048:(i + 1) * 2048], qTd[64:128, i * 2048:(i + 1) * 2048], writes=["qT"])
        P.dma(q, kT[:, i * 2112:(i + 1) * 2112], kTd[:, i * 2112:(i + 1) * 2112], writes=["kT"])
    P.dma("sp", vs[:, 0:33, :], vd[0:33 * 128, :].rearrange("(t p) e -> p t e", p=128), writes=["vs"])
    P.dma("act", vs[:, 33:66, :], vd[33 * 128:, :].rearrange("(t p) e -> p t e", p=128), writes=["vs"])
    P.dma("sp", lamv[:], lamd[:, :, :], writes=["lamv"])
    P.dma("sp", gs[:], gsd[:, :], writes=["gs"])
    P.op("pool", lambda: nc.gpsimd.memset(epsb[:], EPS), writes=["epsb"])
    P.op("pool", lambda: nc.gpsimd.memset(onesb[:], 1.0), writes=["onesb"])
    P.op("pool", lambda: nc.gpsimd.memset(onesf[:], 1.0 / 128), writes=["onesf"])
    P.op("pool", lambda: nc.gpsimd.memset(ones1[:], 1.0), writes=["ones1"])
    P.op("pool", lambda: nc.gpsimd.memset(qz[0][64:128, :], 0.0), writes=["qz0pad"])
    P.op("dve", lambda: nc.vector.memset(qz[1][0:64, :], 0.0), writes=["qz1pad"])
    P.op("dve", lambda: nc.vector.tensor_tensor(out=lt[:, 0, :], in0=lamv[:, 0, :], in1=lamv[:, 1, :], op=ALU.mult), reads=["lamv"], writes=["lt"])
    P.op("dve", lambda: nc.vector.tensor_tensor(out=lt[:, 1, :], in0=lamv[:, 2, :], in1=lamv[:, 3, :], op=ALU.mult), reads=["lamv", "lt"], writes=["lt"])
    P.op("dve", lambda: nc.vector.reduce_sum(out=ls[:, 0:2], in_=lt[:], axis=AX.X), reads=["lt"], writes=["ls"])
    P.op("act", lambda: nc.scalar.activation(out=ls[:, 2:4], in_=ls[:, 0:2], func=AF.Exp), reads=["ls"], writes=["ls"])
    P.op("dve", lambda: nc.vector.tensor_tensor(out=ls[:, 4:5], in0=ls[:, 3:4], in1=ls[:, 2:3], op=ALU.subtract), reads=["ls"], writes=["ls"])
    P.op("dve", lambda: nc.vector.tensor_scalar(out=ls[:, 4:5], in0=ls[:, 4:5], scalar1=-LAM_INIT0, scalar2=None, op0=ALU.add), reads=["ls"], writes=["ls"])
    P.op("dve", lambda: nc.vector.tensor_scalar(out=gs[:], in0=gs[:], scalar1=1.0 - LAM_INIT0, scalar2=None, op0=ALU.mult), reads=["gs"], writes=["gs"])

    def epilogue(qb):
        q0 = qb * 512
        for c in range(2):
            P.op("dve", lambda c=c: nc.vector.reciprocal(out=rz[c][:], in_=zs[c][:]), reads=[("zs", c)], writes=[("rz", c)])
        P.op("dve", lambda: nc.vector.tensor_tensor(out=o[:], in0=os_[0][:], in1=rz[0][:], op=ALU.mult), reads=[("os", 0), ("rz", 0)], writes=["o"])
        P.op("dve", lambda: nc.vector.tensor_tensor(out=t1[:], in0=os_[1][:], in1=rz[1][:], op=ALU.mult), reads=[("os", 1), ("rz", 1)], writes=["t1"])
        P.op("dve", lambda: nc.vector.scalar_tensor_tensor(out=o[:], in0=t1[:], scalar=ls[:, 4:5], in1=o[:], op0=ALU.mult, op1=ALU.add),
             reads=["t1", "ls", "o"], writes=["o"])
        P.op("dve", lambda: nc.vector.tensor_tensor(out=osq[:], in0=o[:], in1=o[:], op=ALU.mult), reads=["o"], writes=["osq"])
        P.op("pe", lambda: nc.tensor.matmul(pr[:], lhsT=onesf[:], rhs=osq[:], start=True, stop=True), reads=["onesf", "osq"], writes=["pr"])
        P.op("act", lambda: nc.scalar.activation(out=rstd[:], in_=pr[:], func=AF.Sqrt, bias=epsb[:, 0:1]), reads=["pr", "epsb"], writes=["rstd"])
        P.op("dve", lambda: nc.vector.reciprocal(out=rstd[:], in_=rstd[:]), reads=["rstd"], writes=["rstd"])
        a = ab[qb % 2]
        P.op("dve", lambda: nc.vector.scalar_tensor_tensor(out=a[:], in0=o[:], scalar=gs[:, 0:1], in1=rstd[:], op0=ALU.mult, op1=ALU.mult),
             reads=["o", "gs", "rstd"], writes=[("ab", qb % 2)])
        P.dma("sp", aTo[:, q0:q0 + 512], a[:], reads=[("ab", qb % 2)], writes=["aTo"])

    it = 0
    NQB = SEQ // 512
    for qb in range(NQB):
        q0 = qb * 512
        steps = [(kt, c) for kt in range(NKT) for c in range(2)]

        def qk(i, kt, c):
            j = i % 3
            P.op("pe", lambda: nc.tensor.matmul(ps[j][:], lhsT=kT[:, kt * 128:(kt + 1) * 128],
                                                rhs=qz[c][:, q0:q0 + 512], start=True, stop=True),
                 reads=["qT", "kT", "qz0pad", "qz1pad"], writes=[("ps", j)])

        for pre in range(2):
            qk(it + pre, *steps[pre])
        for si, (kt, c) in enumerate(steps):
            j = it % 3
            P.op("act", lambda: nc.scalar.activation(out=pt[j][:], in_=ps[j][:], func=AF.Exp, scale=0.125),
                 reads=[("ps", j)], writes=[("pt", j)])
            if si + 2 < len(steps):
                qk(it + 2, *steps[si + 2])
            P.op("pe", lambda: nc.tensor.matmul(po[c][:], lhsT=vs[:, kt, :], rhs=pt[j][:], start=(kt == 0), stop=(kt == NKT - 1)),
                 reads=["vs", ("pt", j)], writes=[("po", c)])
            if kt % 2 == 0:
                P.op("pe", lambda: nc.tensor.matmul(pzz[c][:], lhsT=onesb[:], rhs=pt[j][:], start=(kt == 0), stop=False),
                     reads=["onesb", ("pt", j)], writes=[("pzz", c)])
            elif kt == 1:
                P.op("dve", lambda: nc.vector.tensor_copy(out=zacc[c][:], in_=pt[j][:]), reads=[("pt", j)], writes=[("zacc", c)])
            else:
                P.op("dve", lambda: nc.vector.tensor_tensor(out=zacc[c][:], in0=zacc[c][:], in1=pt[j][:], op=ALU.add),
                     reads=[("pt", j), ("zacc", c)], writes=[("zacc", c)])
            it += 1
            if si == 12 and qb > 0:
                epilogue(qb - 1)
        for c in range(2):
            P.op("pe", lambda c=c: nc.tensor.matmul(pzz[c][:], lhsT=ones1[:], rhs=zacc[c][:], start=False, stop=True),
                 reads=["ones1", ("zacc", c)], writes=[("pzz", c)])
        for c in range(2):
            P.op("dve", lambda c=c: nc.vector.tensor_copy(out=zs[c][:], in_=pzz[c][:]), reads=[("pzz", c)], writes=[("zs", c)])
            P.op("dve", lambda c=c: nc.vector.tensor_copy(out=os_[c][:], in_=po[c][:]), reads=[("po", c)], writes=[("os", c)])
    epilogue(NQB - 1)
    return P.finish(["aTo"])


def run_B(inp, ra):
    nc = build_B()
    lamv = np.stack([inp["lam_q1"][0], inp["lam_k1"][0], inp["lam_q2"][0], inp["lam_k2"][0]], 0)
    lamv = np.ascontiguousarray(np.broadcast_to(lamv[None], (128, 4, 64))).astype(np.float32)
    gsub = np.ascontiguousarray(inp["g_subln"][0].reshape(128, 1))
    maps = []
    for h in range(NCORES):
        qT = np.concatenate([r["qT"][h] for r in ra], axis=1)
        kT = np.concatenate([r["kT"][h][:, :TPC] for r in ra] + [r["kT"][h][:, TPC:] for r in ra], axis=1)
        v = np.concatenate([r["v"][:TPC, h * 128:(h + 1) * 128] for r in ra] + [r["v"][TPC:, h * 128:(h + 1) * 128] for r in ra], axis=0)
        maps.append({"qT": np.ascontiguousarray(qT), "kT": np.ascontiguousarray(kT), "v": np.ascontiguousarray(v),
                     "lamv": lamv, "gsub": gsub})
    res = run_bass_kernel_spmd(nc, maps, core_ids=list(range(NCORES)))
    return [r["aT"] for r in res.results]


def build_proj():
    P = Prog()
    nc = P.nc
    fTd = P.din("fT", [16, 128, TPC], BF16)
    Wd = P.din("W", [D, D], F32)
    xd = P.din("x", [TPC, D], F32)
    gmd = P.din("gm", [128, D], F32)
    gfd = P.din("gffn", [128, D], F32)
    cfd = P.din("cf", [128, D], F32)
    sfd = P.din("sf", [128, D], F32)
    wrd = P.din("wr", [D, 16], F32)
    x1o = P.dout("x1", [TPC, D], F32)
    hfo = P.dout("hf", [TPC, D], BF16)
    affo = P.dout("aff", [TPC, 16], F32)

    identf, idk = make_ident(P, F32)
    Wb = P.sb("Wb", [128, 16, D], BF16)
    fT = [P.sb("fTs%d" % i, [128, 16, 128], BF16) for i in range(2)]
    gm = P.sb("gms", [128, D], F32)
    gam = P.sb("gam", [128, D], F32)
    sh = P.sb("sh", [128, D], F32)
    xs = [P.sb("xs%d" % i, [128, D], F32) for i in range(2)]
    hf32s = [P.sb("hf32_%d" % i, [128, D], F32) for i in range(2)]
    hfb = [P.sb("hfb%d" % i, [128, D], BF16) for i in range(2)]
    hT32 = P.sb("hT32", [128, 16, 128], F32)
    tq = [P.sb("tq%d" % i, [128, 512], F32) for i in range(2)]
    wr = P.sb("wrs", [128, 16, 16], F32)
    ss = [P.sb("ss%d" % i, [128, 4], F32) for i in range(2)]
    sm = [P.sb("smx%d" % i, [128, 4], F32) for i in range(2)]
    ex = [P.sb("ex%d" % i, [128, 16], F32) for i in range(2)]
    epsb = P.sb("epsb", [128, 1], F32)
    pz = [P.ps("pz%d" % i, [128, 512], F32) for i in range(2)]
    pT = P.ps("pT", [128, D], F32)
    pl = P.ps("pl", [128, 16], F32)

    P.op("pool", lambda: nc.gpsimd.memset(epsb[:], EPS), writes=["epsb"])
    wv = Wd.rearrange("(kc p) n -> p kc n", p=128)
    CL = CastLoader(P, 1024, engines=("pool", "dve"))
    for hh in range(2):
        for kc in range(16):
            CL.load(Wb[:, kc:kc + 1, hh * 1024:(hh + 1) * 1024], ("Wb", hh), wv[:, kc:kc + 1, hh * 1024:(hh + 1) * 1024], (1, 1024))
    P.dma("sp", gm[:], gmd[:, :], writes=["gm"])
    P.dma("sp", gam[:], cfd[:, :], writes=["gam"])
    P.dma("act", xs[1][:], gfd[:, :], writes=[("xs", 1)])
    P.dma("act", sh[:], sfd[:, :], writes=["sh"])
    P.dma("act", wr[:], wrd.rearrange("(kc p) e -> p kc e", p=128), writes=["wr"])
    P.op("dve", lambda: nc.vector.scalar_tensor_tensor(out=gam[:], in0=gam[:], scalar=1.0, in1=xs[1][:], op0=ALU.add, op1=ALU.mult),
         reads=["gam", ("xs", 1)], writes=["gam"])

    pending = []

    def router(ti, b):
        tok0 = ti * 128
        hf32 = hf32s[b]
        hk = ("hf32", b)
        for kc in range(16):
            P.op("pe", lambda kc=kc: nc.tensor.transpose(pT[:, kc * 128:(kc + 1) * 128], hf32[:, kc * 128:(kc + 1) * 128], identf[:]),
                 reads=[hk, idk], writes=["pT"])
        P.op("dve", lambda: nc.vector.tensor_copy(out=hT32[:], in_=pT.rearrange("p (k t) -> p k t", k=16)), reads=["pT"], writes=["hT32"])
        for kc in range(16):
            P.op("pe", lambda kc=kc: nc.tensor.matmul(pl[:], lhsT=hT32[:, kc, :], rhs=wr[:, kc, :], start=(kc == 0), stop=(kc == 15)),
                 reads=["hT32", "wr"], writes=["pl"])
        m_ = sm[b]
        mk = ("sm", b)
        P.op("dve", lambda: nc.vector.reduce_max(out=m_[:, 0:1], in_=pl[:], axis=AX.X), reads=["pl"], writes=[mk])
        P.op("dve", lambda: nc.vector.tensor_scalar(out=m_[:, 1:2], in0=m_[:, 0:1], scalar1=-1.0, scalar2=None, op0=ALU.mult), reads=[mk], writes=[mk])
        P.op("act", lambda: nc.scalar.activation(out=ex[b][:], in_=pl[:], func=AF.Exp, bias=m_[:, 1:2], accum_out=m_[:, 2:3]),
             reads=["pl", mk], writes=[("ex", b), mk])
        P.op("dve", lambda: nc.vector.reciprocal(out=m_[:, 3:4], in_=m_[:, 2:3]), reads=[mk], writes=[mk])
        P.op("dve", lambda: nc.vector.tensor_scalar(out=ex[b][:], in0=ex[b][:], scalar1=m_[:, 3:4], scalar2=None, op0=ALU.mult),
             reads=[("ex", b), mk], writes=[("ex", b)])
        P.dma("pool", affo[tok0:tok0 + 128, :], ex[b][:], reads=[("ex", b)], writes=["affo"])

    zc = 0
    for ti in range(NT):
        b = ti % 2
        tok0 = ti * 128
        xk = ("xs", b)
        hf32 = hf32s[b]
        P.dma("sp", xs[b][:], xd[tok0:tok0 + 128, :], writes=[xk])
        P.dma("sp", fT[b][:], fTd[:, :, tok0:tok0 + 128].rearrange("k d t -> d k t"), writes=[("fT", b)])
        for nb in range(4):
            j = zc % 2
            zc += 1
            for kc in range(16):
                P.op("pe", lambda kc=kc: nc.tensor.matmul(pz[j][:], lhsT=fT[b][:, kc, :], rhs=Wb[:, kc, nb * 512:(nb + 1) * 512],
                                                           start=(kc == 0), stop=(kc == 15)),
                     reads=[("fT", b), ("Wb", nb // 2)], writes=[("pz", j)])
            P.op("dve", lambda: nc.vector.tensor_tensor(out=tq[j][:], in0=pz[j][:], in1=gm[:, nb * 512:(nb + 1) * 512], op=ALU.mult),
                 reads=[("pz", j), "gm"], writes=[("tq", j)])
            P.op("pool", lambda: nc.gpsimd.tensor_tensor(out=xs[b][:, nb * 512:(nb + 1) * 512], in0=xs[b][:, nb * 512:(nb + 1) * 512],
                                                         in1=tq[j][:], op=ALU.add),
                 reads=[xk, ("tq", j)], writes=[xk])
        P.dma("pool", x1o[tok0:tok0 + 128, :], xs[b][:], reads=[xk], writes=["x1o"])
        s_ = ss[b]
        sk = ("ss", b)
        P.op("act", lambda: nc.scalar.activation(out=hfb[b][:], in_=xs[b][:], func=AF.Square, accum_out=s_[:, 0:1]),
             reads=[xk], writes=[("hfb", b), sk])
        P.op("act", lambda: nc.scalar.activation(out=s_[:, 1:2], in_=s_[:, 0:1], func=AF.Sqrt, scale=1.0 / D, bias=epsb[:, 0:1]),
             reads=[sk, "epsb"], writes=[sk])
        P.op("dve", lambda: nc.vector.reciprocal(out=s_[:, 2:3], in_=s_[:, 1:2]), reads=[sk], writes=[sk])
        P.op("dve", lambda: nc.vector.scalar_tensor_tensor(out=hf32[:], in0=xs[b][:], scalar=s_[:, 2:3], in1=gam[:], op0=ALU.mult, op1=ALU.mult),
             reads=[xk, sk, "gam"], writes=[("hf32", b)])
        P.op("pool", lambda: nc.gpsimd.tensor_tensor(out=hf32[:], in0=hf32[:], in1=sh[:], op=ALU.add), reads=[("hf32", b), "sh"], writes=[("hf32", b)])
        P.op("act", lambda: nc.scalar.copy(out=hfb[b][:], in_=hf32[:]), reads=[("hf32", b)], writes=[("hfb", b)])
        P.dma("act", hfo[tok0:tok0 + 128, :], hfb[b][:], reads=[("hfb", b)], writes=["hfo"])
        pending.append((ti, b))
        if len(pending) > 1:
            router(*pending.pop(0))
    while pending:
        router(*pending.pop(0))
    return P.finish(["x1o", "hfo", "affo"])


_PROJ_NC = [None]


def run_proj(fT_list, W, x_full, gm_, gffn, cf_, sf_, w_r):
    nc = build_proj()
    common = {"W": np.ascontiguousarray(W), "gm": bc128(gm_), "gffn": bc128(gffn), "cf": bc128(cf_), "sf": bc128(sf_),
              "wr": np.ascontiguousarray(w_r)}
    maps = []
    for i in range(NCORES):
        d = dict(common)
        d["fT"] = np.ascontiguousarray(fT_list[i])
        d["x"] = np.ascontiguousarray(x_full[i * TPC:(i + 1) * TPC])
        maps.append(d)
    res = run_bass_kernel_spmd(nc, maps, core_ids=list(range(NCORES)))
    x1 = np.concatenate([r["x1"] for r in res.results], 0)
    hf = np.concatenate([r["hf"] for r in res.results], 0)
    aff = np.concatenate([r["aff"] for r in res.results], 0)
    return x1, hf, aff


CAP = 1024
FE = 1024
NBIS = 32
OOB = 30000.0


def build_E():
    P = Prog()
    nc = P.nc
    affd = P.din("affT", [128, 2, 64], F32)
    ebd = P.din("ebase", [128, 2], F32)
    hfd = P.din("hf", [SEQ, D], BF16)
    wgd = P.din("wg", [2, D, FE], F32)
    wud = P.din("wu", [2, D, FE], F32)
    wdd = P.din("wd", [2, FE, D], F32)
    Yo = P.dout("Y", [2, CAP, D], F32)
    sloto = P.dout("slot", [128, 2, 64], I32)

    ident, idk = make_ident(P)
    onesf = P.sb("onesf", [128, 128], F32)
    UT = P.sb("UT", [128, 128], F32)
    Lb = P.sb("Lb", [128, 128], F32)
    iot_i = P.sb("iot_i", [128, 1024], I32)
    iot = P.sb("iot", [128, 1024], F32)
    jp_i = P.sb("jp_i", [128, 64, 2], I32)
    jp = P.sb("jp", [128, 64, 2], BF16)
    a = P.sb("a", [128, 2, 64], F32)
    eb = P.sb("eb", [128, 2], F32)
    msk = P.sb("msk", [128, 2, 64], F32)
    bs = P.sb("bs", [128, 16], F32)
    exs = P.sb("exs", [128, 128], F32)
    kv_i = P.sb("kv_i", [128, 2, 16], I32)
    kv = P.sb("kv", [128, 2, 16], F32)
    thr = P.sb("thr", [128, 2, 16], F32)
    mk4 = P.sb("mk4", [128, 2, 16, 64], F32)
    cn4 = P.sb("cn4", [128, 2, 16], F32)
    ge4 = P.sb("ge4", [128, 2, 16], F32)
    ct = P.sb("ct", [128, 1], F32)
    CB = P.sb("CB", [128, 128], F32)
    rk = P.sb("rk", [128, 2, 64], F32)
    sg = P.sb("sg", [128, 2, 64], F32)
    sgi = P.sb("sgi", [128, 2, 64], I32)
    sel = [P.sb("sel%d" % i, [128, 1024], BF16) for i in range(3)]
    idxf = P.sb("idxf", [128, 2, 8], F32)
    pselS = P.sb("pselS", [128, 8, 2], F32)
    zb = P.sb("zb", [128, 128], BF16)
    idxi = P.sb("idxi", [128, 2, 8], I32)
    xg = [P.sb("xg%d" % i, [128, D], BF16) for i in range(2)]
    xsT = P.sb("xsT", [128, 16, CAP], BF16)
    hT = P.sb("hTe", [128, 8, CAP], BF16)
    wgb = [P.sb("wgb%d" % i, [128, 16, 256], BF16) for i in range(2)]
    wub = [P.sb("wub%d" % i, [128, 16, 256], BF16) for i in range(2)]
    wdb = [P.sb("wdb%d" % i, [128, 8, 512], BF16) for i in range(2)]
    sa = [P.sb("sa%d" % i, [128, 512], F32) for i in range(2)]
    ys = [P.sb("ys%d" % i, [128, 512], F32) for i in range(2)]
    pa = [P.ps("pa%d" % i, [128, 512], F32) for i in range(2)]
    pu = [P.ps("pu%d" % i, [128, 512], F32) for i in range(2)]
    py = [P.ps("py%d" % i, [128, 512], F32) for i in range(2)]
    pT = P.ps("pT", [128, D], BF16)

    P.op("pool", lambda: nc.gpsimd.memset(onesf[:], 1.0), writes=["onesf"])
    P.op("pool", lambda: nc.gpsimd.memset(UT[:], 1.0), writes=["UT"])
    P.op("pool", lambda: nc.gpsimd.affine_select(out=UT[:], in_=UT[:], pattern=[[1, 128]], compare_op=ALU.is_ge, fill=0.0, base=0,
                                                 channel_multiplier=-1), reads=["UT"], writes=["UT"])
    P.op("pool", lambda: nc.gpsimd.memset(Lb[:], 1.0), writes=["Lb"])
    P.op("pool", lambda: nc.gpsimd.affine_select(out=Lb[:], in_=Lb[:], pattern=[[1, 128]], compare_op=ALU.is_gt, fill=0.0, base=0,
                                                 channel_multiplier=-1), reads=["Lb"], writes=["Lb"])
    P.op("pool", lambda: nc.gpsimd.memset(Lb[0:64, 64:128], 0.0), reads=["Lb"], writes=["Lb"])
    P.op("pool", lambda: nc.gpsimd.iota(iot_i[:], pattern=[[1, 1024]], base=0, channel_multiplier=0), writes=["iot_i"])
    P.op("dve", lambda: nc.vector.tensor_copy(out=iot[:], in_=iot_i[:]), reads=["iot_i"], writes=["iot"])
    P.op("pool", lambda: nc.gpsimd.iota(jp_i[:, :, 0], pattern=[[1, 64]], base=0, channel_multiplier=0), writes=["jp_i"])
    P.op("pool", lambda: nc.gpsimd.iota(jp_i[:, :, 1], pattern=[[0, 64]], base=0, channel_multiplier=1), reads=["jp_i"], writes=["jp_i"])
    P.op("dve", lambda: nc.vector.tensor_copy(out=jp[:], in_=jp_i[:]), reads=["jp_i"], writes=["jp"])
    P.dma("sp", a[:], affd[:, :, :], writes=["a"])
    P.dma("sp", eb[:], ebd[:, :], writes=["eb"])
    P.op("pool", lambda: nc.gpsimd.memset(zb[:], 0.0), writes=["zb"])

    lo, stp, nn, tmp = bs[:, 0:2], bs[:, 2:4], bs[:, 4:6], bs[:, 6:8]
    P.op("dve", lambda: nc.vector.memset(bs[:, 0:2], 0.0), writes=["bs"])
    P.op("dve", lambda: nc.vector.memset(bs[:, 2:4], 1.001 / 16), reads=["bs"], writes=["bs"])
    P.op("pool", lambda: nc.gpsimd.iota(kv_i[:], pattern=[[0, 2], [1, 16]], base=0, channel_multiplier=0), writes=["kv_i"])
    P.op("dve", lambda: nc.vector.tensor_copy(out=kv[:], in_=kv_i[:]), reads=["kv_i"], writes=["kv"])
    tot = pa[0][:, 0:32].rearrange("p (e k) -> p e k", e=2)

    def V(fn, reads=("bs",), writes=("bs",)):
        P.op("dve", fn, reads=list(reads), writes=list(writes))

    for itb in range(8):
        V(lambda: nc.vector.tensor_tensor(out=thr[:], in0=kv[:], in1=stp.unsqueeze(2).to_broadcast([128, 2, 16]), op=ALU.mult),
          reads=["kv", "bs"], writes=["thr"])
        V(lambda: nc.vector.tensor_tensor(out=thr[:], in0=thr[:], in1=lo.unsqueeze(2).to_broadcast([128, 2, 16]), op=ALU.add),
          reads=["thr", "bs"], writes=["thr"])
        V(lambda: nc.vector.tensor_tensor(out=mk4[:], in0=a[:, :, :].unsqueeze(2).to_broadcast([128, 2, 16, 64]),
                                          in1=thr[:, :, :].unsqueeze(3).to_broadcast([128, 2, 16, 64]), op=ALU.is_ge),
          reads=["a", "thr"], writes=["mk4"])
        V(lambda: nc.vector.reduce_sum(out=cn4[:], in_=mk4[:], axis=AX.X), reads=["mk4"], writes=["cn4"])
        P.op("pe", lambda: nc.tensor.matmul(pa[0][:, 0:32], lhsT=onesf[:], rhs=cn4.rearrange("p e k -> p (e k)"), start=True, stop=True),
             reads=["onesf", "cn4"], writes=[("pa", 0)])
        V(lambda: nc.vector.tensor_scalar(out=ge4[:], in0=tot, scalar1=float(CAP) - 0.5, scalar2=None, op0=ALU.is_ge),
          reads=[("pa", 0)], writes=["ge4"])
        V(lambda: nc.vector.reduce_sum(out=nn, in_=ge4[:], axis=AX.X), reads=["ge4", "bs"], writes=["bs"])
        V(lambda: nc.vector.scalar_tensor_tensor(out=tmp, in0=nn, scalar=-1.0, in1=stp, op0=ALU.add, op1=ALU.mult))
        V(lambda: nc.vector.tensor_tensor(out=lo, in0=lo, in1=tmp, op=ALU.add))
        V(lambda: nc.vector.tensor_scalar(out=stp, in0=stp, scalar1=0.0625, scalar2=None, op0=ALU.mult))
    V(lambda: nc.vector.tensor_tensor(out=msk[:], in0=a[:], in1=lo.unsqueeze(2).to_broadcast([128, 2, 64]), op=ALU.is_ge),
      reads=["a", "bs"], writes=["msk"])

    mflat = msk.rearrange("p e j -> p (e j)")
    pc = pa[1][:, 0:128]
    pex = pu[0][:, 0:128]
    pct = pu[1][:, 0:1]
    P.op("pe", lambda: nc.tensor.matmul(pc, lhsT=UT[:], rhs=mflat, start=True, stop=True), reads=["UT", "msk"], writes=[("pa", 1)])
    P.op("pe", lambda: nc.tensor.matmul(pct, lhsT=mflat, rhs=onesf[:, 0:1], start=True, stop=True), reads=["onesf", "msk"], writes=[("pu", 1)])
    P.op("dve", lambda: nc.vector.tensor_copy(out=ct[:], in_=pct), reads=[("pu", 1)], writes=["ct"])
    P.op("dve", lambda: nc.vector.tensor_scalar(out=CB[:], in0=onesf[:], scalar1=ct[:, 0:1], scalar2=None, op0=ALU.mult),
         reads=["onesf", "ct"], writes=["CB"])
    P.op("pe", lambda: nc.tensor.matmul(pex, lhsT=CB[:], rhs=Lb[:], start=True, stop=True), reads=["CB", "Lb"], writes=[("pu", 0)])
    P.op("dve", lambda: nc.vector.tensor_copy(out=exs[:], in_=pex), reads=[("pu", 0)], writes=["exs"])
    rkf = rk.rearrange("p e j -> p (e j)")
    P.op("dve", lambda: nc.vector.tensor_tensor(out=rkf, in0=pc, in1=exs[:], op=ALU.add), reads=[("pa", 1), "exs"], writes=["rk"])
    P.op("dve", lambda: nc.vector.tensor_tensor(out=rkf, in0=rkf, in1=mflat, op=ALU.mult), reads=["rk", "msk"], writes=["rk"])
    P.op("dve", lambda: nc.vector.tensor_scalar(out=rkf, in0=rkf, scalar1=-1.0, scalar2=None, op0=ALU.add), reads=["rk"], writes=["rk"])
    P.op("dve", lambda: nc.vector.tensor_tensor(out=sg[:], in0=rk[:], in1=eb[:, :].unsqueeze(2).to_broadcast([128, 2, 64]), op=ALU.add),
         reads=["rk", "eb"], writes=["sg"])
    P.op("dve", lambda: nc.vector.tensor_scalar(out=sg[:], in0=sg[:], scalar1=-OOB, scalar2=None, op0=ALU.add), reads=["sg"], writes=["sg"])
    P.op("dve", lambda: nc.vector.tensor_tensor(out=sg[:], in0=sg[:], in1=msk[:], op=ALU.mult), reads=["sg", "msk"], writes=["sg"])
    P.op("dve", lambda: nc.vector.tensor_scalar(out=sg[:], in0=sg[:], scalar1=OOB, scalar2=None, op0=ALU.add), reads=["sg"], writes=["sg"])
    P.op("dve", lambda: nc.vector.tensor_copy(out=sgi[:], in_=sg[:]), reads=["sg"], writes=["sgi"])
    P.dma("sp", sloto[:, :, :], sgi[:], reads=["sgi"], writes=["sloto"])

    for e in range(2):
        psel = py[e][:, 0:16].rearrange("p (s c) -> p s c", c=2)
        P.op("pe", lambda: nc.tensor.matmul(py[e][:, 0:16], lhsT=zb[:, 0:128], rhs=zb[:, 0:16], start=True, stop=False),
             reads=["zb"], writes=[("py", e)])
        for j in range(64):
            sb_ = sel[j % 3]
            P.op("dve", lambda: nc.vector.tensor_scalar(out=sb_[:], in0=iot[:], scalar1=rk[:, e, j:j + 1], scalar2=None, op0=ALU.is_equal),
                 reads=["iot", "rk"], writes=[("sel", j % 3)])
            for s in range(8):
                P.op("pe", lambda s=s: nc.tensor.matmul(psel[:, s, :], lhsT=sb_[:, s * 128:(s + 1) * 128], rhs=jp[:, j, :],
                                                         start=False, stop=(j == 63), skip_group_check=True),
                     reads=[("sel", j % 3), "jp"], writes=[("py", e)])
        P.op("dve", lambda: nc.vector.tensor_copy(out=pselS[:], in_=psel), reads=[("py", e)], writes=["pselS"])
        P.op("dve", lambda: nc.vector.scalar_tensor_tensor(out=idxf[:, e, :], in0=pselS[:, :, 0], scalar=128.0, in1=pselS[:, :, 1],
                                                           op0=ALU.mult, op1=ALU.add),
             reads=["pselS"], writes=["idxf"])
    P.op("dve", lambda: nc.vector.tensor_copy(out=idxi[:], in_=idxf[:]), reads=["idxf"], writes=["idxi"])

    CL = CastLoader(P, 1024)
    gi = 0
    for e in range(2):
        for s in range(8):
            g_ = gi % 2
            gi += 1
            P.idma(xg[g_][:], None, hfd[:, :], bass.IndirectOffsetOnAxis(ap=idxi[:, e, s:s + 1], axis=0),
                   reads=["idxi"], writes=[("xg", g_)])
            for kc in range(16):
                P.op("pe", lambda kc=kc: nc.tensor.transpose(pT[:, kc * 128:(kc + 1) * 128], xg[g_][:, kc * 128:(kc + 1) * 128], ident[:]),
                     reads=[("xg", g_), idk], writes=["pT"])
            P.op("act", lambda: nc.scalar.copy(out=xsT[:, :, s * 128:(s + 1) * 128], in_=pT.rearrange("p (k t) -> p k t", k=16)),
                 reads=["pT"], writes=["xsT"])
        wgv = wgd[e].rearrange("(kc p) f -> p kc f", p=128)
        wuv = wud[e].rearrange("(kc p) f -> p kc f", p=128)
        wdv = wdd[e].rearrange("(fc p) d -> p fc d", p=128)
        zi = 0

        def queue_gu(f2, wgv=wgv, wuv=wuv):
            b = f2 % 2
            for kq in range(4):
                CL.enqueue(wgb[b][:, kq * 4:(kq + 1) * 4, :], ("wgb", b), wgv[:, kq * 4:(kq + 1) * 4, f2 * 256:(f2 + 1) * 256], (4, 256))
                CL.enqueue(wub[b][:, kq * 4:(kq + 1) * 4, :], ("wub", b), wuv[:, kq * 4:(kq + 1) * 4, f2 * 256:(f2 + 1) * 256], (4, 256))

        def queue_d(dc, wdv=wdv):
            b = dc % 2
            for fq in range(4):
                CL.enqueue(wdb[b][:, fq * 2:(fq + 1) * 2, :], ("wdb", b), wdv[:, fq * 2:(fq + 1) * 2, dc * 512:(dc + 1) * 512], (2, 512))

        if e == 0:
            queue_gu(0)
        for f2 in range(4):
            b = f2 % 2
            CL.flush()
            if f2 + 1 < 4:
                queue_gu(f2 + 1)
            else:
                queue_d(0)
            for fs in range(2):
                fc = f2 * 2 + fs
                for hh in range(2):
                    z = zi % 2
                    zi += 1
                    for kc in range(16):
                        P.op("pe", lambda kc=kc: nc.tensor.matmul(pa[z][:], lhsT=wgb[b][:, kc, fs * 128:(fs + 1) * 128],
                                                                   rhs=xsT[:, kc, hh * 512:(hh + 1) * 512], start=(kc == 0), stop=(kc == 15)),
                             reads=[("wgb", b), "xsT"], writes=[("pa", z)])
                    for kc in range(16):
                        P.op("pe", lambda kc=kc: nc.tensor.matmul(pu[z][:], lhsT=wub[b][:, kc, fs * 128:(fs + 1) * 128],
                                                                   rhs=xsT[:, kc, hh * 512:(hh + 1) * 512], start=(kc == 0), stop=(kc == 15)),
                             reads=[("wub", b), "xsT"], writes=[("pu", z)])
                    P.op("act", lambda: nc.scalar.activation(out=sa[z][:], in_=pa[z][:], func=AF.Silu), reads=[("pa", z)], writes=[("sa", z)])
                    P.op("dve", lambda: nc.vector.tensor_tensor(out=hT[:, fc, hh * 512:(hh + 1) * 512], in0=pu[z][:], in1=sa[z][:], op=ALU.mult),
                         reads=[("pu", z), ("sa", z)], writes=["hT"])
                    CL.pump(2)
        yi = 0
        for dc in range(4):
            b = dc % 2
            CL.flush()
            if dc + 1 < 4:
                queue_d(dc + 1)
            elif e == 0:
                wgv1 = wgd[1].rearrange("(kc p) f -> p kc f", p=128)
                wuv1 = wud[1].rearrange("(kc p) f -> p kc f", p=128)
                queue_gu(0, wgv1, wuv1)
            for s in range(8):
                z = yi % 2
                yi += 1
                for fc in range(8):
                    P.op("pe", lambda fc=fc: nc.tensor.matmul(py[z][:], lhsT=hT[:, fc, s * 128:(s + 1) * 128], rhs=wdb[b][:, fc, :],
                                                               start=(fc == 0), stop=(fc == 7)),
                         reads=["hT", ("wdb", b)], writes=[("py", z)])
                P.op("act", lambda: nc.scalar.copy(out=ys[z][:], in_=py[z][:]), reads=[("py", z)], writes=[("ys", z)])
                P.dma("act", Yo[e, s * 128:(s + 1) * 128, dc * 512:(dc + 1) * 512], ys[z][:], reads=[("ys", z)], writes=["Yo"])
                CL.pump(1)
    return P.finish(["Yo", "sloto"])


def run_E(aff, hf, wg, wu, wd):
    nc = build_E()
    hf = np.ascontiguousarray(hf)
    maps = []
    for c in range(NCORES):
        es = slice(2 * c, 2 * c + 2)
        affT = np.ascontiguousarray(aff[:, es].reshape(64, 128, 2).transpose(1, 2, 0))
        ebase = np.ascontiguousarray(np.broadcast_to(np.array([[2 * c * CAP, (2 * c + 1) * CAP]], np.float32), (128, 2)))
        maps.append({"affT": affT, "ebase": ebase, "hf": hf, "wg": np.ascontiguousarray(wg[es]), "wu": np.ascontiguousarray(wu[es]),
                     "wd": np.ascontiguousarray(wd[es])})
    res = run_bass_kernel_spmd(nc, maps, core_ids=list(range(NCORES)))
    Yall = np.concatenate([r["Y"].reshape(2 * CAP, D) for r in res.results], 0)
    slots = np.concatenate([r["slot"].transpose(2, 0, 1).reshape(SEQ, 2) for r in res.results], 1)
    return Yall, np.ascontiguousarray(slots)


NYROWS = 16 * CAP
NGT = 5


def combine_setup(P):
    nc = P.nc
    c = {}
    c["x1d"] = P.din("x1", [TPC, D], F32)
    c["Yd"] = P.din("Yall", [NYROWS, D], F32)
    c["sld"] = P.din("slots", [TPC, 16], I32)
    c["afd"] = P.din("aff", [TPC, 16], F32)
    c["gfd"] = P.din("gf", [128, D], F32)
    c["sl"] = P.sb("sl", [128, NT, 16], I32)
    c["af"] = P.sb("af", [128, NT, 16], F32)
    c["gf"] = P.sb("gfs", [128, D], F32)
    c["gt"] = [P.sb("gt%d" % i, [128, D], F32) for i in range(NGT)]
    c["acc"] = [P.sb("acc%d" % i, [128, D], F32) for i in range(2)]
    c["xs"] = [P.sb("cxs%d" % i, [128, D], F32) for i in range(2)]
    P.dma("sp", c["sl"][:], c["sld"].rearrange("(t p) e -> p t e", p=128), writes=["sl"])
    P.dma("sp", c["af"][:], c["afd"].rearrange("(t p) e -> p t e", p=128), writes=["af"])
    P.dma("sp", c["gf"][:], c["gfd"][:, :], writes=["gfs"])
    c["gi"] = 0
    c["breg"] = nc.gpsimd.to_reg(NYROWS - 1)
    return c


def combine_tile(P, c, ti):
    nc = P.nc
    b = ti % 2
    acc = c["acc"][b]
    ak = ("acc", b)
    xs = c["xs"][b]
    xk = ("cxs", b)
    P.dma("sp", xs[:], c["x1d"][ti * 128:(ti + 1) * 128, :], writes=[xk])
    for e in range(16):
        g = c["gi"] % NGT
        c["gi"] += 1
        gt = c["gt"][g]
        gk = ("gt", g)
        P.op("act", lambda: nc.scalar.memzero(gt[:]), writes=[gk])
        P.idma(gt[:], None, c["Yd"][:, :], bass.IndirectOffsetOnAxis(ap=c["sl"][:, ti, e:e + 1], axis=0),
               reads=["sl"], writes=[gk], semkey=("dg", g), bounds_check=c["breg"], oob_is_err=False)
        if e == 0:
            P.op("dve", lambda: nc.vector.tensor_scalar(out=acc[:], in0=gt[:], scalar1=c["af"][:, ti, e:e + 1], scalar2=None, op0=ALU.mult),
                 reads=[gk, "af"], writes=[ak])
        else:
            P.op("dve", lambda: nc.vector.scalar_tensor_tensor(out=acc[:], in0=gt[:], scalar=c["af"][:, ti, e:e + 1], in1=acc[:],
                                                               op0=ALU.mult, op1=ALU.add),
                 reads=[gk, "af", ak], writes=[ak])
    P.op("dve", lambda: nc.vector.tensor_tensor(out=acc[:], in0=acc[:], in1=c["gf"][:], op=ALU.mult), reads=[ak, "gfs"], writes=[ak])
    P.op("pool", lambda: nc.gpsimd.tensor_tensor(out=xs[:], in0=xs[:], in1=acc[:], op=ALU.add), reads=[xk, ak], writes=[xk])
    return xs, xk


def build_F():
    P = Prog()
    nc = P.nc
    c = combine_setup(P)
    gmixd = P.din("gmix", [128, D], F32)
    cmd = P.din("cm", [128, D], F32)
    smd = P.din("sm", [128, D], F32)
    Cd = P.din("Cch", [512, 512], BF16)
    Sd = P.din("Sch", [512, 512], BF16)
    x2o = P.dout("x2", [TPC, D], F32)
    Ao = P.dout("A", [TPC, D], BF16)
    Bo = P.dout("B", [TPC, D], BF16)
    ident, idk = make_ident(P)
    gam = P.sb("gam", [128, D], F32)
    sh = P.sb("sh", [128, D], F32)
    hb = [P.sb("hb%d" % i, [128, D], BF16) for i in range(2)]
    hTt = P.sb("hTt", [128, 16, 128], BF16)
    Cs = P.sb("Cs", [128, 4, 512], BF16)
    Ss = P.sb("Ss", [128, 4, 512], BF16)
    ao = [P.sb("ao%d" % i, [128, 512], BF16) for i in range(2)]
    ss = [P.sb("ss%d" % i, [128, 4], F32) for i in range(2)]
    epsb = P.sb("epsb", [128, 1], F32)
    pT = P.ps("pT", [128, D], BF16)
    pz = [P.ps("pz%d" % i, [128, 512], F32) for i in range(2)]
    P.op("pool", lambda: nc.gpsimd.memset(epsb[:], EPS), writes=["epsb"])
    P.dma("act", gam[:], cmd[:, :], writes=["gam"])
    P.dma("act", c["gt"][0][:], gmixd[:, :], writes=[("gt", 0)], semkey=("dg", 0))
    P.dma("act", sh[:], smd[:, :], writes=["sh"])
    P.dma("act", Cs[:], Cd.rearrange("(k p) n -> p k n", p=128), writes=["Cs"])
    P.dma("act", Ss[:], Sd.rearrange("(k p) n -> p k n", p=128), writes=["Ss"])
    P.op("dve", lambda: nc.vector.scalar_tensor_tensor(out=gam[:], in0=gam[:], scalar=1.0, in1=c["gt"][0][:], op0=ALU.add, op1=ALU.mult),
         reads=["gam", ("gt", 0)], writes=["gam"])
    zc = 0
    nxt = combine_tile(P, c, 0)
    for ti in range(NT):
        b = ti % 2
        xs, xk = nxt
        if ti + 1 < NT:
            nxt = combine_tile(P, c, ti + 1)
        P.dma("act", x2o[ti * 128:(ti + 1) * 128, :], xs[:], reads=[xk], writes=["x2o"])
        rms_modulate_tile(P, nc, xs, xk, 128, ss[b], ("ss", b), epsb, gam, sh, hb[b], ("hb", b))
        for kc in range(16):
            P.op("pe", lambda kc=kc: nc.tensor.transpose(pT[:, kc * 128:(kc + 1) * 128], hb[b][:, kc * 128:(kc + 1) * 128], ident[:]),
                 reads=[("hb", b), idk], writes=["pT"])
        P.op("act", lambda: nc.scalar.copy(out=hTt[:], in_=pT.rearrange("p (k t) -> p k t", k=16)), reads=["pT"], writes=["hTt"])
        for g in range(4):
            for (W_, wk, outd) in ((Cs, "Cs", Ao), (Ss, "Ss", Bo)):
                j = zc % 2
                zc += 1
                for k in range(4):
                    P.op("pe", lambda k=k: nc.tensor.matmul(pz[j][:], lhsT=hTt[:, g * 4 + k, :], rhs=W_[:, k, :], start=(k == 0), stop=(k == 3)),
                         reads=["hTt", wk], writes=[("pz", j)])
                P.op("act", lambda: nc.scalar.copy(out=ao[j][:], in_=pz[j][:]), reads=[("pz", j)], writes=[("ao", j)])
                P.dma("act", outd[ti * 128:(ti + 1) * 128, g * 512:(g + 1) * 512], ao[j][:], reads=[("ao", j)], writes=["AB"])
    return P.finish(["x2o", "AB"])


def bf16_np(a):
    import ml_dtypes
    return np.ascontiguousarray(np.asarray(a, np.float32).astype(ml_dtypes.bfloat16))


def dft_consts():
    c = np.arange(512)
    ang = 2 * np.pi * np.outer(c, c) / 512
    Cch = np.cos(ang) / math.sqrt(512)
    Sch = np.sin(ang) / math.sqrt(512)
    n1 = np.arange(128)
    a1 = 2 * np.pi * np.outer(n1, n1) / 128
    Cc = np.cos(a1) / math.sqrt(128)
    Ssn = np.sin(a1) / math.sqrt(128)
    n2 = np.arange(64)[:, None, None]
    k1 = np.arange(128)[None, :, None]
    k2 = np.arange(64)[None, None, :]
    th = 2 * np.pi * (n2 * k1 / 8192.0 + n2 * k2 / 64.0)
    Hr = np.cos(th) / 8.0
    Hni = np.sin(th) / 8.0
    return dict(Cch=bf16_np(Cch), Sch=bf16_np(Sch), Cc=bf16_np(Cc), nSs=bf16_np(-Ssn), nCc=bf16_np(-Cc), Hr=bf16_np(Hr), Hni=bf16_np(Hni))


def run_F(x1, Yall, slots, aff, gf_, gmix, cm_, sm_):
    nc = build_F()
    k = dft_consts()
    common = {"Yall": Yall, "gf": bc128(gf_), "gmix": bc128(gmix), "cm": bc128(cm_), "sm": bc128(sm_), "Cch": k["Cch"], "Sch": k["Sch"]}
    maps = []
    for i in range(NCORES):
        d = dict(common)
        sl = slice(i * TPC, (i + 1) * TPC)
        d["x1"] = np.ascontiguousarray(x1[sl])
        d["slots"] = np.ascontiguousarray(slots[sl])
        d["aff"] = np.ascontiguousarray(aff[sl])
        maps.append(d)
    res = run_bass_kernel_spmd(nc, maps, core_ids=list(range(NCORES)))
    x2 = np.concatenate([r["x2"] for r in res.results], 0)
    A = np.concatenate([r["A"] for r in res.results], 0)
    B = np.concatenate([r["B"] for r in res.results], 0)
    return x2, A, B


CPC = D // NCORES


def build_G():
    P = Prog()
    nc = P.nc
    zAd = P.din("zA", [128, 64, CPC], BF16)
    zBd = P.din("zB", [128, 64, CPC], BF16)
    Ccd = P.din("Cc", [128, 128], BF16)
    nSsd = P.din("nSs", [128, 128], BF16)
    nCcd = P.din("nCc", [128, 128], BF16)
    Hrd = P.din("Hr", [64, 128, 64], BF16)
    Hnid = P.din("Hni", [64, 128, 64], BF16)
    YTo = P.dout("YT", [CPC, SEQ], BF16)
    zA = P.sb("zAs", [128, 64, 128], BF16)
    zB = P.sb("zBs", [128, 64, 128], BF16)
    Cc = P.sb("Ccs", [128, 128], BF16)
    nSs = P.sb("nSss", [128, 128], BF16)
    nCc = P.sb("nCcs", [128, 128], BF16)
    Hr = P.sb("Hrs", [64, 128, 64], BF16)
    Hni = P.sb("Hnis", [64, 128, 64], BF16)
    Tr = P.sb("Tr", [64, 128, 128], BF16)
    Ti = P.sb("Ti", [64, 128, 128], BF16)
    YTs = P.sb("YTs", [128, 64, 128], BF16)
    pTr = [P.ps("pTr%d" % i, [64, 4, 128], F32) for i in range(2)]
    pTi = [P.ps("pTi%d" % i, [64, 4, 128], F32) for i in range(2)]
    pY = [P.ps("pY%d" % i, [128, 8, 64], F32) for i in range(2)]
    P.dma("sp", Cc[:], Ccd[:, :], writes=["Cc"])
    P.dma("sp", nSs[:], nSsd[:, :], writes=["nSs"])
    P.dma("sp", nCc[:], nCcd[:, :], writes=["nCc"])
    P.dma("act", Hr[:], Hrd[:, :, :], writes=["Hr"])
    P.dma("act", Hni[:], Hnid[:, :, :], writes=["Hni"])
    for hc in range(2):
        c0 = hc * 128
        P.dma("sp", zA[:], zAd[:, :, c0:c0 + 128], writes=["zA"])
        P.dma("act", zB[:], zBd[:, :, c0:c0 + 128], writes=["zB"])
        for cb in range(32):
            j = cb % 2
            for cc in range(4):
                c = cb * 4 + cc
                P.op("pe", lambda: nc.tensor.matmul(pTr[j][:, cc, :], lhsT=zA[:, :, c], rhs=Cc[:], start=True, stop=False),
                     reads=["zA", "Cc"], writes=[("pTr", j)])
                P.op("pe", lambda: nc.tensor.matmul(pTr[j][:, cc, :], lhsT=zB[:, :, c], rhs=nSs[:], start=False, stop=True),
                     reads=["zB", "nSs"], writes=[("pTr", j)])
                P.op("pe", lambda: nc.tensor.matmul(pTi[j][:, cc, :], lhsT=zB[:, :, c], rhs=nCc[:], start=True, stop=False),
                     reads=["zB", "nCc"], writes=[("pTi", j)])
                P.op("pe", lambda: nc.tensor.matmul(pTi[j][:, cc, :], lhsT=zA[:, :, c], rhs=nSs[:], start=False, stop=True),
                     reads=["zA", "nSs"], writes=[("pTi", j)])
            P.op("act", lambda: nc.scalar.copy(out=Tr[:, cb * 4:(cb + 1) * 4, :], in_=pTr[j][:]), reads=[("pTr", j)], writes=["Tr"])
            P.op("dve", lambda: nc.vector.tensor_copy(out=Ti[:, cb * 4:(cb + 1) * 4, :], in_=pTi[j][:]), reads=[("pTi", j)], writes=["Ti"])
        for kb in range(16):
            j = kb % 2
            for kk in range(8):
                k1 = kb * 8 + kk
                P.op("pe", lambda: nc.tensor.matmul(pY[j][:, kk, :], lhsT=Tr[:, :, k1], rhs=Hr[:, k1, :], start=True, stop=False),
                     reads=["Tr", "Hr"], writes=[("pY", j)])
                P.op("pe", lambda: nc.tensor.matmul(pY[j][:, kk, :], lhsT=Ti[:, :, k1], rhs=Hni[:, k1, :], start=False, stop=True),
                     reads=["Ti", "Hni"], writes=[("pY", j)])
            eng = "act" if kb % 2 == 0 else "dve"
            if eng == "act":
                P.op("act", lambda: nc.scalar.copy(out=YTs[:, :, kb * 8:(kb + 1) * 8], in_=pY[j].rearrange("p a b -> p b a")),
                     reads=[("pY", j)], writes=["YTs"])
            else:
                P.op("dve", lambda: nc.vector.tensor_copy(out=YTs[:, :, kb * 8:(kb + 1) * 8], in_=pY[j].rearrange("p a b -> p b a")),
                     reads=[("pY", j)], writes=["YTs"])
        P.dma("sp", YTo[c0:c0 + 128, :], YTs.rearrange("p a b -> p (a b)"), reads=["YTs"], writes=["YTo"])
    return P.finish(["YTo"])


def run_G(A, B):
    nc = build_G()
    k = dft_consts()
    maps = []
    for i in range(NCORES):
        cs = slice(i * CPC, (i + 1) * CPC)
        maps.append({"zA": np.ascontiguousarray(A[:, cs]).reshape(128, 64, CPC), "zB": np.ascontiguousarray(B[:, cs]).reshape(128, 64, CPC),
                     "Cc": k["Cc"], "nSs": k["nSs"], "nCc": k["nCc"], "Hr": k["Hr"], "Hni": k["Hni"]})
    res = run_bass_kernel_spmd(nc, maps, core_ids=list(range(NCORES)))
    YT = np.concatenate([r["YT"] for r in res.results], 0)
    return YT


def build_I():
    P = Prog()
    nc = P.nc
    c = combine_setup(P)
    gfin_d = P.din("gfin", [128, D], F32)
    outd = P.dout("out", [TPC, D], F32)
    gfin = P.sb("gfin_s", [128, D], F32)
    jk = P.sb("junk", [128, D], BF16)
    ss = [P.sb("ss%d" % i, [128, 4], F32) for i in range(2)]
    epsb = P.sb("epsb", [128, 1], F32)
    P.op("pool", lambda: nc.gpsimd.memset(epsb[:], EPS), writes=["epsb"])
    P.dma("act", gfin[:], gfin_d[:, :], writes=["gfin"])
    nxt = combine_tile(P, c, 0)
    for ti in range(NT):
        b = ti % 2
        xs, xk = nxt
        if ti + 1 < NT:
            nxt = combine_tile(P, c, ti + 1)
        s_ = ss[b]
        sk = ("ss", b)
        P.op("act", lambda: nc.scalar.activation(out=jk[:], in_=xs[:], func=AF.Square, accum_out=s_[:, 0:1]), reads=[xk], writes=["junk", sk])
        P.op("act", lambda: nc.scalar.activation(out=s_[:, 1:2], in_=s_[:, 0:1], func=AF.Sqrt, scale=1.0 / D, bias=epsb[:, 0:1]),
             reads=[sk, "epsb"], writes=[sk])
        P.op("dve", lambda: nc.vector.reciprocal(out=s_[:, 2:3], in_=s_[:, 1:2]), reads=[sk], writes=[sk])
        P.op("dve", lambda: nc.vector.scalar_tensor_tensor(out=xs[:], in0=xs[:], scalar=s_[:, 2:3], in1=gfin[:], op0=ALU.mult, op1=ALU.mult),
             reads=[xk, sk, "gfin"], writes=[xk])
        P.dma("act", outd[ti * 128:(ti + 1) * 128, :], xs[:], reads=[xk], writes=["outd"])
    return P.finish(["outd"])


def run_I(x1, Yall, slots, aff, gf_, gfin):
    nc = build_I()
    common = {"Yall": Yall, "gf": bc128(gf_), "gfin": bc128(gfin)}
    maps = []
    for i in range(NCORES):
        d = dict(common)
        sl = slice(i * TPC, (i + 1) * TPC)
        d["x1"] = np.ascontiguousarray(x1[sl])
        d["slots"] = np.ascontiguousarray(slots[sl])
        d["aff"] = np.ascontiguousarray(aff[sl])
        maps.append(d)
    res = run_bass_kernel_spmd(nc, maps, core_ids=list(range(NCORES)))
    return np.concatenate([r["out"] for r in res.results], 0)


def kernel(**inp):
    inp = {k: np.asarray(v) for k, v in inp.items()}
    m = run_mod(inp)
    x0 = inp["x"][0]
    ra = run_A(inp, m)
    aT = run_B(inp, ra)
    fT = [np.concatenate([np.stack([aT[h][:, i * TPC:(i + 1) * TPC] for h in range(8)], 0), ra[i]["gT"]], 0) for i in range(NCORES)]
    m0 = m[0, 0]
    x1, hf, aff = run_proj(fT, inp["w_out"][0], x0, m0[2 * D:3 * D], inp["g_norm_ffn"][0], m0[4 * D:5 * D], m0[3 * D:4 * D],
                           inp["w_router"][0])
    Yall, slots = run_E(aff, hf, inp["w_gate"][0], inp["w_up"][0], inp["w_down"][0])
    m1 = m[1, 0]
    x2, A, B = run_F(x1, Yall, slots, aff, m0[5 * D:6 * D], inp["g_norm_mix"][1], m1[D:2 * D], m1[0:D])
    YT = run_G(A, B)
    fT = [np.ascontiguousarray(YT[:, i * TPC:(i + 1) * TPC]).reshape(16, 128, TPC) for i in range(NCORES)]
    x1, hf, aff = run_proj(fT, inp["w_fourier_out"][0], x2, m1[2 * D:3 * D], inp["g_norm_ffn"][1], m1[4 * D:5 * D], m1[3 * D:4 * D],
                           inp["w_router"][1])
    Yall, slots = run_E(aff, hf, inp["w_gate"][1], inp["w_up"][1], inp["w_down"][1])
    out = run_I(x1, Yall, slots, aff, m1[5 * D:6 * D], inp["g_final"])
    return np.ascontiguousarray(out.reshape(1, SEQ, D).astype(np.float32))
```

```python
import math
import numpy as np
from contextlib import ExitStack
import concourse.bass as bass
import concourse.mybir as mybir
from concourse.bass_utils import run_bass_kernel_spmd

F32 = mybir.dt.float32
BF16 = mybir.dt.bfloat16
I32 = mybir.dt.int32
AF = mybir.ActivationFunctionType
ALU = mybir.AluOpType
AX = mybir.AxisListType
NCORES = 8
D = 2048
SEQ = 8192
TPC = SEQ // NCORES
NT = TPC // 128
EPS = 1e-6


class Res:
    __slots__ = ("w", "r")

    def __init__(self):
        self.w = {}
        self.r = {}


class Prog:
    def __init__(self):
        self.nc = bass.Bass("TRN2", target_bir_lowering=False)
        self.es = ExitStack()
        nc = self.nc
        self.eng = {"pe": nc.tensor, "act": nc.scalar, "dve": nc.vector,
                    "pool": nc.gpsimd, "sp": nc.sync}
        self.sems = {}
        self.cnt = {}
        for e in self.eng:
            self.sems[e] = self.es.enter_context(nc.semaphore("s_" + e))
            self.cnt[e] = 0
        self.known = {e: {} for e in self.eng}
        self.res = {}
        self.nd = 0

    def sb(self, name, shape, dt):
        return self.es.enter_context(self.nc.sbuf_tensor(name, list(shape), dt))

    def ps(self, name, shape, dt):
        return self.es.enter_context(self.nc.psum_tensor(name, list(shape), dt))

    def din(self, name, shape, dt):
        return self.nc.dram_tensor(name, list(shape), dt, kind="ExternalInput").ap()

    def dout(self, name, shape, dt):
        return self.nc.dram_tensor(name, list(shape), dt, kind="ExternalOutput").ap()

    def dtmp(self, name, shape, dt):
        return self.nc.dram_tensor(name, list(shape), dt, kind="Internal").ap()

    def R(self, key):
        r = self.res.get(key)
        if r is None:
            r = self.res[key] = Res()
        return r

    def _wait(self, e, semkey, val):
        if val <= 0:
            return
        k = self.known[e]
        if k.get(semkey, 0) >= val:
            return
        k[semkey] = val
        if getattr(self, "_pend", None) is not None:
            self._pend.append((semkey, val))
        else:
            self.eng[e].wait_ge(self.sems[semkey], val)

    def _deps(self, e, reads, writes, skip=None):
        for key in reads:
            for sk, v in self.R(key).w.items():
                if e == "pe" and sk == "pe":
                    continue
                self._wait(e, sk, v)
        for key in writes:
            r = self.R(key)
            for sk, v in r.w.items():
                if sk == skip or (e == "pe" and sk == "pe"):
                    continue
                self._wait(e, sk, v)
            for sk, v in r.r.items():
                if e == "pe" and sk == "pe":
                    continue
                self._wait(e, sk, v)

    def op(self, e, fn, reads=(), writes=()):
        self._pend = []
        self._deps(e, reads, writes)
        pend, self._pend = self._pend, None
        for (sk, v) in pend[:-1]:
            self.eng[e].wait_ge(self.sems[sk], v)
        ins = fn()
        if pend:
            ins._wait_ge(self.sems[pend[-1][0]], pend[-1][1])
        self.cnt[e] += 1
        c = self.cnt[e]
        ins.then_inc(self.sems[e], 1)
        for key in reads:
            self.R(key).r[e] = c
        for key in writes:
            r = self.R(key)
            r.w = {e: c}
            r.r = {}
        return ins

    def _dma_post(self, ins, sk, reads, writes):
        self.cnt[sk] += 16
        c = self.cnt[sk]
        ins.then_inc(self.sems[sk], 16)
        for key in reads:
            self.R(key).r[sk] = c
        for key in writes:
            r = self.R(key)
            if sk in r.w and len(r.w) == 1:
                r.w[sk] = c
            else:
                r.w = {sk: c}
            r.r = {}

    def _dsem(self, semkey, writes):
        sk = semkey if semkey is not None else ("d", writes[0])
        if sk not in self.sems:
            self.sems[sk] = self.es.enter_context(self.nc.semaphore("sd%d" % self.nd))
            self.nd += 1
            self.cnt[sk] = 0
        return sk

    def dma(self, q, out, in_, reads=(), writes=(), semkey=None, **kw):
        sk = self._dsem(semkey, writes)
        self._deps(q, reads, writes, skip=sk)
        ins = self.eng[q].dma_start(out=out, in_=in_, **kw)
        self._dma_post(ins, sk, reads, writes)
        return ins

    def idma(self, out, out_off, in_, in_off, reads=(), writes=(), semkey=None, **kw):
        sk = self._dsem(semkey, writes)
        self._deps("pool", reads, writes, skip=sk)
        ins = self.nc.gpsimd.indirect_dma_start(out, out_off, in_, in_off, **kw)
        self._dma_post(ins, sk, reads, writes)
        return ins

    def finish(self, out_keys, e="sp"):
        for key in out_keys:
            for sk, v in self.R(key).w.items():
                self._wait(e, sk, v)
        for o in self.eng:
            if o != e:
                self._wait(e, o, self.cnt[o])
        self.es.close()
        return self.nc


class CastLoader:
    def __init__(self, P, nelem, nbuf=4, engines=("pool", "dve", "pool")):
        self.P = P
        self.n = nelem
        self.stg = [P.sb("cstg%d" % i, [128, nelem], F32) for i in range(nbuf)]
        self.i = 0
        self.engines = engines
        self.fifo = []

    def load(self, dst_ap, dst_key, src_ap, shape):
        P, nc = self.P, self.P.nc
        k = self.i % len(self.stg)
        e = self.engines[self.i % len(self.engines)]
        self.i += 1
        a, b = shape
        sv = self.stg[k].rearrange("p (a b) -> p a b", a=a)
        P.dma("sp", sv, src_ap, writes=[("cstg", k)])
        if e == "pool":
            P.op("pool", lambda: nc.gpsimd.tensor_copy(out=dst_ap, in_=sv), reads=[("cstg", k)], writes=[dst_key])
        else:
            P.op("dve", lambda: nc.vector.tensor_copy(out=dst_ap, in_=sv), reads=[("cstg", k)], writes=[dst_key])

    def enqueue(self, *args):
        self.fifo.append(args)

    def pump(self, n=1):
        for _ in range(n):
            if not self.fifo:
                return
            self.load(*self.fifo.pop(0))

    def flush(self):
        self.pump(len(self.fifo))


def make_ident(P, dt=BF16, name="ident"):
    nc = P.nc
    idf = P.sb(name + "f", [128, 128], F32)
    P.op("pool", lambda: nc.gpsimd.memset(idf[:], 1.0), writes=[name + "f"])
    P.op("pool", lambda: nc.gpsimd.affine_select(out=idf[:], in_=idf[:], pattern=[[-1, 128]],
                                                 compare_op=ALU.is_equal, fill=0.0, base=0,
                                                 channel_multiplier=1),
         reads=[name + "f"], writes=[name + "f"])
    if dt == F32:
        return idf, name + "f"
    idb = P.sb(name, [128, 128], dt)
    P.op("dve", lambda: nc.vector.tensor_copy(out=idb[:], in_=idf[:]), reads=[name + "f"], writes=[name])
    return idb, name


def bc128(v):
    v = np.asarray(v, dtype=np.float32).reshape(1, -1)
    return np.ascontiguousarray(np.broadcast_to(v, (128, v.shape[1])))


MC = 6 * D // NCORES


def build_mod():
    P = Prog()
    nc = P.nc
    c1 = P.din("c1", [128, 16], F32)
    c2 = P.din("c2", [128, 16], F32)
    wm = P.din("wm", [2, D, MC], F32)
    bm = P.din("bm", [2, 2, MC], F32)
    mo = P.dout("mo", [2, 2, MC], F32)
    c1s = P.sb("c1s", [128, 16], F32)
    c2s = P.sb("c2s", [128, 16], F32)
    cc = P.sb("cc", [128, 16, 2], F32)
    bs = P.sb("bs", [2, 2, MC], F32)
    ms = P.sb("ms", [2, 2, MC], F32)
    HC = MC // 2
    wb = [P.sb("wmb%d" % i, [128, 16, HC], F32) for i in range(2)]
    pz = [P.ps("pz%d" % i, [2, 384], F32) for i in range(2)]
    P.dma("sp", c1s[:], c1[:, :], writes=["c1s"])
    P.dma("sp", c2s[:], c2[:, :], writes=["c2s"])
    P.dma("sp", bs[:], bm.rearrange("l s n -> s l n"), writes=["bs"])
    P.op("act", lambda: nc.scalar.activation(out=cc[:, :, 0], in_=c1s[:], func=AF.Silu), reads=["c1s"], writes=["cc0"])
    P.op("act", lambda: nc.scalar.activation(out=cc[:, :, 1], in_=c2s[:], func=AF.Silu), reads=["c2s"], writes=["cc1"])
    ci = 0
    for l in range(2):
        wv = wm[l].rearrange("(p kc) n -> p kc n", kc=16)
        for hf in range(2):
            b = ci % 2
            q = "sp" if ci % 2 == 0 else "act"
            for kq in range(4):
                P.dma(q, wb[b][:, kq * 4:(kq + 1) * 4, :], wv[:, kq * 4:(kq + 1) * 4, hf * HC:(hf + 1) * HC], writes=[("wmb", b)])
            for nb in range(HC // 384):
                pb = (ci * 2 + nb) % 2
                for kc in range(16):
                    P.op("pe", lambda kc=kc: nc.tensor.matmul(pz[pb][:], lhsT=cc[:, kc, :], rhs=wb[b][:, kc, nb * 384:(nb + 1) * 384],
                                                               start=(kc == 0), stop=(kc == 15)),
                         reads=["cc0", "cc1", ("wmb", b)], writes=[("pz", pb)])
                n0 = hf * HC + nb * 384
                P.op("dve", lambda: nc.vector.tensor_tensor(out=ms[:, l, n0:n0 + 384], in0=pz[pb][:], in1=bs[:, l, n0:n0 + 384], op=ALU.add),
                     reads=[("pz", pb), "bs"], writes=["ms"])
            ci += 1
    P.dma("sp", mo.rearrange("l s n -> s l n"), ms[:], reads=["ms"], writes=["mo"])
    return P.finish(["mo"])


def run_mod(inp):
    nc = build_mod()
    c1 = np.ascontiguousarray(inp["c"].reshape(128, 16))
    c2 = np.ascontiguousarray(inp["c_ctx"].reshape(128, 16))
    maps = []
    for i in range(NCORES):
        wm = np.ascontiguousarray(inp["w_mod"][:, :, i * MC:(i + 1) * MC])
        bm = np.ascontiguousarray(np.broadcast_to(inp["b_mod"][:, None, i * MC:(i + 1) * MC], (2, 2, MC)))
        maps.append({"c1": c1, "c2": c2, "wm": wm, "bm": bm})
    res = run_bass_kernel_spmd(nc, maps, core_ids=list(range(NCORES)))
    m = np.concatenate([r["mo"] for r in res.results], axis=2)
    return m


NTK = TPC + 32
INC = 5120


def rms_modulate_tile(P, nc, xs, xkey, rows, ss, sskey, epsb, gam, sh, hb, hbkey):
    P.op("act", lambda: nc.scalar.activation(out=hb[:rows, :], in_=xs[:rows, :], func=AF.Square, accum_out=ss[:rows, 0:1]),
         reads=[xkey], writes=[hbkey, sskey])
    P.op("act", lambda: nc.scalar.activation(out=ss[:rows, 1:2], in_=ss[:rows, 0:1], func=AF.Sqrt, scale=1.0 / D, bias=epsb[:rows, 0:1]),
         reads=[sskey, "epsb"], writes=[sskey])
    P.op("dve", lambda: nc.vector.reciprocal(out=ss[:rows, 2:3], in_=ss[:rows, 1:2]), reads=[sskey], writes=[sskey])
    P.op("dve", lambda: nc.vector.scalar_tensor_tensor(out=xs[:rows, :], in0=xs[:rows, :], scalar=ss[:rows, 2:3], in1=gam[:rows, :],
                                                       op0=ALU.mult, op1=ALU.mult),
         reads=[xkey, sskey, "gam"], writes=[xkey])
    P.op("pool", lambda: nc.gpsimd.tensor_tensor(out=hb[:rows, :], in0=xs[:rows, :], in1=sh[:rows, :], op=ALU.add),
         reads=[xkey, "sh"], writes=[hbkey])


def build_A():
    P = Prog()
    nc = P.nc
    x = P.din("x", [TPC, D], F32)
    cx = P.din("cx", [32, D], F32)
    gmix = P.din("gmix", [128, D], F32)
    cm = P.din("cm", [128, D], F32)
    sm = P.din("sm", [128, D], F32)
    cmc = P.din("cmc", [128, D], F32)
    smc = P.din("smc", [128, D], F32)
    w_in = P.din("w_in", [D, INC], F32)
    cosd = P.din("cosE", [TPC, 256], F32)
    sind = P.din("sinE", [TPC, 256], F32)
    lngd = P.din("lng", [128, 1024], F32)
    lnbd = P.din("lnb", [128, 1024], F32)
    wsTd = P.din("wsT", [8, 128, 128], F32)
    bsbd = P.din("bsb", [128, 1024], F32)
    qT = P.dout("qT", [8, 128, TPC], BF16)
    kT = P.dout("kT", [8, 128, NTK], BF16)
    vo = P.dout("v", [NTK, 1024], BF16)
    gTo = P.dout("gT", [8, 128, TPC], BF16)

    ident, idk = make_ident(P)
    xs = [P.sb("xs%d" % i, [128, D], F32) for i in range(2)]
    gam = P.sb("gam", [128, D], F32)
    sh = P.sb("sh", [128, D], F32)
    hb = [P.sb("hb%d" % i, [128, D], BF16) for i in range(2)]
    hT = P.sb("hT", [128, 16, NTK], BF16)
    wblk = [P.sb("wblk%d" % i, [128, 16, 512], BF16) for i in range(2)]
    big = P.sb("big", [128, 4096], F32)
    cosE = big[:, 0:2048].rearrange("p (t f) -> p t f", t=8)
    sinE = big[:, 2048:4096].rearrange("p (t f) -> p t f", t=8)
    uT = big.bitcast(BF16).rearrange("p (g t) -> p g t", g=8)
    lng = P.sb("lngs", [128, 1024], F32)
    lnb = P.sb("lnbs", [128, 1024], F32)
    wsTf = P.sb("wsTf", [128, 8, 128], F32)
    wsTb = P.sb("wsTb", [128, 8, 128], BF16)
    bsb = P.sb("bsbs", [128, 8, 128], F32)
    stg = P.sb("stg", [128, 4, NTK], BF16)
    zf = [P.sb("zf%d" % i, [128, 512], F32) for i in range(2)]
    zsqs = [P.sb("zsq%d" % i, [128, 512], F32) for i in range(2)]
    rt = [P.sb("rt%d" % i, [128, 256], F32) for i in range(8)]
    qr = [P.sb("qr%d" % i, [128, 512], BF16) for i in range(2)]
    vs = [P.sb("vs%d" % i, [128, 512], BF16) for i in range(2)]
    vlfs = [P.sb("vlf%d" % i, [128, 512], F32) for i in range(2)]
    vlb = [P.sb("vlb%d" % i, [128, 512], BF16) for i in range(2)]
    tt = [P.sb("tt%d" % i, [128, 128], F32) for i in range(2)]
    ss = [P.sb("ss%d" % i, [128, 4], F32) for i in range(2)]
    st = [P.sb("st%d" % i, [128, 24], F32) for i in range(2)]
    epsb = P.sb("epsb", [128, 1], F32)
    pT = P.ps("pT", [128, D], BF16)
    pz = [P.ps("pz%d" % i, [128, 512], F32) for i in range(2)]
    pq = [P.ps("pq%d" % i, [128, 512], BF16) for i in range(2)]
    pm = [P.ps("pm%d" % i, [128, 128], F32) for i in range(2)]

    P.op("pool", lambda: nc.gpsimd.memset(epsb[:], EPS), writes=["epsb"])

    def load_mod(cmd, smd):
        P.dma("sp", gam[:], cmd[:, :], writes=["gam"])
        P.dma("act", xs[1][:], gmix[:, :], writes=[("xs", 1)])
        P.dma("sp", sh[:], smd[:, :], writes=["sh"])
        P.op("dve", lambda: nc.vector.scalar_tensor_tensor(out=gam[:], in0=gam[:], scalar=1.0, in1=xs[1][:], op0=ALU.add, op1=ALU.mult),
             reads=["gam", ("xs", 1)], writes=["gam"])

    def front_tile(src_ap, rows, tok0, b):
        P.dma("sp", xs[b][:rows, :], src_ap, writes=[("xs", b)])
        rms_modulate_tile(P, nc, xs[b], ("xs", b), rows, ss[b], ("ss", b), epsb, gam, sh, hb[b], ("hb", b))
        for kc in range(16):
            P.op("pe", lambda kc=kc: nc.tensor.transpose(pT[:, kc * 128:kc * 128 + rows], hb[b][:rows, kc * 128:(kc + 1) * 128], ident[:rows, :rows]),
                 reads=[("hb", b), idk], writes=["pT"])
        P.op("act", lambda: nc.scalar.copy(out=hT[:, :, tok0:tok0 + rows],
                                           in_=pT.rearrange("p (k t) -> p k t", k=16)[:, :, 0:rows]),
             reads=["pT"], writes=["hT"])

    load_mod(cmc, smc)
    front_tile(cx[:, :], 32, TPC, 0)
    load_mod(cm, sm)
    for ti in range(NT):
        front_tile(x[ti * 128:(ti + 1) * 128, :], 128, ti * 128, ti % 2)

    P.dma("sp", big[:, 0:2048].rearrange("p (t f) -> p t f", t=8), cosd.rearrange("(t p) f -> p t f", p=128), writes=["big"])
    P.dma("sp", big[:, 2048:4096].rearrange("p (t f) -> p t f", t=8), sind.rearrange("(t p) f -> p t f", p=128), writes=["big"])
    P.dma("act", lng[:], lngd[:, :], writes=["lng"])
    P.dma("act", lnb[:], lnbd[:, :], writes=["lnb"])
    P.dma("act", wsTf[:], wsTd.rearrange("g q p -> q g p"), writes=["wsTf"])
    P.dma("act", bsb[:], bsbd.rearrange("q (g p) -> q g p", g=8), writes=["bsb"])
    P.op("dve", lambda: nc.vector.tensor_copy(out=wsTb[:], in_=wsTf[:]), reads=["wsTf"], writes=["wsTb"])

    wv = w_in.rearrange("(kc p) n -> p kc n", p=128)
    CL = CastLoader(P, 1024, engines=("dve",))
    zc = [0]

    def proj_tok(b, tok0, rows):
        j = zc[0] % 2
        zc[0] += 1
        for kc in range(16):
            P.op("pe", lambda kc=kc: nc.tensor.matmul(pz[j][:rows, :], lhsT=hT[:, kc, tok0:tok0 + rows], rhs=wblk[b][:, kc, :],
                                                       start=(kc == 0), stop=(kc == 15)),
                 reads=["hT", ("wblk", b)], writes=[("pz", j)])
        CL.pump(1)
        return j

    def queue_block(cb):
        b = cb % 2
        for kq in range(8):
            CL.enqueue(wblk[b][:, kq * 2:(kq + 1) * 2, :], ("wblk", b), wv[:, kq * 2:(kq + 1) * 2, cb * 512:(cb + 1) * 512], (2, 512))

    queue_block(0)
    CL.flush()
    for cb in range(10):
        b = cb % 2
        CL.flush()
        if cb + 1 < 10:
            queue_block(cb + 1)
        if cb < 4:
            isk = cb >= 2
            jn = proj_tok(b, 0, 128)
            for ti in range(NT):
                j = jn
                zv = pz[j].rearrange("p (a h x f) -> p a h x f", a=8, h=2, x=2, f=16)
                x1 = zv[:, :, :, 0, :]
                x2 = zv[:, :, :, 1, :]
                cv = cosE[:, ti, :].rearrange("p (a h f) -> p a h f", a=8, h=2)
                sv = sinE[:, ti, :].rearrange("p (a h f) -> p a h f", a=8, h=2)
                ro = (ti % 2) * 4
                r4 = [t.rearrange("p (a h f) -> p a h f", a=8, h=2) for t in rt[ro:ro + 4]]
                rk_ = ["rt%d" % (ro + i) for i in range(4)]
                qv = qr[j].rearrange("p (a h x f) -> p a h x f", a=8, h=2, x=2, f=16)
                zk = ("pz", j)
                P.op("dve", lambda: nc.vector.tensor_tensor(out=r4[0], in0=x1, in1=cv, op=ALU.mult), reads=[zk, "big"], writes=[rk_[0]])
                P.op("dve", lambda: nc.vector.tensor_tensor(out=r4[1], in0=x2, in1=sv, op=ALU.mult), reads=[zk, "big"], writes=[rk_[1]])
                P.op("dve", lambda: nc.vector.tensor_tensor(out=r4[2], in0=x2, in1=cv, op=ALU.mult), reads=[zk, "big"], writes=[rk_[2]])
                P.op("dve", lambda: nc.vector.tensor_tensor(out=r4[3], in0=x1, in1=sv, op=ALU.mult), reads=[zk, "big"], writes=[rk_[3]])
                P.op("dve", lambda: nc.vector.tensor_tensor(out=qv[:, :, :, 0, :], in0=r4[0], in1=r4[1], op=ALU.subtract),
                     reads=[rk_[0], rk_[1]], writes=[("qr", j)])
                P.op("dve", lambda: nc.vector.tensor_tensor(out=qv[:, :, :, 1, :], in0=r4[2], in1=r4[3], op=ALU.add),
                     reads=[rk_[2], rk_[3]], writes=[("qr", j)])
                if ti + 1 < NT:
                    jn = proj_tok(b, (ti + 1) * 128, 128)
                for s in range(4):
                    P.op("pe", lambda s=s: nc.tensor.transpose(pq[j][:, s * 128:(s + 1) * 128], qr[j][:, s * 128:(s + 1) * 128], ident[:]),
                         reads=[("qr", j), idk], writes=[("pq", j)])
                P.op("act", lambda: nc.scalar.copy(out=stg[:, :, ti * 128:(ti + 1) * 128], in_=pq[j].rearrange("p (s t) -> p s t", s=4)),
                     reads=[("pq", j)], writes=["stg"])
            if isk:
                j = proj_tok(b, TPC, 32)
                P.op("act", lambda: nc.scalar.copy(out=qr[j][:32, :], in_=pz[j][:32, :]), reads=[("pz", j)], writes=[("qr", j)])
                for s in range(4):
                    P.op("pe", lambda s=s: nc.tensor.transpose(pq[j][:, s * 128:s * 128 + 32], qr[j][:32, s * 128:(s + 1) * 128], ident[:32, :32]),
                         reads=[("qr", j), idk], writes=[("pq", j)])
                P.op("act", lambda: nc.scalar.copy(out=stg[:, :, TPC:TPC + 32], in_=pq[j].rearrange("p (s t) -> p s t", s=4)[:, :, 0:32]),
                     reads=[("pq", j)], writes=["stg"])
            h0 = (cb % 2) * 4
            if isk:
                P.dma("act", kT[h0:h0 + 4].rearrange("h d t -> d h t"), stg[:], reads=["stg"], writes=["kT"])
            else:
                P.dma("act", qT[h0:h0 + 4].rearrange("h d t -> d h t"), stg[:, :, 0:TPC], reads=["stg"], writes=["qT"])
        elif cb < 6:
            for ti in range(NT + 1):
                rows = 128 if ti < NT else 32
                tok0 = ti * 128
                j = proj_tok(b, tok0, rows)
                P.op("act", lambda: nc.scalar.copy(out=vs[j][:rows, :], in_=pz[j][:rows, :]), reads=[("pz", j)], writes=[("vs", j)])
                P.dma("act", vo[tok0:tok0 + rows, (cb - 4) * 512:(cb - 3) * 512], vs[j][:rows, :], reads=[("vs", j)], writes=["vo"])
        elif cb < 8:
            for sub in range(4):
                G = (cb - 6) * 4 + sub
                for tg in range(2):
                    j = zc[0] % 2
                    zc[0] += 1
                    for kc in range(16):
                        P.op("pe", lambda kc=kc: nc.tensor.matmul(pz[j][:], lhsT=wblk[b][:, kc, sub * 128:(sub + 1) * 128],
                                                                   rhs=hT[:, kc, tg * 512:(tg + 1) * 512],
                                                                   start=(kc == 0), stop=(kc == 15)),
                             reads=["hT", ("wblk", b)], writes=[("pz", j)])
                    P.op("act", lambda: nc.scalar.activation(out=uT[:, G, tg * 512:(tg + 1) * 512], in_=pz[j][:], func=AF.Gelu_apprx_tanh),
                         reads=[("pz", j)], writes=["big"])
                    CL.pump(1)
        else:
            jn = proj_tok(b, 0, 128)
            for ti in range(NT):
                tok0 = ti * 128
                j = jn
                zsq = zsqs[ti % 2]
                zsqk = ("zsq", ti % 2)
                vlf = vlfs[ti % 2]
                vlfk = ("vlf", ti % 2)
                z = zf[j]
                zk = ("zf", j)
                s_ = st[j]
                sk = ("st", j)
                P.op("act", lambda: nc.scalar.activation(out=z[:], in_=pz[j][:], func=AF.Gelu_apprx_tanh), reads=[("pz", j)], writes=[zk])
                P.op("dve", lambda: nc.vector.reduce_sum(out=s_[:, 0:4], in_=z.rearrange("p (g d) -> p g d", g=4), axis=AX.X),
                     reads=[zk], writes=[sk])
                P.op("act", lambda: nc.scalar.activation(out=zsq[:], in_=z[:], func=AF.Square), reads=[zk], writes=[zsqk])
                P.op("dve", lambda: nc.vector.reduce_sum(out=s_[:, 4:8], in_=zsq.rearrange("p (g d) -> p g d", g=4), axis=AX.X),
                     reads=[zsqk], writes=[sk])
                P.op("dve", lambda: nc.vector.tensor_scalar(out=s_[:, 8:12], in0=s_[:, 0:4], scalar1=1.0 / 128, scalar2=None, op0=ALU.mult),
                     reads=[sk], writes=[sk])
                P.op("dve", lambda: nc.vector.tensor_tensor(out=s_[:, 12:16], in0=s_[:, 8:12], in1=s_[:, 8:12], op=ALU.mult),
                     reads=[sk], writes=[sk])
                P.op("dve", lambda: nc.vector.scalar_tensor_tensor(out=s_[:, 16:20], in0=s_[:, 4:8], scalar=1.0 / 128, in1=s_[:, 12:16],
                                                                   op0=ALU.mult, op1=ALU.subtract),
                     reads=[sk], writes=[sk])
                P.op("act", lambda: nc.scalar.activation(out=s_[:, 20:24], in_=s_[:, 16:20], func=AF.Sqrt, bias=epsb[:, 0:1]),
                     reads=[sk, "epsb"], writes=[sk])
                P.op("dve", lambda: nc.vector.reciprocal(out=s_[:, 20:24], in_=s_[:, 20:24]), reads=[sk], writes=[sk])
                for g in range(4):
                    P.op("dve", lambda g=g: nc.vector.tensor_scalar(out=vlf[:, g * 128:(g + 1) * 128], in0=z[:, g * 128:(g + 1) * 128],
                                                                    scalar1=s_[:, 8 + g:9 + g], scalar2=s_[:, 20 + g:21 + g],
                                                                    op0=ALU.subtract, op1=ALU.mult),
                         reads=[zk, sk], writes=[vlfk])
                c0 = (cb - 8) * 512
                P.op("dve", lambda: nc.vector.tensor_tensor(out=vlf[:], in0=vlf[:], in1=lng[:, c0:c0 + 512], op=ALU.mult),
                     reads=[vlfk, "lng"], writes=[vlfk])
                P.op("dve", lambda: nc.vector.tensor_tensor(out=vlb[j][:], in0=vlf[:], in1=lnb[:, c0:c0 + 512], op=ALU.add),
                     reads=[vlfk, "lnb"], writes=[("vlb", j)])
                if ti + 1 < NT:
                    jn = proj_tok(b, (ti + 1) * 128, 128)
                for g in range(4):
                    G = (cb - 8) * 4 + g
                    m = (ti * 4 + g) % 2
                    P.op("pe", lambda g=g, G=G, m=m: nc.tensor.matmul(pm[m][:], lhsT=vlb[j][:, g * 128:(g + 1) * 128], rhs=wsTb[:, G, :],
                                                                      start=True, stop=True),
                         reads=[("vlb", j), "wsTb"], writes=[("pm", m)])
                    P.op("dve", lambda G=G, m=m: nc.vector.tensor_tensor(out=tt[m][:], in0=pm[m][:], in1=bsb[:, G, :], op=ALU.add),
                         reads=[("pm", m), "bsb"], writes=[("tt", m)])
                    P.op("dve", lambda g=g, G=G, m=m: nc.vector.tensor_tensor(out=stg[:, g, tok0:tok0 + 128], in0=tt[m][:],
                                                                              in1=uT[:, G, tok0:tok0 + 128], op=ALU.mult),
                         reads=[("tt", m), "big"], writes=["stg"])
            g0 = (cb - 8) * 4
            P.dma("pool", gTo[g0:g0 + 4].rearrange("g d t -> d g t"), stg[:, :, 0:TPC], reads=["stg"], writes=["gTo"])
    return P.finish(["qT", "kT", "vo", "gTo"])


def rope_tables():
    t = np.arange(SEQ)
    row = (t // 64).astype(np.float32)
    col = (t % 64).astype(np.float32)
    inv = (10000.0 ** (-np.arange(0, 32, 2, dtype=np.float32) / 32)).astype(np.float32)
    ang = np.stack([row[:, None] * inv[None, :], col[:, None] * inv[None, :]], axis=1)
    cosE = np.tile(np.cos(ang).astype(np.float32).reshape(SEQ, 1, 32), (1, 8, 1)).reshape(SEQ, 256)
    sinE = np.tile(np.sin(ang).astype(np.float32).reshape(SEQ, 1, 32), (1, 8, 1)).reshape(SEQ, 256)
    return np.ascontiguousarray(cosE), np.ascontiguousarray(sinE)


def run_A(inp, m):
    nc = build_A()
    cosE, sinE = rope_tables()
    m0 = m[0]
    sm_, cm_ = m0[0, 0:D], m0[0, D:2 * D]
    smc_, cmc_ = m0[1, 0:D], m0[1, D:2 * D]
    common = {
        "gmix": bc128(inp["g_norm_mix"][0]), "cm": bc128(cm_), "sm": bc128(sm_), "cmc": bc128(cmc_), "smc": bc128(smc_),
        "w_in": np.ascontiguousarray(inp["w_in"][0]),
        "lng": bc128(inp["sgu_ln_g"][0]), "lnb": bc128(inp["sgu_ln_b"][0]),
        "wsT": np.ascontiguousarray(np.transpose(inp["w_spatial"][0], (0, 2, 1))),
        "bsb": bc128(inp["b_spatial"][0].reshape(-1)),
    }
    maps = []
    for i in range(NCORES):
        d = dict(common)
        d["x"] = np.ascontiguousarray(inp["x"][0, i * TPC:(i + 1) * TPC])
        d["cx"] = np.ascontiguousarray(inp["ctx"][0, i * 32:(i + 1) * 32])
        d["cosE"] = cosE[i * TPC:(i + 1) * TPC]
        d["sinE"] = sinE[i * TPC:(i + 1) * TPC]
        maps.append(d)
    res = run_bass_kernel_spmd(nc, maps, core_ids=list(range(NCORES)))
    return [r for r in res.results]


NKEY = SEQ + 256
NKT = NKEY // 128
LAM_INIT0 = 0.8 - 0.6 * math.exp(-0.3 * 0)


def build_B():
    P = Prog()
    nc = P.nc
    qTd = P.din("qT", [128, SEQ], BF16)
    kTd = P.din("kT", [128, NKEY], BF16)
    vd = P.din("v", [NKEY, 128], BF16)
    lamd = P.din("lamv", [128, 4, 64], F32)
    gsd = P.din("gsub", [128, 1], F32)
    aTo = P.dout("aT", [128, SEQ], BF16)

    qz = [P.sb("qz%d" % i, [128, SEQ], BF16) for i in range(2)]
    kT = P.sb("kTs", [128, NKEY], BF16)
    vs = P.sb("vsb", [128, NKT, 128], BF16)
    lamv = P.sb("lamvs", [128, 4, 64], F32)
    lt = P.sb("lt", [128, 2, 64], F32)
    ls = P.sb("ls", [128, 8], F32)
    gs = P.sb("gs", [128, 1], F32)
    epsb = P.sb("epsb", [128, 1], F32)
    onesb = P.sb("onesb", [128, 128], BF16)
    onesf = P.sb("onesf", [128, 128], F32)
    pt = [P.sb("pt%d" % i, [128, 512], BF16) for i in range(3)]
    rz = [P.sb("rz%d" % i, [128, 512], F32) for i in range(2)]
    o = P.sb("o", [128, 512], F32)
    t1 = P.sb("t1", [128, 512], F32)
    osq = P.sb("osq", [128, 512], F32)
    rstd = P.sb("rstd", [128, 512], F32)
    ab = [P.sb("ab%d" % i, [128, 512], BF16) for i in range(2)]
    ps = [P.ps("ps%d" % i, [128, 512], F32) for i in range(3)]
    po = [P.ps("po%d" % i, [128, 512], F32) for i in range(2)]
    pzz = [P.ps("pzz%d" % i, [128, 512], F32) for i in range(2)]
    pr = P.ps("pr", [128, 512], F32)
    zacc = [P.sb("zacc%d" % i, [128, 512], F32) for i in range(2)]
    zs = [P.sb("zs%d" % i, [128, 512], F32) for i in range(2)]
    os_ = [P.sb("os%d" % i, [128, 512], F32) for i in range(2)]
    ones1 = P.sb("ones1", [128, 128], F32)

    for i in range(4):
        q = "sp" if i % 2 == 0 else "act"
        P.dma(q, qz[0][0:64, i * 2048:(i + 1) * 2048], qTd[0:64, i * 2048:(i + 1) * 2048], writes=["qT"])
        P.dma(q, qz[1][64:128, i * 2048:(i + 1) * 2048], qTd[64:128, i * 2048:(i + 1) * 2048], writes=["qT"])
        P.dma(q, kT[:, i * 2112:(i + 1) * 2112], kTd[:, i * 2112:(i + 1) * 2112], writes=["kT"])
    P.dma("sp", vs[:, 0:33, :], vd[0:33 * 128, :].rearrange("(t p) e -> p t e", p=128), writes=["vs"])
    P.dma("act", vs[:, 33:66, :], vd[33 * 128:, :].rearrange("(t p) e -> p t e", p=128), writes=["vs"])
    P.dma("sp", lamv[:], lamd[:, :, :], writes=["lamv"])
    P.dma("sp", gs[:], gsd[:, :], writes=["gs"])
    P.op("pool", lambda: nc.gpsimd.memset(epsb[:], EPS), writes=["epsb"])
    P.op("pool", lambda: nc.gpsimd.memset(onesb[:], 1.0), writes=["onesb"])
    P.op("pool", lambda: nc.gpsimd.memset(onesf[:], 1.0 / 128), writes=["onesf"])
    P.op("pool", lambda: nc.gpsimd.memset(ones1[:], 1.0), writes=["ones1"])
    P.op("pool", lambda: nc.gpsimd.memset(qz[0][64:128, :], 0.0), writes=["qz0pad"])
    P.op("dve", lambda: nc.vector.memset(qz[1][0:64, :], 0.0), writes=["qz1pad"])
    P.op("dve", lambda: nc.vector.tensor_tensor(out=lt[:, 0, :], in0=lamv[:, 0, :], in1=lamv[:, 1, :], op=ALU.mult), reads=["lamv"], writes=["lt"])
    P.op("dve", lambda: nc.vector.tensor_tensor(out=lt[:, 1, :], in0=lamv[:, 2, :], in1=lamv[:, 3, :], op=ALU.mult), reads=["lamv", "lt"], writes=["lt"])
    P.op("dve", lambda: nc.vector.reduce_sum(out=ls[:, 0:2], in_=lt[:], axis=AX.X), reads=["lt"], writes=["ls"])
    P.op("act", lambda: nc.scalar.activation(out=ls[:, 2:4], in_=ls[:, 0:2], func=AF.Exp), reads=["ls"], writes=["ls"])
    P.op("dve", lambda: nc.vector.tensor_tensor(out=ls[:, 4:5], in0=ls[:, 3:4], in1=ls[:, 2:3], op=ALU.subtract), reads=["ls"], writes=["ls"])
    P.op("dve", lambda: nc.vector.tensor_scalar(out=ls[:, 4:5], in0=ls[:, 4:5], scalar1=-LAM_INIT0, scalar2=None, op0=ALU.add), reads=["ls"], writes=["ls"])
    P.op("dve", lambda: nc.vector.tensor_scalar(out=gs[:], in0=gs[:], scalar1=1.0 - LAM_INIT0, scalar2=None, op0=ALU.mult), reads=["gs"], writes=["gs"])

    def epilogue(qb):
        q0 = qb * 512
        for c in range(2):
            P.op("dve", lambda c=c: nc.vector.reciprocal(out=rz[c][:], in_=zs[c][:]), reads=[("zs", c)], writes=[("rz", c)])
        P.op("dve", lambda: nc.vector.tensor_tensor(out=o[:], in0=os_[0][:], in1=rz[0][:], op=ALU.mult), reads=[("os", 0), ("rz", 0)], writes=["o"])
        P.op("dve", lambda: nc.vector.tensor_tensor(out=t1[:], in0=os_[1][:], in1=rz[1][:], op=ALU.mult), reads=[("os", 1), ("rz", 1)], writes=["t1"])
        P.op("dve", lambda: nc.vector.scalar_tensor_tensor(out=o[:], in0=t1[:], scalar=ls[:, 4:5], in1=o[:], op0=ALU.mult, op1=ALU.add),
             reads=["t1", "ls", "o"], writes=["o"])
        P.op("dve", lambda: nc.vector.tensor_tensor(out=osq[:], in0=o[:], in1=o[:], op=ALU.mult), reads=["o"], writes=["osq"])
        P.op("pe", lambda: nc.tensor.matmul(pr[:], lhsT=onesf[:], rhs=osq[:], start=True, stop=True), reads=["onesf", "osq"], writes=["pr"])
        P.op("act", lambda: nc.scalar.activation(out=rstd[:], in_=pr[:], func=AF.Sqrt, bias=epsb[:, 0:1]), reads=["pr", "epsb"], writes=["rstd"])
        P.op("dve", lambda: nc.vector.reciprocal(out=rstd[:], in_=rstd[:]), reads=["rstd"], writes=["rstd"])
        a = ab[qb % 2]
        P.op("dve", lambda: nc.vector.scalar_tensor_tensor(out=a[:], in0=o[:], scalar=gs[:, 0:1], in1=rstd[:], op0=ALU.mult, op1=ALU.mult),
             reads=["o", "gs", "rstd"], writes=[("ab", qb % 2)])
        P.dma("sp", aTo[:, q0:q0 + 512], a[:], reads=[("ab", qb % 2)], writes=["aTo"])

    it = 0
    NQB = SEQ // 512
    for qb in range(NQB):
        q0 = qb * 512
        steps = [(kt, c) for kt in range(NKT) for c in range(2)]

        def qk(i, kt, c):
            j = i % 3
            P.op("pe", lambda: nc.tensor.matmul(ps[j][:], lhsT=kT[:, kt * 128:(kt + 1) * 128],
                                                rhs=qz[c][:, q0:q0 + 512], start=True, stop=True),
                 reads=["qT", "kT", "qz0pad", "qz1pad"], writes=[("ps", j)])

        for pre in range(2):
            qk(it + pre, *steps[pre])
        for si, (kt, c) in enumerate(steps):
            j = it % 3
            P.op("act", lambda: nc.scalar.activation(out=pt[j][:], in_=ps[j][:], func=AF.Exp, scale=0.125),
                 reads=[("ps", j)], writes=[("pt", j)])
            if si + 2 < len(steps):
                qk(it + 2, *steps[si + 2])
            P.op("pe", lambda: nc.tensor.matmul(po[c][:], lhsT=vs[:, kt, :], rhs=pt[j][:], start=(kt == 0), stop=(kt == NKT - 1)),
                 reads=["vs", ("pt", j)], writes=[("po", c)])
            if kt % 2 == 0:
                P.op("pe", lambda: nc.tensor.matmul(pzz[c][:], lhsT=onesb[:], rhs=pt[j][:], start=(kt == 0), stop=False),
                     reads=["onesb", ("pt", j)], writes=[("pzz", c)])
            elif kt == 1:
                P.op("dve", lambda: nc.vector.tensor_copy(out=zacc[c][:], in_=pt[j][:]), reads=[("pt", j)], writes=[("zacc", c)])
            else:
                P.op("dve", lambda: nc.vector.tensor_tensor(out=zacc[c][:], in0=zacc[c][:], in1=pt[j][:], op=ALU.add),
                     reads=[("pt", j), ("zacc", c)], writes=[("zacc", c)])
            it += 1
            if si == 12 and qb > 0:
                epilogue(qb - 1)
        for c in range(2):
            P.op("pe", lambda c=c: nc.tensor.matmul(pzz[c][:], lhsT=ones1[:], rhs=zacc[c][:], start=False, stop=True),
                 reads=["ones1", ("zacc", c)], writes=[("pzz", c)])
        for c in range(2):
            P.op("dve", lambda c=c: nc.vector.tensor_copy(out=zs[c][:], in_=pzz[c][:]), reads=[("pzz", c)], writes=[("zs", c)])
            P.op("dve", lambda c=c: nc.vector.tensor_copy(out=os_[c][:], in_=po[c][:]), reads=[("po", c)], writes=[("os", c)])
    epilogue(NQB - 1)
    return P.finish(["aTo"])


def run_B(inp, ra):
    nc = build_B()
    lamv = np.stack([inp["lam_q1"][0], inp["lam_k1"][0], inp["lam_q2"][0], inp["lam_k2"][0]], 0)
    lamv = np.ascontiguousarray(np.broadcast_to(lamv[None], (128, 4, 64))).astype(np.float32)
    gsub = np.ascontiguousarray(inp["g_subln"][0].reshape(128, 1))
    maps = []
    for h in range(NCORES):
        qT = np.concatenate([r["qT"][h] for r in ra], axis=1)
        kT = np.concatenate([r["kT"][h][:, :TPC] for r in ra] + [r["kT"][h][:, TPC:] for r in ra], axis=1)
        v = np.concatenate([r["v"][:TPC, h * 128:(h + 1) * 128] for r in ra] + [r["v"][TPC:, h * 128:(h + 1) * 128] for r in ra], axis=0)
        maps.append({"qT": np.ascontiguousarray(qT), "kT": np.ascontiguousarray(kT), "v": np.ascontiguousarray(v),
                     "lamv": lamv, "gsub": gsub})
    res = run_bass_kernel_spmd(nc, maps, core_ids=list(range(NCORES)))
    return [r["aT"] for r in res.results]


def build_proj():
    P = Prog()
    nc = P.nc
    fTd = P.din("fT", [16, 128, TPC], BF16)
    Wd = P.din("W", [D, D], F32)
    xd = P.din("x", [TPC, D], F32)
    gmd = P.din("gm", [128, D], F32)
    gfd = P.din("gffn", [128, D], F32)
    cfd = P.din("cf", [128, D], F32)
    sfd = P.din("sf", [128, D], F32)
    wrd = P.din("wr", [D, 16], F32)
    x1o = P.dout("x1", [TPC, D], F32)
    hfo = P.dout("hf", [TPC, D], BF16)
    affo = P.dout("aff", [TPC, 16], F32)

    identf, idk = make_ident(P, F32)
    Wb = P.sb("Wb", [128, 16, D], BF16)
    fT = [P.sb("fTs%d" % i, [128, 16, 128], BF16) for i in range(2)]
    gm = P.sb("gms", [128, D], F32)
    gam = P.sb("gam", [128, D], F32)
    sh = P.sb("sh", [128, D], F32)
    xs = [P.sb("xs%d" % i, [128, D], F32) for i in range(2)]
    hf32s = [P.sb("hf32_%d" % i, [128, D], F32) for i in range(2)]
    hfb = [P.sb("hfb%d" % i, [128, D], BF16) for i in range(2)]
    hT32 = P.sb("hT32", [128, 16, 128], F32)
    tq = [P.sb("tq%d" % i, [128, 512], F32) for i in range(2)]
    wr = P.sb("wrs", [128, 16, 16], F32)
    ss = [P.sb("ss%d" % i, [128, 4], F32) for i in range(2)]
    sm = [P.sb("smx%d" % i, [128, 4], F32) for i in range(2)]
    ex = [P.sb("ex%d" % i, [128, 16], F32) for i in range(2)]
    epsb = P.sb("epsb", [128, 1], F32)
    pz = [P.ps("pz%d" % i, [128, 512], F32) for i in range(2)]
    pT = P.ps("pT", [128, D], F32)
    pl = P.ps("pl", [128, 16], F32)

    P.op("pool", lambda: nc.gpsimd.memset(epsb[:], EPS), writes=["epsb"])
    wv = Wd.rearrange("(kc p) n -> p kc n", p=128)
    CL = CastLoader(P, 1024, engines=("dve",))
    for hh in range(2):
        for kc in range(16):
            CL.load(Wb[:, kc:kc + 1, hh * 1024:(hh + 1) * 1024], ("Wb", hh), wv[:, kc:kc + 1, hh * 1024:(hh + 1) * 1024], (1, 1024))
    P.dma("sp", gm[:], gmd[:, :], writes=["gm"])
    P.dma("sp", gam[:], cfd[:, :], writes=["gam"])
    P.dma("act", xs[1][:], gfd[:, :], writes=[("xs", 1)])
    P.dma("act", sh[:], sfd[:, :], writes=["sh"])
    P.dma("act", wr[:], wrd.rearrange("(kc p) e -> p kc e", p=128), writes=["wr"])
    P.op("dve", lambda: nc.vector.scalar_tensor_tensor(out=gam[:], in0=gam[:], scalar=1.0, in1=xs[1][:], op0=ALU.add, op1=ALU.mult),
         reads=["gam", ("xs", 1)], writes=["gam"])

    pending = []

    def router(ti, b):
        tok0 = ti * 128
        hf32 = hf32s[b]
        hk = ("hf32", b)
        for kc in range(16):
            P.op("pe", lambda kc=kc: nc.tensor.transpose(pT[:, kc * 128:(kc + 1) * 128], hf32[:, kc * 128:(kc + 1) * 128], identf[:]),
                 reads=[hk, idk], writes=["pT"])
        P.op("dve", lambda: nc.vector.tensor_copy(out=hT32[:], in_=pT.rearrange("p (k t) -> p k t", k=16)), reads=["pT"], writes=["hT32"])
        for kc in range(16):
            P.op("pe", lambda kc=kc: nc.tensor.matmul(pl[:], lhsT=hT32[:, kc, :], rhs=wr[:, kc, :], start=(kc == 0), stop=(kc == 15)),
                 reads=["hT32", "wr"], writes=["pl"])
        m_ = sm[b]
        mk = ("sm", b)
        P.op("dve", lambda: nc.vector.reduce_max(out=m_[:, 0:1], in_=pl[:], axis=AX.X), reads=["pl"], writes=[mk])
        P.op("dve", lambda: nc.vector.tensor_scalar(out=m_[:, 1:2], in0=m_[:, 0:1], scalar1=-1.0, scalar2=None, op0=ALU.mult), reads=[mk], writes=[mk])
        P.op("act", lambda: nc.scalar.activation(out=ex[b][:], in_=pl[:], func=AF.Exp, bias=m_[:, 1:2], accum_out=m_[:, 2:3]),
             reads=["pl", mk], writes=[("ex", b), mk])
        P.op("dve", lambda: nc.vector.reciprocal(out=m_[:, 3:4], in_=m_[:, 2:3]), reads=[mk], writes=[mk])
        P.op("dve", lambda: nc.vector.tensor_scalar(out=ex[b][:], in0=ex[b][:], scalar1=m_[:, 3:4], scalar2=None, op0=ALU.mult),
             reads=[("ex", b), mk], writes=[("ex", b)])
        P.dma("pool", affo[tok0:tok0 + 128, :], ex[b][:], reads=[("ex", b)], writes=["affo"])

    zc = 0
    for ti in range(NT):
        b = ti % 2
        tok0 = ti * 128
        xk = ("xs", b)
        hf32 = hf32s[b]
        P.dma("sp", xs[b][:], xd[tok0:tok0 + 128, :], writes=[xk])
        P.dma("sp", fT[b][:], fTd[:, :, tok0:tok0 + 128].rearrange("k d t -> d k t"), writes=[("fT", b)])
        for nb in range(4):
            j = zc % 2
            zc += 1
            for kc in range(16):
                P.op("pe", lambda kc=kc: nc.tensor.matmul(pz[j][:], lhsT=fT[b][:, kc, :], rhs=Wb[:, kc, nb * 512:(nb + 1) * 512],
                                                           start=(kc == 0), stop=(kc == 15)),
                     reads=[("fT", b), ("Wb", nb // 2)], writes=[("pz", j)])
            P.op("dve", lambda: nc.vector.tensor_tensor(out=tq[j][:], in0=pz[j][:], in1=gm[:, nb * 512:(nb + 1) * 512], op=ALU.mult),
                 reads=[("pz", j), "gm"], writes=[("tq", j)])
            P.op("dve", lambda: nc.vector.tensor_tensor(out=xs[b][:, nb * 512:(nb + 1) * 512], in0=xs[b][:, nb * 512:(nb + 1) * 512],
                                                        in1=tq[j][:], op=ALU.add),
                 reads=[xk, ("tq", j)], writes=[xk])
        P.dma("pool", x1o[tok0:tok0 + 128, :], xs[b][:], reads=[xk], writes=["x1o"])
        s_ = ss[b]
        sk = ("ss", b)
        P.op("act", lambda: nc.scalar.activation(out=hfb[b][:], in_=xs[b][:], func=AF.Square, accum_out=s_[:, 0:1]),
             reads=[xk], writes=[("hfb", b), sk])
        P.op("act", lambda: nc.scalar.activation(out=s_[:, 1:2], in_=s_[:, 0:1], func=AF.Sqrt, scale=1.0 / D, bias=epsb[:, 0:1]),
             reads=[sk, "epsb"], writes=[sk])
        P.op("dve", lambda: nc.vector.reciprocal(out=s_[:, 2:3], in_=s_[:, 1:2]), reads=[sk], writes=[sk])
        P.op("dve", lambda: nc.vector.scalar_tensor_tensor(out=hf32[:], in0=xs[b][:], scalar=s_[:, 2:3], in1=gam[:], op0=ALU.mult, op1=ALU.mult),
             reads=[xk, sk, "gam"], writes=[("hf32", b)])
        P.op("dve", lambda: nc.vector.tensor_tensor(out=hf32[:], in0=hf32[:], in1=sh[:], op=ALU.add), reads=[("hf32", b), "sh"], writes=[("hf32", b)])
        P.op("act", lambda: nc.scalar.copy(out=hfb[b][:], in_=hf32[:]), reads=[("hf32", b)], writes=[("hfb", b)])
        P.dma("act", hfo[tok0:tok0 + 128, :], hfb[b][:], reads=[("hfb", b)], writes=["hfo"])
        pending.append((ti, b))
        if len(pending) > 1:
            router(*pending.pop(0))
    while pending:
        router(*pending.pop(0))
    return P.finish(["x1o", "hfo", "affo"])


_PROJ_NC = [None]


def run_proj(fT_list, W, x_full, gm_, gffn, cf_, sf_, w_r):
    nc = build_proj()
    common = {"W": np.ascontiguousarray(W), "gm": bc128(gm_), "gffn": bc128(gffn), "cf": bc128(cf_), "sf": bc128(sf_),
              "wr": np.ascontiguousarray(w_r)}
    maps = []
    for i in range(NCORES):
        d = dict(common)
        d["fT"] = np.ascontiguousarray(fT_list[i])
        d["x"] = np.ascontiguousarray(x_full[i * TPC:(i + 1) * TPC])
        maps.append(d)
    res = run_bass_kernel_spmd(nc, maps, core_ids=list(range(NCORES)))
    x1 = np.concatenate([r["x1"] for r in res.results], 0)
    hf = np.concatenate([r["hf"] for r in res.results], 0)
    aff = np.concatenate([r["aff"] for r in res.results], 0)
    return x1, hf, aff


CAP = 1024
FE = 1024
NBIS = 32
OOB = 30000.0


def build_E():
    P = Prog()
    nc = P.nc
    affd = P.din("affT", [128, 2, 64], F32)
    ebd = P.din("ebase", [128, 2], F32)
    hfd = P.din("hf", [SEQ, D], BF16)
    wgd = P.din("wg", [2, D, FE], F32)
    wud = P.din("wu", [2, D, FE], F32)
    wdd = P.din("wd", [2, FE, D], F32)
    Yo = P.dout("Y", [2, CAP, D], F32)
    sloto = P.dout("slot", [128, 2, 64], I32)

    ident, idk = make_ident(P)
    onesf = P.sb("onesf", [128, 128], F32)
    UT = P.sb("UT", [128, 128], F32)
    Lb = P.sb("Lb", [128, 128], F32)
    iot_i = P.sb("iot_i", [128, 1024], I32)
    iot = P.sb("iot", [128, 1024], F32)
    jp_i = P.sb("jp_i", [128, 64, 2], I32)
    jp = P.sb("jp", [128, 64, 2], BF16)
    a = P.sb("a", [128, 2, 64], F32)
    eb = P.sb("eb", [128, 2], F32)
    msk = P.sb("msk", [128, 2, 64], F32)
    bs = P.sb("bs", [128, 16], F32)
    exs = P.sb("exs", [128, 128], F32)
    kv_i = P.sb("kv_i", [128, 2, 16], I32)
    kv = P.sb("kv", [128, 2, 16], F32)
    thr = P.sb("thr", [128, 2, 16], F32)
    mk4 = P.sb("mk4", [128, 2, 16, 64], F32)
    cn4 = P.sb("cn4", [128, 2, 16], F32)
    ge4 = P.sb("ge4", [128, 2, 16], F32)
    ct = P.sb("ct", [128, 1], F32)
    CB = P.sb("CB", [128, 128], F32)
    rk = P.sb("rk", [128, 2, 64], F32)
    sg = P.sb("sg", [128, 2, 64], F32)
    sgi = P.sb("sgi", [128, 2, 64], I32)
    sel = [P.sb("sel%d" % i, [128, 1024], BF16) for i in range(3)]
    idxf = P.sb("idxf", [128, 2, 8], F32)
    pselS = P.sb("pselS", [128, 8, 2], F32)
    zb = P.sb("zb", [128, 128], BF16)
    idxi = P.sb("idxi", [128, 2, 8], I32)
    xg = [P.sb("xg%d" % i, [128, D], BF16) for i in range(2)]
    xsT = P.sb("xsT", [128, 16, CAP], BF16)
    hT = P.sb("hTe", [128, 8, CAP], BF16)
    wgb = [P.sb("wgb%d" % i, [128, 16, 256], BF16) for i in range(2)]
    wub = [P.sb("wub%d" % i, [128, 16, 256], BF16) for i in range(2)]
    wdb = [P.sb("wdb%d" % i, [128, 8, 512], BF16) for i in range(2)]
    sa = [P.sb("sa%d" % i, [128, 512], F32) for i in range(2)]
    ys = [P.sb("ys%d" % i, [128, 512], F32) for i in range(2)]
    pa = [P.ps("pa%d" % i, [128, 512], F32) for i in range(2)]
    pu = [P.ps("pu%d" % i, [128, 512], F32) for i in range(2)]
    py = [P.ps("py%d" % i, [128, 512], F32) for i in range(2)]
    pT = P.ps("pT", [128, D], BF16)

    P.op("pool", lambda: nc.gpsimd.memset(onesf[:], 1.0), writes=["onesf"])
    P.op("pool", lambda: nc.gpsimd.memset(UT[:], 1.0), writes=["UT"])
    P.op("pool", lambda: nc.gpsimd.affine_select(out=UT[:], in_=UT[:], pattern=[[1, 128]], compare_op=ALU.is_ge, fill=0.0, base=0,
                                                 channel_multiplier=-1), reads=["UT"], writes=["UT"])
    P.op("pool", lambda: nc.gpsimd.memset(Lb[:], 1.0), writes=["Lb"])
    P.op("pool", lambda: nc.gpsimd.affine_select(out=Lb[:], in_=Lb[:], pattern=[[1, 128]], compare_op=ALU.is_gt, fill=0.0, base=0,
                                                 channel_multiplier=-1), reads=["Lb"], writes=["Lb"])
    P.op("pool", lambda: nc.gpsimd.memset(Lb[0:64, 64:128], 0.0), reads=["Lb"], writes=["Lb"])
    P.op("pool", lambda: nc.gpsimd.iota(iot_i[:], pattern=[[1, 1024]], base=0, channel_multiplier=0), writes=["iot_i"])
    P.op("dve", lambda: nc.vector.tensor_copy(out=iot[:], in_=iot_i[:]), reads=["iot_i"], writes=["iot"])
    P.op("pool", lambda: nc.gpsimd.iota(jp_i[:, :, 0], pattern=[[1, 64]], base=0, channel_multiplier=0), writes=["jp_i"])
    P.op("pool", lambda: nc.gpsimd.iota(jp_i[:, :, 1], pattern=[[0, 64]], base=0, channel_multiplier=1), reads=["jp_i"], writes=["jp_i"])
    P.op("dve", lambda: nc.vector.tensor_copy(out=jp[:], in_=jp_i[:]), reads=["jp_i"], writes=["jp"])
    P.dma("sp", a[:], affd[:, :, :], writes=["a"])
    P.dma("sp", eb[:], ebd[:, :], writes=["eb"])
    P.op("pool", lambda: nc.gpsimd.memset(zb[:], 0.0), writes=["zb"])

    lo, stp, nn, tmp = bs[:, 0:2], bs[:, 2:4], bs[:, 4:6], bs[:, 6:8]
    P.op("dve", lambda: nc.vector.memset(bs[:, 0:2], 0.0), writes=["bs"])
    P.op("dve", lambda: nc.vector.memset(bs[:, 2:4], 1.001 / 16), reads=["bs"], writes=["bs"])
    P.op("pool", lambda: nc.gpsimd.iota(kv_i[:], pattern=[[0, 2], [1, 16]], base=0, channel_multiplier=0), writes=["kv_i"])
    P.op("dve", lambda: nc.vector.tensor_copy(out=kv[:], in_=kv_i[:]), reads=["kv_i"], writes=["kv"])
    tot = pa[0][:, 0:32].rearrange("p (e k) -> p e k", e=2)

    def V(fn, reads=("bs",), writes=("bs",)):
        P.op("dve", fn, reads=list(reads), writes=list(writes))

    for itb in range(8):
        V(lambda: nc.vector.tensor_tensor(out=thr[:], in0=kv[:], in1=stp.unsqueeze(2).to_broadcast([128, 2, 16]), op=ALU.mult),
          reads=["kv", "bs"], writes=["thr"])
        V(lambda: nc.vector.tensor_tensor(out=thr[:], in0=thr[:], in1=lo.unsqueeze(2).to_broadcast([128, 2, 16]), op=ALU.add),
          reads=["thr", "bs"], writes=["thr"])
        V(lambda: nc.vector.tensor_tensor(out=mk4[:], in0=a[:, :, :].unsqueeze(2).to_broadcast([128, 2, 16, 64]),
                                          in1=thr[:, :, :].unsqueeze(3).to_broadcast([128, 2, 16, 64]), op=ALU.is_ge),
          reads=["a", "thr"], writes=["mk4"])
        V(lambda: nc.vector.reduce_sum(out=cn4[:], in_=mk4[:], axis=AX.X), reads=["mk4"], writes=["cn4"])
        P.op("pe", lambda: nc.tensor.matmul(pa[0][:, 0:32], lhsT=onesf[:], rhs=cn4.rearrange("p e k -> p (e k)"), start=True, stop=True),
             reads=["onesf", "cn4"], writes=[("pa", 0)])
        V(lambda: nc.vector.tensor_scalar(out=ge4[:], in0=tot, scalar1=float(CAP) - 0.5, scalar2=None, op0=ALU.is_ge),
          reads=[("pa", 0)], writes=["ge4"])
        V(lambda: nc.vector.reduce_sum(out=nn, in_=ge4[:], axis=AX.X), reads=["ge4", "bs"], writes=["bs"])
        V(lambda: nc.vector.scalar_tensor_tensor(out=tmp, in0=nn, scalar=-1.0, in1=stp, op0=ALU.add, op1=ALU.mult))
        V(lambda: nc.vector.tensor_tensor(out=lo, in0=lo, in1=tmp, op=ALU.add))
        V(lambda: nc.vector.tensor_scalar(out=stp, in0=stp, scalar1=0.0625, scalar2=None, op0=ALU.mult))
    V(lambda: nc.vector.tensor_tensor(out=msk[:], in0=a[:], in1=lo.unsqueeze(2).to_broadcast([128, 2, 64]), op=ALU.is_ge),
      reads=["a", "bs"], writes=["msk"])

    mflat = msk.rearrange("p e j -> p (e j)")
    pc = pa[1][:, 0:128]
    pex = pu[0][:, 0:128]
    pct = pu[1][:, 0:1]
    P.op("pe", lambda: nc.tensor.matmul(pc, lhsT=UT[:], rhs=mflat, start=True, stop=True), reads=["UT", "msk"], writes=[("pa", 1)])
    P.op("pe", lambda: nc.tensor.matmul(pct, lhsT=mflat, rhs=onesf[:, 0:1], start=True, stop=True), reads=["onesf", "msk"], writes=[("pu", 1)])
    P.op("dve", lambda: nc.vector.tensor_copy(out=ct[:], in_=pct), reads=[("pu", 1)], writes=["ct"])
    P.op("dve", lambda: nc.vector.tensor_scalar(out=CB[:], in0=onesf[:], scalar1=ct[:, 0:1], scalar2=None, op0=ALU.mult),
         reads=["onesf", "ct"], writes=["CB"])
    P.op("pe", lambda: nc.tensor.matmul(pex, lhsT=CB[:], rhs=Lb[:], start=True, stop=True), reads=["CB", "Lb"], writes=[("pu", 0)])
    P.op("dve", lambda: nc.vector.tensor_copy(out=exs[:], in_=pex), reads=[("pu", 0)], writes=["exs"])
    rkf = rk.rearrange("p e j -> p (e j)")
    P.op("dve", lambda: nc.vector.tensor_tensor(out=rkf, in0=pc, in1=exs[:], op=ALU.add), reads=[("pa", 1), "exs"], writes=["rk"])
    P.op("dve", lambda: nc.vector.tensor_tensor(out=rkf, in0=rkf, in1=mflat, op=ALU.mult), reads=["rk", "msk"], writes=["rk"])
    P.op("dve", lambda: nc.vector.tensor_scalar(out=rkf, in0=rkf, scalar1=-1.0, scalar2=None, op0=ALU.add), reads=["rk"], writes=["rk"])
    P.op("dve", lambda: nc.vector.tensor_tensor(out=sg[:], in0=rk[:], in1=eb[:, :].unsqueeze(2).to_broadcast([128, 2, 64]), op=ALU.add),
         reads=["rk", "eb"], writes=["sg"])
    P.op("dve", lambda: nc.vector.tensor_scalar(out=sg[:], in0=sg[:], scalar1=-OOB, scalar2=None, op0=ALU.add), reads=["sg"], writes=["sg"])
    P.op("dve", lambda: nc.vector.tensor_tensor(out=sg[:], in0=sg[:], in1=msk[:], op=ALU.mult), reads=["sg", "msk"], writes=["sg"])
    P.op("dve", lambda: nc.vector.tensor_scalar(out=sg[:], in0=sg[:], scalar1=OOB, scalar2=None, op0=ALU.add), reads=["sg"], writes=["sg"])
    P.op("dve", lambda: nc.vector.tensor_copy(out=sgi[:], in_=sg[:]), reads=["sg"], writes=["sgi"])
    P.dma("sp", sloto[:, :, :], sgi[:], reads=["sgi"], writes=["sloto"])

    for e in range(2):
        psel = py[e][:, 0:16].rearrange("p (s c) -> p s c", c=2)
        P.op("pe", lambda: nc.tensor.matmul(py[e][:, 0:16], lhsT=zb[:, 0:128], rhs=zb[:, 0:16], start=True, stop=False),
             reads=["zb"], writes=[("py", e)])
        for j in range(64):
            sb_ = sel[j % 3]
            P.op("dve", lambda: nc.vector.tensor_scalar(out=sb_[:], in0=iot[:], scalar1=rk[:, e, j:j + 1], scalar2=None, op0=ALU.is_equal),
                 reads=["iot", "rk"], writes=[("sel", j % 3)])
            for s in range(8):
                P.op("pe", lambda s=s: nc.tensor.matmul(psel[:, s, :], lhsT=sb_[:, s * 128:(s + 1) * 128], rhs=jp[:, j, :],
                                                         start=False, stop=(j == 63), skip_group_check=True),
                     reads=[("sel", j % 3), "jp"], writes=[("py", e)])
        P.op("dve", lambda: nc.vector.tensor_copy(out=pselS[:], in_=psel), reads=[("py", e)], writes=["pselS"])
        P.op("dve", lambda: nc.vector.scalar_tensor_tensor(out=idxf[:, e, :], in0=pselS[:, :, 0], scalar=128.0, in1=pselS[:, :, 1],
                                                           op0=ALU.mult, op1=ALU.add),
             reads=["pselS"], writes=["idxf"])
    P.op("dve", lambda: nc.vector.tensor_copy(out=idxi[:], in_=idxf[:]), reads=["idxf"], writes=["idxi"])

    CL = CastLoader(P, 1024)
    gi = 0
    for e in range(2):
        for s in range(8):
            g_ = gi % 2
            gi += 1
            P.idma(xg[g_][:], None, hfd[:, :], bass.IndirectOffsetOnAxis(ap=idxi[:, e, s:s + 1], axis=0),
                   reads=["idxi"], writes=[("xg", g_)])
            for kc in range(16):
                P.op("pe", lambda kc=kc: nc.tensor.transpose(pT[:, kc * 128:(kc + 1) * 128], xg[g_][:, kc * 128:(kc + 1) * 128], ident[:]),
                     reads=[("xg", g_), idk], writes=["pT"])
            P.op("act", lambda: nc.scalar.copy(out=xsT[:, :, s * 128:(s + 1) * 128], in_=pT.rearrange("p (k t) -> p k t", k=16)),
                 reads=["pT"], writes=["xsT"])
        wgv = wgd[e].rearrange("(kc p) f -> p kc f", p=128)
        wuv = wud[e].rearrange("(kc p) f -> p kc f", p=128)
        wdv = wdd[e].rearrange("(fc p) d -> p fc d", p=128)
        zi = 0

        def queue_gu(f2, wgv=wgv, wuv=wuv):
            b = f2 % 2
            for kq in range(4):
                CL.enqueue(wgb[b][:, kq * 4:(kq + 1) * 4, :], ("wgb", b), wgv[:, kq * 4:(kq + 1) * 4, f2 * 256:(f2 + 1) * 256], (4, 256))
                CL.enqueue(wub[b][:, kq * 4:(kq + 1) * 4, :], ("wub", b), wuv[:, kq * 4:(kq + 1) * 4, f2 * 256:(f2 + 1) * 256], (4, 256))

        def queue_d(dc, wdv=wdv):
            b = dc % 2
            for fq in range(4):
                CL.enqueue(wdb[b][:, fq * 2:(fq + 1) * 2, :], ("wdb", b), wdv[:, fq * 2:(fq + 1) * 2, dc * 512:(dc + 1) * 512], (2, 512))

        if e == 0:
            queue_gu(0)
        for f2 in range(4):
            b = f2 % 2
            CL.flush()
            if f2 + 1 < 4:
                queue_gu(f2 + 1)
            else:
                queue_d(0)
            for fs in range(2):
                fc = f2 * 2 + fs
                for hh in range(2):
                    z = zi % 2
                    zi += 1
                    for kc in range(16):
                        P.op("pe", lambda kc=kc: nc.tensor.matmul(pa[z][:], lhsT=wgb[b][:, kc, fs * 128:(fs + 1) * 128],
                                                                   rhs=xsT[:, kc, hh * 512:(hh + 1) * 512], start=(kc == 0), stop=(kc == 15)),
                             reads=[("wgb", b), "xsT"], writes=[("pa", z)])
                    for kc in range(16):
                        P.op("pe", lambda kc=kc: nc.tensor.matmul(pu[z][:], lhsT=wub[b][:, kc, fs * 128:(fs + 1) * 128],
                                                                   rhs=xsT[:, kc, hh * 512:(hh + 1) * 512], start=(kc == 0), stop=(kc == 15)),
                             reads=[("wub", b), "xsT"], writes=[("pu", z)])
                    P.op("act", lambda: nc.scalar.activation(out=sa[z][:], in_=pa[z][:], func=AF.Silu), reads=[("pa", z)], writes=[("sa", z)])
                    P.op("dve", lambda: nc.vector.tensor_tensor(out=hT[:, fc, hh * 512:(hh + 1) * 512], in0=pu[z][:], in1=sa[z][:], op=ALU.mult),
                         reads=[("pu", z), ("sa", z)], writes=["hT"])
                    CL.pump(2)
        yi = 0
        for dc in range(4):
            b = dc % 2
            CL.flush()
            if dc + 1 < 4:
                queue_d(dc + 1)
            elif e == 0:
                wgv1 = wgd[1].rearrange("(kc p) f -> p kc f", p=128)
                wuv1 = wud[1].rearrange("(kc p) f -> p kc f", p=128)
                queue_gu(0, wgv1, wuv1)
            for s in range(8):
                z = yi % 2
                yi += 1
                for fc in range(8):
                    P.op("pe", lambda fc=fc: nc.tensor.matmul(py[z][:], lhsT=hT[:, fc, s * 128:(s + 1) * 128], rhs=wdb[b][:, fc, :],
                                                               start=(fc == 0), stop=(fc == 7)),
                         reads=["hT", ("wdb", b)], writes=[("py", z)])
                P.op("act", lambda: nc.scalar.copy(out=ys[z][:], in_=py[z][:]), reads=[("py", z)], writes=[("ys", z)])
                P.dma("act", Yo[e, s * 128:(s + 1) * 128, dc * 512:(dc + 1) * 512], ys[z][:], reads=[("ys", z)], writes=["Yo"])
                CL.pump(1)
    return P.finish(["Yo", "sloto"])


def run_E(aff, hf, wg, wu, wd):
    nc = build_E()
    hf = np.ascontiguousarray(hf)
    maps = []
    for c in range(NCORES):
        es = slice(2 * c, 2 * c + 2)
        affT = np.ascontiguousarray(aff[:, es].reshape(64, 128, 2).transpose(1, 2, 0))
        ebase = np.ascontiguousarray(np.broadcast_to(np.array([[2 * c * CAP, (2 * c + 1) * CAP]], np.float32), (128, 2)))
        maps.append({"affT": affT, "ebase": ebase, "hf": hf, "wg": np.ascontiguousarray(wg[es]), "wu": np.ascontiguousarray(wu[es]),
                     "wd": np.ascontiguousarray(wd[es])})
    res = run_bass_kernel_spmd(nc, maps, core_ids=list(range(NCORES)))
    Yall = np.concatenate([r["Y"].reshape(2 * CAP, D) for r in res.results], 0)
    slots = np.concatenate([r["slot"].transpose(2, 0, 1).reshape(SEQ, 2) for r in res.results], 1)
    return Yall, np.ascontiguousarray(slots)


NYROWS = 16 * CAP
NGT = 5


def combine_setup(P):
    nc = P.nc
    c = {}
    c["x1d"] = P.din("x1", [TPC, D], F32)
    c["Yd"] = P.din("Yall", [NYROWS, D], F32)
    c["sld"] = P.din("slots", [TPC, 16], I32)
    c["afd"] = P.din("aff", [TPC, 16], F32)
    c["gfd"] = P.din("gf", [128, D], F32)
    c["sl"] = P.sb("sl", [128, NT, 16], I32)
    c["af"] = P.sb("af", [128, NT, 16], F32)
    c["gf"] = P.sb("gfs", [128, D], F32)
    c["gt"] = [P.sb("gt%d" % i, [128, D], F32) for i in range(NGT)]
    c["acc"] = [P.sb("acc%d" % i, [128, D], F32) for i in range(2)]
    c["xs"] = [P.sb("cxs%d" % i, [128, D], F32) for i in range(2)]
    P.dma("sp", c["sl"][:], c["sld"].rearrange("(t p) e -> p t e", p=128), writes=["sl"])
    P.dma("sp", c["af"][:], c["afd"].rearrange("(t p) e -> p t e", p=128), writes=["af"])
    P.dma("sp", c["gf"][:], c["gfd"][:, :], writes=["gfs"])
    c["gi"] = 0
    c["breg"] = nc.gpsimd.to_reg(NYROWS - 1)
    return c


def combine_tile(P, c, ti):
    nc = P.nc
    b = ti % 2
    acc = c["acc"][b]
    ak = ("acc", b)
    xs = c["xs"][b]
    xk = ("cxs", b)
    P.dma("sp", xs[:], c["x1d"][ti * 128:(ti + 1) * 128, :], writes=[xk])
    for e in range(16):
        g = c["gi"] % NGT
        c["gi"] += 1
        gt = c["gt"][g]
        gk = ("gt", g)
        P.op("act", lambda: nc.scalar.memzero(gt[:]), writes=[gk])
        P.idma(gt[:], None, c["Yd"][:, :], bass.IndirectOffsetOnAxis(ap=c["sl"][:, ti, e:e + 1], axis=0),
               reads=["sl"], writes=[gk], semkey=("dg", g), bounds_check=c["breg"], oob_is_err=False)
        if e == 0:
            P.op("dve", lambda: nc.vector.tensor_scalar(out=acc[:], in0=gt[:], scalar1=c["af"][:, ti, e:e + 1], scalar2=None, op0=ALU.mult),
                 reads=[gk, "af"], writes=[ak])
        else:
            P.op("dve", lambda: nc.vector.scalar_tensor_tensor(out=acc[:], in0=gt[:], scalar=c["af"][:, ti, e:e + 1], in1=acc[:],
                                                               op0=ALU.mult, op1=ALU.add),
                 reads=[gk, "af", ak], writes=[ak])
    P.op("dve", lambda: nc.vector.tensor_tensor(out=acc[:], in0=acc[:], in1=c["gf"][:], op=ALU.mult), reads=[ak, "gfs"], writes=[ak])
    P.op("pool", lambda: nc.gpsimd.tensor_tensor(out=xs[:], in0=xs[:], in1=acc[:], op=ALU.add), reads=[xk, ak], writes=[xk])
    return xs, xk


def build_F():
    P = Prog()
    nc = P.nc
    c = combine_setup(P)
    gmixd = P.din("gmix", [128, D], F32)
    cmd = P.din("cm", [128, D], F32)
    smd = P.din("sm", [128, D], F32)
    Cd = P.din("Cch", [512, 512], BF16)
    Sd = P.din("Sch", [512, 512], BF16)
    x2o = P.dout("x2", [TPC, D], F32)
    Ao = P.dout("A", [TPC, D], BF16)
    Bo = P.dout("B", [TPC, D], BF16)
    ident, idk = make_ident(P)
    gam = P.sb("gam", [128, D], F32)
    sh = P.sb("sh", [128, D], F32)
    hb = [P.sb("hb%d" % i, [128, D], BF16) for i in range(2)]
    hTt = P.sb("hTt", [128, 16, 128], BF16)
    Cs = P.sb("Cs", [128, 4, 512], BF16)
    Ss = P.sb("Ss", [128, 4, 512], BF16)
    ao = [P.sb("ao%d" % i, [128, 512], BF16) for i in range(2)]
    ss = [P.sb("ss%d" % i, [128, 4], F32) for i in range(2)]
    epsb = P.sb("epsb", [128, 1], F32)
    pT = P.ps("pT", [128, D], BF16)
    pz = [P.ps("pz%d" % i, [128, 512], F32) for i in range(2)]
    P.op("pool", lambda: nc.gpsimd.memset(epsb[:], EPS), writes=["epsb"])
    P.dma("act", gam[:], cmd[:, :], writes=["gam"])
    P.dma("act", c["gt"][0][:], gmixd[:, :], writes=[("gt", 0)], semkey=("dg", 0))
    P.dma("act", sh[:], smd[:, :], writes=["sh"])
    P.dma("act", Cs[:], Cd.rearrange("(k p) n -> p k n", p=128), writes=["Cs"])
    P.dma("act", Ss[:], Sd.rearrange("(k p) n -> p k n", p=128), writes=["Ss"])
    P.op("dve", lambda: nc.vector.scalar_tensor_tensor(out=gam[:], in0=gam[:], scalar=1.0, in1=c["gt"][0][:], op0=ALU.add, op1=ALU.mult),
         reads=["gam", ("gt", 0)], writes=["gam"])
    zc = 0
    nxt = combine_tile(P, c, 0)
    for ti in range(NT):
        b = ti % 2
        xs, xk = nxt
        if ti + 1 < NT:
            nxt = combine_tile(P, c, ti + 1)
        P.dma("act", x2o[ti * 128:(ti + 1) * 128, :], xs[:], reads=[xk], writes=["x2o"])
        rms_modulate_tile(P, nc, xs, xk, 128, ss[b], ("ss", b), epsb, gam, sh, hb[b], ("hb", b))
        for kc in range(16):
            P.op("pe", lambda kc=kc: nc.tensor.transpose(pT[:, kc * 128:(kc + 1) * 128], hb[b][:, kc * 128:(kc + 1) * 128], ident[:]),
                 reads=[("hb", b), idk], writes=["pT"])
        P.op("act", lambda: nc.scalar.copy(out=hTt[:], in_=pT.rearrange("p (k t) -> p k t", k=16)), reads=["pT"], writes=["hTt"])
        for g in range(4):
            for (W_, wk, outd) in ((Cs, "Cs", Ao), (Ss, "Ss", Bo)):
                j = zc % 2
                zc += 1
                for k in range(4):
                    P.op("pe", lambda k=k: nc.tensor.matmul(pz[j][:], lhsT=hTt[:, g * 4 + k, :], rhs=W_[:, k, :], start=(k == 0), stop=(k == 3)),
                         reads=["hTt", wk], writes=[("pz", j)])
                P.op("act", lambda: nc.scalar.copy(out=ao[j][:], in_=pz[j][:]), reads=[("pz", j)], writes=[("ao", j)])
                P.dma("act", outd[ti * 128:(ti + 1) * 128, g * 512:(g + 1) * 512], ao[j][:], reads=[("ao", j)], writes=["AB"])
    return P.finish(["x2o", "AB"])


def bf16_np(a):
    import ml_dtypes
    return np.ascontiguousarray(np.asarray(a, np.float32).astype(ml_dtypes.bfloat16))


def dft_consts():
    c = np.arange(512)
    ang = 2 * np.pi * np.outer(c, c) / 512
    Cch = np.cos(ang) / math.sqrt(512)
    Sch = np.sin(ang) / math.sqrt(512)
    n1 = np.arange(128)
    a1 = 2 * np.pi * np.outer(n1, n1) / 128
    Cc = np.cos(a1) / math.sqrt(128)
    Ssn = np.sin(a1) / math.sqrt(128)
    n2 = np.arange(64)[:, None, None]
    k1 = np.arange(128)[None, :, None]
    k2 = np.arange(64)[None, None, :]
    th = 2 * np.pi * (n2 * k1 / 8192.0 + n2 * k2 / 64.0)
    Hr = np.cos(th) / 8.0
    Hni = np.sin(th) / 8.0
    return dict(Cch=bf16_np(Cch), Sch=bf16_np(Sch), Cc=bf16_np(Cc), nSs=bf16_np(-Ssn), nCc=bf16_np(-Cc), Hr=bf16_np(Hr), Hni=bf16_np(Hni))


def run_F(x1, Yall, slots, aff, gf_, gmix, cm_, sm_):
    nc = build_F()
    k = dft_consts()
    common = {"Yall": Yall, "gf": bc128(gf_), "gmix": bc128(gmix), "cm": bc128(cm_), "sm": bc128(sm_), "Cch": k["Cch"], "Sch": k["Sch"]}
    maps = []
    for i in range(NCORES):
        d = dict(common)
        sl = slice(i * TPC, (i + 1) * TPC)
        d["x1"] = np.ascontiguousarray(x1[sl])
        d["slots"] = np.ascontiguousarray(slots[sl])
        d["aff"] = np.ascontiguousarray(aff[sl])
        maps.append(d)
    res = run_bass_kernel_spmd(nc, maps, core_ids=list(range(NCORES)))
    x2 = np.concatenate([r["x2"] for r in res.results], 0)
    A = np.concatenate([r["A"] for r in res.results], 0)
    B = np.concatenate([r["B"] for r in res.results], 0)
    return x2, A, B


CPC = D // NCORES


def build_G():
    P = Prog()
    nc = P.nc
    zAd = P.din("zA", [128, 64, CPC], BF16)
    zBd = P.din("zB", [128, 64, CPC], BF16)
    Ccd = P.din("Cc", [128, 128], BF16)
    nSsd = P.din("nSs", [128, 128], BF16)
    nCcd = P.din("nCc", [128, 128], BF16)
    Hrd = P.din("Hr", [64, 128, 64], BF16)
    Hnid = P.din("Hni", [64, 128, 64], BF16)
    YTo = P.dout("YT", [CPC, SEQ], BF16)
    zA = P.sb("zAs", [128, 64, 128], BF16)
    zB = P.sb("zBs", [128, 64, 128], BF16)
    Cc = P.sb("Ccs", [128, 128], BF16)
    nSs = P.sb("nSss", [128, 128], BF16)
    nCc = P.sb("nCcs", [128, 128], BF16)
    Hr = P.sb("Hrs", [64, 128, 64], BF16)
    Hni = P.sb("Hnis", [64, 128, 64], BF16)
    Tr = P.sb("Tr", [64, 128, 128], BF16)
    Ti = P.sb("Ti", [64, 128, 128], BF16)
    YTs = P.sb("YTs", [128, 64, 128], BF16)
    pTr = [P.ps("pTr%d" % i, [64, 4, 128], F32) for i in range(2)]
    pTi = [P.ps("pTi%d" % i, [64, 4, 128], F32) for i in range(2)]
    pY = [P.ps("pY%d" % i, [128, 8, 64], F32) for i in range(2)]
    P.dma("sp", Cc[:], Ccd[:, :], writes=["Cc"])
    P.dma("sp", nSs[:], nSsd[:, :], writes=["nSs"])
    P.dma("sp", nCc[:], nCcd[:, :], writes=["nCc"])
    P.dma("act", Hr[:], Hrd[:, :, :], writes=["Hr"])
    P.dma("act", Hni[:], Hnid[:, :, :], writes=["Hni"])
    for hc in range(2):
        c0 = hc * 128
        P.dma("sp", zA[:], zAd[:, :, c0:c0 + 128], writes=["zA"])
        P.dma("act", zB[:], zBd[:, :, c0:c0 + 128], writes=["zB"])
        for cb in range(32):
            j = cb % 2
            for cc in range(4):
                c = cb * 4 + cc
                P.op("pe", lambda: nc.tensor.matmul(pTr[j][:, cc, :], lhsT=zA[:, :, c], rhs=Cc[:], start=True, stop=False),
                     reads=["zA", "Cc"], writes=[("pTr", j)])
                P.op("pe", lambda: nc.tensor.matmul(pTr[j][:, cc, :], lhsT=zB[:, :, c], rhs=nSs[:], start=False, stop=True),
                     reads=["zB", "nSs"], writes=[("pTr", j)])
                P.op("pe", lambda: nc.tensor.matmul(pTi[j][:, cc, :], lhsT=zB[:, :, c], rhs=nCc[:], start=True, stop=False),
                     reads=["zB", "nCc"], writes=[("pTi", j)])
                P.op("pe", lambda: nc.tensor.matmul(pTi[j][:, cc, :], lhsT=zA[:, :, c], rhs=nSs[:], start=False, stop=True),
                     reads=["zA", "nSs"], writes=[("pTi", j)])
            P.op("act", lambda: nc.scalar.copy(out=Tr[:, cb * 4:(cb + 1) * 4, :], in_=pTr[j][:]), reads=[("pTr", j)], writes=["Tr"])
            P.op("dve", lambda: nc.vector.tensor_copy(out=Ti[:, cb * 4:(cb + 1) * 4, :], in_=pTi[j][:]), reads=[("pTi", j)], writes=["Ti"])
        for kb in range(16):
            j = kb % 2
            for kk in range(8):
                k1 = kb * 8 + kk
                P.op("pe", lambda: nc.tensor.matmul(pY[j][:, kk, :], lhsT=Tr[:, :, k1], rhs=Hr[:, k1, :], start=True, stop=False),
                     reads=["Tr", "Hr"], writes=[("pY", j)])
                P.op("pe", lambda: nc.tensor.matmul(pY[j][:, kk, :], lhsT=Ti[:, :, k1], rhs=Hni[:, k1, :], start=False, stop=True),
                     reads=["Ti", "Hni"], writes=[("pY", j)])
            eng = "act" if kb % 2 == 0 else "dve"
            if eng == "act":
                P.op("act", lambda: nc.scalar.copy(out=YTs[:, :, kb * 8:(kb + 1) * 8], in_=pY[j].rearrange("p a b -> p b a")),
                     reads=[("pY", j)], writes=["YTs"])
            else:
                P.op("dve", lambda: nc.vector.tensor_copy(out=YTs[:, :, kb * 8:(kb + 1) * 8], in_=pY[j].rearrange("p a b -> p b a")),
                     reads=[("pY", j)], writes=["YTs"])
        P.dma("sp", YTo[c0:c0 + 128, :], YTs.rearrange("p a b -> p (a b)"), reads=["YTs"], writes=["YTo"])
    return P.finish(["YTo"])


def run_G(A, B):
    nc = build_G()
    k = dft_consts()
    maps = []
    for i in range(NCORES):
        cs = slice(i * CPC, (i + 1) * CPC)
        maps.append({"zA": np.ascontiguousarray(A[:, cs]).reshape(128, 64, CPC), "zB": np.ascontiguousarray(B[:, cs]).reshape(128, 64, CPC),
                     "Cc": k["Cc"], "nSs": k["nSs"], "nCc": k["nCc"], "Hr": k["Hr"], "Hni": k["Hni"]})
    res = run_bass_kernel_spmd(nc, maps, core_ids=list(range(NCORES)))
    YT = np.concatenate([r["YT"] for r in res.results], 0)
    return YT


def build_I():
    P = Prog()
    nc = P.nc
    c = combine_setup(P)
    gfin_d = P.din("gfin", [128, D], F32)
    outd = P.dout("out", [TPC, D], F32)
    gfin = P.sb("gfin_s", [128, D], F32)
    jk = P.sb("junk", [128, D], BF16)
    ss = [P.sb("ss%d" % i, [128, 4], F32) for i in range(2)]
    epsb = P.sb("epsb", [128, 1], F32)
    P.op("pool", lambda: nc.gpsimd.memset(epsb[:], EPS), writes=["epsb"])
    P.dma("act", gfin[:], gfin_d[:, :], writes=["gfin"])
    nxt = combine_tile(P, c, 0)
    for ti in range(NT):
        b = ti % 2
        xs, xk = nxt
        if ti + 1 < NT:
            nxt = combine_tile(P, c, ti + 1)
        s_ = ss[b]
        sk = ("ss", b)
        P.op("act", lambda: nc.scalar.activation(out=jk[:], in_=xs[:], func=AF.Square, accum_out=s_[:, 0:1]), reads=[xk], writes=["junk", sk])
        P.op("act", lambda: nc.scalar.activation(out=s_[:, 1:2], in_=s_[:, 0:1], func=AF.Sqrt, scale=1.0 / D, bias=epsb[:, 0:1]),
             reads=[sk, "epsb"], writes=[sk])
        P.op("dve", lambda: nc.vector.reciprocal(out=s_[:, 2:3], in_=s_[:, 1:2]), reads=[sk], writes=[sk])
        P.op("dve", lambda: nc.vector.scalar_tensor_tensor(out=xs[:], in0=xs[:], scalar=s_[:, 2:3], in1=gfin[:], op0=ALU.mult, op1=ALU.mult),
             reads=[xk, sk, "gfin"], writes=[xk])
        P.dma("act", outd[ti * 128:(ti + 1) * 128, :], xs[:], reads=[xk], writes=["outd"])
    return P.finish(["outd"])


def run_I(x1, Yall, slots, aff, gf_, gfin):
    nc = build_I()
    common = {"Yall": Yall, "gf": bc128(gf_), "gfin": bc128(gfin)}
    maps = []
    for i in range(NCORES):
        d = dict(common)
        sl = slice(i * TPC, (i + 1) * TPC)
        d["x1"] = np.ascontiguousarray(x1[sl])
        d["slots"] = np.ascontiguousarray(slots[sl])
        d["aff"] = np.ascontiguousarray(aff[sl])
        maps.append(d)
    res = run_bass_kernel_spmd(nc, maps, core_ids=list(range(NCORES)))
    return np.concatenate([r["out"] for r in res.results], 0)


def kernel(**inp):
    inp = {k: np.asarray(v) for k, v in inp.items()}
    m = run_mod(inp)
    x0 = inp["x"][0]
    ra = run_A(inp, m)
    aT = run_B(inp, ra)
    fT = [np.concatenate([np.stack([aT[h][:, i * TPC:(i + 1) * TPC] for h in range(8)], 0), ra[i]["gT"]], 0) for i in range(NCORES)]
    m0 = m[0, 0]
    x1, hf, aff = run_proj(fT, inp["w_out"][0], x0, m0[2 * D:3 * D], inp["g_norm_ffn"][0], m0[4 * D:5 * D], m0[3 * D:4 * D],
                           inp["w_router"][0])
    Yall, slots = run_E(aff, hf, inp["w_gate"][0], inp["w_up"][0], inp["w_down"][0])
    m1 = m[1, 0]
    x2, A, B = run_F(x1, Yall, slots, aff, m0[5 * D:6 * D], inp["g_norm_mix"][1], m1[D:2 * D], m1[0:D])
    YT = run_G(A, B)
    fT = [np.ascontiguousarray(YT[:, i * TPC:(i + 1) * TPC]).reshape(16, 128, TPC) for i in range(NCORES)]
    x1, hf, aff = run_proj(fT, inp["w_fourier_out"][0], x2, m1[2 * D:3 * D], inp["g_norm_ffn"][1], m1[4 * D:5 * D], m1[3 * D:4 * D],
                           inp["w_router"][1])
    Yall, slots = run_E(aff, hf, inp["w_gate"][1], inp["w_up"][1], inp["w_down"][1])
    out = run_I(x1, Yall, slots, aff, m1[5 * D:6 * D], inp["g_final"])
    return np.ascontiguousarray(out.reshape(1, SEQ, D).astype(np.float32))
```
